# Optimizing a Trainium2 kernel written in Bass

```python
import math
import jax, jax.numpy as jnp
from jax import lax
import numpy as np

D_MODEL = 1024
BATCH = 1
SEQ = 16384
DEPTH = 2

N_MIXERS = 2
N_GLA_LAYERS = (DEPTH + 1) // 2
N_MLA_LAYERS = DEPTH // 2
RMS_EPS = 1e-6

GLA_HEADS = 4
GLA_DK = D_MODEL // 2
GLA_DV = D_MODEL
GLA_HEAD_K = GLA_DK // GLA_HEADS
GLA_HEAD_V = GLA_DV // GLA_HEADS
GLA_GATE_RANK = 16
GLA_TAU = 16.0
GLA_CHUNK = 64
GLA_IN = 2 * GLA_DK + 2 * GLA_DV + 2 * GLA_GATE_RANK

MLA_HEADS = 16
MLA_Q_RANK = 256
MLA_KV_RANK = 128
MLA_NOPE = 128
MLA_ROPE = 64
MLA_V = 128
MLA_IN = MLA_Q_RANK + MLA_KV_RANK + MLA_ROPE
ROPE_BASE = 10000.0
Q_BLOCK = 128

N_EXPERTS = 16
EXPERT_FF = 2048
EC_CAPACITY_FACTOR = 2

kernel_name = 'bidir_gla_mla_ec_moe_hybrid'


def rms_norm(x, g):
    xf = x.astype(jnp.float32)
    y = xf * lax.rsqrt(jnp.mean(xf * xf, axis=-1, keepdims=True) + RMS_EPS)
    return (y * g.astype(jnp.float32)).astype(x.dtype)


def gla_chunked(q, k, v, g, strict):
    B, H, T, dk = q.shape
    dv = v.shape[-1]
    n = T // GLA_CHUNK

    def to_chunks(a):
        return jnp.moveaxis(a.reshape(B, H, n, GLA_CHUNK, a.shape[-1]), 2, 0)

    qc, kc, vc, gc = to_chunks(q), to_chunks(k), to_chunks(v), to_chunks(g)
    idx = jnp.arange(GLA_CHUNK)
    mask = (idx[:, None] > idx[None, :]) if strict else (idx[:, None] >= idx[None, :])

    def step(S, inp):
        qi, ki, vi, gi = inp
        qf = qi.astype(jnp.float32)
        kf = ki.astype(jnp.float32)
        vf = vi.astype(jnp.float32)
        G = jnp.cumsum(gi.astype(jnp.float32), axis=-2)
        o_inter = jnp.einsum('bhtk,bhkv->bhtv', qf * jnp.exp(G), S)
        diff = G[:, :, :, None, :] - G[:, :, None, :, :]
        decay = jnp.exp(jnp.where(mask[None, None, :, :, None], diff, -jnp.inf))
        attn = jnp.einsum('bhtk,bhsk,bhtsk->bhts', qf, kf, decay)
        o_intra = jnp.einsum('bhts,bhsv->bhtv', attn, vf)
        G_last = G[:, :, -1, :]
        S_new = jnp.exp(G_last)[..., None] * S + jnp.einsum(
            'bhsk,bhsv->bhkv', kf * jnp.exp(G_last[:, :, None, :] - G), vf)
        return S_new, o_inter + o_intra

    S0 = jnp.zeros((B, H, dk, dv), jnp.float32)
    _, o = lax.scan(step, S0, (qc, kc, vc, gc))
    return jnp.moveaxis(o, 0, 2).reshape(B, H, T, dv).astype(v.dtype)


def gla_mixer(h, w_in, w_gate_up_f, b_gate_f, w_gate_up_b, b_gate_b, head_norm, w_out):
    B, T, _ = h.shape
    proj = h @ w_in
    s1 = GLA_DK
    s2 = 2 * GLA_DK
    s3 = s2 + GLA_DV
    s4 = s3 + GLA_DV
    s5 = s4 + GLA_GATE_RANK
    q, k, v, r, gd_f, gd_b = jnp.split(proj, [s1, s2, s3, s4, s5], axis=-1)

    def heads(a, d):
        return a.reshape(B, T, GLA_HEADS, d).transpose(0, 2, 1, 3)

    log_a_f = jax.nn.log_sigmoid((gd_f @ w_gate_up_f + b_gate_f).astype(jnp.float32)) / GLA_TAU
    log_a_b = jax.nn.log_sigmoid((gd_b @ w_gate_up_b + b_gate_b).astype(jnp.float32)) / GLA_TAU
    qh = heads(q, GLA_HEAD_K) * (GLA_HEAD_K ** -0.5)
    kh = heads(k, GLA_HEAD_K)
    vh = heads(v, GLA_HEAD_V)
    gf = heads(log_a_f, GLA_HEAD_K)
    gb = heads(log_a_b, GLA_HEAD_K)
    o_fwd = gla_chunked(qh, kh, vh, gf, False)
    o_bwd = jnp.flip(gla_chunked(jnp.flip(qh, 2), jnp.flip(kh, 2), jnp.flip(vh, 2), jnp.flip(gb, 2), True), 2)
    o = (o_fwd + o_bwd).transpose(0, 2, 1, 3)
    o = rms_norm(o, head_norm).reshape(B, T, GLA_DV)
    return (o * jax.nn.silu(r)) @ w_out


def rope(x, cos, sin):
    half = x.shape[-1] // 2
    x1 = x[..., :half].astype(jnp.float32)
    x2 = x[..., half:].astype(jnp.float32)
    return jnp.concatenate([x1 * cos - x2 * sin, x1 * sin + x2 * cos], axis=-1).astype(x.dtype)


def mla_mixer(h, positions, w_in, q_norm, w_uq, kv_norm, w_ukv, w_out):
    B, T, _ = h.shape
    c_q, c_kv, k_r = jnp.split(h @ w_in, [MLA_Q_RANK, MLA_Q_RANK + MLA_KV_RANK], axis=-1)
    q = (rms_norm(c_q, q_norm) @ w_uq).reshape(B, T, MLA_HEADS, MLA_NOPE + MLA_ROPE)
    kv = (rms_norm(c_kv, kv_norm) @ w_ukv).reshape(B, T, MLA_HEADS, MLA_NOPE + MLA_V)
    q_nope, q_rope = q[..., :MLA_NOPE], q[..., MLA_NOPE:]
    k_nope, v = kv[..., :MLA_NOPE], kv[..., MLA_NOPE:]

    half = MLA_ROPE // 2
    inv_freq = ROPE_BASE ** (-jnp.arange(half, dtype=jnp.float32) / half)
    ang = positions.astype(jnp.float32)[..., None] * inv_freq
    cos, sin = jnp.cos(ang), jnp.sin(ang)
    q_rope = rope(q_rope, cos[:, :, None, :], sin[:, :, None, :])
    k_rope = rope(k_r, cos, sin)

    scale = (MLA_NOPE + MLA_ROPE) ** -0.5
    nb = T // Q_BLOCK
    qn_blocks = q_nope.reshape(B, nb, Q_BLOCK, MLA_HEADS, MLA_NOPE).transpose(1, 0, 3, 2, 4)
    qr_blocks = q_rope.reshape(B, nb, Q_BLOCK, MLA_HEADS, MLA_ROPE).transpose(1, 0, 3, 2, 4)
    k_nope_t = k_nope.transpose(0, 2, 1, 3)
    v_t = v.transpose(0, 2, 1, 3)

    def attend(blk):
        qn, qr = blk
        s = jnp.einsum('bhqd,bhkd->bhqk', qn, k_nope_t) + jnp.einsum('bhqr,bkr->bhqk', qr, k_rope)
        p = jax.nn.softmax(s.astype(jnp.float32) * scale, axis=-1)
        return jnp.einsum('bhqk,bhkd->bqhd', p.astype(v_t.dtype), v_t)

    o = lax.map(attend, (qn_blocks, qr_blocks))
    o = o.transpose(1, 0, 2, 3, 4).reshape(B, T, MLA_HEADS * MLA_V)
    return o @ w_out


def ec_moe(h, w_router, w_gate, w_up, w_down):
    B, T, D = h.shape
    cap = max(1, EC_CAPACITY_FACTOR * T // N_EXPERTS)
    aff = jax.nn.softmax((h @ w_router).astype(jnp.float32), axis=-1)
    top_aff, top_idx = lax.top_k(jnp.swapaxes(aff, 1, 2), cap)
    xs = jax.vmap(lambda hb, ib: hb[ib])(h, top_idx)
    a = jnp.einsum('becd,edf->becf', xs, w_gate)
    u = jnp.einsum('becd,edf->becf', xs, w_up)
    y = jnp.einsum('becf,efd->becd', jax.nn.silu(a) * u, w_down)
    y = y * top_aff[..., None].astype(y.dtype)
    return jax.vmap(lambda yb, ib: jnp.zeros((T, D), yb.dtype).at[ib.reshape(-1)].add(yb.reshape(-1, D)))(y, top_idx)


def setup_inputs(seed: int = 0) -> dict:
    key = jax.random.key(seed)
    ks = jax.random.split(key, 24)

    def nrm(k, shape, fan_in):
        return jax.random.normal(k, shape, jnp.float32) * (fan_in ** -0.5)

    def gain(k, shape):
        return 1.0 + 0.02 * jax.random.normal(k, shape, jnp.float32)

    return {
        'x': jax.random.normal(ks[0], (BATCH, SEQ, D_MODEL), jnp.float32),
        'positions': jnp.broadcast_to(jnp.arange(SEQ, dtype=jnp.int32)[None, :], (BATCH, SEQ)),
        'mix_norm': gain(ks[1], (DEPTH, D_MODEL)),
        'ffn_norm': gain(ks[2], (DEPTH, D_MODEL)),
        'final_norm': gain(ks[3], (D_MODEL,)),
        'gla_w_in': nrm(ks[4], (N_GLA_LAYERS, D_MODEL, GLA_IN), D_MODEL),
        'gla_w_gate_up_f': nrm(ks[5], (N_GLA_LAYERS, GLA_GATE_RANK, GLA_DK), GLA_GATE_RANK),
        'gla_b_gate_f': 0.1 * jax.random.normal(ks[6], (N_GLA_LAYERS, GLA_DK), jnp.float32),
        'gla_w_gate_up_b': nrm(ks[7], (N_GLA_LAYERS, GLA_GATE_RANK, GLA_DK), GLA_GATE_RANK),
        'gla_b_gate_b': 0.1 * jax.random.normal(ks[8], (N_GLA_LAYERS, GLA_DK), jnp.float32),
        'gla_head_norm': gain(ks[9], (N_GLA_LAYERS, GLA_HEAD_V)),
        'gla_w_out': nrm(ks[10], (N_GLA_LAYERS, GLA_DV, D_MODEL), GLA_DV),
        'mla_w_in': nrm(ks[11], (N_MLA_LAYERS, D_MODEL, MLA_IN), D_MODEL),
        'mla_q_norm': gain(ks[12], (N_MLA_LAYERS, MLA_Q_RANK)),
        'mla_w_uq': nrm(ks[13], (N_MLA_LAYERS, MLA_Q_RANK, MLA_HEADS * (MLA_NOPE + MLA_ROPE)), MLA_Q_RANK),
        'mla_kv_norm': gain(ks[14], (N_MLA_LAYERS, MLA_KV_RANK)),
        'mla_w_ukv': nrm(ks[15], (N_MLA_LAYERS, MLA_KV_RANK, MLA_HEADS * (MLA_NOPE + MLA_V)), MLA_KV_RANK),
        'mla_w_out': nrm(ks[16], (N_MLA_LAYERS, MLA_HEADS * MLA_V, D_MODEL), MLA_HEADS * MLA_V),
        'moe_w_router': nrm(ks[17], (DEPTH, D_MODEL, N_EXPERTS), D_MODEL),
        'moe_w_gate': nrm(ks[18], (DEPTH, N_EXPERTS, D_MODEL, EXPERT_FF), D_MODEL),
        'moe_w_up': nrm(ks[19], (DEPTH, N_EXPERTS, D_MODEL, EXPERT_FF), D_MODEL),
        'moe_w_down': nrm(ks[20], (DEPTH, N_EXPERTS, EXPERT_FF, D_MODEL), EXPERT_FF),
    }


def reference(x, positions, mix_norm, ffn_norm, final_norm,
              gla_w_in, gla_w_gate_up_f, gla_b_gate_f, gla_w_gate_up_b, gla_b_gate_b, gla_head_norm, gla_w_out,
              mla_w_in, mla_q_norm, mla_w_uq, mla_kv_norm, mla_w_ukv, mla_w_out,
              moe_w_router, moe_w_gate, moe_w_up, moe_w_down):
    h = x
    for i in range(DEPTH):
        j = i // N_MIXERS
        hn = rms_norm(h, mix_norm[i])
        if i % N_MIXERS == 0:
            h = h + gla_mixer(hn, gla_w_in[j], gla_w_gate_up_f[j], gla_b_gate_f[j], gla_w_gate_up_b[j],
                              gla_b_gate_b[j], gla_head_norm[j], gla_w_out[j])
        else:
            h = h + mla_mixer(hn, positions, mla_w_in[j], mla_q_norm[j], mla_w_uq[j], mla_kv_norm[j],
                              mla_w_ukv[j], mla_w_out[j])
        h = h + ec_moe(rms_norm(h, ffn_norm[i]), moe_w_router[i], moe_w_gate[i], moe_w_up[i], moe_w_down[i])
    return rms_norm(h, final_norm)
```

```python
import contextlib
import numpy as np
import ml_dtypes
import concourse.bass as bass
import concourse.mybir as mybir
from concourse.bass_utils import run_bass_kernel_spmd

F32 = mybir.dt.float32
BF16 = mybir.dt.bfloat16
I32 = mybir.dt.int32
AF = mybir.ActivationFunctionType
ALU = mybir.AluOpType
AX = mybir.AxisListType

NCORES = 8
SEQ = 16384
TOK = SEQ // NCORES
NT = TOK // 128
D = 1024
RMS_EPS = 1e-6


class V:
    __slots__ = ("ap", "key")

    def __init__(self, ap, key):
        self.ap = ap
        self.key = key


class Buf:
    def __init__(self, t, name):
        self.t = t
        self.name = name

    def __getitem__(self, idx):
        return V(self.t[idx], self.name)

    def sub(self, s, idx):
        return V(self.t[idx], (self.name, s))

    def v(self, ap, s=None):
        return V(ap, self.name if s is None else (self.name, s))


class Prog:
    ENGS = ("pe", "dve", "act", "pool", "sp")
    NDMA = {"sp": 10, "pool": 8, "act": 4}

    def __init__(self, nc, same_engine_sync=True):
        self.nc = nc
        self.same = same_engine_sync
        self.stack = contextlib.ExitStack()
        self.lists = {e: [] for e in self.ENGS}
        self.cnt = {e: 0 for e in self.ENGS}
        self.seen = {e: {} for e in self.ENGS}
        self.last_w = {}
        self.readers = {}
        self.sem = {}
        self.dma_cnt = {}
        self.dma_rr = {q: 0 for q in self.NDMA}
        for e in ("pe", "dve", "act", "pool"):
            self.sem[("c", e)] = self.stack.enter_context(nc.semaphore("c_" + e))
        for q, n in self.NDMA.items():
            for k in range(n):
                sk = ("d", q, k)
                self.sem[sk] = self.stack.enter_context(nc.semaphore("d_%s%d" % (q, k)))
                self.dma_cnt[sk] = 0
        self.psum = set()
        self.bregs = {}

    def sb(self, name, shape, dt):
        t = self.stack.enter_context(self.nc.sbuf_tensor(name, list(shape), dt))
        return Buf(t, name)

    def ps(self, name, shape, dt):
        t = self.stack.enter_context(self.nc.psum_tensor(name, list(shape), dt))
        self.psum.add(name)
        return Buf(t, name)

    def dram(self, name, shape, dt, kind):
        t = self.nc.dram_tensor(name, list(shape), dt, kind=kind)
        return Buf(t.ap(), name)

    def emit(self, eng, fn, reads, writes, dma=False):
        deps = {}

        def need(tok):
            if tok is None:
                return
            sk, val = tok
            if deps.get(sk, 0) < val:
                deps[sk] = val

        for r in reads:
            need(self.last_w.get(r))
            if r in self.psum:
                for sk, val in self.readers.get(r, {}).items():
                    if sk != ("c", eng):
                        need((sk, val))
        for w in writes:
            need(self.last_w.get(w))
            for sk, val in self.readers.get(w, {}).items():
                need((sk, val))
        if dma:
            k = self.dma_rr[eng]
            self.dma_rr[eng] = (k + 1) % self.NDMA[eng]
            sk = ("d", eng, k)
            if self.dma_cnt[sk] > 0:
                need((sk, self.dma_cnt[sk]))
            self.dma_cnt[sk] += 16
            tok = (sk, self.dma_cnt[sk])
        else:
            self.cnt[eng] += 1
            tok = (("c", eng), self.cnt[eng])
        waits = []
        for sk, val in deps.items():
            if sk == ("c", eng) and (eng == "pe" or not self.same):
                continue
            if self.seen[eng].get(sk, 0) >= val:
                continue
            self.seen[eng][sk] = val
            waits.append((sk, val))
        self.lists[eng].append((waits, fn, tok))
        for r in reads:
            d = self.readers.setdefault(r, {})
            if d.get(tok[0], 0) < tok[1]:
                d[tok[0]] = tok[1]
        for w in writes:
            self.last_w[w] = tok
            self.readers[w] = {}

    @staticmethod
    def _keys(*vs):
        return [v.key for v in vs if isinstance(v, V)]

    @staticmethod
    def _ap(v):
        return v.ap if isinstance(v, V) else v

    def dma(self, q, out, in_, reads=None, writes=None, **kw):
        o, i = out.ap, in_.ap
        self.emit(q, lambda e: e.dma_start(out=o, in_=i, **kw), [in_.key] + (reads or []),
                  [out.key] if writes is None else writes, dma=True)

    def _breg(self, e, val):
        if val not in self.bregs:
            r = e.alloc_register("bchk%d" % val)
            e.reg_mov(r, val)
            self.bregs[val] = r
        return self.bregs[val]

    def gather(self, out, src, idx, nrows):
        o, s, ix = out.ap, src.ap, idx.ap
        self.emit("pool", lambda e: e.indirect_dma_start(
            out=o, out_offset=None, in_=s,
            in_offset=bass.IndirectOffsetOnAxis(ap=ix, axis=0),
            bounds_check=self._breg(e, nrows - 1), oob_is_err=False),
            [src.key, idx.key], [out.key], dma=True)

    def scatter(self, dst, src, idx, nrows, accum=False, reads=None, writes=None):
        d, s, ix = dst.ap, src.ap, idx.ap
        kw = {"compute_op": ALU.add} if accum else {}
        self.emit("pool", lambda e: e.indirect_dma_start(
            out=d, out_offset=bass.IndirectOffsetOnAxis(ap=ix, axis=0), in_=s, in_offset=None,
            bounds_check=self._breg(e, nrows - 1), oob_is_err=False, **kw),
            [src.key, idx.key] + ([dst.key] if accum else []) + (reads or []),
            [dst.key] if writes is None else writes, dma=True)

    def matmul(self, out, lhsT, rhs, start=True, stop=True):
        o, l, r = out.ap, lhsT.ap, rhs.ap
        rd = [lhsT.key, rhs.key] + ([] if start else [out.key])
        self.emit("pe", lambda e: e.matmul(o, l, r, start=start, stop=stop), rd, [out.key])

    def transpose(self, out, in_, ident):
        o, i, d = out.ap, in_.ap, ident.ap
        self.emit("pe", lambda e: e.transpose(o, i, d), [in_.key, ident.key], [out.key])

    def act(self, out, in_, func, bias=0.0, scale=1.0, accum_out=None, eng="act"):
        o, i = out.ap, in_.ap
        b, s = self._ap(bias), self._ap(scale)
        kw = {}
        if accum_out is not None:
            kw["accum_out"] = accum_out.ap
        self.emit(eng, lambda e: e.activation(o, i, func, bias=b, scale=s, **kw),
                  self._keys(in_, bias, scale), self._keys(out, accum_out))

    def copy(self, eng, out, in_):
        o, i = out.ap, in_.ap
        if eng == "act":
            self.emit(eng, lambda e: e.copy(o, i), [in_.key], [out.key])
        else:
            self.emit(eng, lambda e: e.tensor_copy(o, i), [in_.key], [out.key])

    def tt(self, eng, out, in0, in1, op):
        o, a, b = out.ap, in0.ap, in1.ap
        self.emit(eng, lambda e: e.tensor_tensor(o, a, b, op), [in0.key, in1.key], [out.key])

    def ts(self, eng, out, in0, s1, s2, op0, op1=None, accum_out=None):
        o, a = out.ap, in0.ap
        x1, x2 = self._ap(s1), self._ap(s2)
        kw = {}
        if op1 is not None:
            kw["op1"] = op1
        if accum_out is not None:
            kw["accum_out"] = accum_out.ap
        self.emit(eng, lambda e: e.tensor_scalar(o, a, x1, x2, op0, **kw),
                  self._keys(in0, s1, s2), self._keys(out, accum_out))

    def stt(self, eng, out, in0, scalar, in1, op0, op1):
        o, a, b = out.ap, in0.ap, in1.ap
        s = self._ap(scalar)
        self.emit(eng, lambda e: e.scalar_tensor_tensor(o, a, s, b, op0, op1),
                  self._keys(in0, scalar, in1), [out.key])

    def reduce(self, eng, out, in_, op, axis=AX.X):
        o, i = out.ap, in_.ap
        self.emit(eng, lambda e: e.tensor_reduce(o, i, axis, op), [in_.key], [out.key])

    def recip(self, out, in_):
        o, i = out.ap, in_.ap
        self.emit("dve", lambda e: e.reciprocal(o, i), [in_.key], [out.key])

    def memset(self, eng, out, val):
        o = out.ap
        self.emit(eng, lambda e: e.memset(o, val), [], [out.key])

    def wait(self, eng, keys):
        waits = []
        for k_ in keys:
            tok = self.last_w.get(k_)
            if tok is None:
                continue
            sk, val = tok
            if sk == ("c", eng) or self.seen[eng].get(sk, 0) >= val:
                continue
            self.seen[eng][sk] = val
            waits.append((sk, val))
        if waits:
            self.lists[eng].append((waits, None, None))

    def barrier(self):
        allv = {("c", e): self.cnt[e] for e in ("pe", "dve", "act", "pool") if self.cnt[e] > 0}
        allv.update({sk: v for sk, v in self.dma_cnt.items() if v > 0})
        for eng in self.ENGS:
            waits = []
            for sk, val in allv.items():
                if sk == ("c", eng) or self.seen[eng].get(sk, 0) >= val:
                    continue
                self.seen[eng][sk] = val
                waits.append((sk, val))
            if waits:
                self.lists[eng].append((waits, None, None))

    def push_scope(self):
        self._outer = self.stack
        self.stack = contextlib.ExitStack()

    def pop_scope(self):
        self.barrier()
        self.stack.close()
        self.stack = self._outer

    def finish(self):
        nc = self.nc
        prog = self

        def mk(name):
            def body(e):
                for waits, fn, tok in prog.lists[name]:
                    for sk, val in waits:
                        e.wait_ge(prog.sem[sk], val)
                    if fn is None:
                        continue
                    ins = fn(e)
                    ins.then_inc(prog.sem[tok[0]], 16 if tok[0][0] == "d" else 1)
                if name == "sp":
                    for sk, val in prog.dma_cnt.items():
                        if val > 0:
                            e.wait_ge(prog.sem[sk], val)
            return body

        with nc.Block() as block:
            block.tensor(mk("pe"))
            block.vector(mk("dve"))
            block.scalar(mk("act"))
            block.gpsimd(mk("pool"))
            block.sync(mk("sp"))
        self.stack.close()
        return nc


def bcast_rows(ap1d_or_row, nparts):
    return ap1d_or_row.broadcast(0, nparts) if hasattr(ap1d_or_row, "broadcast") else ap1d_or_row


def const_tables():
    i = np.arange(128)
    ui = (i[:, None] <= i[None, :]).astype(np.float32)
    li = (i[:, None] >= i[None, :]).astype(np.float32)
    us = (i[:, None] < i[None, :]).astype(np.float32)
    ls = (i[:, None] > i[None, :]).astype(np.float32)
    ident = np.eye(128, dtype=np.float32)
    cf = np.stack([ui, li, us, ls, ident], axis=1)
    cb = np.eye(128, dtype=np.float32).astype(ml_dtypes.bfloat16)
    return np.ascontiguousarray(cf), cb


GH = 4
GDK = 128
GDV = 256
GIN = 3104


def rmsnorm_tile(p, xt, gbc, hn_out, sq, ss, rstd, width=D):
    p.tt("dve", sq, xt, xt, ALU.mult)
    p.reduce("dve", ss, sq, ALU.add)
    p.ts("dve", rstd, ss, 1.0 / width, RMS_EPS, ALU.mult, ALU.add)
    p.act(rstd, rstd, AF.Sqrt)
    p.recip(rstd, rstd)
    p.stt("dve", hn_out, xt, rstd, gbc, ALU.mult, ALU.mult)


def build_L1():
    nc = bass.Bass("TRN2", target_bir_lowering=False)
    p = Prog(nc)
    x = p.dram("x", [TOK, D], F32, "ExternalInput")
    g_mix = p.dram("g_mix", [1, D], F32, "ExternalInput")
    w_in = p.dram("w_in", [D, GIN], F32, "ExternalInput")
    wg = p.dram("wg", [2, 16, 512], F32, "ExternalInput")
    bg = p.dram("bg", [2, 512], F32, "ExternalInput")
    cf_d = p.dram("cf", [128, 5, 128], F32, "ExternalInput")
    cb_d = p.dram("cb", [128, 128], BF16, "ExternalInput")
    QP = p.dram("QP", [2, 128, GH, TOK], BF16, "ExternalOutput")
    KP = p.dram("KP", [2, 128, GH, TOK], BF16, "Internal")
    K2 = p.dram("K2", [2, TOK, 512], BF16, "Internal")
    VT = p.dram("VT", [TOK, 1024], BF16, "Internal")
    SR = p.dram("SR", [TOK, 1024], F32, "ExternalOutput")
    OL = p.dram("OL", [TOK, 1024], F32, "ExternalOutput")
    AA = p.dram("AA", [2, 128, NT * GH], F32, "ExternalOutput")
    BE = p.dram("BE", [2, GH, 128, GDV], F32, "ExternalOutput")

    W = p.sb("W", [128, 8, GIN], BF16)
    cf = p.sb("cfs", [128, 5, 128], F32)
    cb = p.sb("cbs", [128, 128], BF16)
    gbc = p.sb("gbc", [128, D], F32)
    bgs = p.sb("bgs", [128, 2, 512], F32)
    wgs = p.sb("wgs", [16, 2, 512], BF16)
    xt = [p.sb("xt%d" % i, [128, D], F32) for i in range(2)]
    sq = p.sb("sq", [128, D], F32)
    ss = p.sb("ss", [128, 1], F32)
    rstd = p.sb("rstd", [128, 1], F32)
    hn = p.sb("hn", [128, D], BF16)
    hnT = p.sb("hnT", [128, D], BF16)
    gdT = p.sb("gdT", [16, 256], BF16)
    zb = p.sb("zb", [128, 512], F32)
    spl = [p.sb("spl%d" % i, [128, 512], F32) for i in range(2)]
    EqT = p.sb("EqT", [128, 512], F32)
    EkT = p.sb("EkT", [128, 512], F32)
    Ek2 = p.sb("Ek2", [128, 512], F32)
    a_all = p.sb("a_all", [128, 2, NT * GH], F32)
    c_all = p.sb("c_all", [128, 2, NT * GH], F32)
    qp = [p.sb("qp%d" % i, [128, 512], BF16) for i in range(2)]
    kp = [p.sb("kp%d" % i, [128, 512], BF16) for i in range(2)]
    k2 = [p.sb("k2%d" % i, [128, 512], BF16) for i in range(2)]
    vb = p.sb("vb", [128, 1024], BF16)
    srt = p.sb("srt", [128, 1024], F32)
    S = p.sb("S", [128, GH, GDV], F32)
    Sb = p.sb("Sb", [128, GH, GDV], BF16)
    s_qp = [p.sb("s_qp%d" % i, [128, 512], BF16) for i in range(2)]
    s_kp = [p.sb("s_kp%d" % i, [128, 512], BF16) for i in range(2)]
    s_k2 = [p.sb("s_k2%d" % i, [128, 512], BF16) for i in range(2)]
    s_v = [p.sb("s_v%d" % i, [128, 1024], BF16) for i in range(2)]
    AT = [p.sb("AT%d" % i, [128, 128], BF16) for i in range(2)]
    ot = [p.sb("ot%d" % i, [128, 1024], F32) for i in range(2)]
    of = [p.sb("of%d" % i, [128, 1024], F32) for i in range(2)]
    banks = [p.ps("bk%d" % i, [128, 512], F32) for i in range(7)]
    pT = p.ps("pT", [128, 1024], BF16)
    bi = [0]

    rot = [3, 4]

    def bank():
        b = banks[rot[0] + bi[0] % rot[1]]
        bi[0] += 1
        return b

    p.dma("sp", cf[:], cf_d[:])
    p.dma("sp", cb[:], cb_d[:])
    p.dma("sp", gbc[:], g_mix.v(g_mix.t[0:1, :].broadcast_to([128, D])))
    p.dma("sp", bgs[:, 0, :], bg.v(bg.t[0:1, :].broadcast_to([128, 512])))
    p.dma("sp", bgs[:, 1, :], bg.v(bg.t[1:2, :].broadcast_to([128, 512])))
    for d_ in range(2):
        p.dma("pool", wgs.sub(d_, (slice(None), d_, slice(None))), wg[d_])
    for dc in range(8):
        p.dma("pool", W.sub(dc, (slice(None), dc, slice(None))), w_in[dc * 128:(dc + 1) * 128, :])
    Wk = lambda dc, lo, hi: W.sub(dc, (slice(None), dc, slice(lo, hi)))
    UI, LI, US, LS = (cf[:, i, :] for i in range(4))
    scale_q = float(GDK) ** -0.5

    for j in range(NT):
        X = xt[j % 2]
        p.dma("sp", X[:], x[j * 128:(j + 1) * 128, :])
        rmsnorm_tile(p, X[:], gbc[:], hn[:], sq[:], ss[:], rstd[:])
        for dc in range(8):
            p.transpose(pT[:, dc * 128:(dc + 1) * 128], hn[:, dc * 128:(dc + 1) * 128], cb[:])
        p.copy("act", hnT[:], pT[:])
        hT = lambda dc: hnT[:, dc * 128:(dc + 1) * 128]
        bG = bank()
        for d_ in range(2):
            for dc in range(8):
                p.matmul(bG[0:16, d_ * 128:(d_ + 1) * 128], Wk(dc, 3072 + 16 * d_, 3088 + 16 * d_), hT(dc),
                         start=(dc == 0), stop=(dc == 7))
        p.copy("act", gdT[:], bG[0:16, 0:256])
        bq, bk, bkt = banks[0], banks[1], banks[2]
        for n in range(4):
            for dc in range(8):
                p.matmul(bq[:, n * 128:(n + 1) * 128], Wk(dc, n * 128, (n + 1) * 128), hT(dc),
                         start=(dc == 0), stop=(dc == 7))
        for n in range(4):
            for dc in range(8):
                p.matmul(bk[:, n * 128:(n + 1) * 128], Wk(dc, 512 + n * 128, 512 + (n + 1) * 128), hT(dc),
                         start=(dc == 0), stop=(dc == 7))
        for dc in range(8):
            p.matmul(bkt[:], hT(dc), Wk(dc, 512, 1024), start=(dc == 0), stop=(dc == 7))
        for d_ in range(2):
            bz = bank()
            p.matmul(bz[:], gdT[:, d_ * 128:(d_ + 1) * 128], wgs.sub(d_, (slice(None), d_, slice(None))))
            p.tt("dve", zb[:], bz[:], bgs[:, d_, :], ALU.add)
            p.act(zb[:], zb[:], AF.Exp, scale=-1.0)
            sp_ = spl[d_]
            p.act(sp_[:], zb[:], AF.Ln, bias=1.0)
            bc, bd = bank(), bank()
            tri = UI if d_ == 0 else LI
            for h in range(GH):
                p.matmul(bc[:, h * 128:(h + 1) * 128], sp_[:, h * 128:(h + 1) * 128], tri)
            p.matmul(bd[:], LS if d_ == 0 else US, sp_[:])
            p.act(EqT[:], bc[:], AF.Exp, scale=-1.0 / 16)
            p.act(EkT[:], bc[:], AF.Exp, scale=1.0 / 16)
            p.act(Ek2[:], bd[:], AF.Exp, scale=-1.0 / 16)
            last = 127 if d_ == 0 else 0
            p.copy("dve", a_all.v(a_all.t[:, d_, j * GH:(j + 1) * GH]),
                   EqT.v(EqT.t[:].rearrange("p (h t) -> p h t", h=GH)[:, :, last]))
            p.copy("dve", c_all.v(c_all.t[:, d_, j * GH:(j + 1) * GH]),
                   bc.v(bc.t[:].rearrange("p (h t) -> p h t", h=GH)[:, :, last]))
            Q, K, KK = qp[d_], kp[d_], k2[d_]
            p.stt("dve", Q[:], bq[:], scale_q, EqT[:], ALU.mult, ALU.mult)
            p.tt("dve", K[:], bk[:], EkT[:], ALU.mult)
            p.tt("dve", KK[:], bkt[:], Ek2[:], ALU.mult)
            p.dma("pool", QP.v(QP.t[d_, :, :, j * 128:(j + 1) * 128]),
                  Q.v(Q.t[:].rearrange("p (h t) -> p h t", h=GH)))
            p.dma("pool", KP.v(KP.t[d_, :, :, j * 128:(j + 1) * 128]),
                  K.v(K.t[:].rearrange("p (h t) -> p h t", h=GH)))
            p.dma("pool", K2.v(K2.t[d_, j * 128:(j + 1) * 128, :]), KK[:])
        for half in range(2):
            bv = bank()
            for dc in range(8):
                p.matmul(bv[:], hT(dc), Wk(dc, 1024 + half * 512, 1536 + half * 512), start=(dc == 0), stop=(dc == 7))
            p.copy("act", vb[:, half * 512:(half + 1) * 512], bv[:])
        p.dma("pool", VT[j * 128:(j + 1) * 128, :], vb[:])
        for half in range(2):
            br = bank()
            for dc in range(8):
                p.matmul(br[:], hT(dc), Wk(dc, 2048 + half * 512, 2560 + half * 512), start=(dc == 0), stop=(dc == 7))
            p.act(srt[:, half * 512:(half + 1) * 512], br[:], AF.Silu)
        p.dma("pool", SR[j * 128:(j + 1) * 128, :], srt[:])
    for d_ in range(2):
        p.dma("pool", AA[d_], c_all[:, d_, :])

    rot[0], rot[1] = 0, 7
    for d_ in range(2):
        p.memset("dve", S[:], 0.0)
        p.memset("dve", Sb[:], 0.0)
        order = list(range(NT)) if d_ == 0 else list(range(NT - 1, -1, -1))
        mask = UI if d_ == 0 else LS
        for it, j in enumerate(order):
            b = it % 2
            sq_, sk_, sk2_, sv_ = s_qp[b], s_kp[b], s_k2[b], s_v[b]
            p.dma("sp", sq_.v(sq_.t[:].rearrange("p (h t) -> p h t", h=GH)), QP.v(QP.t[d_, :, :, j * 128:(j + 1) * 128]))
            p.dma("sp", sk_.v(sk_.t[:].rearrange("p (h t) -> p h t", h=GH)), KP.v(KP.t[d_, :, :, j * 128:(j + 1) * 128]))
            p.dma("sp", sk2_[:], K2.v(K2.t[d_, j * 128:(j + 1) * 128, :]))
            p.dma("sp", sv_[:], VT[j * 128:(j + 1) * 128, :])
            O = ot[b]
            if d_ == 1:
                p.dma("sp", of[b][:], OL[j * 128:(j + 1) * 128, :])
            for h in range(GH):
                hs = slice(h * 128, (h + 1) * 128)
                vs = slice(h * GDV, (h + 1) * GDV)
                ba = bank()
                p.matmul(ba[:, 0:128], sk_[:, hs], sq_[:, hs])
                A = AT[h % 2]
                p.tt("dve", A[:], ba[:, 0:128], mask, ALU.mult)
                bo = bank()
                p.matmul(bo[:, 0:GDV], A[:], sv_[:, vs], start=True, stop=False)
                p.matmul(bo[:, 0:GDV], sq_[:, hs], Sb[:, h, :], start=False, stop=True)
                bs = bank()
                p.matmul(bs[:, 0:GDV], sk2_[:, hs], sv_[:, vs])
                if d_ == 0:
                    p.copy("act", O[:, vs], bo[:, 0:GDV])
                else:
                    p.tt("dve", O[:, vs], bo[:, 0:GDV], of[b][:, vs], ALU.add)
                p.stt("dve", S[:, h, :], S[:, h, :], a_all.v(a_all.t[:, d_, j * GH + h:j * GH + h + 1]),
                      bs[:, 0:GDV], ALU.mult, ALU.add)
                p.copy("act", Sb[:, h, :], S[:, h, :])
            p.dma("pool", OL[j * 128:(j + 1) * 128, :], O[:])
        for h in range(GH):
            p.dma("pool", BE.v(BE.t[d_, h]), S[:, h, :])
    return p.finish()


def ffn_prep_tile(p, h1, gf, wr, cff, bufs, HN_out, AFF_out, rows, bank):
    sq, ss, rstd, hnf, hnb, hnT, lg, mx, sm = bufs
    rmsnorm_tile(p, h1, gf[:], hnf[:], sq[:], ss[:], rstd[:])
    p.copy("act", hnb[:], hnf[:])
    p.dma("pool", HN_out[rows, :], hnb[:])
    ident_f = cff[:, 4, :]
    for half in range(2):
        bt = bank()
        for q in range(4):
            dc = half * 4 + q
            p.transpose(bt[:, q * 128:(q + 1) * 128], hnf[:, dc * 128:(dc + 1) * 128], ident_f)
        p.copy("act", hnT[:, half * 512:(half + 1) * 512], bt[:])
    bl = bank()
    for dc in range(8):
        p.matmul(bl[:, 0:16], hnT[:, dc * 128:(dc + 1) * 128], wr[:, dc, :], start=(dc == 0), stop=(dc == 7))
    p.reduce("dve", mx[:], bl[:, 0:16], ALU.max)
    p.ts("dve", mx[:], mx[:], -1.0, None, ALU.mult)
    p.act(lg[:], bl[:, 0:16], AF.Exp, bias=mx[:])
    p.reduce("dve", sm[:], lg[:], ALU.add)
    p.recip(sm[:], sm[:])
    p.ts("dve", lg[:], lg[:], sm[:], None, ALU.mult)
    p.dma("pool", AFF_out[rows, :], lg[:])


def ffn_prep_bufs(p):
    return (p.sb("f_sq", [128, D], F32), p.sb("f_ss", [128, 1], F32), p.sb("f_rstd", [128, 1], F32),
            p.sb("f_hnf", [128, D], F32), p.sb("f_hnb", [128, D], BF16), p.sb("f_hnT", [128, D], F32),
            p.sb("f_lg", [128, 16], F32), p.sb("f_mx", [128, 1], F32), p.sb("f_sm", [128, 1], F32))


def build_L2():
    nc = bass.Bass("TRN2", target_bir_lowering=False)
    p = Prog(nc)
    x = p.dram("x", [TOK, D], F32, "ExternalInput")
    OL = p.dram("OL", [TOK, 1024], F32, "ExternalInput")
    QP = p.dram("QP", [2, 128, GH, TOK], BF16, "ExternalInput")
    SR = p.dram("SR", [TOK, 1024], F32, "ExternalInput")
    AA = p.dram("AA", [2, 128, NT * GH], F32, "ExternalInput")
    BP = p.dram("BP", [2, 7, GH, 128, GDV], F32, "ExternalInput")
    APD = p.dram("APD", [2, 7, 128, NT * GH], F32, "ExternalInput")
    g_head = p.dram("g_head", [1, GDV], F32, "ExternalInput")
    w_out = p.dram("w_out", [D, D], F32, "ExternalInput")
    g_ffn = p.dram("g_ffn", [1, D], F32, "ExternalInput")
    w_r = p.dram("w_r", [D, 16], F32, "ExternalInput")
    cf_d = p.dram("cf", [128, 5, 128], F32, "ExternalInput")
    cb_d = p.dram("cb", [128, 128], BF16, "ExternalInput")
    H1 = p.dram("H1", [TOK, D], F32, "ExternalOutput")
    HN1 = p.dram("HN1", [TOK, D], BF16, "ExternalOutput")
    AFF = p.dram("AFF", [TOK, 16], F32, "ExternalOutput")

    Wo = p.sb("Wo", [128, 8, D], BF16)
    wr = p.sb("wr", [128, 8, 16], F32)
    cf = p.sb("cfs", [128, 5, 128], F32)
    cb = p.sb("cbs", [128, 128], BF16)
    gf = p.sb("gf", [128, D], F32)
    gh = p.sb("gh", [128, D], F32)
    apd = p.sb("apd", [128, 2, 7, NT * GH], F32)
    csum = p.sb("csum", [128, 2, 7, GH], F32)
    aown = p.sb("aown", [128, 2, NT * GH], F32)
    Sin = p.sb("Sin", [128, 2, GH, GDV], F32)
    Bt = [p.sb("Bt%d" % i, [128, GH, GDV], F32) for i in range(2)]
    Sbw = p.sb("Sbw", [128, NT, GH, GDV], BF16)
    Sfb = p.sb("Sfb", [128, GH, GDV], BF16)
    xt = [p.sb("xt%d" % i, [128, D], F32) for i in range(2)]
    olt = [p.sb("olt%d" % i, [128, D], F32) for i in range(2)]
    srt = [p.sb("srt%d" % i, [128, D], F32) for i in range(2)]
    qf = [p.sb("qf%d" % i, [128, 512], BF16) for i in range(2)]
    qb = [p.sb("qb%d" % i, [128, 512], BF16) for i in range(2)]
    o = p.sb("o", [128, D], F32)
    osq = p.sb("osq", [128, D], F32)
    hss = p.sb("hss", [128, GH], F32)
    y = p.sb("y", [128, D], BF16)
    yT = p.sb("yT", [128, D], BF16)
    h1 = p.sb("h1", [128, D], F32)
    fb = ffn_prep_bufs(p)
    banks = [p.ps("bk%d" % i, [128, 512], F32) for i in range(7)]
    pT = p.ps("pT", [128, 1024], BF16)
    bi = [0]

    def bank():
        b = banks[bi[0] % 7]
        bi[0] += 1
        return b

    p.dma("sp", cf[:], cf_d[:])
    p.dma("sp", cb[:], cb_d[:])
    p.dma("sp", gf[:], g_ffn.v(g_ffn.t[0:1, :].broadcast_to([128, D])))
    for h in range(GH):
        p.dma("sp", gh[:, h * GDV:(h + 1) * GDV], g_head.v(g_head.t[0:1, :].broadcast_to([128, GDV])))
    p.dma("sp", wr[:], w_r.v(w_r.t.rearrange("(c p) e -> p c e", p=128)))
    for dc in range(8):
        p.dma("pool", Wo.sub(dc, (slice(None), dc, slice(None))), w_out[dc * 128:(dc + 1) * 128, :])
    for d_ in range(2):
        p.dma("sp", apd.v(apd.t[:, d_]), APD.v(APD.t[d_].rearrange("i p c -> p i c")))
        p.dma("sp", aown.v(aown.t[:, d_, :]), AA[d_])
    p.reduce("dve", csum[:], apd.v(apd.t[:].rearrange("p d i (t h) -> p d i h t", h=GH)), ALU.add)
    p.act(csum[:], csum[:], AF.Exp, scale=-1.0 / 16)
    p.act(aown[:], aown[:], AF.Exp, scale=-1.0 / 16)
    p.memset("dve", Sin[:], 0.0)
    n = 0
    for d_ in range(2):
        for i in range(7):
            B = Bt[n % 2]
            n += 1
            p.dma("sp", B[:], BP.v(BP.t[d_, i].rearrange("h p v -> p h v")))
            for h in range(GH):
                p.stt("dve", Sin[:, d_, h, :], Sin[:, d_, h, :], csum.v(csum.t[:, d_, i, h:h + 1]), B[:, h, :],
                      ALU.mult, ALU.add)
    for j in range(NT - 1, -1, -1):
        p.copy("act", Sbw[:, j], Sin[:, 1])
        for h in range(GH):
            p.ts("dve", Sin[:, 1, h, :], Sin[:, 1, h, :], aown.v(aown.t[:, 1, j * GH + h:j * GH + h + 1]), None, ALU.mult)
    for j in range(NT):
        b = j % 2
        rows = slice(j * 128, (j + 1) * 128)
        p.dma("sp", xt[b][:], x[rows, :])
        p.dma("sp", olt[b][:], OL[rows, :])
        p.dma("sp", srt[b][:], SR[rows, :])
        p.dma("sp", qf[b].v(qf[b].t[:].rearrange("p (h t) -> p h t", h=GH)), QP.v(QP.t[0, :, :, rows]))
        p.dma("sp", qb[b].v(qb[b].t[:].rearrange("p (h t) -> p h t", h=GH)), QP.v(QP.t[1, :, :, rows]))
        p.copy("act", Sfb[:], Sin[:, 0])
        for h in range(GH):
            hs = slice(h * 128, (h + 1) * 128)
            vs = slice(h * GDV, (h + 1) * GDV)
            bo = bank()
            p.matmul(bo[:, 0:GDV], qf[b][:, hs], Sfb[:, h, :], start=True, stop=False)
            p.matmul(bo[:, 0:GDV], qb[b][:, hs], Sbw[:, j, h, :], start=False, stop=True)
            p.tt("dve", o[:, vs], bo[:, 0:GDV], olt[b][:, vs], ALU.add)
            p.ts("dve", Sin[:, 0, h, :], Sin[:, 0, h, :], aown.v(aown.t[:, 0, j * GH + h:j * GH + h + 1]), None, ALU.mult)
        p.tt("dve", osq[:], o[:], o[:], ALU.mult)
        p.reduce("dve", hss[:], osq.v(osq.t[:].rearrange("p (h v) -> p h v", h=GH)), ALU.add)
        p.ts("dve", hss[:], hss[:], 1.0 / GDV, RMS_EPS, ALU.mult, ALU.add)
        p.act(hss[:], hss[:], AF.Sqrt)
        p.recip(hss[:], hss[:])
        p.tt("dve", osq[:], srt[b][:], gh[:], ALU.mult)
        for h in range(GH):
            vs = slice(h * GDV, (h + 1) * GDV)
            p.stt("dve", y[:, vs], o[:, vs], hss[:, h:h + 1], osq[:, vs], ALU.mult, ALU.mult)
        for dc in range(8):
            p.transpose(pT[:, dc * 128:(dc + 1) * 128], y[:, dc * 128:(dc + 1) * 128], cb[:])
        p.copy("act", yT[:], pT[:])
        for half in range(2):
            bm = bank()
            for dc in range(8):
                p.matmul(bm[:], yT[:, dc * 128:(dc + 1) * 128],
                         Wo.sub(dc, (slice(None), dc, slice(half * 512, (half + 1) * 512))),
                         start=(dc == 0), stop=(dc == 7))
            p.tt("dve", h1[:, half * 512:(half + 1) * 512], bm[:], xt[b][:, half * 512:(half + 1) * 512], ALU.add)
        p.dma("pool", H1[rows, :], h1[:])
        ffn_prep_tile(p, h1[:], gf, wr, cf, fb, HN1, AFF, rows, bank)
    return p.finish()


NE = 16
CAP = 2 * SEQ // NE
FF = 2048
BISECT_ITERS = 36


def build_L3():
    nc = bass.Bass("TRN2", target_bir_lowering=False)
    p = Prog(nc)
    AFFE = p.dram("AFFE", [2, 128, 128], F32, "ExternalInput")
    HN = p.dram("HN", [SEQ, D], BF16, "ExternalInput")
    wg = p.dram("wg", [2, D, FF], F32, "ExternalInput")
    wu = p.dram("wu", [2, D, FF], F32, "ExternalInput")
    wd = p.dram("wd", [2, FF, D], F32, "ExternalInput")
    cf_d = p.dram("cf", [128, 5, 128], F32, "ExternalInput")
    cb_d = p.dram("cb", [128, 128], BF16, "ExternalInput")
    tok_d = p.dram("tokid", [128, 128], I32, "ExternalInput")
    DELTA = p.dram("DELTA", [SEQ, D], F32, "ExternalOutput")
    IDXA = [p.dram("IDXA%d" % i, [CAP, 2], I32, "Internal") for i in range(2)]

    Wg = p.sb("Wg", [128, 8, FF], BF16)
    Wu = p.sb("Wu", [128, 8, FF], BF16)
    Wd = p.sb("Wd", [128, 16, D], BF16)
    cf = p.sb("cfs", [128, 5, 128], F32)
    cb = p.sb("cbs", [128, 128], BF16)
    tokid = p.sb("tokid_s", [128, 128], I32)
    zt = p.sb("zt", [128, 4096], F32)
    ones = p.sb("ones", [128, 128], F32)
    aff = p.sb("aff", [128, 2, 128], F32)
    cmp_ = p.sb("cmp", [128, 2, 128], F32)
    st = {n: p.sb("b_" + n, [128, 2], F32) for n in ("lo", "hi", "mid", "cnt", "ge", "nge", "t1", "t2")}
    selT = p.sb("selT", [128, 128], F32)
    rp = p.sb("rp", [128, 1], F32)
    slot = p.sb("slot", [128, 128], F32)
    pen = p.sb("pen", [128, 128], F32)
    slot_i = p.sb("slot_i", [128, 128], I32)
    pk = p.sb("pk", [128, 128, 2], I32)
    idx_sb = p.sb("idx_sb", [128, 2, 16, 2], I32)
    xs = [p.sb("xs%d" % i, [128, D], BF16) for i in range(2)]
    xsT = p.sb("xsT", [128, 8, 512], BF16)
    hT = p.sb("hT", [128, 16, 512], BF16)
    sg = [p.sb("sg%d" % i, [128, 512], F32) for i in range(2)]
    y = [p.sb("y%d" % i, [128, D], F32) for i in range(2)]
    banks = [p.ps("bk%d" % i, [128, 512], F32) for i in range(7)]
    pT = p.ps("pT", [128, 1024], BF16)
    bi = [0]

    def bank():
        b = banks[bi[0] % 7]
        bi[0] += 1
        return b

    p.dma("sp", cf[:], cf_d[:])
    p.dma("sp", cb[:], cb_d[:])
    p.dma("sp", tokid[:], tok_d[:])
    p.dma("sp", aff[:], AFFE.v(AFFE.t.rearrange("e p f -> p e f")))
    p.memset("pool", zt[:], 0.0)
    p.memset("dve", ones[:], 1.0)
    zkeys = []
    dz = DELTA.t.rearrange("(k p r) d -> k p (r d)", p=128, r=4)
    for k in range(SEQ // 512):
        zk = ("DELTA", "z", k)
        zkeys.append(zk)
        p.dma("sp", V(dz[k], zk), zt[:])
    US = cf[:, 2, :]
    ident_f = cf[:, 4, :]

    def load_weights(e):
        for dc in range(8):
            p.dma("pool", Wg.sub(dc, (slice(None), dc, slice(None))), V(wg.t[e, dc * 128:(dc + 1) * 128, :], ("wg", e)))
            p.dma("pool", Wu.sub(dc, (slice(None), dc, slice(None))), V(wu.t[e, dc * 128:(dc + 1) * 128, :], ("wu", e)))
        for fc in range(16):
            p.dma("pool", Wd.sub(fc, (slice(None), fc, slice(None))), V(wd.t[e, fc * 128:(fc + 1) * 128, :], ("wd", e)))

    load_weights(0)
    lo, hi, mid, cnt, ge, nge, t1, t2 = (st[n] for n in ("lo", "hi", "mid", "cnt", "ge", "nge", "t1", "t2"))
    p.memset("dve", lo[:], 0.0)
    p.memset("dve", hi[:], 1.0)
    for it in range(BISECT_ITERS):
        p.tt("dve", mid[:], lo[:], hi[:], ALU.add)
        p.ts("dve", mid[:], mid[:], 0.5, None, ALU.mult)
        for e in range(2):
            p.ts("dve", cmp_[:, e, :], aff[:, e, :], mid[:, e:e + 1], None, ALU.is_ge)
        p.reduce("dve", cnt[:], cmp_[:], ALU.add)
        bt = bank()
        p.matmul(bt[:, 0:2], ones[:], cnt[:])
        p.ts("dve", ge[:], bt[:, 0:2], float(CAP) - 0.5, None, ALU.is_ge)
        p.ts("dve", nge[:], ge[:], -1.0, 1.0, ALU.mult, ALU.add)
        p.tt("dve", lo[:], lo[:], nge[:], ALU.mult)
        p.tt("dve", t1[:], mid[:], ge[:], ALU.mult)
        p.tt("dve", lo[:], lo[:], t1[:], ALU.add)
        p.tt("dve", hi[:], hi[:], ge[:], ALU.mult)
        p.tt("dve", t2[:], mid[:], nge[:], ALU.mult)
        p.tt("dve", hi[:], hi[:], t2[:], ALU.add)
    for e in range(2):
        p.ts("dve", cmp_[:, e, :], aff[:, e, :], lo[:, e:e + 1], None, ALU.is_ge)
    p.reduce("dve", cnt[:], cmp_[:], ALU.add)
    pkf = pk.t[:].bitcast(F32)
    for e in range(2):
        bt = bank()
        p.transpose(bt[:, 0:128], cmp_[:, e, :], ident_f)
        p.copy("act", selT[:], bt[:, 0:128])
        bp = bank()
        p.matmul(bp[:, 0:128], selT[:], US)
        p.matmul(bp[:, 128:129], US, cnt[:, e:e + 1])
        p.copy("act", rp[:], bp[:, 128:129])
        p.ts("dve", slot[:], bp[:, 0:128], rp[:, 0:1], None, ALU.add)
        p.ts("dve", pen[:], cmp_[:, e, :], -4096.0, 4096.0, ALU.mult, ALU.add)
        p.tt("dve", slot[:], slot[:], pen[:], ALU.add)
        p.copy("dve", slot_i[:], slot[:])
        p.copy("dve", pk[:, :, 0], tokid[:])
        p.copy("dve", pk.v(pkf[:, :, 1]), aff[:, e, :])
        skeys = []
        for f in range(128):
            sk = ("IDXA", e, f)
            skeys.append(sk)
            p.scatter(V(IDXA[e].t, sk), pk[:, f, :], slot_i[:, f:f + 1], CAP, writes=[sk])
        p.dma("sp", idx_sb[:, e], V(IDXA[e].t.rearrange("(k p) c -> p k c", p=128), ("IDXA", e, "all")), reads=skeys)
    wts = idx_sb.t[:].bitcast(F32)
    first_scatter = True
    for e in range(2):
        if e > 0:
            load_weights(e)
        for tg in range(4):
            for kk in range(4):
                k = tg * 4 + kk
                X = xs[kk % 2]
                p.gather(X[:], HN[:, :], idx_sb[:, e, k, 0:1], SEQ)
                for dc in range(8):
                    p.transpose(pT[:, dc * 128:(dc + 1) * 128], X[:, dc * 128:(dc + 1) * 128], cb[:])
                p.copy("act", xsT[:, :, kk * 128:(kk + 1) * 128], pT.v(pT.t[:].rearrange("p (c t) -> p c t", c=8)))
            for fc in range(16):
                bg_, bu_ = bank(), bank()
                for dc in range(8):
                    p.matmul(bg_[:], Wg.sub(dc, (slice(None), dc, slice(fc * 128, (fc + 1) * 128))), xsT[:, dc, :],
                             start=(dc == 0), stop=(dc == 7))
                for dc in range(8):
                    p.matmul(bu_[:], Wu.sub(dc, (slice(None), dc, slice(fc * 128, (fc + 1) * 128))), xsT[:, dc, :],
                             start=(dc == 0), stop=(dc == 7))
                G = sg[fc % 2]
                p.act(G[:], bg_[:], AF.Silu)
                p.tt("dve", hT[:, fc, :], G[:], bu_[:], ALU.mult)
            for kk in range(4):
                k = tg * 4 + kk
                Y = y[kk % 2]
                for half in range(2):
                    by = bank()
                    for fc in range(16):
                        p.matmul(by[:], hT[:, fc, kk * 128:(kk + 1) * 128],
                                 Wd.sub(fc, (slice(None), fc, slice(half * 512, (half + 1) * 512))),
                                 start=(fc == 0), stop=(fc == 15))
                    p.ts("dve", Y[:, half * 512:(half + 1) * 512], by[:], idx_sb.v(wts[:, e, k, 1:2]), None, ALU.mult)
                p.scatter(DELTA[:, :], Y[:], idx_sb[:, e, k, 0:1], SEQ, accum=True,
                          reads=zkeys if first_scatter else None)
                first_scatter = False
    return p.finish()


MH = 16
QRANK = 256
KVR = 128
NOPE = 128
ROPE = 64
MLA_SCALE = float(NOPE + ROPE) ** -0.5
TWO_PI = 2.0 * np.pi


def rope_consts():
    half = ROPE // 2
    inv = (10000.0 ** (-np.arange(half, dtype=np.float32) / half)).astype(np.float32)
    rc = np.zeros((64, 4), np.float32)
    rc[:, 0] = np.concatenate([inv, inv])
    rc[:, 1] = np.concatenate([np.ones(half), -np.ones(half)])
    rc[:, 2] = -np.pi
    return rc


def build_L4():
    nc = bass.Bass("TRN2", target_bir_lowering=False)
    p = Prog(nc)
    H1 = p.dram("H1", [TOK, D], F32, "ExternalInput")
    DS = p.dram("DS", [NCORES, TOK, D], F32, "ExternalInput")
    pos = p.dram("pos", [1, TOK], I32, "ExternalInput")
    g_mix = p.dram("g_mix", [1, D], F32, "ExternalInput")
    w_m = p.dram("w_m", [D, 448], F32, "ExternalInput")
    g_q = p.dram("g_q", [1, QRANK], F32, "ExternalInput")
    g_kv = p.dram("g_kv", [1, KVR], F32, "ExternalInput")
    w_uq = p.dram("w_uq", [QRANK, MH * 192], F32, "ExternalInput")
    w_ukv = p.dram("w_ukv", [KVR, MH * 256], F32, "ExternalInput")
    rc_d = p.dram("rc", [64, 4], F32, "ExternalInput")
    cf_d = p.dram("cf", [128, 5, 128], F32, "ExternalInput")
    cb_d = p.dram("cb", [128, 128], BF16, "ExternalInput")
    H2 = p.dram("H2", [TOK, D], F32, "ExternalOutput")
    QL = p.dram("QL", [MH, 128, TOK], BF16, "ExternalOutput")
    QR = p.dram("QR", [MH, 64, TOK], BF16, "ExternalOutput")
    CKVT = p.dram("CKVT", [128, TOK], BF16, "ExternalOutput")
    KRT = p.dram("KRT", [64, TOK], BF16, "ExternalOutput")
    CKV = p.dram("CKV", [TOK, 128], BF16, "ExternalOutput")
    KN2 = p.dram("KN2", [TOK, 1], F32, "ExternalOutput")
    QMAX = p.dram("QMAX", [1, MH], F32, "ExternalOutput")

    cf = p.sb("cfs", [128, 5, 128], F32)
    cb = p.sb("cbs", [128, 128], BF16)
    rc = p.sb("rcs", [64, 4], F32)
    gm = p.sb("gm", [128, D], F32)
    gq = p.sb("gq", [128, QRANK], F32)
    gkv = p.sb("gkv", [128, KVR], F32)
    Wm = p.sb("Wm", [128, 8, 448], BF16)
    Wuq = p.sb("Wuq", [128, 2, MH * 192], BF16)
    Wukv = p.sb("Wukv", [128, MH * 256], BF16)
    WukT = p.sb("WukT", [128, MH, 128], BF16)
    posi = p.sb("posi", [64, TOK], I32)
    ang = p.sb("ang", [64, TOK], F32)
    C2 = p.sb("C2", [64, TOK], F32)
    S2 = p.sb("S2", [64, TOK], F32)
    hnT = p.sb("hnT", [128, 8, TOK], BF16)
    cqT = p.sb("cqT", [128, 2, TOK], BF16)
    ht = [p.sb("ht%d" % i, [128, D], F32) for i in range(2)]
    dt_ = [p.sb("dt%d" % i, [128, D], F32) for i in range(3)]
    sq = p.sb("sq", [128, D], F32)
    ss = p.sb("ss", [128, 1], F32)
    rstd = p.sb("rstd", [128, 1], F32)
    hn = p.sb("hn", [128, D], BF16)
    cs = p.sb("cs", [128, 448], F32)
    cn = p.sb("cn", [128, 384], BF16)
    kn = p.sb("kn", [128, 2], F32)
    qn = p.sb("qn", [128, 512], BF16)
    qlf = p.sb("qlf", [128, 512], F32)
    qlb = p.sb("qlb", [128, 512], BF16)
    qsq = p.sb("qsq", [128, 512], F32)
    r1 = p.sb("r1", [64, 512], F32)
    r2 = p.sb("r2", [64, 512], F32)
    rb = p.sb("rb", [64, 512], BF16)
    qmx = p.sb("qmx", [1, MH], F32)
    qm1 = p.sb("qm1", [1, 1], F32)
    ckT = p.sb("ckT", [128, 128], BF16)
    banks = [p.ps("bk%d" % i, [128, 512], F32) for i in range(7)]
    pT = p.ps("pT", [128, 1024], BF16)
    bi = [0]

    def bank():
        b = banks[bi[0] % 7]
        bi[0] += 1
        return b

    p.dma("sp", cf[:], cf_d[:])
    p.dma("sp", cb[:], cb_d[:])
    p.dma("sp", rc[:], rc_d[:])
    p.dma("sp", gm[:], g_mix.v(g_mix.t[0:1, :].broadcast_to([128, D])))
    p.dma("sp", gq[:], g_q.v(g_q.t[0:1, :].broadcast_to([128, QRANK])))
    p.dma("sp", gkv[:], g_kv.v(g_kv.t[0:1, :].broadcast_to([128, KVR])))
    p.dma("sp", posi[:], pos.v(pos.t[0:1, :].broadcast_to([64, TOK])))
    p.dma("pool", Wm[:], w_m.v(w_m.t.rearrange("(c p) n -> p c n", p=128)))
    p.dma("pool", Wuq[:], w_uq.v(w_uq.t.rearrange("(c p) n -> p c n", p=128)))
    p.dma("pool", Wukv[:], w_ukv[:, :])
    onesf = p.sb("onesf", [128, 1], F32)
    p.memset("dve", onesf[:], 1.0)
    p.memset("dve", qmx[:], 0.0)
    Wm_sw = p.sb("Wm_sw", [128, 8, 64], BF16)
    Wuq_sw = p.sb("Wuq_sw", [128, 2, MH, 64], BF16)
    p.copy("dve", Wm_sw[:, :, 0:32], Wm[:, :, 416:448])
    p.copy("dve", Wm_sw[:, :, 32:64], Wm[:, :, 384:416])
    for kc in range(2):
        wv = Wuq.t[:, kc, :].rearrange("p (h c) -> p h c", h=MH)
        p.copy("dve", Wuq_sw[:, kc, :, 0:32], Wuq.v(wv[:, :, 160:192]))
        p.copy("dve", Wuq_sw[:, kc, :, 32:64], Wuq.v(wv[:, :, 128:160]))
    for h in range(MH):
        p.transpose(pT[:, (h % 8) * 128:(h % 8 + 1) * 128], Wukv[:, h * 256:h * 256 + 128], cb[:])
        if h % 8 == 7:
            g0 = h - 7
            p.copy("act", WukT[:, g0:g0 + 8, :], pT.v(pT.t[:].rearrange("p (h r) -> p h r", h=8)))
    p.copy("dve", ang[:], posi[:])
    p.ts("dve", ang[:], ang[:], rc[:, 0:1], None, ALU.mult)
    C1 = 6.28125
    C2_ = float(TWO_PI - 6.28125)
    ki = p.sb("ki", [64, TOK], I32)
    kf = p.sb("kf", [64, TOK], F32)
    gt = p.sb("gt", [64, TOK], F32)
    p.ts("dve", kf[:], ang[:], float(1.0 / TWO_PI), None, ALU.mult)
    p.copy("dve", ki[:], kf[:])
    p.copy("dve", kf[:], ki[:])
    p.stt("dve", ang[:], kf[:], -C1, ang[:], ALU.mult, ALU.add)
    p.stt("dve", ang[:], kf[:], -C2_, ang[:], ALU.mult, ALU.add)

    def fold(t):
        p.ts("dve", gt[:], t[:], float(np.pi), None, ALU.is_gt)
        p.stt("dve", t[:], gt[:], -TWO_PI, t[:], ALU.mult, ALU.add)
        p.ts("dve", gt[:], t[:], float(-np.pi), None, ALU.is_lt)
        p.stt("dve", t[:], gt[:], TWO_PI, t[:], ALU.mult, ALU.add)
        p.ts("dve", t[:], t[:], float(np.pi), float(-np.pi), ALU.min, ALU.max)

    fold(ang)
    p.ts("dve", C2[:], ang[:], float(np.pi / 2), None, ALU.add)
    fold(C2)
    p.act(S2[:], ang[:], AF.Sin)
    p.ts("dve", S2[:], S2[:], rc[:, 1:2], -1.0, ALU.mult, ALU.mult)
    p.act(C2[:], C2[:], AF.Sin)

    for j in range(NT):
        rows = slice(j * 128, (j + 1) * 128)
        Ht = ht[j % 2]
        p.dma("sp", Ht[:], H1[rows, :])
        for c in range(NCORES):
            Dt = dt_[c % 3]
            p.dma("sp", Dt[:], DS.v(DS.t[c, rows, :]))
            p.tt("dve", Ht[:], Ht[:], Dt[:], ALU.add)
        p.dma("pool", H2[rows, :], Ht[:])
        rmsnorm_tile(p, Ht[:], gm[:], hn[:], sq[:], ss[:], rstd[:])
        for dc in range(8):
            p.transpose(pT[:, dc * 128:(dc + 1) * 128], hn[:, dc * 128:(dc + 1) * 128], cb[:])
        p.copy("act", hnT[:, :, rows], pT.v(pT.t[:].rearrange("p (c t) -> p c t", c=8)))
        bc_ = bank()
        for dc in range(8):
            p.matmul(bc_[:, 0:448], hnT[:, dc, rows], Wm[:, dc, :], start=(dc == 0), stop=(dc == 7))
        p.copy("act", cs[:], bc_[:, 0:448])
        rmsnorm_tile(p, cs[:, 0:256], gq[:], cn[:, 0:256], sq[:, 0:256], ss[:], rstd[:], width=QRANK)
        rmsnorm_tile(p, cs[:, 256:384], gkv[:], cn[:, 256:384], sq[:, 0:128], ss[:], rstd[:], width=KVR)
        p.dma("pool", CKV[rows, :], cn[:, 256:384])
        p.tt("dve", sq[:, 0:128], cn[:, 256:384], cn[:, 256:384], ALU.mult)
        p.reduce("dve", kn[:, 0:1], sq[:, 0:128], ALU.add)
        p.tt("dve", sq[:, 0:64], cs[:, 384:448], cs[:, 384:448], ALU.mult)
        p.reduce("dve", kn[:, 1:2], sq[:, 0:64], ALU.add)
        p.tt("dve", kn[:, 0:1], kn[:, 0:1], kn[:, 1:2], ALU.add)
        p.dma("pool", KN2[rows, :], kn[:, 0:1])
        for q in range(3):
            p.transpose(pT[:, q * 128:(q + 1) * 128], cn[:, q * 128:(q + 1) * 128], cb[:])
        p.copy("act", cqT[:, :, rows], pT.v(pT.t[:, 0:256].rearrange("p (c t) -> p c t", c=2)))
        p.copy("act", ckT[:], pT[:, 256:384])
        p.dma("pool", CKVT[:, rows], ckT[:])

    for tg in range(TOK // 512):
        cols = slice(tg * 512, (tg + 1) * 512)
        bk_, bks = bank(), bank()
        for dc in range(8):
            p.matmul(bk_[0:64, :], Wm[:, dc, 384:448], hnT[:, dc, cols], start=(dc == 0), stop=(dc == 7))
        for dc in range(8):
            p.matmul(bks[0:64, :], Wm_sw[:, dc, :], hnT[:, dc, cols], start=(dc == 0), stop=(dc == 7))
        p.tt("dve", r1[:], bk_[0:64, :], C2[:, cols], ALU.mult)
        p.tt("dve", r2[:], bks[0:64, :], S2[:, cols], ALU.mult)
        p.tt("dve", rb[:], r1[:], r2[:], ALU.add)
        p.dma("pool", KRT[:, cols], rb[:])
        for h in range(MH):
            o = h * 192
            bq = bank()
            for kc in range(2):
                p.matmul(bq[:], Wuq[:, kc, o:o + 128], cqT[:, kc, cols], start=(kc == 0), stop=(kc == 1))
            p.copy("act", qn[:], bq[:])
            bl = bank()
            p.matmul(bl[:], WukT[:, h, :], qn[:])
            p.ts("dve", qlf[:], bl[:], MLA_SCALE, None, ALU.mult)
            p.copy("act", qlb[:], qlf[:])
            p.dma("pool", QL.v(QL.t[h, :, cols]), qlb[:])
            br, brs = bank(), bank()
            for kc in range(2):
                p.matmul(br[0:64, :], Wuq[:, kc, o + 128:o + 192], cqT[:, kc, cols], start=(kc == 0), stop=(kc == 1))
            for kc in range(2):
                p.matmul(brs[0:64, :], Wuq_sw[:, kc, h, :], cqT[:, kc, cols], start=(kc == 0), stop=(kc == 1))
            p.tt("dve", r1[:], br[0:64, :], C2[:, cols], ALU.mult)
            p.tt("dve", r2[:], brs[0:64, :], S2[:, cols], ALU.mult)
            p.tt("dve", r1[:], r1[:], r2[:], ALU.add)
            p.ts("dve", r1[:], r1[:], MLA_SCALE, None, ALU.mult)
            p.copy("act", rb[:], r1[:])
            p.dma("pool", QR.v(QR.t[h, :, cols]), rb[:])
            p.tt("dve", qsq[:], qlf[:], qlf[:], ALU.mult)
            p.tt("dve", r2[:], r1[:], r1[:], ALU.mult)
            bn = bank()
            p.matmul(bn[0:1, :], onesf[:, 0:1], qsq[:], start=True, stop=False)
            p.matmul(bn[0:1, :], onesf[0:64, 0:1], r2[:], start=False, stop=True)
            p.reduce("dve", qm1[:], bn[0:1, :], ALU.max)
            p.tt("dve", qmx[:, h:h + 1], qmx[:, h:h + 1], qm1[:], ALU.max)
    p.dma("pool", QMAX[:, :], qmx[:])
    return p.finish()


NKT = SEQ // 128


def build_L5(nheads=None, same=True):
    nc = bass.Bass("TRN2", target_bir_lowering=False)
    p = Prog(nc)
    QL = p.dram("QL", [MH, 128, TOK], BF16, "ExternalInput")
    QR = p.dram("QR", [MH, 64, TOK], BF16, "ExternalInput")
    p.same = same
    KTd = p.dram("KT", [128, SEQ], BF16, "ExternalInput")
    KRd = p.dram("KR", [64, SEQ], BF16, "ExternalInput")
    Vd = p.dram("VK", [SEQ, 128], BF16, "ExternalInput")
    KN2 = p.dram("KN2", [128, 128], F32, "ExternalInput")
    QMAX = p.dram("QMAX", [1, MH], F32, "ExternalInput")
    H2 = p.dram("H2", [TOK, D], F32, "ExternalInput")
    w_ukv = p.dram("w_ukv", [KVR, MH * 256], F32, "ExternalInput")
    w_o = p.dram("w_o", [MH * 128, D], F32, "ExternalInput")
    g_ffn = p.dram("g_ffn", [1, D], F32, "ExternalInput")
    w_r = p.dram("w_r", [D, 16], F32, "ExternalInput")
    cf_d = p.dram("cf", [128, 5, 128], F32, "ExternalInput")
    cb_d = p.dram("cb", [128, 128], BF16, "ExternalInput")
    H3 = p.dram("H3", [TOK, D], F32, "ExternalOutput")
    HN3 = p.dram("HN3", [TOK, D], BF16, "ExternalOutput")
    AFF = p.dram("AFF", [TOK, 16], F32, "ExternalOutput")
    OTD = p.dram("OTD", [MH, 128, TOK], BF16, "Internal")

    KT = p.sb("KTs", [128, SEQ], BF16)
    KR2 = p.sb("KR2", [128, SEQ // 2], BF16)
    Vt = p.sb("Vt", [128, NKT, 128], BF16)
    Wo = p.sb("Wo", [128, MH, D], BF16)
    Wuv = p.sb("Wuv", [128, MH, 128], BF16)
    wr = p.sb("wr", [128, 8, 16], F32)
    cf = p.sb("cfs", [128, 5, 128], F32)
    cb = p.sb("cbs", [128, 128], BF16)
    gf = p.sb("gf", [128, D], F32)
    onesf = p.sb("onesf", [128, 128], F32)
    kn = p.sb("kn", [128, 128], F32)
    km = p.sb("km", [128, 1], F32)
    km1 = p.sb("km1", [1, 1], F32)
    qmx = p.sb("qmx", [1, MH], F32)
    negm = p.sb("negm", [128, MH], F32)
    QW = 1024
    sbk = [p.ps("sbk%d" % i, [128, QW], F32) for i in range(2)]
    obk = p.ps("obk", [128, QW], F32)
    ebk = [p.ps("ebk%d" % i, [128, 512], F32) for i in range(2)]
    mi = [0]

    def bank():
        b = ebk[mi[0] % 2]
        mi[0] += 1
        return b

    p.dma("sp", cf[:], cf_d[:])
    p.dma("sp", cb[:], cb_d[:])
    p.dma("sp", kn[:], KN2[:, :])
    p.dma("sp", qmx[:], QMAX[:, :])
    p.dma("sp", gf[:], g_ffn.v(g_ffn.t[0:1, :].broadcast_to([128, D])))
    p.dma("sp", wr[:], w_r.v(w_r.t.rearrange("(c p) e -> p c e", p=128)))
    for q in range(8):
        cs_ = slice(q * 2048, (q + 1) * 2048)
        p.dma("sp", KT.sub(q, (slice(None), cs_)), V(KTd.t[:, cs_], ("KTd", q)))
    for half in range(2):
        for q in range(4):
            cs_ = slice(q * 2048, (q + 1) * 2048)
            p.dma("sp", KR2.sub((half, q), (slice(half * 64, (half + 1) * 64), cs_)),
                  V(KRd.t[:, half * 8192 + q * 2048: half * 8192 + (q + 1) * 2048], ("KRd", half, q)))
    for q in range(8):
        p.dma("sp", Vt.sub(q, (slice(None), slice(q * 16, (q + 1) * 16), slice(None))),
              V(Vd.t[q * 2048:(q + 1) * 2048, :].rearrange("(k p) r -> p k r", p=128), ("Vd", q)))
    p.dma("pool", Wuv[:], w_ukv.v(w_ukv.t.rearrange("r (h c) -> r h c", h=MH)[:, :, 128:256]))
    for h in range(MH):
        p.dma("pool", Wo.sub(h, (slice(None), h, slice(None))), V(w_o.t[h * 128:(h + 1) * 128, :], ("w_o", h)))
    p.memset("dve", onesf[:], 1.0)
    p.reduce("dve", km[:], kn[:], ALU.max)
    bt = bank()
    p.transpose(bt[0:1, 0:128], km[:, 0:1], cf[:, 4, :])
    p.reduce("dve", km1[:], bt[0:1, 0:128], ALU.max)
    p.ts("dve", qmx[:], qmx[:], km1[0:1, 0:1], None, ALU.mult)
    p.act(qmx[:], qmx[:], AF.Sqrt)
    p.ts("dve", qmx[:], qmx[:], -1.0, None, ALU.mult)
    bb = bank()
    p.matmul(bb[:, 0:MH], onesf[0:1, :], qmx[:])
    p.copy("act", negm[:], bb[:, 0:MH])

    p.push_scope()
    ql = [p.sb("ql%d" % i, [128, TOK], BF16) for i in range(2)]
    qr = [p.sb("qr%d" % i, [128, TOK], BF16) for i in range(2)]
    PT = [p.sb("PT%d" % i, [128, QW], BF16) for i in range(3)]
    accD = [p.sb("accD%d" % i, [128, QW], F32) for i in range(2)]
    accP = [p.sb("accP%d" % i, [128, QW], F32) for i in range(2)]
    rl = p.sb("rl", [128, QW], F32)
    OTn = p.sb("OTn", [128, QW], BF16)
    oTs = [p.sb("oTs%d" % i, [128, QW], BF16) for i in range(2)]
    NH = MH if nheads is None else nheads
    it = 0
    for h in range(NH):
        Ql, Qr = ql[h % 2], qr[h % 2]
        p.dma("sp", Ql[:], QL.v(QL.t[h]))
        p.dma("sp", Qr[0:64, :], QR.v(QR.t[h]))
        p.dma("sp", Qr[64:128, :], QR.v(QR.t[h]))
        for qg in range(TOK // QW):
            q0 = qg * QW
            bo = obk
            AD, AP_ = accD[it % 2], accP[it % 2]
            it += 1
            first = {"dve": True, "pool": True}

            def s_mm(kt):
                bs = sbk[kt % 2]
                ktile = KT.sub(kt // 16, (slice(None), slice(kt * 128, (kt + 1) * 128)))
                hf = kt // 64
                kc = kt % 64
                rtile = KR2.sub((hf, kc // 16), (slice(hf * 64, (hf + 1) * 64), slice(kc * 128, (kc + 1) * 128)))
                for hh in range(QW // 512):
                    p.matmul(bs[:, hh * 512:(hh + 1) * 512], ktile, Ql[:, q0 + hh * 512:q0 + (hh + 1) * 512],
                             start=True, stop=False)
                if kt >= 1:
                    p.wait("pe", [PT[(kt - 1) % 3].name])
                for hh in range(QW // 512):
                    p.matmul(bs[:, hh * 512:(hh + 1) * 512], rtile,
                             Qr[hf * 64:(hf + 1) * 64, q0 + hh * 512:q0 + (hh + 1) * 512], start=False, stop=True)

            s_mm(0)
            for kt in range(NKT):
                if kt + 1 < NKT:
                    s_mm(kt + 1)
                P_ = PT[kt % 3]
                p.act(P_[:], sbk[kt % 2][:], AF.Exp, bias=negm[:, h:h + 1])
                vt = Vt.sub(kt // 16, (slice(None), kt, slice(None)))
                for hh in range(QW // 512):
                    p.matmul(bo[:, hh * 512:(hh + 1) * 512], vt, P_[:, hh * 512:(hh + 1) * 512],
                             start=(kt == 0), stop=(kt == NKT - 1))
                eng = "pool" if kt % 3 == 2 else "dve"
                A = AP_ if eng == "pool" else AD
                if first[eng]:
                    p.copy(eng, A[:], P_[:])
                    first[eng] = False
                else:
                    p.tt(eng, A[:], A[:], P_[:], ALU.add)
            p.tt("dve", AD[:], AD[:], AP_[:], ALU.add)
            for hh in range(QW // 512):
                hs = slice(hh * 512, (hh + 1) * 512)
                bl = ebk[hh]
                p.matmul(bl[:], onesf[:], AD[:, hs])
                p.recip(rl[:, hs], bl[:])
                p.tt("dve", OTn[:, hs], bo[:, hs], rl[:, hs], ALU.mult)
            oT = oTs[it % 2]
            for hh in range(QW // 512):
                hs = slice(hh * 512, (hh + 1) * 512)
                bv = ebk[hh]
                p.matmul(bv[:], Wuv[:, h, :], OTn[:, hs])
                p.copy("act", oT[:, hs], bv[:])
            p.dma("pool", V(OTD.t[h, :, q0:q0 + QW], ("OTD", h, qg)), oT[:])
    otd_keys = [("OTD", h, qg) for h in range(NH) for qg in range(TOK // QW)]
    p.pop_scope()
    oTt = [p.sb("oTt%d" % i, [128, MH, 128], BF16) for i in range(2)]
    h2t = [p.sb("h2t%d" % i, [128, D], F32) for i in range(2)]
    h3 = p.sb("h3", [128, D], F32)
    fb = ffn_prep_bufs(p)

    for j in range(NT):
        rows = slice(j * 128, (j + 1) * 128)
        b = j % 2
        p.dma("sp", h2t[b][:], H2[rows, :])
        p.dma("sp", oTt[b][:], V(OTD.t[:, :, rows].rearrange("h d t -> d h t"), ("OTD", "rd", j)),
              reads=[k_ for k_ in otd_keys if k_[2] == j // (QW // 128)])
        for half in range(2):
            bm = bank()
            for h in range(MH):
                p.matmul(bm[:], oTt[b][:, h, :], Wo.sub(h, (slice(None), h, slice(half * 512, (half + 1) * 512))),
                         start=(h == 0), stop=(h == MH - 1))
            p.tt("dve", h3[:, half * 512:(half + 1) * 512], bm[:], h2t[b][:, half * 512:(half + 1) * 512], ALU.add)
        p.dma("pool", H3[rows, :], h3[:])
        ffn_prep_tile(p, h3[:], gf, wr, cf, fb, HN3, AFF, rows, bank)
    return p.finish()


def build_L7():
    nc = bass.Bass("TRN2", target_bir_lowering=False)
    p = Prog(nc)
    H3 = p.dram("H3", [TOK, D], F32, "ExternalInput")
    DS = p.dram("DS", [NCORES, TOK, D], F32, "ExternalInput")
    g_fin = p.dram("g_fin", [1, D], F32, "ExternalInput")
    OUT = p.dram("OUT", [TOK, D], F32, "ExternalOutput")
    gm = p.sb("gm", [128, D], F32)
    ht = [p.sb("ht%d" % i, [128, D], F32) for i in range(2)]
    dt_ = [p.sb("dt%d" % i, [128, D], F32) for i in range(3)]
    sq = p.sb("sq", [128, D], F32)
    ss = p.sb("ss", [128, 1], F32)
    rstd = p.sb("rstd", [128, 1], F32)
    ot = [p.sb("ot%d" % i, [128, D], F32) for i in range(2)]
    p.dma("sp", gm[:], g_fin.v(g_fin.t[0:1, :].broadcast_to([128, D])))
    for j in range(NT):
        rows = slice(j * 128, (j + 1) * 128)
        Ht = ht[j % 2]
        p.dma("sp", Ht[:], H3[rows, :])
        for c in range(NCORES):
            Dt = dt_[c % 3]
            p.dma("sp", Dt[:], DS.v(DS.t[c, rows, :]))
            p.tt("dve", Ht[:], Ht[:], Dt[:], ALU.add)
        rmsnorm_tile(p, Ht[:], gm[:], ot[j % 2][:], sq[:], ss[:], rstd[:])
        p.dma("pool", OUT[rows, :], ot[j % 2][:])
    return p.finish()


def run(nc, in_maps):
    res = run_bass_kernel_spmd(nc, in_maps, core_ids=list(range(NCORES)))
    return res.results


_CACHE = {}


def _prog(name, builder):
    if name not in _CACHE:
        _CACHE[name] = builder()
    return _CACHE[name]


def _c(a):
    return np.ascontiguousarray(a)


def stage_L1(inp):
    cf, cb = const_tables()
    x = inp["x"][0]
    wg = _c(np.stack([inp["gla_w_gate_up_f"][0], inp["gla_w_gate_up_b"][0]]))
    bg = _c(np.stack([inp["gla_b_gate_f"][0], inp["gla_b_gate_b"][0]]))
    maps = [dict(x=_c(x[c * TOK:(c + 1) * TOK]), g_mix=_c(inp["mix_norm"][0:1]), w_in=_c(inp["gla_w_in"][0]),
                 wg=wg, bg=bg, cf=cf, cb=cb) for c in range(NCORES)]
    return run(_prog("L1", build_L1), maps)


def stage_L2(inp, r1):
    cf, cb = const_tables()
    x = inp["x"][0]
    maps = []
    for c in range(NCORES):
        BP = np.zeros((2, 7, GH, 128, GDV), np.float32)
        APD = np.zeros((2, 7, 128, NT * GH), np.float32)
        for i in range(7):
            cf_ = c - 7 + i
            if cf_ >= 0:
                BP[0, i] = r1[cf_]["BE"][0]
                APD[0, i] = r1[cf_]["AA"][0]
            cb_ = c + 7 - i
            if cb_ <= NCORES - 1:
                BP[1, i] = r1[cb_]["BE"][1]
                APD[1, i] = r1[cb_]["AA"][1]
        maps.append(dict(x=_c(x[c * TOK:(c + 1) * TOK]), OL=r1[c]["OL"], QP=r1[c]["QP"], SR=r1[c]["SR"],
                         AA=r1[c]["AA"], BP=BP, APD=APD, g_head=_c(inp["gla_head_norm"][0:1]),
                         w_out=_c(inp["gla_w_out"][0]), g_ffn=_c(inp["ffn_norm"][0:1]),
                         w_r=_c(inp["moe_w_router"][0]), cf=cf, cb=cb))
    return run(_prog("L2", build_L2), maps)


def stage_L3(inp, layer, aff_full, hn_full):
    cf, cb = const_tables()
    tokid = np.arange(SEQ, dtype=np.int32).reshape(128, 128)
    maps = []
    for c in range(NCORES):
        affe = _c(aff_full[:, 2 * c:2 * c + 2].T.reshape(2, 128, 128))
        maps.append(dict(AFFE=affe, HN=hn_full, wg=_c(inp["moe_w_gate"][layer, 2 * c:2 * c + 2]),
                         wu=_c(inp["moe_w_up"][layer, 2 * c:2 * c + 2]), wd=_c(inp["moe_w_down"][layer, 2 * c:2 * c + 2]),
                         cf=cf, cb=cb, tokid=tokid))
    return run(_prog("L3", build_L3), maps)


def stage_L4(inp, h_prev, deltas):
    cf, cb = const_tables()
    rc = rope_consts()
    maps = []
    for c in range(NCORES):
        DS = _c(np.stack([d[c * TOK:(c + 1) * TOK] for d in deltas]))
        maps.append(dict(H1=h_prev[c], DS=DS, pos=_c(inp["positions"][0:1, c * TOK:(c + 1) * TOK]),
                         g_mix=_c(inp["mix_norm"][1:2]), w_m=_c(inp["mla_w_in"][0]), g_q=_c(inp["mla_q_norm"][0:1]),
                         g_kv=_c(inp["mla_kv_norm"][0:1]), w_uq=_c(inp["mla_w_uq"][0]), w_ukv=_c(inp["mla_w_ukv"][0]),
                         rc=rc, cf=cf, cb=cb))
    return run(_prog("L4", build_L4), maps)


def stage_L5(inp, r4):
    cf, cb = const_tables()
    KT = _c(np.concatenate([r4[c]["CKVT"] for c in range(NCORES)], axis=1))
    KR = _c(np.concatenate([r4[c]["KRT"] for c in range(NCORES)], axis=1))
    VK = _c(np.concatenate([r4[c]["CKV"] for c in range(NCORES)], axis=0))
    KN2 = _c(np.concatenate([r4[c]["KN2"] for c in range(NCORES)], axis=0).reshape(128, 128))
    maps = []
    for c in range(NCORES):
        maps.append(dict(QL=r4[c]["QL"], QR=r4[c]["QR"], KT=KT, KR=KR, VK=VK, KN2=KN2, QMAX=r4[c]["QMAX"],
                         H2=r4[c]["H2"], w_ukv=_c(inp["mla_w_ukv"][0]), w_o=_c(inp["mla_w_out"][0]),
                         g_ffn=_c(inp["ffn_norm"][1:2]), w_r=_c(inp["moe_w_router"][1]), cf=cf, cb=cb))
    return run(_prog("L5", build_L5), maps)


def stage_L7(inp, h_prev, deltas):
    maps = []
    for c in range(NCORES):
        DS = _c(np.stack([d[c * TOK:(c + 1) * TOK] for d in deltas]))
        maps.append(dict(H3=h_prev[c], DS=DS, g_fin=_c(inp["final_norm"].reshape(1, D))))
    return run(_prog("L7", build_L7), maps)


def kernel(**inputs):
    inp = {k: np.asarray(v) for k, v in inputs.items()}
    r1 = stage_L1(inp)
    r2 = stage_L2(inp, r1)
    aff0 = _c(np.concatenate([r2[c]["AFF"] for c in range(NCORES)], axis=0))
    hn1 = _c(np.concatenate([r2[c]["HN1"] for c in range(NCORES)], axis=0))
    r3 = stage_L3(inp, 0, aff0, hn1)
    r4 = stage_L4(inp, [r2[c]["H1"] for c in range(NCORES)], [r3[c]["DELTA"] for c in range(NCORES)])
    del r3
    r5 = stage_L5(inp, r4)
    aff1 = _c(np.concatenate([r5[c]["AFF"] for c in range(NCORES)], axis=0))
    hn3 = _c(np.concatenate([r5[c]["HN3"] for c in range(NCORES)], axis=0))
    r6 = stage_L3(inp, 1, aff1, hn3)
    r7 = stage_L7(inp, [r5[c]["H3"] for c in range(NCORES)], [r6[c]["DELTA"] for c in range(NCORES)])
    out = np.concatenate([r7[c]["OUT"] for c in range(NCORES)], axis=0).astype(np.float32)
    return out.reshape(1, SEQ, D)
```

```python
import contextlib
import numpy as np
import ml_dtypes
import concourse.bass as bass
import concourse.mybir as mybir
from concourse.bass_utils import run_bass_kernel_spmd

F32 = mybir.dt.float32
BF16 = mybir.dt.bfloat16
I32 = mybir.dt.int32
AF = mybir.ActivationFunctionType
ALU = mybir.AluOpType
AX = mybir.AxisListType

NCORES = 8
SEQ = 16384
TOK = SEQ // NCORES
NT = TOK // 128
D = 1024
RMS_EPS = 1e-6


class V:
    __slots__ = ("ap", "key")

    def __init__(self, ap, key):
        self.ap = ap
        self.key = key


class Buf:
    def __init__(self, t, name):
        self.t = t
        self.name = name

    def __getitem__(self, idx):
        return V(self.t[idx], self.name)

    def sub(self, s, idx):
        return V(self.t[idx], (self.name, s))

    def v(self, ap, s=None):
        return V(ap, self.name if s is None else (self.name, s))


class Prog:
    ENGS = ("pe", "dve", "act", "pool", "sp")
    NDMA = {"sp": 10, "pool": 8, "act": 4}

    def __init__(self, nc, same_engine_sync=True):
        self.nc = nc
        self.same = same_engine_sync
        self.stack = contextlib.ExitStack()
        self.lists = {e: [] for e in self.ENGS}
        self.cnt = {e: 0 for e in self.ENGS}
        self.seen = {e: {} for e in self.ENGS}
        self.last_w = {}
        self.readers = {}
        self.sem = {}
        self.dma_cnt = {}
        self.dma_rr = {q: 0 for q in self.NDMA}
        for e in ("pe", "dve", "act", "pool"):
            self.sem[("c", e)] = self.stack.enter_context(nc.semaphore("c_" + e))
        for q, n in self.NDMA.items():
            for k in range(n):
                sk = ("d", q, k)
                self.sem[sk] = self.stack.enter_context(nc.semaphore("d_%s%d" % (q, k)))
                self.dma_cnt[sk] = 0
        self.psum = set()
        self.bregs = {}

    def sb(self, name, shape, dt):
        t = self.stack.enter_context(self.nc.sbuf_tensor(name, list(shape), dt))
        return Buf(t, name)

    def ps(self, name, shape, dt):
        t = self.stack.enter_context(self.nc.psum_tensor(name, list(shape), dt))
        self.psum.add(name)
        return Buf(t, name)

    def dram(self, name, shape, dt, kind):
        t = self.nc.dram_tensor(name, list(shape), dt, kind=kind)
        return Buf(t.ap(), name)

    def emit(self, eng, fn, reads, writes, dma=False):
        deps = {}

        def need(tok):
            if tok is None:
                return
            sk, val = tok
            if deps.get(sk, 0) < val:
                deps[sk] = val

        for r in reads:
            need(self.last_w.get(r))
            if r in self.psum:
                for sk, val in self.readers.get(r, {}).items():
                    if sk != ("c", eng):
                        need((sk, val))
        for w in writes:
            need(self.last_w.get(w))
            for sk, val in self.readers.get(w, {}).items():
                need((sk, val))
        if dma:
            k = self.dma_rr[eng]
            self.dma_rr[eng] = (k + 1) % self.NDMA[eng]
            sk = ("d", eng, k)
            if self.dma_cnt[sk] > 0:
                need((sk, self.dma_cnt[sk]))
            self.dma_cnt[sk] += 16
            tok = (sk, self.dma_cnt[sk])
        else:
            self.cnt[eng] += 1
            tok = (("c", eng), self.cnt[eng])
        waits = []
        for sk, val in deps.items():
            if sk == ("c", eng) and (eng == "pe" or not self.same):
                continue
            if self.seen[eng].get(sk, 0) >= val:
                continue
            self.seen[eng][sk] = val
            waits.append((sk, val))
        self.lists[eng].append((waits, fn, tok))
        for r in reads:
            d = self.readers.setdefault(r, {})
            if d.get(tok[0], 0) < tok[1]:
                d[tok[0]] = tok[1]
        for w in writes:
            self.last_w[w] = tok
            self.readers[w] = {}

    @staticmethod
    def _keys(*vs):
        return [v.key for v in vs if isinstance(v, V)]

    @staticmethod
    def _ap(v):
        return v.ap if isinstance(v, V) else v

    def dma(self, q, out, in_, reads=None, writes=None, **kw):
        o, i = out.ap, in_.ap
        self.emit(q, lambda e: e.dma_start(out=o, in_=i, **kw), [in_.key] + (reads or []),
                  [out.key] if writes is None else writes, dma=True)

    def _breg(self, e, val):
        if val not in self.bregs:
            r = e.alloc_register("bchk%d" % val)
            e.reg_mov(r, val)
            self.bregs[val] = r
        return self.bregs[val]

    def gather(self, out, src, idx, nrows):
        o, s, ix = out.ap, src.ap, idx.ap
        self.emit("pool", lambda e: e.indirect_dma_start(
            out=o, out_offset=None, in_=s,
            in_offset=bass.IndirectOffsetOnAxis(ap=ix, axis=0),
            bounds_check=self._breg(e, nrows - 1), oob_is_err=False),
            [src.key, idx.key], [out.key], dma=True)

    def scatter(self, dst, src, idx, nrows, accum=False, reads=None, writes=None):
        d, s, ix = dst.ap, src.ap, idx.ap
        kw = {"compute_op": ALU.add} if accum else {}
        self.emit("pool", lambda e: e.indirect_dma_start(
            out=d, out_offset=bass.IndirectOffsetOnAxis(ap=ix, axis=0), in_=s, in_offset=None,
            bounds_check=self._breg(e, nrows - 1), oob_is_err=False, **kw),
            [src.key, idx.key] + ([dst.key] if accum else []) + (reads or []),
            [dst.key] if writes is None else writes, dma=True)

    def matmul(self, out, lhsT, rhs, start=True, stop=True):
        o, l, r = out.ap, lhsT.ap, rhs.ap
        rd = [lhsT.key, rhs.key] + ([] if start else [out.key])
        self.emit("pe", lambda e: e.matmul(o, l, r, start=start, stop=stop), rd, [out.key])

    def transpose(self, out, in_, ident):
        o, i, d = out.ap, in_.ap, ident.ap
        self.emit("pe", lambda e: e.transpose(o, i, d), [in_.key, ident.key], [out.key])

    def act(self, out, in_, func, bias=0.0, scale=1.0, accum_out=None, eng="act"):
        o, i = out.ap, in_.ap
        b, s = self._ap(bias), self._ap(scale)
        kw = {}
        if accum_out is not None:
            kw["accum_out"] = accum_out.ap
        self.emit(eng, lambda e: e.activation(o, i, func, bias=b, scale=s, **kw),
                  self._keys(in_, bias, scale), self._keys(out, accum_out))

    def copy(self, eng, out, in_):
        o, i = out.ap, in_.ap
        if eng == "act":
            self.emit(eng, lambda e: e.copy(o, i), [in_.key], [out.key])
        else:
            self.emit(eng, lambda e: e.tensor_copy(o, i), [in_.key], [out.key])

    def tt(self, eng, out, in0, in1, op):
        o, a, b = out.ap, in0.ap, in1.ap
        self.emit(eng, lambda e: e.tensor_tensor(o, a, b, op), [in0.key, in1.key], [out.key])

    def ts(self, eng, out, in0, s1, s2, op0, op1=None, accum_out=None):
        o, a = out.ap, in0.ap
        x1, x2 = self._ap(s1), self._ap(s2)
        kw = {}
        if op1 is not None:
            kw["op1"] = op1
        if accum_out is not None:
            kw["accum_out"] = accum_out.ap
        self.emit(eng, lambda e: e.tensor_scalar(o, a, x1, x2, op0, **kw),
                  self._keys(in0, s1, s2), self._keys(out, accum_out))

    def stt(self, eng, out, in0, scalar, in1, op0, op1):
        o, a, b = out.ap, in0.ap, in1.ap
        s = self._ap(scalar)
        self.emit(eng, lambda e: e.scalar_tensor_tensor(o, a, s, b, op0, op1),
                  self._keys(in0, scalar, in1), [out.key])

    def reduce(self, eng, out, in_, op, axis=AX.X):
        o, i = out.ap, in_.ap
        self.emit(eng, lambda e: e.tensor_reduce(o, i, axis, op), [in_.key], [out.key])

    def recip(self, out, in_):
        o, i = out.ap, in_.ap
        self.emit("dve", lambda e: e.reciprocal(o, i), [in_.key], [out.key])

    def memset(self, eng, out, val):
        o = out.ap
        self.emit(eng, lambda e: e.memset(o, val), [], [out.key])

    def wait(self, eng, keys):
        waits = []
        for k_ in keys:
            tok = self.last_w.get(k_)
            if tok is None:
                continue
            sk, val = tok
            if sk == ("c", eng) or self.seen[eng].get(sk, 0) >= val:
                continue
            self.seen[eng][sk] = val
            waits.append((sk, val))
        if waits:
            self.lists[eng].append((waits, None, None))

    def barrier(self):
        allv = {("c", e): self.cnt[e] for e in ("pe", "dve", "act", "pool") if self.cnt[e] > 0}
        allv.update({sk: v for sk, v in self.dma_cnt.items() if v > 0})
        for eng in self.ENGS:
            waits = []
            for sk, val in allv.items():
                if sk == ("c", eng) or self.seen[eng].get(sk, 0) >= val:
                    continue
                self.seen[eng][sk] = val
                waits.append((sk, val))
            if waits:
                self.lists[eng].append((waits, None, None))

    def push_scope(self):
        self._outer = self.stack
        self.stack = contextlib.ExitStack()

    def pop_scope(self):
        self.barrier()
        self.stack.close()
        self.stack = self._outer

    def finish(self):
        nc = self.nc
        prog = self

        def mk(name):
            def body(e):
                for waits, fn, tok in prog.lists[name]:
                    for sk, val in waits:
                        e.wait_ge(prog.sem[sk], val)
                    if fn is None:
                        continue
                    ins = fn(e)
                    ins.then_inc(prog.sem[tok[0]], 16 if tok[0][0] == "d" else 1)
                if name == "sp":
                    for sk, val in prog.dma_cnt.items():
                        if val > 0:
                            e.wait_ge(prog.sem[sk], val)
            return body

        with nc.Block() as block:
            block.tensor(mk("pe"))
            block.vector(mk("dve"))
            block.scalar(mk("act"))
            block.gpsimd(mk("pool"))
            block.sync(mk("sp"))
        self.stack.close()
        return nc


def bcast_rows(ap1d_or_row, nparts):
    return ap1d_or_row.broadcast(0, nparts) if hasattr(ap1d_or_row, "broadcast") else ap1d_or_row


def const_tables():
    i = np.arange(128)
    ui = (i[:, None] <= i[None, :]).astype(np.float32)
    li = (i[:, None] >= i[None, :]).astype(np.float32)
    us = (i[:, None] < i[None, :]).astype(np.float32)
    ls = (i[:, None] > i[None, :]).astype(np.float32)
    ident = np.eye(128, dtype=np.float32)
    cf = np.stack([ui, li, us, ls, ident], axis=1)
    cb = np.eye(128, dtype=np.float32).astype(ml_dtypes.bfloat16)
    return np.ascontiguousarray(cf), cb


GH = 4
GDK = 128
GDV = 256
GIN = 3104


def rmsnorm_tile(p, xt, gbc, hn_out, sq, ss, rstd, width=D):
    p.act(sq, xt, AF.Square, accum_out=ss)
    p.ts("dve", rstd, ss, 1.0 / width, RMS_EPS, ALU.mult, ALU.add)
    p.act(rstd, rstd, AF.Sqrt)
    p.recip(rstd, rstd)
    p.stt("dve", hn_out, xt, rstd, gbc, ALU.mult, ALU.mult)


def build_L1():
    nc = bass.Bass("TRN2", target_bir_lowering=False)
    p = Prog(nc)
    x = p.dram("x", [TOK, D], F32, "ExternalInput")
    g_mix = p.dram("g_mix", [1, D], F32, "ExternalInput")
    w_in = p.dram("w_in", [D, GIN], F32, "ExternalInput")
    wg = p.dram("wg", [2, 16, 512], F32, "ExternalInput")
    bg = p.dram("bg", [2, 512], F32, "ExternalInput")
    cf_d = p.dram("cf", [128, 5, 128], F32, "ExternalInput")
    cb_d = p.dram("cb", [128, 128], BF16, "ExternalInput")
    QP = p.dram("QP", [2, 128, GH, TOK], BF16, "ExternalOutput")
    KP = p.dram("KP", [2, 128, GH, TOK], BF16, "Internal")
    K2 = p.dram("K2", [2, TOK, 512], BF16, "Internal")
    VT = p.dram("VT", [TOK, 1024], BF16, "Internal")
    SR = p.dram("SR", [TOK, 1024], F32, "ExternalOutput")
    OL = p.dram("OL", [TOK, 1024], F32, "ExternalOutput")
    AA = p.dram("AA", [2, 128, NT * GH], F32, "ExternalOutput")
    BE = p.dram("BE", [2, GH, 128, GDV], F32, "ExternalOutput")

    W = p.sb("W", [128, 8, GIN], BF16)
    cf = p.sb("cfs", [128, 5, 128], F32)
    cb = p.sb("cbs", [128, 128], BF16)
    gbc = p.sb("gbc", [128, D], F32)
    bgs = p.sb("bgs", [128, 2, 512], F32)
    wgs = p.sb("wgs", [16, 2, 512], BF16)
    xt = [p.sb("xt%d" % i, [128, D], F32) for i in range(2)]
    sq = p.sb("sq", [128, D], F32)
    ss = p.sb("ss", [128, 1], F32)
    rstd = p.sb("rstd", [128, 1], F32)
    hn = p.sb("hn", [128, D], BF16)
    hnT = p.sb("hnT", [128, D], BF16)
    gdT = p.sb("gdT", [16, 256], BF16)
    zb = p.sb("zb", [128, 512], F32)
    spl = [p.sb("spl%d" % i, [128, 512], F32) for i in range(2)]
    EqT = p.sb("EqT", [128, 512], F32)
    EkT = p.sb("EkT", [128, 512], F32)
    Ek2 = p.sb("Ek2", [128, 512], F32)
    a_all = p.sb("a_all", [128, 2, NT * GH], F32)
    c_all = p.sb("c_all", [128, 2, NT * GH], F32)
    qp = [p.sb("qp%d" % i, [128, 512], BF16) for i in range(2)]
    kp = [p.sb("kp%d" % i, [128, 512], BF16) for i in range(2)]
    k2 = [p.sb("k2%d" % i, [128, 512], BF16) for i in range(2)]
    vb = p.sb("vb", [128, 1024], BF16)
    srt = p.sb("srt", [128, 1024], F32)
    S = p.sb("S", [128, GH, GDV], F32)
    Sb = p.sb("Sb", [128, GH, GDV], BF16)
    s_qp = [p.sb("s_qp%d" % i, [128, 512], BF16) for i in range(2)]
    s_kp = [p.sb("s_kp%d" % i, [128, 512], BF16) for i in range(2)]
    s_k2 = [p.sb("s_k2%d" % i, [128, 512], BF16) for i in range(2)]
    s_v = [p.sb("s_v%d" % i, [128, 1024], BF16) for i in range(2)]
    AT = [p.sb("AT%d" % i, [128, 128], BF16) for i in range(2)]
    ot = [p.sb("ot%d" % i, [128, 1024], F32) for i in range(2)]
    of = [p.sb("of%d" % i, [128, 1024], F32) for i in range(2)]
    banks = [p.ps("bk%d" % i, [128, 512], F32) for i in range(7)]
    pT = p.ps("pT", [128, 1024], BF16)
    bi = [0]

    rot = [3, 4]

    def bank():
        b = banks[rot[0] + bi[0] % rot[1]]
        bi[0] += 1
        return b

    p.dma("sp", cf[:], cf_d[:])
    p.dma("sp", cb[:], cb_d[:])
    p.dma("sp", gbc[:], g_mix.v(g_mix.t[0:1, :].broadcast_to([128, D])))
    p.dma("sp", bgs[:, 0, :], bg.v(bg.t[0:1, :].broadcast_to([128, 512])))
    p.dma("sp", bgs[:, 1, :], bg.v(bg.t[1:2, :].broadcast_to([128, 512])))
    for d_ in range(2):
        p.dma("pool", wgs.sub(d_, (slice(None), d_, slice(None))), wg[d_])
    for dc in range(8):
        p.dma("pool", W.sub(dc, (slice(None), dc, slice(None))), w_in[dc * 128:(dc + 1) * 128, :])
    Wk = lambda dc, lo, hi: W.sub(dc, (slice(None), dc, slice(lo, hi)))
    UI, LI, US, LS = (cf[:, i, :] for i in range(4))
    scale_q = float(GDK) ** -0.5

    def scan_init(d_):
        p.memset("dve", S[:], 0.0)
        p.memset("dve", Sb[:], 0.0)

    def scan_step(d_, j, it, sq_, sk_, sk2_, sv_):
        b = it % 2
        mask = UI if d_ == 0 else LS
        O = ot[b]
        for h in range(GH):
            hs = slice(h * 128, (h + 1) * 128)
            vs = slice(h * GDV, (h + 1) * GDV)
            ba = bank()
            p.matmul(ba[:, 0:128], sk_[:, hs], sq_[:, hs])
            A = AT[h % 2]
            p.tt("dve", A[:], ba[:, 0:128], mask, ALU.mult)
            bo = bank()
            p.matmul(bo[:, 0:GDV], A[:], sv_[:, vs], start=True, stop=False)
            p.matmul(bo[:, 0:GDV], sq_[:, hs], Sb[:, h, :], start=False, stop=True)
            bs = bank()
            p.matmul(bs[:, 0:GDV], sk2_[:, hs], sv_[:, vs])
            if d_ == 0:
                p.copy("act", O[:, vs], bo[:, 0:GDV])
            else:
                p.tt("dve", O[:, vs], bo[:, 0:GDV], of[b][:, vs], ALU.add)
            p.stt("dve", S[:, h, :], S[:, h, :], a_all.v(a_all.t[:, d_, j * GH + h:j * GH + h + 1]),
                  bs[:, 0:GDV], ALU.mult, ALU.add)
            p.copy("act", Sb[:, h, :], S[:, h, :])
        p.dma("pool", OL[j * 128:(j + 1) * 128, :], O[:])

    def scan_fin(d_):
        for h in range(GH):
            p.dma("pool", BE.v(BE.t[d_, h]), S[:, h, :])

    scan_init(0)
    for j in range(NT):
        X = xt[j % 2]
        p.dma("sp", X[:], x[j * 128:(j + 1) * 128, :])
        rmsnorm_tile(p, X[:], gbc[:], hn[:], sq[:], ss[:], rstd[:])
        for dc in range(8):
            p.transpose(pT[:, dc * 128:(dc + 1) * 128], hn[:, dc * 128:(dc + 1) * 128], cb[:])
        p.copy("act", hnT[:], pT[:])
        hT = lambda dc: hnT[:, dc * 128:(dc + 1) * 128]
        bG = bank()
        for d_ in range(2):
            for dc in range(8):
                p.matmul(bG[0:16, d_ * 128:(d_ + 1) * 128], Wk(dc, 3072 + 16 * d_, 3088 + 16 * d_), hT(dc),
                         start=(dc == 0), stop=(dc == 7))
        p.copy("act", gdT[:], bG[0:16, 0:256])
        bq, bk, bkt = banks[0], banks[1], banks[2]
        for n in range(4):
            for dc in range(8):
                p.matmul(bq[:, n * 128:(n + 1) * 128], Wk(dc, n * 128, (n + 1) * 128), hT(dc),
                         start=(dc == 0), stop=(dc == 7))
        for n in range(4):
            for dc in range(8):
                p.matmul(bk[:, n * 128:(n + 1) * 128], Wk(dc, 512 + n * 128, 512 + (n + 1) * 128), hT(dc),
                         start=(dc == 0), stop=(dc == 7))
        for dc in range(8):
            p.matmul(bkt[:], hT(dc), Wk(dc, 512, 1024), start=(dc == 0), stop=(dc == 7))
        for d_ in range(2):
            bz = bank()
            p.matmul(bz[:], gdT[:, d_ * 128:(d_ + 1) * 128], wgs.sub(d_, (slice(None), d_, slice(None))))
            p.tt("dve", zb[:], bz[:], bgs[:, d_, :], ALU.add)
            p.act(zb[:], zb[:], AF.Exp, scale=-1.0)
            sp_ = spl[d_]
            p.act(sp_[:], zb[:], AF.Ln, bias=1.0)
            bc, bd = bank(), bank()
            tri = UI if d_ == 0 else LI
            for h in range(GH):
                p.matmul(bc[:, h * 128:(h + 1) * 128], sp_[:, h * 128:(h + 1) * 128], tri)
            p.matmul(bd[:], LS if d_ == 0 else US, sp_[:])
            p.act(EqT[:], bc[:], AF.Exp, scale=-1.0 / 16)
            p.act(EkT[:], bc[:], AF.Exp, scale=1.0 / 16)
            p.act(Ek2[:], bd[:], AF.Exp, scale=-1.0 / 16)
            last = 127 if d_ == 0 else 0
            p.copy("dve", a_all.v(a_all.t[:, d_, j * GH:(j + 1) * GH]),
                   EqT.v(EqT.t[:].rearrange("p (h t) -> p h t", h=GH)[:, :, last]))
            p.copy("dve", c_all.v(c_all.t[:, d_, j * GH:(j + 1) * GH]),
                   bc.v(bc.t[:].rearrange("p (h t) -> p h t", h=GH)[:, :, last]))
            Q, K, KK = qp[d_], kp[d_], k2[d_]
            p.stt("dve", Q[:], bq[:], scale_q, EqT[:], ALU.mult, ALU.mult)
            p.tt("dve", K[:], bk[:], EkT[:], ALU.mult)
            p.tt("dve", KK[:], bkt[:], Ek2[:], ALU.mult)
            p.dma("pool", QP.v(QP.t[d_, :, :, j * 128:(j + 1) * 128]),
                  Q.v(Q.t[:].rearrange("p (h t) -> p h t", h=GH)))
            p.dma("pool", KP.v(KP.t[d_, :, :, j * 128:(j + 1) * 128]),
                  K.v(K.t[:].rearrange("p (h t) -> p h t", h=GH)))
            p.dma("pool", K2.v(K2.t[d_, j * 128:(j + 1) * 128, :]), KK[:])
        for half in range(2):
            bv = bank()
            for dc in range(8):
                p.matmul(bv[:], hT(dc), Wk(dc, 1024 + half * 512, 1536 + half * 512), start=(dc == 0), stop=(dc == 7))
            p.copy("act", vb[:, half * 512:(half + 1) * 512], bv[:])
        p.dma("pool", VT[j * 128:(j + 1) * 128, :], vb[:])
        scan_step(0, j, j, qp[0], kp[0], k2[0], vb)
        for half in range(2):
            br = bank()
            for dc in range(8):
                p.matmul(br[:], hT(dc), Wk(dc, 2048 + half * 512, 2560 + half * 512), start=(dc == 0), stop=(dc == 7))
            p.act(srt[:, half * 512:(half + 1) * 512], br[:], AF.Silu)
        p.dma("pool", SR[j * 128:(j + 1) * 128, :], srt[:])
    scan_fin(0)
    for d_ in range(2):
        p.dma("pool", AA[d_], c_all[:, d_, :])

    rot[0], rot[1] = 0, 7
    scan_init(1)
    for it, j in enumerate(range(NT - 1, -1, -1)):
        b = it % 2
        sq_, sk_, sk2_, sv_ = s_qp[b], s_kp[b], s_k2[b], s_v[b]
        p.dma("sp", sq_.v(sq_.t[:].rearrange("p (h t) -> p h t", h=GH)), QP.v(QP.t[1, :, :, j * 128:(j + 1) * 128]))
        p.dma("sp", sk_.v(sk_.t[:].rearrange("p (h t) -> p h t", h=GH)), KP.v(KP.t[1, :, :, j * 128:(j + 1) * 128]))
        p.dma("sp", sk2_[:], K2.v(K2.t[1, j * 128:(j + 1) * 128, :]))
        p.dma("sp", sv_[:], VT[j * 128:(j + 1) * 128, :])
        p.dma("sp", of[b][:], OL[j * 128:(j + 1) * 128, :])
        scan_step(1, j, it, sq_, sk_, sk2_, sv_)
    scan_fin(1)
    return p.finish()


def ffn_prep_tile(p, h1, gf, wr, cff, bufs, HN_out, AFF_out, rows, bank):
    sq, ss, rstd, hnf, hnb, hnT, lg, mx, sm = bufs
    rmsnorm_tile(p, h1, gf[:], hnf[:], sq[:], ss[:], rstd[:])
    p.copy("act", hnb[:], hnf[:])
    p.dma("pool", HN_out[rows, :], hnb[:])
    ident_f = cff[:, 4, :]
    for half in range(2):
        bt = bank()
        for q in range(4):
            dc = half * 4 + q
            p.transpose(bt[:, q * 128:(q + 1) * 128], hnf[:, dc * 128:(dc + 1) * 128], ident_f)
        p.copy("act", hnT[:, half * 512:(half + 1) * 512], bt[:])
    bl = bank()
    for dc in range(8):
        p.matmul(bl[:, 0:16], hnT[:, dc * 128:(dc + 1) * 128], wr[:, dc, :], start=(dc == 0), stop=(dc == 7))
    p.reduce("dve", mx[:], bl[:, 0:16], ALU.max)
    p.ts("dve", mx[:], mx[:], -1.0, None, ALU.mult)
    p.act(lg[:], bl[:, 0:16], AF.Exp, bias=mx[:])
    p.reduce("dve", sm[:], lg[:], ALU.add)
    p.recip(sm[:], sm[:])
    p.ts("dve", lg[:], lg[:], sm[:], None, ALU.mult)
    p.dma("pool", AFF_out[rows, :], lg[:])


def ffn_prep_bufs(p):
    return (p.sb("f_sq", [128, D], F32), p.sb("f_ss", [128, 1], F32), p.sb("f_rstd", [128, 1], F32),
            p.sb("f_hnf", [128, D], F32), p.sb("f_hnb", [128, D], BF16), p.sb("f_hnT", [128, D], F32),
            p.sb("f_lg", [128, 16], F32), p.sb("f_mx", [128, 1], F32), p.sb("f_sm", [128, 1], F32))


def build_L2():
    nc = bass.Bass("TRN2", target_bir_lowering=False)
    p = Prog(nc)
    x = p.dram("x", [TOK, D], F32, "ExternalInput")
    OL = p.dram("OL", [TOK, 1024], F32, "ExternalInput")
    QP = p.dram("QP", [2, 128, GH, TOK], BF16, "ExternalInput")
    SR = p.dram("SR", [TOK, 1024], F32, "ExternalInput")
    AA = p.dram("AA", [2, 128, NT * GH], F32, "ExternalInput")
    BP = p.dram("BP", [2, 7, GH, 128, GDV], F32, "ExternalInput")
    APD = p.dram("APD", [2, 7, 128, NT * GH], F32, "ExternalInput")
    g_head = p.dram("g_head", [1, GDV], F32, "ExternalInput")
    w_out = p.dram("w_out", [D, D], F32, "ExternalInput")
    g_ffn = p.dram("g_ffn", [1, D], F32, "ExternalInput")
    w_r = p.dram("w_r", [D, 16], F32, "ExternalInput")
    cf_d = p.dram("cf", [128, 5, 128], F32, "ExternalInput")
    cb_d = p.dram("cb", [128, 128], BF16, "ExternalInput")
    H1 = p.dram("H1", [TOK, D], F32, "ExternalOutput")
    HN1 = p.dram("HN1", [TOK, D], BF16, "ExternalOutput")
    AFF = p.dram("AFF", [TOK, 16], F32, "ExternalOutput")

    Wo = p.sb("Wo", [128, 8, D], BF16)
    wr = p.sb("wr", [128, 8, 16], F32)
    cf = p.sb("cfs", [128, 5, 128], F32)
    cb = p.sb("cbs", [128, 128], BF16)
    gf = p.sb("gf", [128, D], F32)
    gh = p.sb("gh", [128, D], F32)
    apd = p.sb("apd", [128, 2, 7, NT * GH], F32)
    csum = p.sb("csum", [128, 2, 7, GH], F32)
    aown = p.sb("aown", [128, 2, NT * GH], F32)
    Sin = p.sb("Sin", [128, 2, GH, GDV], F32)
    Bt = [p.sb("Bt%d" % i, [128, GH, GDV], F32) for i in range(2)]
    Sbw = p.sb("Sbw", [128, NT, GH, GDV], BF16)
    Sfb = p.sb("Sfb", [128, GH, GDV], BF16)
    xt = [p.sb("xt%d" % i, [128, D], F32) for i in range(2)]
    olt = [p.sb("olt%d" % i, [128, D], F32) for i in range(2)]
    srt = [p.sb("srt%d" % i, [128, D], F32) for i in range(2)]
    qf = [p.sb("qf%d" % i, [128, 512], BF16) for i in range(2)]
    qb = [p.sb("qb%d" % i, [128, 512], BF16) for i in range(2)]
    o = p.sb("o", [128, D], F32)
    osq = p.sb("osq", [128, D], F32)
    hss = p.sb("hss", [128, GH], F32)
    y = p.sb("y", [128, D], BF16)
    yT = p.sb("yT", [128, D], BF16)
    h1 = p.sb("h1", [128, D], F32)
    fb = ffn_prep_bufs(p)
    banks = [p.ps("bk%d" % i, [128, 512], F32) for i in range(7)]
    pT = p.ps("pT", [128, 1024], BF16)
    bi = [0]

    def bank():
        b = banks[bi[0] % 7]
        bi[0] += 1
        return b

    p.dma("sp", cf[:], cf_d[:])
    p.dma("sp", cb[:], cb_d[:])
    p.dma("sp", gf[:], g_ffn.v(g_ffn.t[0:1, :].broadcast_to([128, D])))
    for h in range(GH):
        p.dma("sp", gh[:, h * GDV:(h + 1) * GDV], g_head.v(g_head.t[0:1, :].broadcast_to([128, GDV])))
    p.dma("sp", wr[:], w_r.v(w_r.t.rearrange("(c p) e -> p c e", p=128)))
    for dc in range(8):
        p.dma("pool", Wo.sub(dc, (slice(None), dc, slice(None))), w_out[dc * 128:(dc + 1) * 128, :])
    for d_ in range(2):
        p.dma("sp", apd.v(apd.t[:, d_]), APD.v(APD.t[d_].rearrange("i p c -> p i c")))
        p.dma("sp", aown.v(aown.t[:, d_, :]), AA[d_])
    p.reduce("dve", csum[:], apd.v(apd.t[:].rearrange("p d i (t h) -> p d i h t", h=GH)), ALU.add)
    p.act(csum[:], csum[:], AF.Exp, scale=-1.0 / 16)
    p.act(aown[:], aown[:], AF.Exp, scale=-1.0 / 16)
    p.memset("dve", Sin[:], 0.0)
    n = 0
    for d_ in range(2):
        for i in range(7):
            B = Bt[n % 2]
            n += 1
            p.dma("sp", B[:], BP.v(BP.t[d_, i].rearrange("h p v -> p h v")))
            for h in range(GH):
                p.stt("dve", Sin[:, d_, h, :], Sin[:, d_, h, :], csum.v(csum.t[:, d_, i, h:h + 1]), B[:, h, :],
                      ALU.mult, ALU.add)
    for j in range(NT - 1, -1, -1):
        p.copy("act", Sbw[:, j], Sin[:, 1])
        for h in range(GH):
            p.ts("dve", Sin[:, 1, h, :], Sin[:, 1, h, :], aown.v(aown.t[:, 1, j * GH + h:j * GH + h + 1]), None, ALU.mult)
    for j in range(NT):
        b = j % 2
        rows = slice(j * 128, (j + 1) * 128)
        p.dma("sp", xt[b][:], x[rows, :])
        p.dma("sp", olt[b][:], OL[rows, :])
        p.dma("sp", srt[b][:], SR[rows, :])
        p.dma("sp", qf[b].v(qf[b].t[:].rearrange("p (h t) -> p h t", h=GH)), QP.v(QP.t[0, :, :, rows]))
        p.dma("sp", qb[b].v(qb[b].t[:].rearrange("p (h t) -> p h t", h=GH)), QP.v(QP.t[1, :, :, rows]))
        p.copy("act", Sfb[:], Sin[:, 0])
        for h in range(GH):
            hs = slice(h * 128, (h + 1) * 128)
            vs = slice(h * GDV, (h + 1) * GDV)
            bo = bank()
            p.matmul(bo[:, 0:GDV], qf[b][:, hs], Sfb[:, h, :], start=True, stop=False)
            p.matmul(bo[:, 0:GDV], qb[b][:, hs], Sbw[:, j, h, :], start=False, stop=True)
            p.tt("dve", o[:, vs], bo[:, 0:GDV], olt[b][:, vs], ALU.add)
            p.ts("dve", Sin[:, 0, h, :], Sin[:, 0, h, :], aown.v(aown.t[:, 0, j * GH + h:j * GH + h + 1]), None, ALU.mult)
        p.tt("dve", osq[:], o[:], o[:], ALU.mult)
        p.reduce("dve", hss[:], osq.v(osq.t[:].rearrange("p (h v) -> p h v", h=GH)), ALU.add)
        p.ts("dve", hss[:], hss[:], 1.0 / GDV, RMS_EPS, ALU.mult, ALU.add)
        p.act(hss[:], hss[:], AF.Sqrt)
        p.recip(hss[:], hss[:])
        p.tt("dve", osq[:], srt[b][:], gh[:], ALU.mult)
        for h in range(GH):
            vs = slice(h * GDV, (h + 1) * GDV)
            p.stt("dve", y[:, vs], o[:, vs], hss[:, h:h + 1], osq[:, vs], ALU.mult, ALU.mult)
        for dc in range(8):
            p.transpose(pT[:, dc * 128:(dc + 1) * 128], y[:, dc * 128:(dc + 1) * 128], cb[:])
        p.copy("act", yT[:], pT[:])
        for half in range(2):
            bm = bank()
            for dc in range(8):
                p.matmul(bm[:], yT[:, dc * 128:(dc + 1) * 128],
                         Wo.sub(dc, (slice(None), dc, slice(half * 512, (half + 1) * 512))),
                         start=(dc == 0), stop=(dc == 7))
            p.tt("dve", h1[:, half * 512:(half + 1) * 512], bm[:], xt[b][:, half * 512:(half + 1) * 512], ALU.add)
        p.dma("pool", H1[rows, :], h1[:])
        ffn_prep_tile(p, h1[:], gf, wr, cf, fb, HN1, AFF, rows, bank)
    return p.finish()


NE = 16
CAP = 2 * SEQ // NE
FF = 2048
BISECT_ITERS = 36


def build_L3():
    nc = bass.Bass("TRN2", target_bir_lowering=False)
    p = Prog(nc)
    AFFE = p.dram("AFFE", [2, 128, 128], F32, "ExternalInput")
    HN = p.dram("HN", [SEQ, D], BF16, "ExternalInput")
    wg = p.dram("wg", [2, D, FF], F32, "ExternalInput")
    wu = p.dram("wu", [2, D, FF], F32, "ExternalInput")
    wd = p.dram("wd", [2, FF, D], F32, "ExternalInput")
    cf_d = p.dram("cf", [128, 5, 128], F32, "ExternalInput")
    cb_d = p.dram("cb", [128, 128], BF16, "ExternalInput")
    tok_d = p.dram("tokid", [128, 128], I32, "ExternalInput")
    DELTA = p.dram("DELTA", [SEQ, D], F32, "ExternalOutput")
    IDXA = [p.dram("IDXA%d" % i, [CAP, 2], I32, "Internal") for i in range(2)]

    Wg = p.sb("Wg", [128, 8, FF], BF16)
    Wu = p.sb("Wu", [128, 8, FF], BF16)
    Wd = p.sb("Wd", [128, 16, D], BF16)
    cf = p.sb("cfs", [128, 5, 128], F32)
    cb = p.sb("cbs", [128, 128], BF16)
    tokid = p.sb("tokid_s", [128, 128], I32)
    zt = p.sb("zt", [128, 4096], F32)
    ones = p.sb("ones", [128, 128], F32)
    aff = p.sb("aff", [128, 2, 128], F32)
    cmp_ = p.sb("cmp", [128, 2, 128], F32)
    st = {n: p.sb("b_" + n, [128, 2], F32) for n in ("lo", "hi", "mid", "cnt", "ge", "nge", "t1", "t2")}
    selT = p.sb("selT", [128, 128], F32)
    rp = p.sb("rp", [128, 1], F32)
    slot = p.sb("slot", [128, 128], F32)
    pen = p.sb("pen", [128, 128], F32)
    slot_i = p.sb("slot_i", [128, 128], I32)
    pk = p.sb("pk", [128, 128, 2], I32)
    idx_sb = p.sb("idx_sb", [128, 2, 16, 2], I32)
    xs = [p.sb("xs%d" % i, [128, D], BF16) for i in range(2)]
    xsT = p.sb("xsT", [128, 8, 512], BF16)
    hT = p.sb("hT", [128, 16, 512], BF16)
    sg = [p.sb("sg%d" % i, [128, 512], F32) for i in range(2)]
    y = [p.sb("y%d" % i, [128, D], F32) for i in range(2)]
    banks = [p.ps("bk%d" % i, [128, 512], F32) for i in range(7)]
    pT = p.ps("pT", [128, 1024], BF16)
    bi = [0]

    def bank():
        b = banks[bi[0] % 7]
        bi[0] += 1
        return b

    p.dma("sp", cf[:], cf_d[:])
    p.dma("sp", cb[:], cb_d[:])
    p.dma("sp", tokid[:], tok_d[:])
    p.dma("sp", aff[:], AFFE.v(AFFE.t.rearrange("e p f -> p e f")))
    p.memset("pool", zt[:], 0.0)
    p.memset("dve", ones[:], 1.0)
    zkeys = []
    dz = DELTA.t.rearrange("(k p r) d -> k p (r d)", p=128, r=4)
    for k in range(SEQ // 512):
        zk = ("DELTA", "z", k)
        zkeys.append(zk)
        p.dma("sp", V(dz[k], zk), zt[:])
    US = cf[:, 2, :]
    ident_f = cf[:, 4, :]

    def load_weights(e):
        for dc in range(8):
            p.dma("pool", Wg.sub(dc, (slice(None), dc, slice(None))), V(wg.t[e, dc * 128:(dc + 1) * 128, :], ("wg", e)))
            p.dma("pool", Wu.sub(dc, (slice(None), dc, slice(None))), V(wu.t[e, dc * 128:(dc + 1) * 128, :], ("wu", e)))
        for fc in range(16):
            p.dma("pool", Wd.sub(fc, (slice(None), fc, slice(None))), V(wd.t[e, fc * 128:(fc + 1) * 128, :], ("wd", e)))

    load_weights(0)
    lo, hi, mid, cnt, ge, nge, t1, t2 = (st[n] for n in ("lo", "hi", "mid", "cnt", "ge", "nge", "t1", "t2"))
    p.memset("dve", lo[:], 0.0)
    p.memset("dve", hi[:], 1.0)
    for it in range(BISECT_ITERS):
        p.tt("dve", mid[:], lo[:], hi[:], ALU.add)
        p.ts("dve", mid[:], mid[:], 0.5, None, ALU.mult)
        for e in range(2):
            p.ts("dve", cmp_[:, e, :], aff[:, e, :], mid[:, e:e + 1], None, ALU.is_ge)
        p.reduce("dve", cnt[:], cmp_[:], ALU.add)
        bt = bank()
        p.matmul(bt[:, 0:2], ones[:], cnt[:])
        p.ts("dve", ge[:], bt[:, 0:2], float(CAP) - 0.5, None, ALU.is_ge)
        p.ts("dve", nge[:], ge[:], -1.0, 1.0, ALU.mult, ALU.add)
        p.tt("dve", lo[:], lo[:], nge[:], ALU.mult)
        p.tt("dve", t1[:], mid[:], ge[:], ALU.mult)
        p.tt("dve", lo[:], lo[:], t1[:], ALU.add)
        p.tt("dve", hi[:], hi[:], ge[:], ALU.mult)
        p.tt("dve", t2[:], mid[:], nge[:], ALU.mult)
        p.tt("dve", hi[:], hi[:], t2[:], ALU.add)
    for e in range(2):
        p.ts("dve", cmp_[:, e, :], aff[:, e, :], lo[:, e:e + 1], None, ALU.is_ge)
    p.reduce("dve", cnt[:], cmp_[:], ALU.add)
    pkf = pk.t[:].bitcast(F32)
    for e in range(2):
        bt = bank()
        p.transpose(bt[:, 0:128], cmp_[:, e, :], ident_f)
        p.copy("act", selT[:], bt[:, 0:128])
        bp = bank()
        p.matmul(bp[:, 0:128], selT[:], US)
        p.matmul(bp[:, 128:129], US, cnt[:, e:e + 1])
        p.copy("act", rp[:], bp[:, 128:129])
        p.ts("dve", slot[:], bp[:, 0:128], rp[:, 0:1], None, ALU.add)
        p.ts("dve", pen[:], cmp_[:, e, :], -4096.0, 4096.0, ALU.mult, ALU.add)
        p.tt("dve", slot[:], slot[:], pen[:], ALU.add)
        p.copy("dve", slot_i[:], slot[:])
        p.copy("dve", pk[:, :, 0], tokid[:])
        p.copy("dve", pk.v(pkf[:, :, 1]), aff[:, e, :])
        skeys = []
        for f in range(128):
            sk = ("IDXA", e, f)
            skeys.append(sk)
            p.scatter(V(IDXA[e].t, sk), pk[:, f, :], slot_i[:, f:f + 1], CAP, writes=[sk])
        p.dma("sp", idx_sb[:, e], V(IDXA[e].t.rearrange("(k p) c -> p k c", p=128), ("IDXA", e, "all")), reads=skeys)
    wts = idx_sb.t[:].bitcast(F32)
    first_scatter = True
    for e in range(2):
        if e > 0:
            load_weights(e)
        for tg in range(4):
            for kk in range(4):
                k = tg * 4 + kk
                X = xs[kk % 2]
                p.gather(X[:], HN[:, :], idx_sb[:, e, k, 0:1], SEQ)
                for dc in range(8):
                    p.transpose(pT[:, dc * 128:(dc + 1) * 128], X[:, dc * 128:(dc + 1) * 128], cb[:])
                p.copy("act", xsT[:, :, kk * 128:(kk + 1) * 128], pT.v(pT.t[:].rearrange("p (c t) -> p c t", c=8)))
            for fc in range(16):
                bg_, bu_ = bank(), bank()
                for dc in range(8):
                    p.matmul(bg_[:], Wg.sub(dc, (slice(None), dc, slice(fc * 128, (fc + 1) * 128))), xsT[:, dc, :],
                             start=(dc == 0), stop=(dc == 7))
                for dc in range(8):
                    p.matmul(bu_[:], Wu.sub(dc, (slice(None), dc, slice(fc * 128, (fc + 1) * 128))), xsT[:, dc, :],
                             start=(dc == 0), stop=(dc == 7))
                G = sg[fc % 2]
                p.act(G[:], bg_[:], AF.Silu)
                p.tt("dve", hT[:, fc, :], G[:], bu_[:], ALU.mult)
            for kk in range(4):
                k = tg * 4 + kk
                Y = y[kk % 2]
                for half in range(2):
                    by = bank()
                    for fc in range(16):
                        p.matmul(by[:], hT[:, fc, kk * 128:(kk + 1) * 128],
                                 Wd.sub(fc, (slice(None), fc, slice(half * 512, (half + 1) * 512))),
                                 start=(fc == 0), stop=(fc == 15))
                    p.ts("dve", Y[:, half * 512:(half + 1) * 512], by[:], idx_sb.v(wts[:, e, k, 1:2]), None, ALU.mult)
                p.scatter(DELTA[:, :], Y[:], idx_sb[:, e, k, 0:1], SEQ, accum=True,
                          reads=zkeys if first_scatter else None)
                first_scatter = False
    return p.finish()


MH = 16
QRANK = 256
KVR = 128
NOPE = 128
ROPE = 64
MLA_SCALE = float(NOPE + ROPE) ** -0.5
TWO_PI = 2.0 * np.pi


def rope_consts():
    half = ROPE // 2
    inv = (10000.0 ** (-np.arange(half, dtype=np.float32) / half)).astype(np.float32)
    rc = np.zeros((64, 4), np.float32)
    rc[:, 0] = np.concatenate([inv, inv])
    rc[:, 1] = np.concatenate([np.ones(half), -np.ones(half)])
    rc[:, 2] = -np.pi
    return rc


def build_L4():
    nc = bass.Bass("TRN2", target_bir_lowering=False)
    p = Prog(nc)
    H1 = p.dram("H1", [TOK, D], F32, "ExternalInput")
    DS = p.dram("DS", [NCORES, TOK, D], F32, "ExternalInput")
    pos = p.dram("pos", [1, TOK], I32, "ExternalInput")
    g_mix = p.dram("g_mix", [1, D], F32, "ExternalInput")
    w_m = p.dram("w_m", [D, 448], F32, "ExternalInput")
    g_q = p.dram("g_q", [1, QRANK], F32, "ExternalInput")
    g_kv = p.dram("g_kv", [1, KVR], F32, "ExternalInput")
    w_uq = p.dram("w_uq", [QRANK, MH * 192], F32, "ExternalInput")
    w_ukv = p.dram("w_ukv", [KVR, MH * 256], F32, "ExternalInput")
    rc_d = p.dram("rc", [64, 4], F32, "ExternalInput")
    cf_d = p.dram("cf", [128, 5, 128], F32, "ExternalInput")
    cb_d = p.dram("cb", [128, 128], BF16, "ExternalInput")
    H2 = p.dram("H2", [TOK, D], F32, "ExternalOutput")
    QL = p.dram("QL", [MH, 128, TOK], BF16, "ExternalOutput")
    QR = p.dram("QR", [MH, 64, TOK], BF16, "ExternalOutput")
    CKVT = p.dram("CKVT", [128, TOK], BF16, "ExternalOutput")
    KRT = p.dram("KRT", [64, TOK], BF16, "ExternalOutput")
    CKV = p.dram("CKV", [TOK, 128], BF16, "ExternalOutput")
    KN2 = p.dram("KN2", [TOK, 1], F32, "ExternalOutput")
    QMAX = p.dram("QMAX", [1, MH], F32, "ExternalOutput")

    cf = p.sb("cfs", [128, 5, 128], F32)
    cb = p.sb("cbs", [128, 128], BF16)
    rc = p.sb("rcs", [64, 4], F32)
    gm = p.sb("gm", [128, D], F32)
    gq = p.sb("gq", [128, QRANK], F32)
    gkv = p.sb("gkv", [128, KVR], F32)
    Wm = p.sb("Wm", [128, 8, 448], BF16)
    Wuq = p.sb("Wuq", [128, 2, MH * 192], BF16)
    Wukv = p.sb("Wukv", [128, MH * 256], BF16)
    WukT = p.sb("WukT", [128, MH, 128], BF16)
    posi = p.sb("posi", [64, TOK], I32)
    ang = p.sb("ang", [64, TOK], F32)
    C2 = p.sb("C2", [64, TOK], F32)
    S2 = p.sb("S2", [64, TOK], F32)
    hnT = p.sb("hnT", [128, 8, TOK], BF16)
    cqT = p.sb("cqT", [128, 2, TOK], BF16)
    ht = [p.sb("ht%d" % i, [128, D], F32) for i in range(2)]
    dt_ = [p.sb("dt%d" % i, [128, D], F32) for i in range(3)]
    sq = p.sb("sq", [128, D], F32)
    ss = p.sb("ss", [128, 1], F32)
    rstd = p.sb("rstd", [128, 1], F32)
    hn = p.sb("hn", [128, D], BF16)
    cs = p.sb("cs", [128, 448], F32)
    cn = p.sb("cn", [128, 384], BF16)
    kn = p.sb("kn", [128, 2], F32)
    qn = p.sb("qn", [128, 512], BF16)
    qlf = p.sb("qlf", [128, 512], F32)
    qlb = p.sb("qlb", [128, 512], BF16)
    qsq = p.sb("qsq", [128, 512], F32)
    r1 = p.sb("r1", [64, 512], F32)
    r2 = p.sb("r2", [64, 512], F32)
    rb = p.sb("rb", [64, 512], BF16)
    qmx = p.sb("qmx", [1, MH], F32)
    qm1 = p.sb("qm1", [1, 1], F32)
    ckT = p.sb("ckT", [128, 128], BF16)
    banks = [p.ps("bk%d" % i, [128, 512], F32) for i in range(7)]
    pT = p.ps("pT", [128, 1024], BF16)
    bi = [0]

    def bank():
        b = banks[bi[0] % 7]
        bi[0] += 1
        return b

    p.dma("sp", cf[:], cf_d[:])
    p.dma("sp", cb[:], cb_d[:])
    p.dma("sp", rc[:], rc_d[:])
    p.dma("sp", gm[:], g_mix.v(g_mix.t[0:1, :].broadcast_to([128, D])))
    p.dma("sp", gq[:], g_q.v(g_q.t[0:1, :].broadcast_to([128, QRANK])))
    p.dma("sp", gkv[:], g_kv.v(g_kv.t[0:1, :].broadcast_to([128, KVR])))
    p.dma("sp", posi[:], pos.v(pos.t[0:1, :].broadcast_to([64, TOK])))
    p.dma("pool", Wm[:], w_m.v(w_m.t.rearrange("(c p) n -> p c n", p=128)))
    p.dma("pool", Wuq[:], w_uq.v(w_uq.t.rearrange("(c p) n -> p c n", p=128)))
    p.dma("pool", Wukv[:], w_ukv[:, :])
    onesf = p.sb("onesf", [128, 1], F32)
    p.memset("dve", onesf[:], 1.0)
    p.memset("dve", qmx[:], 0.0)
    Wm_sw = p.sb("Wm_sw", [128, 8, 64], BF16)
    Wuq_sw = p.sb("Wuq_sw", [128, 2, MH, 64], BF16)
    p.copy("dve", Wm_sw[:, :, 0:32], Wm[:, :, 416:448])
    p.copy("dve", Wm_sw[:, :, 32:64], Wm[:, :, 384:416])
    for kc in range(2):
        wv = Wuq.t[:, kc, :].rearrange("p (h c) -> p h c", h=MH)
        p.copy("dve", Wuq_sw[:, kc, :, 0:32], Wuq.v(wv[:, :, 160:192]))
        p.copy("dve", Wuq_sw[:, kc, :, 32:64], Wuq.v(wv[:, :, 128:160]))
    for h in range(MH):
        p.transpose(pT[:, (h % 8) * 128:(h % 8 + 1) * 128], Wukv[:, h * 256:h * 256 + 128], cb[:])
        if h % 8 == 7:
            g0 = h - 7
            p.copy("act", WukT[:, g0:g0 + 8, :], pT.v(pT.t[:].rearrange("p (h r) -> p h r", h=8)))
    p.copy("dve", ang[:], posi[:])
    p.ts("dve", ang[:], ang[:], rc[:, 0:1], None, ALU.mult)
    C1 = 6.28125
    C2_ = float(TWO_PI - 6.28125)
    ki = p.sb("ki", [64, TOK], I32)
    kf = p.sb("kf", [64, TOK], F32)
    gt = p.sb("gt", [64, TOK], F32)
    p.ts("dve", kf[:], ang[:], float(1.0 / TWO_PI), None, ALU.mult)
    p.copy("dve", ki[:], kf[:])
    p.copy("dve", kf[:], ki[:])
    p.stt("dve", ang[:], kf[:], -C1, ang[:], ALU.mult, ALU.add)
    p.stt("dve", ang[:], kf[:], -C2_, ang[:], ALU.mult, ALU.add)

    def fold(t):
        p.ts("dve", gt[:], t[:], float(np.pi), None, ALU.is_gt)
        p.stt("dve", t[:], gt[:], -TWO_PI, t[:], ALU.mult, ALU.add)
        p.ts("dve", gt[:], t[:], float(-np.pi), None, ALU.is_lt)
        p.stt("dve", t[:], gt[:], TWO_PI, t[:], ALU.mult, ALU.add)
        p.ts("dve", t[:], t[:], float(np.pi), float(-np.pi), ALU.min, ALU.max)

    fold(ang)
    p.ts("dve", C2[:], ang[:], float(np.pi / 2), None, ALU.add)
    fold(C2)
    p.act(S2[:], ang[:], AF.Sin)
    p.ts("dve", S2[:], S2[:], rc[:, 1:2], -1.0, ALU.mult, ALU.mult)
    p.act(C2[:], C2[:], AF.Sin)

    for j in range(NT):
        rows = slice(j * 128, (j + 1) * 128)
        Ht = ht[j % 2]
        p.dma("sp", Ht[:], H1[rows, :])
        for c in range(NCORES):
            Dt = dt_[c % 3]
            p.dma("sp", Dt[:], DS.v(DS.t[c, rows, :]))
            p.tt("dve", Ht[:], Ht[:], Dt[:], ALU.add)
        p.dma("pool", H2[rows, :], Ht[:])
        rmsnorm_tile(p, Ht[:], gm[:], hn[:], sq[:], ss[:], rstd[:])
        for dc in range(8):
            p.transpose(pT[:, dc * 128:(dc + 1) * 128], hn[:, dc * 128:(dc + 1) * 128], cb[:])
        p.copy("act", hnT[:, :, rows], pT.v(pT.t[:].rearrange("p (c t) -> p c t", c=8)))
        bc_ = bank()
        for dc in range(8):
            p.matmul(bc_[:, 0:448], hnT[:, dc, rows], Wm[:, dc, :], start=(dc == 0), stop=(dc == 7))
        p.copy("act", cs[:], bc_[:, 0:448])
        rmsnorm_tile(p, cs[:, 0:256], gq[:], cn[:, 0:256], sq[:, 0:256], ss[:], rstd[:], width=QRANK)
        rmsnorm_tile(p, cs[:, 256:384], gkv[:], cn[:, 256:384], sq[:, 0:128], ss[:], rstd[:], width=KVR)
        p.dma("pool", CKV[rows, :], cn[:, 256:384])
        p.tt("dve", sq[:, 0:128], cn[:, 256:384], cn[:, 256:384], ALU.mult)
        p.reduce("dve", kn[:, 0:1], sq[:, 0:128], ALU.add)
        p.tt("dve", sq[:, 0:64], cs[:, 384:448], cs[:, 384:448], ALU.mult)
        p.reduce("dve", kn[:, 1:2], sq[:, 0:64], ALU.add)
        p.tt("dve", kn[:, 0:1], kn[:, 0:1], kn[:, 1:2], ALU.add)
        p.dma("pool", KN2[rows, :], kn[:, 0:1])
        for q in range(3):
            p.transpose(pT[:, q * 128:(q + 1) * 128], cn[:, q * 128:(q + 1) * 128], cb[:])
        p.copy("act", cqT[:, :, rows], pT.v(pT.t[:, 0:256].rearrange("p (c t) -> p c t", c=2)))
        p.copy("act", ckT[:], pT[:, 256:384])
        p.dma("pool", CKVT[:, rows], ckT[:])

    for tg in range(TOK // 512):
        cols = slice(tg * 512, (tg + 1) * 512)
        bk_, bks = bank(), bank()
        for dc in range(8):
            p.matmul(bk_[0:64, :], Wm[:, dc, 384:448], hnT[:, dc, cols], start=(dc == 0), stop=(dc == 7))
        for dc in range(8):
            p.matmul(bks[0:64, :], Wm_sw[:, dc, :], hnT[:, dc, cols], start=(dc == 0), stop=(dc == 7))
        p.tt("dve", r1[:], bk_[0:64, :], C2[:, cols], ALU.mult)
        p.tt("dve", r2[:], bks[0:64, :], S2[:, cols], ALU.mult)
        p.tt("dve", rb[:], r1[:], r2[:], ALU.add)
        p.dma("pool", KRT[:, cols], rb[:])
        for h in range(MH):
            o = h * 192
            bq = bank()
            for kc in range(2):
                p.matmul(bq[:], Wuq[:, kc, o:o + 128], cqT[:, kc, cols], start=(kc == 0), stop=(kc == 1))
            p.copy("act", qn[:], bq[:])
            bl = bank()
            p.matmul(bl[:], WukT[:, h, :], qn[:])
            p.ts("dve", qlf[:], bl[:], MLA_SCALE, None, ALU.mult)
            p.copy("act", qlb[:], qlf[:])
            p.dma("pool", QL.v(QL.t[h, :, cols]), qlb[:])
            br, brs = bank(), bank()
            for kc in range(2):
                p.matmul(br[0:64, :], Wuq[:, kc, o + 128:o + 192], cqT[:, kc, cols], start=(kc == 0), stop=(kc == 1))
            for kc in range(2):
                p.matmul(brs[0:64, :], Wuq_sw[:, kc, h, :], cqT[:, kc, cols], start=(kc == 0), stop=(kc == 1))
            p.tt("dve", r1[:], br[0:64, :], C2[:, cols], ALU.mult)
            p.tt("dve", r2[:], brs[0:64, :], S2[:, cols], ALU.mult)
            p.tt("dve", r1[:], r1[:], r2[:], ALU.add)
            p.ts("dve", r1[:], r1[:], MLA_SCALE, None, ALU.mult)
            p.copy("act", rb[:], r1[:])
            p.dma("pool", QR.v(QR.t[h, :, cols]), rb[:])
            p.tt("dve", qsq[:], qlf[:], qlf[:], ALU.mult)
            p.tt("dve", r2[:], r1[:], r1[:], ALU.mult)
            bn = bank()
            p.matmul(bn[0:1, :], onesf[:, 0:1], qsq[:], start=True, stop=False)
            p.matmul(bn[0:1, :], onesf[0:64, 0:1], r2[:], start=False, stop=True)
            p.reduce("dve", qm1[:], bn[0:1, :], ALU.max)
            p.tt("dve", qmx[:, h:h + 1], qmx[:, h:h + 1], qm1[:], ALU.max)
    p.dma("pool", QMAX[:, :], qmx[:])
    return p.finish()


NKT = SEQ // 128


def build_L5(nheads=None, same=True):
    nc = bass.Bass("TRN2", target_bir_lowering=False)
    p = Prog(nc)
    QL = p.dram("QL", [MH, 128, TOK], BF16, "ExternalInput")
    QR = p.dram("QR", [MH, 64, TOK], BF16, "ExternalInput")
    p.same = same
    KTd = p.dram("KT", [128, SEQ], BF16, "ExternalInput")
    KRd = p.dram("KR", [64, SEQ], BF16, "ExternalInput")
    Vd = p.dram("VK", [SEQ, 128], BF16, "ExternalInput")
    KN2 = p.dram("KN2", [128, 128], F32, "ExternalInput")
    QMAX = p.dram("QMAX", [1, MH], F32, "ExternalInput")
    H2 = p.dram("H2", [TOK, D], F32, "ExternalInput")
    w_ukv = p.dram("w_ukv", [KVR, MH * 256], F32, "ExternalInput")
    w_o = p.dram("w_o", [MH * 128, D], F32, "ExternalInput")
    g_ffn = p.dram("g_ffn", [1, D], F32, "ExternalInput")
    w_r = p.dram("w_r", [D, 16], F32, "ExternalInput")
    cf_d = p.dram("cf", [128, 5, 128], F32, "ExternalInput")
    cb_d = p.dram("cb", [128, 128], BF16, "ExternalInput")
    H3 = p.dram("H3", [TOK, D], F32, "ExternalOutput")
    HN3 = p.dram("HN3", [TOK, D], BF16, "ExternalOutput")
    AFF = p.dram("AFF", [TOK, 16], F32, "ExternalOutput")
    OTD = p.dram("OTD", [MH, 128, TOK], BF16, "Internal")

    KT = p.sb("KTs", [128, SEQ], BF16)
    KR2 = p.sb("KR2", [128, SEQ // 2], BF16)
    Vt = p.sb("Vt", [128, NKT, 128], BF16)
    Wo = p.sb("Wo", [128, MH, D], BF16)
    Wuv = p.sb("Wuv", [128, MH, 128], BF16)
    wr = p.sb("wr", [128, 8, 16], F32)
    cf = p.sb("cfs", [128, 5, 128], F32)
    cb = p.sb("cbs", [128, 128], BF16)
    gf = p.sb("gf", [128, D], F32)
    onesf = p.sb("onesf", [128, 128], F32)
    kn = p.sb("kn", [128, 128], F32)
    km = p.sb("km", [128, 1], F32)
    km1 = p.sb("km1", [1, 1], F32)
    qmx = p.sb("qmx", [1, MH], F32)
    negm = p.sb("negm", [128, MH], F32)
    QW = 1024
    sbk = [p.ps("sbk%d" % i, [128, QW], F32) for i in range(2)]
    obk = p.ps("obk", [128, QW], F32)
    ebk = [p.ps("ebk%d" % i, [128, 512], F32) for i in range(2)]
    mi = [0]

    def bank():
        b = ebk[mi[0] % 2]
        mi[0] += 1
        return b

    p.dma("sp", cf[:], cf_d[:])
    p.dma("sp", cb[:], cb_d[:])
    p.dma("sp", kn[:], KN2[:, :])
    p.dma("sp", qmx[:], QMAX[:, :])
    p.dma("sp", gf[:], g_ffn.v(g_ffn.t[0:1, :].broadcast_to([128, D])))
    p.dma("sp", wr[:], w_r.v(w_r.t.rearrange("(c p) e -> p c e", p=128)))
    for q in range(8):
        cs_ = slice(q * 2048, (q + 1) * 2048)
        p.dma("sp", KT.sub(q, (slice(None), cs_)), V(KTd.t[:, cs_], ("KTd", q)))
    for half in range(2):
        for q in range(4):
            cs_ = slice(q * 2048, (q + 1) * 2048)
            p.dma("sp", KR2.sub((half, q), (slice(half * 64, (half + 1) * 64), cs_)),
                  V(KRd.t[:, half * 8192 + q * 2048: half * 8192 + (q + 1) * 2048], ("KRd", half, q)))
    for q in range(8):
        p.dma("sp", Vt.sub(q, (slice(None), slice(q * 16, (q + 1) * 16), slice(None))),
              V(Vd.t[q * 2048:(q + 1) * 2048, :].rearrange("(k p) r -> p k r", p=128), ("Vd", q)))
    p.dma("pool", Wuv[:], w_ukv.v(w_ukv.t.rearrange("r (h c) -> r h c", h=MH)[:, :, 128:256]))
    for h in range(MH):
        p.dma("pool", Wo.sub(h, (slice(None), h, slice(None))), V(w_o.t[h * 128:(h + 1) * 128, :], ("w_o", h)))
    p.memset("dve", onesf[:], 1.0)
    p.reduce("dve", km[:], kn[:], ALU.max)
    bt = bank()
    p.transpose(bt[0:1, 0:128], km[:, 0:1], cf[:, 4, :])
    p.reduce("dve", km1[:], bt[0:1, 0:128], ALU.max)
    p.ts("dve", qmx[:], qmx[:], km1[0:1, 0:1], None, ALU.mult)
    p.act(qmx[:], qmx[:], AF.Sqrt)
    p.ts("dve", qmx[:], qmx[:], -1.0, None, ALU.mult)
    bb = bank()
    p.matmul(bb[:, 0:MH], onesf[0:1, :], qmx[:])
    p.copy("act", negm[:], bb[:, 0:MH])

    p.push_scope()
    ql = [p.sb("ql%d" % i, [128, TOK], BF16) for i in range(2)]
    qr = [p.sb("qr%d" % i, [128, TOK], BF16) for i in range(2)]
    PT = [p.sb("PT%d" % i, [128, QW], BF16) for i in range(3)]
    accD = [p.sb("accD%d" % i, [128, QW], F32) for i in range(2)]
    accP = [p.sb("accP%d" % i, [128, QW], F32) for i in range(2)]
    rl = p.sb("rl", [128, QW], F32)
    OTn = p.sb("OTn", [128, QW], BF16)
    oTs = [p.sb("oTs%d" % i, [128, QW], BF16) for i in range(2)]
    NH = MH if nheads is None else nheads
    it = 0
    for h in range(NH):
        Ql, Qr = ql[h % 2], qr[h % 2]
        p.dma("sp", Ql[:], QL.v(QL.t[h]))
        p.dma("sp", Qr[0:64, :], QR.v(QR.t[h]))
        p.dma("sp", Qr[64:128, :], QR.v(QR.t[h]))
        for qg in range(TOK // QW):
            q0 = qg * QW
            bo = obk
            AD, AP_ = accD[it % 2], accP[it % 2]
            it += 1
            first = {"dve": True, "pool": True}

            def s_mm(kt):
                bs = sbk[kt % 2]
                ktile = KT.sub(kt // 16, (slice(None), slice(kt * 128, (kt + 1) * 128)))
                hf = kt // 64
                kc = kt % 64
                rtile = KR2.sub((hf, kc // 16), (slice(hf * 64, (hf + 1) * 64), slice(kc * 128, (kc + 1) * 128)))
                for hh in range(QW // 512):
                    p.matmul(bs[:, hh * 512:(hh + 1) * 512], ktile, Ql[:, q0 + hh * 512:q0 + (hh + 1) * 512],
                             start=True, stop=False)
                if kt >= 1:
                    p.wait("pe", [PT[(kt - 1) % 3].name])
                for hh in range(QW // 512):
                    p.matmul(bs[:, hh * 512:(hh + 1) * 512], rtile,
                             Qr[hf * 64:(hf + 1) * 64, q0 + hh * 512:q0 + (hh + 1) * 512], start=False, stop=True)

            s_mm(0)
            for kt in range(NKT):
                if kt + 1 < NKT:
                    s_mm(kt + 1)
                P_ = PT[kt % 3]
                p.act(P_[:], sbk[kt % 2][:], AF.Exp, bias=negm[:, h:h + 1])
                vt = Vt.sub(kt // 16, (slice(None), kt, slice(None)))
                for hh in range(QW // 512):
                    p.matmul(bo[:, hh * 512:(hh + 1) * 512], vt, P_[:, hh * 512:(hh + 1) * 512],
                             start=(kt == 0), stop=(kt == NKT - 1))
                eng = "pool" if kt % 3 == 2 else "dve"
                A = AP_ if eng == "pool" else AD
                if first[eng]:
                    p.copy(eng, A[:], P_[:])
                    first[eng] = False
                else:
                    p.tt(eng, A[:], A[:], P_[:], ALU.add)
            p.tt("dve", AD[:], AD[:], AP_[:], ALU.add)
            for hh in range(QW // 512):
                hs = slice(hh * 512, (hh + 1) * 512)
                bl = ebk[hh]
                p.matmul(bl[:], onesf[:], AD[:, hs])
                p.recip(rl[:, hs], bl[:])
                p.tt("dve", OTn[:, hs], bo[:, hs], rl[:, hs], ALU.mult)
            oT = oTs[it % 2]
            for hh in range(QW // 512):
                hs = slice(hh * 512, (hh + 1) * 512)
                bv = ebk[hh]
                p.matmul(bv[:], Wuv[:, h, :], OTn[:, hs])
                p.copy("act", oT[:, hs], bv[:])
            p.dma("pool", V(OTD.t[h, :, q0:q0 + QW], ("OTD", h, qg)), oT[:])
    otd_keys = [("OTD", h, qg) for h in range(NH) for qg in range(TOK // QW)]
    p.pop_scope()
    oTt = [p.sb("oTt%d" % i, [128, MH, 128], BF16) for i in range(2)]
    h2t = [p.sb("h2t%d" % i, [128, D], F32) for i in range(2)]
    h3 = p.sb("h3", [128, D], F32)
    fb = ffn_prep_bufs(p)

    for j in range(NT):
        rows = slice(j * 128, (j + 1) * 128)
        b = j % 2
        p.dma("sp", h2t[b][:], H2[rows, :])
        p.dma("sp", oTt[b][:], V(OTD.t[:, :, rows].rearrange("h d t -> d h t"), ("OTD", "rd", j)),
              reads=[k_ for k_ in otd_keys if k_[2] == j // (QW // 128)])
        for half in range(2):
            bm = bank()
            for h in range(MH):
                p.matmul(bm[:], oTt[b][:, h, :], Wo.sub(h, (slice(None), h, slice(half * 512, (half + 1) * 512))),
                         start=(h == 0), stop=(h == MH - 1))
            p.tt("dve", h3[:, half * 512:(half + 1) * 512], bm[:], h2t[b][:, half * 512:(half + 1) * 512], ALU.add)
        p.dma("pool", H3[rows, :], h3[:])
        ffn_prep_tile(p, h3[:], gf, wr, cf, fb, HN3, AFF, rows, bank)
    return p.finish()


def build_L7():
    nc = bass.Bass("TRN2", target_bir_lowering=False)
    p = Prog(nc)
    H3 = p.dram("H3", [TOK, D], F32, "ExternalInput")
    DS = p.dram("DS", [NCORES, TOK, D], F32, "ExternalInput")
    g_fin = p.dram("g_fin", [1, D], F32, "ExternalInput")
    OUT = p.dram("OUT", [TOK, D], F32, "ExternalOutput")
    gm = p.sb("gm", [128, D], F32)
    ht = [p.sb("ht%d" % i, [128, D], F32) for i in range(2)]
    dt_ = [p.sb("dt%d" % i, [128, D], F32) for i in range(3)]
    sq = p.sb("sq", [128, D], F32)
    ss = p.sb("ss", [128, 1], F32)
    rstd = p.sb("rstd", [128, 1], F32)
    ot = [p.sb("ot%d" % i, [128, D], F32) for i in range(2)]
    p.dma("sp", gm[:], g_fin.v(g_fin.t[0:1, :].broadcast_to([128, D])))
    for j in range(NT):
        rows = slice(j * 128, (j + 1) * 128)
        Ht = ht[j % 2]
        p.dma("sp", Ht[:], H3[rows, :])
        for c in range(NCORES):
            Dt = dt_[c % 3]
            p.dma("sp", Dt[:], DS.v(DS.t[c, rows, :]))
            p.tt("dve", Ht[:], Ht[:], Dt[:], ALU.add)
        rmsnorm_tile(p, Ht[:], gm[:], ot[j % 2][:], sq[:], ss[:], rstd[:])
        p.dma("pool", OUT[rows, :], ot[j % 2][:])
    return p.finish()


def run(nc, in_maps):
    res = run_bass_kernel_spmd(nc, in_maps, core_ids=list(range(NCORES)))
    return res.results


_CACHE = {}


def _prog(name, builder):
    if name not in _CACHE:
        _CACHE[name] = builder()
    return _CACHE[name]


def _c(a):
    return np.ascontiguousarray(a)


def stage_L1(inp):
    cf, cb = const_tables()
    x = inp["x"][0]
    wg = _c(np.stack([inp["gla_w_gate_up_f"][0], inp["gla_w_gate_up_b"][0]]))
    bg = _c(np.stack([inp["gla_b_gate_f"][0], inp["gla_b_gate_b"][0]]))
    maps = [dict(x=_c(x[c * TOK:(c + 1) * TOK]), g_mix=_c(inp["mix_norm"][0:1]), w_in=_c(inp["gla_w_in"][0]),
                 wg=wg, bg=bg, cf=cf, cb=cb) for c in range(NCORES)]
    return run(_prog("L1", build_L1), maps)


def stage_L2(inp, r1):
    cf, cb = const_tables()
    x = inp["x"][0]
    maps = []
    for c in range(NCORES):
        BP = np.zeros((2, 7, GH, 128, GDV), np.float32)
        APD = np.zeros((2, 7, 128, NT * GH), np.float32)
        for i in range(7):
            cf_ = c - 7 + i
            if cf_ >= 0:
                BP[0, i] = r1[cf_]["BE"][0]
                APD[0, i] = r1[cf_]["AA"][0]
            cb_ = c + 7 - i
            if cb_ <= NCORES - 1:
                BP[1, i] = r1[cb_]["BE"][1]
                APD[1, i] = r1[cb_]["AA"][1]
        maps.append(dict(x=_c(x[c * TOK:(c + 1) * TOK]), OL=r1[c]["OL"], QP=r1[c]["QP"], SR=r1[c]["SR"],
                         AA=r1[c]["AA"], BP=BP, APD=APD, g_head=_c(inp["gla_head_norm"][0:1]),
                         w_out=_c(inp["gla_w_out"][0]), g_ffn=_c(inp["ffn_norm"][0:1]),
                         w_r=_c(inp["moe_w_router"][0]), cf=cf, cb=cb))
    return run(_prog("L2", build_L2), maps)


def stage_L3(inp, layer, aff_full, hn_full):
    cf, cb = const_tables()
    tokid = np.arange(SEQ, dtype=np.int32).reshape(128, 128)
    maps = []
    for c in range(NCORES):
        affe = _c(aff_full[:, 2 * c:2 * c + 2].T.reshape(2, 128, 128))
        maps.append(dict(AFFE=affe, HN=hn_full, wg=_c(inp["moe_w_gate"][layer, 2 * c:2 * c + 2]),
                         wu=_c(inp["moe_w_up"][layer, 2 * c:2 * c + 2]), wd=_c(inp["moe_w_down"][layer, 2 * c:2 * c + 2]),
                         cf=cf, cb=cb, tokid=tokid))
    return run(_prog("L3", build_L3), maps)


def stage_L4(inp, h_prev, deltas):
    cf, cb = const_tables()
    rc = rope_consts()
    maps = []
    for c in range(NCORES):
        DS = _c(np.stack([d[c * TOK:(c + 1) * TOK] for d in deltas]))
        maps.append(dict(H1=h_prev[c], DS=DS, pos=_c(inp["positions"][0:1, c * TOK:(c + 1) * TOK]),
                         g_mix=_c(inp["mix_norm"][1:2]), w_m=_c(inp["mla_w_in"][0]), g_q=_c(inp["mla_q_norm"][0:1]),
                         g_kv=_c(inp["mla_kv_norm"][0:1]), w_uq=_c(inp["mla_w_uq"][0]), w_ukv=_c(inp["mla_w_ukv"][0]),
                         rc=rc, cf=cf, cb=cb))
    return run(_prog("L4", build_L4), maps)


def stage_L5(inp, r4):
    cf, cb = const_tables()
    KT = _c(np.concatenate([r4[c]["CKVT"] for c in range(NCORES)], axis=1))
    KR = _c(np.concatenate([r4[c]["KRT"] for c in range(NCORES)], axis=1))
    VK = _c(np.concatenate([r4[c]["CKV"] for c in range(NCORES)], axis=0))
    KN2 = _c(np.concatenate([r4[c]["KN2"] for c in range(NCORES)], axis=0).reshape(128, 128))
    maps = []
    for c in range(NCORES):
        maps.append(dict(QL=r4[c]["QL"], QR=r4[c]["QR"], KT=KT, KR=KR, VK=VK, KN2=KN2, QMAX=r4[c]["QMAX"],
                         H2=r4[c]["H2"], w_ukv=_c(inp["mla_w_ukv"][0]), w_o=_c(inp["mla_w_out"][0]),
                         g_ffn=_c(inp["ffn_norm"][1:2]), w_r=_c(inp["moe_w_router"][1]), cf=cf, cb=cb))
    return run(_prog("L5", build_L5), maps)


def stage_L7(inp, h_prev, deltas):
    maps = []
    for c in range(NCORES):
        DS = _c(np.stack([d[c * TOK:(c + 1) * TOK] for d in deltas]))
        maps.append(dict(H3=h_prev[c], DS=DS, g_fin=_c(inp["final_norm"].reshape(1, D))))
    return run(_prog("L7", build_L7), maps)


def kernel(**inputs):
    inp = {k: np.asarray(v) for k, v in inputs.items()}
    r1 = stage_L1(inp)
    r2 = stage_L2(inp, r1)
    aff0 = _c(np.concatenate([r2[c]["AFF"] for c in range(NCORES)], axis=0))
    hn1 = _c(np.concatenate([r2[c]["HN1"] for c in range(NCORES)], axis=0))
    r3 = stage_L3(inp, 0, aff0, hn1)
    r4 = stage_L4(inp, [r2[c]["H1"] for c in range(NCORES)], [r3[c]["DELTA"] for c in range(NCORES)])
    del r3
    r5 = stage_L5(inp, r4)
    aff1 = _c(np.concatenate([r5[c]["AFF"] for c in range(NCORES)], axis=0))
    hn3 = _c(np.concatenate([r5[c]["HN3"] for c in range(NCORES)], axis=0))
    r6 = stage_L3(inp, 1, aff1, hn3)
    r7 = stage_L7(inp, [r5[c]["H3"] for c in range(NCORES)], [r6[c]["DELTA"] for c in range(NCORES)])
    out = np.concatenate([r7[c]["OUT"] for c in range(NCORES)], axis=0).astype(np.float32)
    return out.reshape(1, SEQ, D)
```

```python
import contextlib
import numpy as np
import ml_dtypes
import concourse.bass as bass
import concourse.mybir as mybir
from concourse.bass_utils import run_bass_kernel_spmd

F32 = mybir.dt.float32
BF16 = mybir.dt.bfloat16
I32 = mybir.dt.int32
AF = mybir.ActivationFunctionType
ALU = mybir.AluOpType
AX = mybir.AxisListType

NCORES = 8
SEQ = 16384
TOK = SEQ // NCORES
NT = TOK // 128
D = 1024
RMS_EPS = 1e-6


class V:
    __slots__ = ("ap", "key")

    def __init__(self, ap, key):
        self.ap = ap
        self.key = key


class Buf:
    def __init__(self, t, name):
        self.t = t
        self.name = name

    def __getitem__(self, idx):
        return V(self.t[idx], self.name)

    def sub(self, s, idx):
        return V(self.t[idx], (self.name, s))

    def v(self, ap, s=None):
        return V(ap, self.name if s is None else (self.name, s))


class Prog:
    ENGS = ("pe", "dve", "act", "pool", "sp")
    NDMA = {"sp": 10, "pool": 8, "act": 4}

    def __init__(self, nc, same_engine_sync=True):
        self.nc = nc
        self.same = same_engine_sync
        self.stack = contextlib.ExitStack()
        self.lists = {e: [] for e in self.ENGS}
        self.cnt = {e: 0 for e in self.ENGS}
        self.seen = {e: {} for e in self.ENGS}
        self.last_w = {}
        self.readers = {}
        self.sem = {}
        self.dma_cnt = {}
        self.dma_rr = {q: 0 for q in self.NDMA}
        for e in ("pe", "dve", "act", "pool"):
            self.sem[("c", e)] = self.stack.enter_context(nc.semaphore("c_" + e))
        for q, n in self.NDMA.items():
            for k in range(n):
                sk = ("d", q, k)
                self.sem[sk] = self.stack.enter_context(nc.semaphore("d_%s%d" % (q, k)))
                self.dma_cnt[sk] = 0
        self.psum = set()
        self.bregs = {}
        self.same_dist = 2

    def sb(self, name, shape, dt):
        t = self.stack.enter_context(self.nc.sbuf_tensor(name, list(shape), dt))
        return Buf(t, name)

    def ps(self, name, shape, dt):
        t = self.stack.enter_context(self.nc.psum_tensor(name, list(shape), dt))
        self.psum.add(name)
        return Buf(t, name)

    def dram(self, name, shape, dt, kind):
        t = self.nc.dram_tensor(name, list(shape), dt, kind=kind)
        return Buf(t.ap(), name)

    def emit(self, eng, fn, reads, writes, dma=False):
        deps = {}

        def need(tok):
            if tok is None:
                return
            sk, val = tok
            if deps.get(sk, 0) < val:
                deps[sk] = val

        for r in reads:
            need(self.last_w.get(r))
            if r in self.psum:
                for sk, val in self.readers.get(r, {}).items():
                    if sk != ("c", eng):
                        need((sk, val))
        for w in writes:
            need(self.last_w.get(w))
            for sk, val in self.readers.get(w, {}).items():
                need((sk, val))
        if dma:
            k = self.dma_rr[eng]
            self.dma_rr[eng] = (k + 1) % self.NDMA[eng]
            sk = ("d", eng, k)
            if self.dma_cnt[sk] > 0:
                need((sk, self.dma_cnt[sk]))
            self.dma_cnt[sk] += 16
            tok = (sk, self.dma_cnt[sk])
        else:
            self.cnt[eng] += 1
            tok = (("c", eng), self.cnt[eng])
        waits = []
        for sk, val in deps.items():
            if sk == ("c", eng) and (eng == "pe" or not self.same):
                continue
            if sk == ("c", eng) and tok[1] - val > self.same_dist:
                continue
            if self.seen[eng].get(sk, 0) >= val:
                continue
            self.seen[eng][sk] = val
            waits.append((sk, val))
        self.lists[eng].append((waits, fn, tok))
        for r in reads:
            d = self.readers.setdefault(r, {})
            if d.get(tok[0], 0) < tok[1]:
                d[tok[0]] = tok[1]
        for w in writes:
            self.last_w[w] = tok
            self.readers[w] = {}

    @staticmethod
    def _keys(*vs):
        return [v.key for v in vs if isinstance(v, V)]

    @staticmethod
    def _ap(v):
        return v.ap if isinstance(v, V) else v

    def dma(self, q, out, in_, reads=None, writes=None, **kw):
        o, i = out.ap, in_.ap
        self.emit(q, lambda e: e.dma_start(out=o, in_=i, **kw), [in_.key] + (reads or []),
                  [out.key] if writes is None else writes, dma=True)

    def _breg(self, e, val):
        if val not in self.bregs:
            r = e.alloc_register("bchk%d" % val)
            e.reg_mov(r, val)
            self.bregs[val] = r
        return self.bregs[val]

    def gather(self, out, src, idx, nrows):
        o, s, ix = out.ap, src.ap, idx.ap
        self.emit("pool", lambda e: e.indirect_dma_start(
            out=o, out_offset=None, in_=s,
            in_offset=bass.IndirectOffsetOnAxis(ap=ix, axis=0),
            bounds_check=self._breg(e, nrows - 1), oob_is_err=False),
            [src.key, idx.key], [out.key], dma=True)

    def scatter(self, dst, src, idx, nrows, accum=False, reads=None, writes=None):
        d, s, ix = dst.ap, src.ap, idx.ap
        kw = {"compute_op": ALU.add} if accum else {}
        self.emit("pool", lambda e: e.indirect_dma_start(
            out=d, out_offset=bass.IndirectOffsetOnAxis(ap=ix, axis=0), in_=s, in_offset=None,
            bounds_check=self._breg(e, nrows - 1), oob_is_err=False, **kw),
            [src.key, idx.key] + ([dst.key] if accum else []) + (reads or []),
            [dst.key] if writes is None else writes, dma=True)

    def matmul(self, out, lhsT, rhs, start=True, stop=True):
        o, l, r = out.ap, lhsT.ap, rhs.ap
        rd = [lhsT.key, rhs.key] + ([] if start else [out.key])
        self.emit("pe", lambda e: e.matmul(o, l, r, start=start, stop=stop), rd, [out.key])

    def transpose(self, out, in_, ident):
        o, i, d = out.ap, in_.ap, ident.ap
        self.emit("pe", lambda e: e.transpose(o, i, d), [in_.key, ident.key], [out.key])

    def act(self, out, in_, func, bias=0.0, scale=1.0, accum_out=None, eng="act"):
        o, i = out.ap, in_.ap
        b, s = self._ap(bias), self._ap(scale)
        kw = {}
        if accum_out is not None:
            kw["accum_out"] = accum_out.ap
        self.emit(eng, lambda e: e.activation(o, i, func, bias=b, scale=s, **kw),
                  self._keys(in_, bias, scale), self._keys(out, accum_out))

    def copy(self, eng, out, in_):
        o, i = out.ap, in_.ap
        if eng == "act":
            self.emit(eng, lambda e: e.copy(o, i), [in_.key], [out.key])
        else:
            self.emit(eng, lambda e: e.tensor_copy(o, i), [in_.key], [out.key])

    def tt(self, eng, out, in0, in1, op):
        o, a, b = out.ap, in0.ap, in1.ap
        self.emit(eng, lambda e: e.tensor_tensor(o, a, b, op), [in0.key, in1.key], [out.key])

    def ts(self, eng, out, in0, s1, s2, op0, op1=None, accum_out=None):
        o, a = out.ap, in0.ap
        x1, x2 = self._ap(s1), self._ap(s2)
        kw = {}
        if op1 is not None:
            kw["op1"] = op1
        if accum_out is not None:
            kw["accum_out"] = accum_out.ap
        self.emit(eng, lambda e: e.tensor_scalar(o, a, x1, x2, op0, **kw),
                  self._keys(in0, s1, s2), self._keys(out, accum_out))

    def stt(self, eng, out, in0, scalar, in1, op0, op1):
        o, a, b = out.ap, in0.ap, in1.ap
        s = self._ap(scalar)
        self.emit(eng, lambda e: e.scalar_tensor_tensor(o, a, s, b, op0, op1),
                  self._keys(in0, scalar, in1), [out.key])

    def reduce(self, eng, out, in_, op, axis=AX.X):
        o, i = out.ap, in_.ap
        self.emit(eng, lambda e: e.tensor_reduce(o, i, axis, op), [in_.key], [out.key])

    def recip(self, out, in_):
        o, i = out.ap, in_.ap
        self.emit("dve", lambda e: e.reciprocal(o, i), [in_.key], [out.key])

    def memset(self, eng, out, val):
        o = out.ap
        self.emit(eng, lambda e: e.memset(o, val), [], [out.key])

    def wait(self, eng, keys):
        waits = []
        for k_ in keys:
            tok = self.last_w.get(k_)
            if tok is None:
                continue
            sk, val = tok
            if sk == ("c", eng) or self.seen[eng].get(sk, 0) >= val:
                continue
            self.seen[eng][sk] = val
            waits.append((sk, val))
        if waits:
            self.lists[eng].append((waits, None, None))

    def barrier(self):
        allv = {("c", e): self.cnt[e] for e in ("pe", "dve", "act", "pool") if self.cnt[e] > 0}
        allv.update({sk: v for sk, v in self.dma_cnt.items() if v > 0})
        for eng in self.ENGS:
            waits = []
            for sk, val in allv.items():
                if sk == ("c", eng) or self.seen[eng].get(sk, 0) >= val:
                    continue
                self.seen[eng][sk] = val
                waits.append((sk, val))
            if waits:
                self.lists[eng].append((waits, None, None))

    def push_scope(self):
        self._outer = self.stack
        self.stack = contextlib.ExitStack()

    def pop_scope(self):
        self.barrier()
        self.stack.close()
        self.stack = self._outer

    def finish(self):
        nc = self.nc
        prog = self

        def mk(name):
            def body(e):
                for waits, fn, tok in prog.lists[name]:
                    for sk, val in waits:
                        e.wait_ge(prog.sem[sk], val)
                    if fn is None:
                        continue
                    ins = fn(e)
                    ins.then_inc(prog.sem[tok[0]], 16 if tok[0][0] == "d" else 1)
                if name == "sp":
                    for sk, val in prog.dma_cnt.items():
                        if val > 0:
                            e.wait_ge(prog.sem[sk], val)
            return body

        with nc.Block() as block:
            block.tensor(mk("pe"))
            block.vector(mk("dve"))
            block.scalar(mk("act"))
            block.gpsimd(mk("pool"))
            block.sync(mk("sp"))
        self.stack.close()
        return nc


def bcast_rows(ap1d_or_row, nparts):
    return ap1d_or_row.broadcast(0, nparts) if hasattr(ap1d_or_row, "broadcast") else ap1d_or_row


def const_tables():
    i = np.arange(128)
    ui = (i[:, None] <= i[None, :]).astype(np.float32)
    li = (i[:, None] >= i[None, :]).astype(np.float32)
    us = (i[:, None] < i[None, :]).astype(np.float32)
    ls = (i[:, None] > i[None, :]).astype(np.float32)
    ident = np.eye(128, dtype=np.float32)
    cf = np.stack([ui, li, us, ls, ident], axis=1)
    cb = np.eye(128, dtype=np.float32).astype(ml_dtypes.bfloat16)
    return np.ascontiguousarray(cf), cb


GH = 4
GDK = 128
GDV = 256
GIN = 3104


def rmsnorm_tile(p, xt, gbc, hn_out, sq, ss, rstd, width=D):
    p.act(sq, xt, AF.Square, accum_out=ss)
    p.ts("dve", rstd, ss, 1.0 / width, RMS_EPS, ALU.mult, ALU.add)
    p.act(rstd, rstd, AF.Sqrt)
    p.recip(rstd, rstd)
    p.stt("dve", hn_out, xt, rstd, gbc, ALU.mult, ALU.mult)


def build_L1():
    nc = bass.Bass("TRN2", target_bir_lowering=False)
    p = Prog(nc)
    x = p.dram("x", [TOK, D], F32, "ExternalInput")
    g_mix = p.dram("g_mix", [1, D], F32, "ExternalInput")
    w_in = p.dram("w_in", [D, GIN], F32, "ExternalInput")
    wg = p.dram("wg", [2, 16, 512], F32, "ExternalInput")
    bg = p.dram("bg", [2, 512], F32, "ExternalInput")
    cf_d = p.dram("cf", [128, 5, 128], F32, "ExternalInput")
    cb_d = p.dram("cb", [128, 128], BF16, "ExternalInput")
    QP = p.dram("QP", [2, 128, GH, TOK], BF16, "ExternalOutput")
    KP = p.dram("KP", [2, 128, GH, TOK], BF16, "Internal")
    K2 = p.dram("K2", [2, TOK, 512], BF16, "Internal")
    VT = p.dram("VT", [TOK, 1024], BF16, "Internal")
    SR = p.dram("SR", [TOK, 1024], F32, "ExternalOutput")
    OL = p.dram("OL", [TOK, 1024], F32, "ExternalOutput")
    AA = p.dram("AA", [2, 128, NT * GH], F32, "ExternalOutput")
    BE = p.dram("BE", [2, GH, 128, GDV], F32, "ExternalOutput")

    W = p.sb("W", [128, 8, GIN], BF16)
    cf = p.sb("cfs", [128, 5, 128], F32)
    cb = p.sb("cbs", [128, 128], BF16)
    gbc = p.sb("gbc", [128, D], F32)
    bgs = p.sb("bgs", [128, 2, 512], F32)
    wgs = p.sb("wgs", [16, 2, 512], BF16)
    xt = [p.sb("xt%d" % i, [128, D], F32) for i in range(2)]
    sq = p.sb("sq", [128, D], F32)
    ss = p.sb("ss", [128, 1], F32)
    rstd = p.sb("rstd", [128, 1], F32)
    hn = p.sb("hn", [128, D], BF16)
    hnT = p.sb("hnT", [128, D], BF16)
    gdT = p.sb("gdT", [16, 256], BF16)
    zb = p.sb("zb", [128, 512], F32)
    spl = [p.sb("spl%d" % i, [128, 512], F32) for i in range(2)]
    EqT = p.sb("EqT", [128, 512], F32)
    EkT = p.sb("EkT", [128, 512], F32)
    Ek2 = p.sb("Ek2", [128, 512], F32)
    a_all = p.sb("a_all", [128, 2, NT * GH], F32)
    c_all = p.sb("c_all", [128, 2, NT * GH], F32)
    qp = [p.sb("qp%d" % i, [128, 512], BF16) for i in range(2)]
    kp = [p.sb("kp%d" % i, [128, 512], BF16) for i in range(2)]
    k2 = [p.sb("k2%d" % i, [128, 512], BF16) for i in range(2)]
    vb = p.sb("vb", [128, 1024], BF16)
    srt = p.sb("srt", [128, 1024], F32)
    S = p.sb("S", [128, GH, GDV], F32)
    Sb = p.sb("Sb", [128, GH, GDV], BF16)
    s_qp = [p.sb("s_qp%d" % i, [128, 512], BF16) for i in range(2)]
    s_kp = [p.sb("s_kp%d" % i, [128, 512], BF16) for i in range(2)]
    s_k2 = [p.sb("s_k2%d" % i, [128, 512], BF16) for i in range(2)]
    s_v = [p.sb("s_v%d" % i, [128, 1024], BF16) for i in range(2)]
    AT = [p.sb("AT%d" % i, [128, 128], BF16) for i in range(2)]
    ot = [p.sb("ot%d" % i, [128, 1024], F32) for i in range(2)]
    of = [p.sb("of%d" % i, [128, 1024], F32) for i in range(2)]
    banks = [p.ps("bk%d" % i, [128, 512], F32) for i in range(7)]
    pT = p.ps("pT", [128, 1024], BF16)
    bi = [0]

    rot = [3, 4]

    def bank():
        b = banks[rot[0] + bi[0] % rot[1]]
        bi[0] += 1
        return b

    p.dma("sp", cf[:], cf_d[:])
    p.dma("sp", cb[:], cb_d[:])
    p.dma("sp", gbc[:], g_mix.v(g_mix.t[0:1, :].broadcast_to([128, D])))
    p.dma("sp", bgs[:, 0, :], bg.v(bg.t[0:1, :].broadcast_to([128, 512])))
    p.dma("sp", bgs[:, 1, :], bg.v(bg.t[1:2, :].broadcast_to([128, 512])))
    for d_ in range(2):
        p.dma("pool", wgs.sub(d_, (slice(None), d_, slice(None))), wg[d_])
    for dc in range(8):
        p.dma("pool", W.sub(dc, (slice(None), dc, slice(None))), w_in[dc * 128:(dc + 1) * 128, :])
    Wk = lambda dc, lo, hi: W.sub(dc, (slice(None), dc, slice(lo, hi)))
    UI, LI, US, LS = (cf[:, i, :] for i in range(4))
    scale_q = float(GDK) ** -0.5

    def scan_init(d_):
        p.memset("dve", S[:], 0.0)
        p.memset("dve", Sb[:], 0.0)

    def scan_step(d_, j, it, sq_, sk_, sk2_, sv_):
        b = it % 2
        mask = UI if d_ == 0 else LS
        O = ot[b]
        for h in range(GH):
            hs = slice(h * 128, (h + 1) * 128)
            vs = slice(h * GDV, (h + 1) * GDV)
            ba = bank()
            p.matmul(ba[:, 0:128], sk_[:, hs], sq_[:, hs])
            A = AT[h % 2]
            p.tt("dve", A[:], ba[:, 0:128], mask, ALU.mult)
            bo = bank()
            p.matmul(bo[:, 0:GDV], A[:], sv_[:, vs], start=True, stop=False)
            p.matmul(bo[:, 0:GDV], sq_[:, hs], Sb[:, h, :], start=False, stop=True)
            bs = bank()
            p.matmul(bs[:, 0:GDV], sk2_[:, hs], sv_[:, vs])
            if d_ == 0:
                p.copy("act", O[:, vs], bo[:, 0:GDV])
            else:
                p.tt("dve", O[:, vs], bo[:, 0:GDV], of[b][:, vs], ALU.add)
            p.stt("dve", S[:, h, :], S[:, h, :], a_all.v(a_all.t[:, d_, j * GH + h:j * GH + h + 1]),
                  bs[:, 0:GDV], ALU.mult, ALU.add)
            p.copy("act", Sb[:, h, :], S[:, h, :])
        p.dma("pool", OL[j * 128:(j + 1) * 128, :], O[:])

    def scan_fin(d_):
        for h in range(GH):
            p.dma("pool", BE.v(BE.t[d_, h]), S[:, h, :])

    scan_init(0)
    for j in range(NT):
        X = xt[j % 2]
        p.dma("sp", X[:], x[j * 128:(j + 1) * 128, :])
        rmsnorm_tile(p, X[:], gbc[:], hn[:], sq[:], ss[:], rstd[:])
        for dc in range(8):
            p.transpose(pT[:, dc * 128:(dc + 1) * 128], hn[:, dc * 128:(dc + 1) * 128], cb[:])
        p.copy("act", hnT[:], pT[:])
        hT = lambda dc: hnT[:, dc * 128:(dc + 1) * 128]
        bG = bank()
        for d_ in range(2):
            for dc in range(8):
                p.matmul(bG[0:16, d_ * 128:(d_ + 1) * 128], Wk(dc, 3072 + 16 * d_, 3088 + 16 * d_), hT(dc),
                         start=(dc == 0), stop=(dc == 7))
        p.copy("act", gdT[:], bG[0:16, 0:256])
        bq, bk, bkt = banks[0], banks[1], banks[2]
        for n in range(4):
            for dc in range(8):
                p.matmul(bq[:, n * 128:(n + 1) * 128], Wk(dc, n * 128, (n + 1) * 128), hT(dc),
                         start=(dc == 0), stop=(dc == 7))
        for n in range(4):
            for dc in range(8):
                p.matmul(bk[:, n * 128:(n + 1) * 128], Wk(dc, 512 + n * 128, 512 + (n + 1) * 128), hT(dc),
                         start=(dc == 0), stop=(dc == 7))
        for dc in range(8):
            p.matmul(bkt[:], hT(dc), Wk(dc, 512, 1024), start=(dc == 0), stop=(dc == 7))
        for d_ in range(2):
            bz = bank()
            p.matmul(bz[:], gdT[:, d_ * 128:(d_ + 1) * 128], wgs.sub(d_, (slice(None), d_, slice(None))))
            p.tt("dve", zb[:], bz[:], bgs[:, d_, :], ALU.add)
            p.act(zb[:], zb[:], AF.Exp, scale=-1.0)
            sp_ = spl[d_]
            p.act(sp_[:], zb[:], AF.Ln, bias=1.0)
            bc, bd = bank(), bank()
            tri = UI if d_ == 0 else LI
            for h in range(GH):
                p.matmul(bc[:, h * 128:(h + 1) * 128], sp_[:, h * 128:(h + 1) * 128], tri)
            p.matmul(bd[:], LS if d_ == 0 else US, sp_[:])
            p.act(EqT[:], bc[:], AF.Exp, scale=-1.0 / 16)
            p.act(EkT[:], bc[:], AF.Exp, scale=1.0 / 16)
            p.act(Ek2[:], bd[:], AF.Exp, scale=-1.0 / 16)
            last = 127 if d_ == 0 else 0
            p.copy("dve", a_all.v(a_all.t[:, d_, j * GH:(j + 1) * GH]),
                   EqT.v(EqT.t[:].rearrange("p (h t) -> p h t", h=GH)[:, :, last]))
            p.copy("dve", c_all.v(c_all.t[:, d_, j * GH:(j + 1) * GH]),
                   bc.v(bc.t[:].rearrange("p (h t) -> p h t", h=GH)[:, :, last]))
            Q, K, KK = qp[d_], kp[d_], k2[d_]
            p.stt("dve", Q[:], bq[:], scale_q, EqT[:], ALU.mult, ALU.mult)
            p.tt("dve", K[:], bk[:], EkT[:], ALU.mult)
            p.tt("dve", KK[:], bkt[:], Ek2[:], ALU.mult)
            p.dma("pool", QP.v(QP.t[d_, :, :, j * 128:(j + 1) * 128]),
                  Q.v(Q.t[:].rearrange("p (h t) -> p h t", h=GH)))
            p.dma("pool", KP.v(KP.t[d_, :, :, j * 128:(j + 1) * 128]),
                  K.v(K.t[:].rearrange("p (h t) -> p h t", h=GH)))
            p.dma("pool", K2.v(K2.t[d_, j * 128:(j + 1) * 128, :]), KK[:])
        for half in range(2):
            bv = bank()
            for dc in range(8):
                p.matmul(bv[:], hT(dc), Wk(dc, 1024 + half * 512, 1536 + half * 512), start=(dc == 0), stop=(dc == 7))
            p.copy("act", vb[:, half * 512:(half + 1) * 512], bv[:])
        p.dma("pool", VT[j * 128:(j + 1) * 128, :], vb[:])
        scan_step(0, j, j, qp[0], kp[0], k2[0], vb)
        for half in range(2):
            br = bank()
            for dc in range(8):
                p.matmul(br[:], hT(dc), Wk(dc, 2048 + half * 512, 2560 + half * 512), start=(dc == 0), stop=(dc == 7))
            p.act(srt[:, half * 512:(half + 1) * 512], br[:], AF.Silu)
        p.dma("pool", SR[j * 128:(j + 1) * 128, :], srt[:])
    scan_fin(0)
    for d_ in range(2):
        p.dma("pool", AA[d_], c_all[:, d_, :])

    rot[0], rot[1] = 0, 7
    scan_init(1)
    for it, j in enumerate(range(NT - 1, -1, -1)):
        b = it % 2
        sq_, sk_, sk2_, sv_ = s_qp[b], s_kp[b], s_k2[b], s_v[b]
        p.dma("sp", sq_.v(sq_.t[:].rearrange("p (h t) -> p h t", h=GH)), QP.v(QP.t[1, :, :, j * 128:(j + 1) * 128]))
        p.dma("sp", sk_.v(sk_.t[:].rearrange("p (h t) -> p h t", h=GH)), KP.v(KP.t[1, :, :, j * 128:(j + 1) * 128]))
        p.dma("sp", sk2_[:], K2.v(K2.t[1, j * 128:(j + 1) * 128, :]))
        p.dma("sp", sv_[:], VT[j * 128:(j + 1) * 128, :])
        p.dma("sp", of[b][:], OL[j * 128:(j + 1) * 128, :])
        scan_step(1, j, it, sq_, sk_, sk2_, sv_)
    scan_fin(1)
    return p.finish()


def ffn_prep_tile(p, h1, gf, wr, cff, bufs, HN_out, AFF_out, rows, bank):
    sq, ss, rstd, hnf, hnb, hnT, lg, mx, sm = bufs
    rmsnorm_tile(p, h1, gf[:], hnf[:], sq[:], ss[:], rstd[:])
    p.copy("act", hnb[:], hnf[:])
    p.dma("pool", HN_out[rows, :], hnb[:])
    ident_f = cff[:, 4, :]
    for half in range(2):
        bt = bank()
        for q in range(4):
            dc = half * 4 + q
            p.transpose(bt[:, q * 128:(q + 1) * 128], hnf[:, dc * 128:(dc + 1) * 128], ident_f)
        p.copy("act", hnT[:, half * 512:(half + 1) * 512], bt[:])
    bl = bank()
    for dc in range(8):
        p.matmul(bl[:, 0:16], hnT[:, dc * 128:(dc + 1) * 128], wr[:, dc, :], start=(dc == 0), stop=(dc == 7))
    p.reduce("dve", mx[:], bl[:, 0:16], ALU.max)
    p.ts("dve", mx[:], mx[:], -1.0, None, ALU.mult)
    p.act(lg[:], bl[:, 0:16], AF.Exp, bias=mx[:])
    p.reduce("dve", sm[:], lg[:], ALU.add)
    p.recip(sm[:], sm[:])
    p.ts("dve", lg[:], lg[:], sm[:], None, ALU.mult)
    p.dma("pool", AFF_out[rows, :], lg[:])


def ffn_prep_bufs(p):
    return (p.sb("f_sq", [128, D], F32), p.sb("f_ss", [128, 1], F32), p.sb("f_rstd", [128, 1], F32),
            p.sb("f_hnf", [128, D], F32), p.sb("f_hnb", [128, D], BF16), p.sb("f_hnT", [128, D], F32),
            p.sb("f_lg", [128, 16], F32), p.sb("f_mx", [128, 1], F32), p.sb("f_sm", [128, 1], F32))


def build_L2():
    nc = bass.Bass("TRN2", target_bir_lowering=False)
    p = Prog(nc)
    x = p.dram("x", [TOK, D], F32, "ExternalInput")
    OL = p.dram("OL", [TOK, 1024], F32, "ExternalInput")
    QP = p.dram("QP", [2, 128, GH, TOK], BF16, "ExternalInput")
    SR = p.dram("SR", [TOK, 1024], F32, "ExternalInput")
    AA = p.dram("AA", [2, 128, NT * GH], F32, "ExternalInput")
    BP = p.dram("BP", [2, 7, GH, 128, GDV], F32, "ExternalInput")
    APD = p.dram("APD", [2, 7, 128, NT * GH], F32, "ExternalInput")
    g_head = p.dram("g_head", [1, GDV], F32, "ExternalInput")
    w_out = p.dram("w_out", [D, D], F32, "ExternalInput")
    g_ffn = p.dram("g_ffn", [1, D], F32, "ExternalInput")
    w_r = p.dram("w_r", [D, 16], F32, "ExternalInput")
    cf_d = p.dram("cf", [128, 5, 128], F32, "ExternalInput")
    cb_d = p.dram("cb", [128, 128], BF16, "ExternalInput")
    H1 = p.dram("H1", [TOK, D], F32, "ExternalOutput")
    HN1 = p.dram("HN1", [TOK, D], BF16, "ExternalOutput")
    AFF = p.dram("AFF", [TOK, 16], F32, "ExternalOutput")

    Wo = p.sb("Wo", [128, 8, D], BF16)
    wr = p.sb("wr", [128, 8, 16], F32)
    cf = p.sb("cfs", [128, 5, 128], F32)
    cb = p.sb("cbs", [128, 128], BF16)
    gf = p.sb("gf", [128, D], F32)
    gh = p.sb("gh", [128, D], F32)
    apd = p.sb("apd", [128, 2, 7, NT * GH], F32)
    csum = p.sb("csum", [128, 2, 7, GH], F32)
    aown = p.sb("aown", [128, 2, NT * GH], F32)
    Sin = p.sb("Sin", [128, 2, GH, GDV], F32)
    Bt = [p.sb("Bt%d" % i, [128, GH, GDV], F32) for i in range(2)]
    Sbw = p.sb("Sbw", [128, NT, GH, GDV], BF16)
    Sfb = p.sb("Sfb", [128, GH, GDV], BF16)
    xt = [p.sb("xt%d" % i, [128, D], F32) for i in range(2)]
    olt = [p.sb("olt%d" % i, [128, D], F32) for i in range(2)]
    srt = [p.sb("srt%d" % i, [128, D], F32) for i in range(2)]
    qf = [p.sb("qf%d" % i, [128, 512], BF16) for i in range(2)]
    qb = [p.sb("qb%d" % i, [128, 512], BF16) for i in range(2)]
    o = p.sb("o", [128, D], F32)
    osq = p.sb("osq", [128, D], F32)
    hss = p.sb("hss", [128, GH], F32)
    y = p.sb("y", [128, D], BF16)
    yT = p.sb("yT", [128, D], BF16)
    h1 = p.sb("h1", [128, D], F32)
    fb = ffn_prep_bufs(p)
    banks = [p.ps("bk%d" % i, [128, 512], F32) for i in range(7)]
    pT = p.ps("pT", [128, 1024], BF16)
    bi = [0]

    def bank():
        b = banks[bi[0] % 7]
        bi[0] += 1
        return b

    p.dma("sp", cf[:], cf_d[:])
    p.dma("sp", cb[:], cb_d[:])
    p.dma("sp", gf[:], g_ffn.v(g_ffn.t[0:1, :].broadcast_to([128, D])))
    for h in range(GH):
        p.dma("sp", gh[:, h * GDV:(h + 1) * GDV], g_head.v(g_head.t[0:1, :].broadcast_to([128, GDV])))
    p.dma("sp", wr[:], w_r.v(w_r.t.rearrange("(c p) e -> p c e", p=128)))
    for dc in range(8):
        p.dma("pool", Wo.sub(dc, (slice(None), dc, slice(None))), w_out[dc * 128:(dc + 1) * 128, :])
    for d_ in range(2):
        p.dma("sp", apd.v(apd.t[:, d_]), APD.v(APD.t[d_].rearrange("i p c -> p i c")))
        p.dma("sp", aown.v(aown.t[:, d_, :]), AA[d_])
    p.reduce("dve", csum[:], apd.v(apd.t[:].rearrange("p d i (t h) -> p d i h t", h=GH)), ALU.add)
    p.act(csum[:], csum[:], AF.Exp, scale=-1.0 / 16)
    p.act(aown[:], aown[:], AF.Exp, scale=-1.0 / 16)
    p.memset("dve", Sin[:], 0.0)
    n = 0
    for d_ in range(2):
        for i in range(7):
            B = Bt[n % 2]
            n += 1
            p.dma("sp", B[:], BP.v(BP.t[d_, i].rearrange("h p v -> p h v")))
            for h in range(GH):
                p.stt("dve", Sin[:, d_, h, :], Sin[:, d_, h, :], csum.v(csum.t[:, d_, i, h:h + 1]), B[:, h, :],
                      ALU.mult, ALU.add)
    for j in range(NT - 1, -1, -1):
        p.copy("act", Sbw[:, j], Sin[:, 1])
        for h in range(GH):
            p.ts("dve", Sin[:, 1, h, :], Sin[:, 1, h, :], aown.v(aown.t[:, 1, j * GH + h:j * GH + h + 1]), None, ALU.mult)
    for j in range(NT):
        b = j % 2
        rows = slice(j * 128, (j + 1) * 128)
        p.dma("sp", xt[b][:], x[rows, :])
        p.dma("sp", olt[b][:], OL[rows, :])
        p.dma("sp", srt[b][:], SR[rows, :])
        p.dma("sp", qf[b].v(qf[b].t[:].rearrange("p (h t) -> p h t", h=GH)), QP.v(QP.t[0, :, :, rows]))
        p.dma("sp", qb[b].v(qb[b].t[:].rearrange("p (h t) -> p h t", h=GH)), QP.v(QP.t[1, :, :, rows]))
        p.copy("act", Sfb[:], Sin[:, 0])
        for h in range(GH):
            hs = slice(h * 128, (h + 1) * 128)
            vs = slice(h * GDV, (h + 1) * GDV)
            bo = bank()
            p.matmul(bo[:, 0:GDV], qf[b][:, hs], Sfb[:, h, :], start=True, stop=False)
            p.matmul(bo[:, 0:GDV], qb[b][:, hs], Sbw[:, j, h, :], start=False, stop=True)
            p.tt("dve", o[:, vs], bo[:, 0:GDV], olt[b][:, vs], ALU.add)
            p.ts("dve", Sin[:, 0, h, :], Sin[:, 0, h, :], aown.v(aown.t[:, 0, j * GH + h:j * GH + h + 1]), None, ALU.mult)
        p.tt("dve", osq[:], o[:], o[:], ALU.mult)
        p.reduce("dve", hss[:], osq.v(osq.t[:].rearrange("p (h v) -> p h v", h=GH)), ALU.add)
        p.ts("dve", hss[:], hss[:], 1.0 / GDV, RMS_EPS, ALU.mult, ALU.add)
        p.act(hss[:], hss[:], AF.Sqrt)
        p.recip(hss[:], hss[:])
        p.tt("dve", osq[:], srt[b][:], gh[:], ALU.mult)
        for h in range(GH):
            vs = slice(h * GDV, (h + 1) * GDV)
            p.stt("dve", y[:, vs], o[:, vs], hss[:, h:h + 1], osq[:, vs], ALU.mult, ALU.mult)
        for dc in range(8):
            p.transpose(pT[:, dc * 128:(dc + 1) * 128], y[:, dc * 128:(dc + 1) * 128], cb[:])
        p.copy("act", yT[:], pT[:])
        for half in range(2):
            bm = bank()
            for dc in range(8):
                p.matmul(bm[:], yT[:, dc * 128:(dc + 1) * 128],
                         Wo.sub(dc, (slice(None), dc, slice(half * 512, (half + 1) * 512))),
                         start=(dc == 0), stop=(dc == 7))
            p.tt("dve", h1[:, half * 512:(half + 1) * 512], bm[:], xt[b][:, half * 512:(half + 1) * 512], ALU.add)
        p.dma("pool", H1[rows, :], h1[:])
        ffn_prep_tile(p, h1[:], gf, wr, cf, fb, HN1, AFF, rows, bank)
    return p.finish()


NE = 16
CAP = 2 * SEQ // NE
FF = 2048
BISECT_ITERS = 36


def build_L3():
    nc = bass.Bass("TRN2", target_bir_lowering=False)
    p = Prog(nc)
    AFFE = p.dram("AFFE", [2, 128, 128], F32, "ExternalInput")
    HN = p.dram("HN", [SEQ, D], BF16, "ExternalInput")
    wg = p.dram("wg", [2, D, FF], F32, "ExternalInput")
    wu = p.dram("wu", [2, D, FF], F32, "ExternalInput")
    wd = p.dram("wd", [2, FF, D], F32, "ExternalInput")
    cf_d = p.dram("cf", [128, 5, 128], F32, "ExternalInput")
    cb_d = p.dram("cb", [128, 128], BF16, "ExternalInput")
    tok_d = p.dram("tokid", [128, 128], I32, "ExternalInput")
    DELTA = p.dram("DELTA", [SEQ, D], F32, "ExternalOutput")
    IDXA = [p.dram("IDXA%d" % i, [CAP, 2], I32, "Internal") for i in range(2)]

    Wg = p.sb("Wg", [128, 8, FF], BF16)
    Wu = p.sb("Wu", [128, 8, FF], BF16)
    Wd = p.sb("Wd", [128, 16, D], BF16)
    cf = p.sb("cfs", [128, 5, 128], F32)
    cb = p.sb("cbs", [128, 128], BF16)
    tokid = p.sb("tokid_s", [128, 128], I32)
    zt = p.sb("zt", [128, 4096], F32)
    ones = p.sb("ones", [128, 128], F32)
    aff = p.sb("aff", [128, 2, 128], F32)
    cmp_ = p.sb("cmp", [128, 2, 128], F32)
    st = {n: p.sb("b_" + n, [128, 2], F32) for n in ("lo", "hi", "mid", "cnt", "ge", "nge", "t1", "t2")}
    selT = p.sb("selT", [128, 128], F32)
    rp = p.sb("rp", [128, 1], F32)
    slot = p.sb("slot", [128, 128], F32)
    pen = p.sb("pen", [128, 128], F32)
    slot_is = [p.sb("slot_i%d" % i, [128, 128], I32) for i in range(2)]
    pks = [p.sb("pk%d" % i, [128, 128, 2], I32) for i in range(2)]
    idx_sb = p.sb("idx_sb", [128, 2, 16, 2], I32)
    xs = [p.sb("xs%d" % i, [128, D], BF16) for i in range(2)]
    xsT = p.sb("xsT", [128, 8, 512], BF16)
    hT = p.sb("hT", [128, 16, 512], BF16)
    sg = [p.sb("sg%d" % i, [128, 512], F32) for i in range(2)]
    y = [p.sb("y%d" % i, [128, D], F32) for i in range(2)]
    banks = [p.ps("bk%d" % i, [128, 512], F32) for i in range(7)]
    pT = p.ps("pT", [128, 1024], BF16)
    bi = [0]

    def bank():
        b = banks[bi[0] % 7]
        bi[0] += 1
        return b

    p.dma("sp", cf[:], cf_d[:])
    p.dma("sp", cb[:], cb_d[:])
    p.dma("sp", tokid[:], tok_d[:])
    p.dma("sp", aff[:], AFFE.v(AFFE.t.rearrange("e p f -> p e f")))
    p.memset("pool", zt[:], 0.0)
    p.memset("dve", ones[:], 1.0)
    zkeys = []
    dz = DELTA.t.rearrange("(k p r) d -> k p (r d)", p=128, r=4)
    for k in range(SEQ // 512):
        zk = ("DELTA", "z", k)
        zkeys.append(zk)
        p.dma("sp", V(dz[k], zk), zt[:])
    US = cf[:, 2, :]
    ident_f = cf[:, 4, :]

    def load_weights(e):
        for dc in range(8):
            p.dma("pool", Wg.sub(dc, (slice(None), dc, slice(None))), V(wg.t[e, dc * 128:(dc + 1) * 128, :], ("wg", e)))
            p.dma("pool", Wu.sub(dc, (slice(None), dc, slice(None))), V(wu.t[e, dc * 128:(dc + 1) * 128, :], ("wu", e)))
        for fc in range(16):
            p.dma("pool", Wd.sub(fc, (slice(None), fc, slice(None))), V(wd.t[e, fc * 128:(fc + 1) * 128, :], ("wd", e)))

    load_weights(0)
    lo, hi, mid, cnt, ge, nge, t1, t2 = (st[n] for n in ("lo", "hi", "mid", "cnt", "ge", "nge", "t1", "t2"))
    p.memset("dve", lo[:], 0.0)
    p.memset("dve", hi[:], 1.0)
    for it in range(BISECT_ITERS):
        p.tt("dve", mid[:], lo[:], hi[:], ALU.add)
        p.ts("dve", mid[:], mid[:], 0.5, None, ALU.mult)
        for e in range(2):
            p.ts("dve", cmp_[:, e, :], aff[:, e, :], mid[:, e:e + 1], None, ALU.is_ge)
        p.reduce("dve", cnt[:], cmp_[:], ALU.add)
        bt = bank()
        p.matmul(bt[:, 0:2], ones[:], cnt[:])
        p.ts("dve", ge[:], bt[:, 0:2], float(CAP) - 0.5, None, ALU.is_ge)
        p.ts("dve", nge[:], ge[:], -1.0, 1.0, ALU.mult, ALU.add)
        p.tt("dve", lo[:], lo[:], nge[:], ALU.mult)
        p.tt("dve", t1[:], mid[:], ge[:], ALU.mult)
        p.tt("dve", lo[:], lo[:], t1[:], ALU.add)
        p.tt("dve", hi[:], hi[:], ge[:], ALU.mult)
        p.tt("dve", t2[:], mid[:], nge[:], ALU.mult)
        p.tt("dve", hi[:], hi[:], t2[:], ALU.add)
    for e in range(2):
        p.ts("dve", cmp_[:, e, :], aff[:, e, :], lo[:, e:e + 1], None, ALU.is_ge)
    p.reduce("dve", cnt[:], cmp_[:], ALU.add)
    pending = []

    def compaction(e):
        pk, slot_i = pks[e], slot_is[e]
        pkf = pk.t[:].bitcast(F32)
        bt = bank()
        p.transpose(bt[:, 0:128], cmp_[:, e, :], ident_f)
        p.copy("act", selT[:], bt[:, 0:128])
        bp = bank()
        p.matmul(bp[:, 0:128], selT[:], US)
        p.matmul(bp[:, 128:129], US, cnt[:, e:e + 1])
        p.copy("act", rp[:], bp[:, 128:129])
        p.ts("dve", slot[:], bp[:, 0:128], rp[:, 0:1], None, ALU.add)
        p.ts("dve", pen[:], cmp_[:, e, :], -4096.0, 4096.0, ALU.mult, ALU.add)
        p.tt("dve", slot[:], slot[:], pen[:], ALU.add)
        p.copy("dve", slot_i[:], slot[:])
        p.copy("dve", pk[:, :, 0], tokid[:])
        p.copy("dve", pk.v(pkf[:, :, 1]), aff[:, e, :])

    skeys = {0: [], 1: []}

    def scatter_cols(e, f0, f1):
        pk, slot_i = pks[e], slot_is[e]
        for f in range(f0, f1):
            sk = ("IDXA", e, f)
            skeys[e].append(sk)
            p.scatter(V(IDXA[e].t, sk), pk[:, f, :], slot_i[:, f:f + 1], CAP, writes=[sk])

    def load_idx(e):
        p.dma("sp", idx_sb[:, e], V(IDXA[e].t.rearrange("(k p) c -> p k c", p=128), ("IDXA", e, "all")), reads=skeys[e])

    compaction(0)
    compaction(1)
    scatter_cols(0, 0, 128)
    load_idx(0)
    wts = idx_sb.t[:].bitcast(F32)
    first_scatter = True
    for e in range(2):
        if e > 0:
            load_idx(1)
            load_weights(e)
        for tg in range(4):
            for kk in range(4):
                k = tg * 4 + kk
                X = xs[kk % 2]
                if e == 0:
                    scatter_cols(1, k * 8, (k + 1) * 8)
                p.gather(X[:], HN[:, :], idx_sb[:, e, k, 0:1], SEQ)
                for dc in range(8):
                    p.transpose(pT[:, dc * 128:(dc + 1) * 128], X[:, dc * 128:(dc + 1) * 128], cb[:])
                p.copy("act", xsT[:, :, kk * 128:(kk + 1) * 128], pT.v(pT.t[:].rearrange("p (c t) -> p c t", c=8)))
            for fc in range(16):
                bg_, bu_ = bank(), bank()
                for dc in range(8):
                    p.matmul(bg_[:], Wg.sub(dc, (slice(None), dc, slice(fc * 128, (fc + 1) * 128))), xsT[:, dc, :],
                             start=(dc == 0), stop=(dc == 7))
                for dc in range(8):
                    p.matmul(bu_[:], Wu.sub(dc, (slice(None), dc, slice(fc * 128, (fc + 1) * 128))), xsT[:, dc, :],
                             start=(dc == 0), stop=(dc == 7))
                G = sg[fc % 2]
                p.act(G[:], bg_[:], AF.Silu)
                p.tt("dve", hT[:, fc, :], G[:], bu_[:], ALU.mult)
            for kk in range(4):
                k = tg * 4 + kk
                Y = y[kk % 2]
                for half in range(2):
                    by = bank()
                    for fc in range(16):
                        p.matmul(by[:], hT[:, fc, kk * 128:(kk + 1) * 128],
                                 Wd.sub(fc, (slice(None), fc, slice(half * 512, (half + 1) * 512))),
                                 start=(fc == 0), stop=(fc == 15))
                    p.ts("dve", Y[:, half * 512:(half + 1) * 512], by[:], idx_sb.v(wts[:, e, k, 1:2]), None, ALU.mult)
                p.scatter(DELTA[:, :], Y[:], idx_sb[:, e, k, 0:1], SEQ, accum=True,
                          reads=zkeys if first_scatter else None)
                first_scatter = False
    return p.finish()


MH = 16
QRANK = 256
KVR = 128
NOPE = 128
ROPE = 64
MLA_SCALE = float(NOPE + ROPE) ** -0.5
TWO_PI = 2.0 * np.pi


def rope_consts():
    half = ROPE // 2
    inv = (10000.0 ** (-np.arange(half, dtype=np.float32) / half)).astype(np.float32)
    rc = np.zeros((64, 4), np.float32)
    rc[:, 0] = np.concatenate([inv, inv])
    rc[:, 1] = np.concatenate([np.ones(half), -np.ones(half)])
    rc[:, 2] = -np.pi
    return rc


def build_L4():
    nc = bass.Bass("TRN2", target_bir_lowering=False)
    p = Prog(nc)
    H1 = p.dram("H1", [TOK, D], F32, "ExternalInput")
    DS = p.dram("DS", [NCORES, TOK, D], F32, "ExternalInput")
    pos = p.dram("pos", [1, TOK], I32, "ExternalInput")
    g_mix = p.dram("g_mix", [1, D], F32, "ExternalInput")
    w_m = p.dram("w_m", [D, 448], F32, "ExternalInput")
    g_q = p.dram("g_q", [1, QRANK], F32, "ExternalInput")
    g_kv = p.dram("g_kv", [1, KVR], F32, "ExternalInput")
    w_uq = p.dram("w_uq", [QRANK, MH * 192], F32, "ExternalInput")
    w_ukv = p.dram("w_ukv", [KVR, MH * 256], F32, "ExternalInput")
    rc_d = p.dram("rc", [64, 4], F32, "ExternalInput")
    cf_d = p.dram("cf", [128, 5, 128], F32, "ExternalInput")
    cb_d = p.dram("cb", [128, 128], BF16, "ExternalInput")
    H2 = p.dram("H2", [TOK, D], F32, "ExternalOutput")
    QL = p.dram("QL", [MH, 128, TOK], BF16, "ExternalOutput")
    QR = p.dram("QR", [MH, 64, TOK], BF16, "ExternalOutput")
    CKVT = p.dram("CKVT", [128, TOK], BF16, "ExternalOutput")
    KRT = p.dram("KRT", [64, TOK], BF16, "ExternalOutput")
    CKV = p.dram("CKV", [TOK, 128], BF16, "ExternalOutput")
    KN2 = p.dram("KN2", [TOK, 1], F32, "ExternalOutput")
    QMAX = p.dram("QMAX", [1, MH], F32, "ExternalOutput")

    cf = p.sb("cfs", [128, 5, 128], F32)
    cb = p.sb("cbs", [128, 128], BF16)
    rc = p.sb("rcs", [64, 4], F32)
    gm = p.sb("gm", [128, D], F32)
    gq = p.sb("gq", [128, QRANK], F32)
    gkv = p.sb("gkv", [128, KVR], F32)
    Wm = p.sb("Wm", [128, 8, 448], BF16)
    Wuq = p.sb("Wuq", [128, 2, MH * 192], BF16)
    Wukv = p.sb("Wukv", [128, MH * 256], BF16)
    WukT = p.sb("WukT", [128, MH, 128], BF16)
    posi = p.sb("posi", [64, TOK], I32)
    ang = p.sb("ang", [64, TOK], F32)
    C2 = p.sb("C2", [64, TOK], F32)
    S2 = p.sb("S2", [64, TOK], F32)
    hnT = p.sb("hnT", [128, 8, TOK], BF16)
    cqT = p.sb("cqT", [128, 2, TOK], BF16)
    ht = [p.sb("ht%d" % i, [128, D], F32) for i in range(2)]
    dt_ = [p.sb("dt%d" % i, [128, D], F32) for i in range(3)]
    sq = p.sb("sq", [128, D], F32)
    ss = p.sb("ss", [128, 1], F32)
    rstd = p.sb("rstd", [128, 1], F32)
    hn = p.sb("hn", [128, D], BF16)
    cs = p.sb("cs", [128, 448], F32)
    cn = p.sb("cn", [128, 384], BF16)
    kn = p.sb("kn", [128, 2], F32)
    qn = p.sb("qn", [128, 512], BF16)
    qlf = p.sb("qlf", [128, 512], F32)
    qlb = p.sb("qlb", [128, 512], BF16)
    qsq = p.sb("qsq", [128, 512], F32)
    r1 = p.sb("r1", [64, 512], F32)
    r2 = p.sb("r2", [64, 512], F32)
    rb = p.sb("rb", [64, 512], BF16)
    qmx = p.sb("qmx", [1, MH], F32)
    qm1 = p.sb("qm1", [1, 1], F32)
    ckT = p.sb("ckT", [128, 128], BF16)
    banks = [p.ps("bk%d" % i, [128, 512], F32) for i in range(7)]
    pT = p.ps("pT", [128, 1024], BF16)
    bi = [0]

    def bank():
        b = banks[bi[0] % 7]
        bi[0] += 1
        return b

    p.dma("sp", cf[:], cf_d[:])
    p.dma("sp", cb[:], cb_d[:])
    p.dma("sp", rc[:], rc_d[:])
    p.dma("sp", gm[:], g_mix.v(g_mix.t[0:1, :].broadcast_to([128, D])))
    p.dma("sp", gq[:], g_q.v(g_q.t[0:1, :].broadcast_to([128, QRANK])))
    p.dma("sp", gkv[:], g_kv.v(g_kv.t[0:1, :].broadcast_to([128, KVR])))
    p.dma("sp", posi[:], pos.v(pos.t[0:1, :].broadcast_to([64, TOK])))
    p.dma("pool", Wm[:], w_m.v(w_m.t.rearrange("(c p) n -> p c n", p=128)))
    p.dma("pool", Wuq[:], w_uq.v(w_uq.t.rearrange("(c p) n -> p c n", p=128)))
    p.dma("pool", Wukv[:], w_ukv[:, :])
    onesf = p.sb("onesf", [128, 1], F32)
    p.memset("dve", onesf[:], 1.0)
    p.memset("dve", qmx[:], 0.0)
    Wm_sw = p.sb("Wm_sw", [128, 8, 64], BF16)
    Wuq_sw = p.sb("Wuq_sw", [128, 2, MH, 64], BF16)
    p.copy("dve", Wm_sw[:, :, 0:32], Wm[:, :, 416:448])
    p.copy("dve", Wm_sw[:, :, 32:64], Wm[:, :, 384:416])
    for kc in range(2):
        wv = Wuq.t[:, kc, :].rearrange("p (h c) -> p h c", h=MH)
        p.copy("dve", Wuq_sw[:, kc, :, 0:32], Wuq.v(wv[:, :, 160:192]))
        p.copy("dve", Wuq_sw[:, kc, :, 32:64], Wuq.v(wv[:, :, 128:160]))
    for h in range(MH):
        p.transpose(pT[:, (h % 8) * 128:(h % 8 + 1) * 128], Wukv[:, h * 256:h * 256 + 128], cb[:])
        if h % 8 == 7:
            g0 = h - 7
            p.copy("act", WukT[:, g0:g0 + 8, :], pT.v(pT.t[:].rearrange("p (h r) -> p h r", h=8)))
    p.copy("dve", ang[:], posi[:])
    p.ts("dve", ang[:], ang[:], rc[:, 0:1], None, ALU.mult)
    C1 = 6.28125
    C2_ = float(TWO_PI - 6.28125)
    ki = p.sb("ki", [64, TOK], I32)
    kf = p.sb("kf", [64, TOK], F32)
    gt = p.sb("gt", [64, TOK], F32)
    p.ts("dve", kf[:], ang[:], float(1.0 / TWO_PI), None, ALU.mult)
    p.copy("dve", ki[:], kf[:])
    p.copy("dve", kf[:], ki[:])
    p.stt("dve", ang[:], kf[:], -C1, ang[:], ALU.mult, ALU.add)
    p.stt("dve", ang[:], kf[:], -C2_, ang[:], ALU.mult, ALU.add)

    def fold(t):
        p.ts("dve", gt[:], t[:], float(np.pi), None, ALU.is_gt)
        p.stt("dve", t[:], gt[:], -TWO_PI, t[:], ALU.mult, ALU.add)
        p.ts("dve", gt[:], t[:], float(-np.pi), None, ALU.is_lt)
        p.stt("dve", t[:], gt[:], TWO_PI, t[:], ALU.mult, ALU.add)
        p.ts("dve", t[:], t[:], float(np.pi), float(-np.pi), ALU.min, ALU.max)

    fold(ang)
    p.ts("dve", C2[:], ang[:], float(np.pi / 2), None, ALU.add)
    fold(C2)
    p.act(S2[:], ang[:], AF.Sin)
    p.ts("dve", S2[:], S2[:], rc[:, 1:2], -1.0, ALU.mult, ALU.mult)
    p.act(C2[:], C2[:], AF.Sin)

    for j in range(NT):
        rows = slice(j * 128, (j + 1) * 128)
        Ht = ht[j % 2]
        p.dma("sp", Ht[:], H1[rows, :])
        for c in range(NCORES):
            Dt = dt_[c % 3]
            p.dma("sp", Dt[:], DS.v(DS.t[c, rows, :]))
            p.tt("dve", Ht[:], Ht[:], Dt[:], ALU.add)
        p.dma("pool", H2[rows, :], Ht[:])
        rmsnorm_tile(p, Ht[:], gm[:], hn[:], sq[:], ss[:], rstd[:])
        for dc in range(8):
            p.transpose(pT[:, dc * 128:(dc + 1) * 128], hn[:, dc * 128:(dc + 1) * 128], cb[:])
        p.copy("act", hnT[:, :, rows], pT.v(pT.t[:].rearrange("p (c t) -> p c t", c=8)))
        bc_ = bank()
        for dc in range(8):
            p.matmul(bc_[:, 0:448], hnT[:, dc, rows], Wm[:, dc, :], start=(dc == 0), stop=(dc == 7))
        p.copy("act", cs[:], bc_[:, 0:448])
        rmsnorm_tile(p, cs[:, 0:256], gq[:], cn[:, 0:256], sq[:, 0:256], ss[:], rstd[:], width=QRANK)
        rmsnorm_tile(p, cs[:, 256:384], gkv[:], cn[:, 256:384], sq[:, 0:128], ss[:], rstd[:], width=KVR)
        p.dma("pool", CKV[rows, :], cn[:, 256:384])
        p.tt("dve", sq[:, 0:128], cn[:, 256:384], cn[:, 256:384], ALU.mult)
        p.reduce("dve", kn[:, 0:1], sq[:, 0:128], ALU.add)
        p.tt("dve", sq[:, 0:64], cs[:, 384:448], cs[:, 384:448], ALU.mult)
        p.reduce("dve", kn[:, 1:2], sq[:, 0:64], ALU.add)
        p.tt("dve", kn[:, 0:1], kn[:, 0:1], kn[:, 1:2], ALU.add)
        p.dma("pool", KN2[rows, :], kn[:, 0:1])
        for q in range(3):
            p.transpose(pT[:, q * 128:(q + 1) * 128], cn[:, q * 128:(q + 1) * 128], cb[:])
        p.copy("act", cqT[:, :, rows], pT.v(pT.t[:, 0:256].rearrange("p (c t) -> p c t", c=2)))
        p.copy("act", ckT[:], pT[:, 256:384])
        p.dma("pool", CKVT[:, rows], ckT[:])

    for tg in range(TOK // 512):
        cols = slice(tg * 512, (tg + 1) * 512)
        bk_, bks = bank(), bank()
        for dc in range(8):
            p.matmul(bk_[0:64, :], Wm[:, dc, 384:448], hnT[:, dc, cols], start=(dc == 0), stop=(dc == 7))
        for dc in range(8):
            p.matmul(bks[0:64, :], Wm_sw[:, dc, :], hnT[:, dc, cols], start=(dc == 0), stop=(dc == 7))
        p.tt("dve", r1[:], bk_[0:64, :], C2[:, cols], ALU.mult)
        p.tt("dve", r2[:], bks[0:64, :], S2[:, cols], ALU.mult)
        p.tt("dve", rb[:], r1[:], r2[:], ALU.add)
        p.dma("pool", KRT[:, cols], rb[:])
        for h in range(MH):
            o = h * 192
            bq = bank()
            for kc in range(2):
                p.matmul(bq[:], Wuq[:, kc, o:o + 128], cqT[:, kc, cols], start=(kc == 0), stop=(kc == 1))
            p.copy("act", qn[:], bq[:])
            bl = bank()
            p.matmul(bl[:], WukT[:, h, :], qn[:])
            p.ts("dve", qlf[:], bl[:], MLA_SCALE, None, ALU.mult)
            p.copy("act", qlb[:], qlf[:])
            p.dma("pool", QL.v(QL.t[h, :, cols]), qlb[:])
            br, brs = bank(), bank()
            for kc in range(2):
                p.matmul(br[0:64, :], Wuq[:, kc, o + 128:o + 192], cqT[:, kc, cols], start=(kc == 0), stop=(kc == 1))
            for kc in range(2):
                p.matmul(brs[0:64, :], Wuq_sw[:, kc, h, :], cqT[:, kc, cols], start=(kc == 0), stop=(kc == 1))
            p.tt("dve", r1[:], br[0:64, :], C2[:, cols], ALU.mult)
            p.tt("dve", r2[:], brs[0:64, :], S2[:, cols], ALU.mult)
            p.tt("dve", r1[:], r1[:], r2[:], ALU.add)
            p.ts("dve", r1[:], r1[:], MLA_SCALE, None, ALU.mult)
            p.copy("act", rb[:], r1[:])
            p.dma("pool", QR.v(QR.t[h, :, cols]), rb[:])
            p.tt("dve", qsq[:], qlf[:], qlf[:], ALU.mult)
            p.tt("dve", r2[:], r1[:], r1[:], ALU.mult)
            bn = bank()
            p.matmul(bn[0:1, :], onesf[:, 0:1], qsq[:], start=True, stop=False)
            p.matmul(bn[0:1, :], onesf[0:64, 0:1], r2[:], start=False, stop=True)
            p.reduce("dve", qm1[:], bn[0:1, :], ALU.max)
            p.tt("dve", qmx[:, h:h + 1], qmx[:, h:h + 1], qm1[:], ALU.max)
    p.dma("pool", QMAX[:, :], qmx[:])
    return p.finish()


NKT = SEQ // 128


def build_L5(nheads=None, same=True):
    nc = bass.Bass("TRN2", target_bir_lowering=False)
    p = Prog(nc)
    QL = p.dram("QL", [MH, 128, TOK], BF16, "ExternalInput")
    QR = p.dram("QR", [MH, 64, TOK], BF16, "ExternalInput")
    p.same = same
    KTd = p.dram("KT", [128, SEQ], BF16, "ExternalInput")
    KRd = p.dram("KR", [64, SEQ], BF16, "ExternalInput")
    Vd = p.dram("VK", [SEQ, 128], BF16, "ExternalInput")
    KN2 = p.dram("KN2", [128, 128], F32, "ExternalInput")
    QMAX = p.dram("QMAX", [1, MH], F32, "ExternalInput")
    H2 = p.dram("H2", [TOK, D], F32, "ExternalInput")
    w_ukv = p.dram("w_ukv", [KVR, MH * 256], F32, "ExternalInput")
    w_o = p.dram("w_o", [MH * 128, D], F32, "ExternalInput")
    g_ffn = p.dram("g_ffn", [1, D], F32, "ExternalInput")
    w_r = p.dram("w_r", [D, 16], F32, "ExternalInput")
    cf_d = p.dram("cf", [128, 5, 128], F32, "ExternalInput")
    cb_d = p.dram("cb", [128, 128], BF16, "ExternalInput")
    H3 = p.dram("H3", [TOK, D], F32, "ExternalOutput")
    HN3 = p.dram("HN3", [TOK, D], BF16, "ExternalOutput")
    AFF = p.dram("AFF", [TOK, 16], F32, "ExternalOutput")
    OTD = p.dram("OTD", [MH, 128, TOK], BF16, "Internal")

    KT = p.sb("KTs", [128, SEQ], BF16)
    KR2 = p.sb("KR2", [128, SEQ], BF16)
    Vt = p.sb("Vt", [128, NKT, 128], BF16)
    Wo = p.sb("Wo", [128, MH, D], BF16)
    Wuv = p.sb("Wuv", [128, MH, 128], BF16)
    wr = p.sb("wr", [128, 8, 16], F32)
    cf = p.sb("cfs", [128, 5, 128], F32)
    cb = p.sb("cbs", [128, 128], BF16)
    gf = p.sb("gf", [128, D], F32)
    onesf = p.sb("onesf", [128, 128], F32)
    kn = p.sb("kn", [128, 128], F32)
    km = p.sb("km", [128, 1], F32)
    km1 = p.sb("km1", [1, 1], F32)
    qmx = p.sb("qmx", [1, MH], F32)
    negm = p.sb("negm", [128, MH], F32)
    QW = 1024
    sbk = [p.ps("sbk%d" % i, [128, QW], F32) for i in range(2)]
    obk = p.ps("obk", [128, QW], F32)
    ebk = [p.ps("ebk%d" % i, [128, 512], F32) for i in range(2)]
    mi = [0]

    def bank():
        b = ebk[mi[0] % 2]
        mi[0] += 1
        return b

    p.dma("sp", cf[:], cf_d[:])
    p.dma("sp", cb[:], cb_d[:])
    p.dma("sp", kn[:], KN2[:, :])
    p.dma("sp", qmx[:], QMAX[:, :])
    p.dma("sp", gf[:], g_ffn.v(g_ffn.t[0:1, :].broadcast_to([128, D])))
    p.dma("sp", wr[:], w_r.v(w_r.t.rearrange("(c p) e -> p c e", p=128)))
    for q in range(8):
        cs_ = slice(q * 2048, (q + 1) * 2048)
        p.dma("sp", KT.sub(q, (slice(None), cs_)), V(KTd.t[:, cs_], ("KTd", q)))
    for half in range(2):
        for q in range(8):
            cs_ = slice(q * 2048, (q + 1) * 2048)
            p.dma("sp", KR2.sub((half, q), (slice(half * 64, (half + 1) * 64), cs_)), V(KRd.t[:, cs_], ("KRd", half, q)))
    for q in range(8):
        p.dma("sp", Vt.sub(q, (slice(None), slice(q * 16, (q + 1) * 16), slice(None))),
              V(Vd.t[q * 2048:(q + 1) * 2048, :].rearrange("(k p) r -> p k r", p=128), ("Vd", q)))
    p.dma("pool", Wuv[:], w_ukv.v(w_ukv.t.rearrange("r (h c) -> r h c", h=MH)[:, :, 128:256]))
    for h in range(MH):
        p.dma("pool", Wo.sub(h, (slice(None), h, slice(None))), V(w_o.t[h * 128:(h + 1) * 128, :], ("w_o", h)))
    p.memset("dve", onesf[:], 1.0)
    p.reduce("dve", km[:], kn[:], ALU.max)
    bt = bank()
    p.transpose(bt[0:1, 0:128], km[:, 0:1], cf[:, 4, :])
    p.reduce("dve", km1[:], bt[0:1, 0:128], ALU.max)
    p.ts("dve", qmx[:], qmx[:], km1[0:1, 0:1], None, ALU.mult)
    p.act(qmx[:], qmx[:], AF.Sqrt)
    p.ts("dve", qmx[:], qmx[:], -1.0, None, ALU.mult)
    bb = bank()
    p.matmul(bb[:, 0:MH], onesf[0:1, :], qmx[:])
    p.copy("act", negm[:], bb[:, 0:MH])

    p.push_scope()
    ql = [p.sb("ql%d" % i, [128, TOK], BF16) for i in range(2)]
    qr = [p.sb("qr%d" % i, [128, TOK], BF16) for i in range(2)]
    PT = [p.sb("PT%d" % i, [128, QW], BF16) for i in range(3)]
    accD = [p.sb("accD%d" % i, [128, QW], F32) for i in range(1)]
    accP = [p.sb("accP%d" % i, [128, QW], F32) for i in range(1)]
    rl = p.sb("rl", [128, QW], F32)
    OTn = p.sb("OTn", [128, QW], BF16)
    oTs = [p.sb("oTs%d" % i, [128, QW], BF16) for i in range(2)]
    NH = MH if nheads is None else nheads
    it = 0
    for h in range(NH):
        Ql, Qr = ql[h % 2], qr[h % 2]
        p.dma("sp", Ql[:], QL.v(QL.t[h]))
        p.dma("sp", Qr[0:64, :], QR.v(QR.t[h]))
        p.dma("sp", Qr[64:128, :], QR.v(QR.t[h]))
        for qg in range(TOK // QW):
            q0 = qg * QW
            bo = obk
            AD, AP_ = accD[0], accP[0]
            it += 1
            first = {"dve": True, "pool": True}

            def s_mm(kt):
                bs = sbk[kt % 2]
                ktile = KT.sub(kt // 16, (slice(None), slice(kt * 128, (kt + 1) * 128)))
                rtiles = [KR2.sub((hf, kt // 16), (slice(hf * 64, (hf + 1) * 64), slice(kt * 128, (kt + 1) * 128)))
                          for hf in range(2)]
                for hh in range(QW // 512):
                    p.matmul(bs[:, hh * 512:(hh + 1) * 512], ktile, Ql[:, q0 + hh * 512:q0 + (hh + 1) * 512],
                             start=True, stop=False)
                if kt >= 1:
                    p.wait("pe", [PT[(kt - 1) % 3].name])
                for hh in range(QW // 512):
                    p.matmul(bs[:, hh * 512:(hh + 1) * 512], rtiles[hh],
                             Qr[hh * 64:(hh + 1) * 64, q0 + hh * 512:q0 + (hh + 1) * 512], start=False, stop=True)

            s_mm(0)
            for kt in range(NKT):
                if kt + 1 < NKT:
                    s_mm(kt + 1)
                P_ = PT[kt % 3]
                p.act(P_[:], sbk[kt % 2][:], AF.Exp, bias=negm[:, h:h + 1])
                vt = Vt.sub(kt // 16, (slice(None), kt, slice(None)))
                for hh in range(QW // 512):
                    p.matmul(bo[:, hh * 512:(hh + 1) * 512], vt, P_[:, hh * 512:(hh + 1) * 512],
                             start=(kt == 0), stop=(kt == NKT - 1))
                eng = "pool" if kt % 3 == 2 else "dve"
                A = AP_ if eng == "pool" else AD
                if first[eng]:
                    p.copy(eng, A[:], P_[:])
                    first[eng] = False
                else:
                    p.tt(eng, A[:], A[:], P_[:], ALU.add)
            p.tt("dve", AD[:], AD[:], AP_[:], ALU.add)
            for hh in range(QW // 512):
                hs = slice(hh * 512, (hh + 1) * 512)
                bl = ebk[hh]
                p.matmul(bl[:], onesf[:], AD[:, hs])
                p.recip(rl[:, hs], bl[:])
                p.tt("dve", OTn[:, hs], bo[:, hs], rl[:, hs], ALU.mult)
            oT = oTs[it % 2]
            for hh in range(QW // 512):
                hs = slice(hh * 512, (hh + 1) * 512)
                bv = ebk[hh]
                p.matmul(bv[:], Wuv[:, h, :], OTn[:, hs])
                p.copy("act", oT[:, hs], bv[:])
            p.dma("pool", V(OTD.t[h, :, q0:q0 + QW], ("OTD", h, qg)), oT[:])
    otd_keys = [("OTD", h, qg) for h in range(NH) for qg in range(TOK // QW)]
    p.pop_scope()
    oTt = [p.sb("oTt%d" % i, [128, MH, 128], BF16) for i in range(2)]
    h2t = [p.sb("h2t%d" % i, [128, D], F32) for i in range(2)]
    h3 = p.sb("h3", [128, D], F32)
    fb = ffn_prep_bufs(p)

    for j in range(NT):
        rows = slice(j * 128, (j + 1) * 128)
        b = j % 2
        p.dma("sp", h2t[b][:], H2[rows, :])
        p.dma("sp", oTt[b][:], V(OTD.t[:, :, rows].rearrange("h d t -> d h t"), ("OTD", "rd", j)),
              reads=[k_ for k_ in otd_keys if k_[2] == j // (QW // 128)])
        for half in range(2):
            bm = bank()
            for h in range(MH):
                p.matmul(bm[:], oTt[b][:, h, :], Wo.sub(h, (slice(None), h, slice(half * 512, (half + 1) * 512))),
                         start=(h == 0), stop=(h == MH - 1))
            p.tt("dve", h3[:, half * 512:(half + 1) * 512], bm[:], h2t[b][:, half * 512:(half + 1) * 512], ALU.add)
        p.dma("pool", H3[rows, :], h3[:])
        ffn_prep_tile(p, h3[:], gf, wr, cf, fb, HN3, AFF, rows, bank)
    return p.finish()


def build_L7():
    nc = bass.Bass("TRN2", target_bir_lowering=False)
    p = Prog(nc)
    H3 = p.dram("H3", [TOK, D], F32, "ExternalInput")
    DS = p.dram("DS", [NCORES, TOK, D], F32, "ExternalInput")
    g_fin = p.dram("g_fin", [1, D], F32, "ExternalInput")
    OUT = p.dram("OUT", [TOK, D], F32, "ExternalOutput")
    gm = p.sb("gm", [128, D], F32)
    ht = [p.sb("ht%d" % i, [128, D], F32) for i in range(2)]
    dt_ = [p.sb("dt%d" % i, [128, D], F32) for i in range(3)]
    sq = p.sb("sq", [128, D], F32)
    ss = p.sb("ss", [128, 1], F32)
    rstd = p.sb("rstd", [128, 1], F32)
    ot = [p.sb("ot%d" % i, [128, D], F32) for i in range(2)]
    p.dma("sp", gm[:], g_fin.v(g_fin.t[0:1, :].broadcast_to([128, D])))
    for j in range(NT):
        rows = slice(j * 128, (j + 1) * 128)
        Ht = ht[j % 2]
        p.dma("sp", Ht[:], H3[rows, :])
        for c in range(NCORES):
            Dt = dt_[c % 3]
            p.dma("sp", Dt[:], DS.v(DS.t[c, rows, :]))
            p.tt("dve", Ht[:], Ht[:], Dt[:], ALU.add)
        rmsnorm_tile(p, Ht[:], gm[:], ot[j % 2][:], sq[:], ss[:], rstd[:])
        p.dma("pool", OUT[rows, :], ot[j % 2][:])
    return p.finish()


def run(nc, in_maps):
    res = run_bass_kernel_spmd(nc, in_maps, core_ids=list(range(NCORES)))
    return res.results


_CACHE = {}


def _prog(name, builder):
    if name not in _CACHE:
        _CACHE[name] = builder()
    return _CACHE[name]


def _c(a):
    return np.ascontiguousarray(a)


def stage_L1(inp):
    cf, cb = const_tables()
    x = inp["x"][0]
    wg = _c(np.stack([inp["gla_w_gate_up_f"][0], inp["gla_w_gate_up_b"][0]]))
    bg = _c(np.stack([inp["gla_b_gate_f"][0], inp["gla_b_gate_b"][0]]))
    maps = [dict(x=_c(x[c * TOK:(c + 1) * TOK]), g_mix=_c(inp["mix_norm"][0:1]), w_in=_c(inp["gla_w_in"][0]),
                 wg=wg, bg=bg, cf=cf, cb=cb) for c in range(NCORES)]
    return run(_prog("L1", build_L1), maps)


def stage_L2(inp, r1):
    cf, cb = const_tables()
    x = inp["x"][0]
    maps = []
    for c in range(NCORES):
        BP = np.zeros((2, 7, GH, 128, GDV), np.float32)
        APD = np.zeros((2, 7, 128, NT * GH), np.float32)
        for i in range(7):
            cf_ = c - 7 + i
            if cf_ >= 0:
                BP[0, i] = r1[cf_]["BE"][0]
                APD[0, i] = r1[cf_]["AA"][0]
            cb_ = c + 7 - i
            if cb_ <= NCORES - 1:
                BP[1, i] = r1[cb_]["BE"][1]
                APD[1, i] = r1[cb_]["AA"][1]
        maps.append(dict(x=_c(x[c * TOK:(c + 1) * TOK]), OL=r1[c]["OL"], QP=r1[c]["QP"], SR=r1[c]["SR"],
                         AA=r1[c]["AA"], BP=BP, APD=APD, g_head=_c(inp["gla_head_norm"][0:1]),
                         w_out=_c(inp["gla_w_out"][0]), g_ffn=_c(inp["ffn_norm"][0:1]),
                         w_r=_c(inp["moe_w_router"][0]), cf=cf, cb=cb))
    return run(_prog("L2", build_L2), maps)


def stage_L3(inp, layer, aff_full, hn_full):
    cf, cb = const_tables()
    tokid = np.arange(SEQ, dtype=np.int32).reshape(128, 128)
    maps = []
    for c in range(NCORES):
        affe = _c(aff_full[:, 2 * c:2 * c + 2].T.reshape(2, 128, 128))
        maps.append(dict(AFFE=affe, HN=hn_full, wg=_c(inp["moe_w_gate"][layer, 2 * c:2 * c + 2]),
                         wu=_c(inp["moe_w_up"][layer, 2 * c:2 * c + 2]), wd=_c(inp["moe_w_down"][layer, 2 * c:2 * c + 2]),
                         cf=cf, cb=cb, tokid=tokid))
    return run(_prog("L3", build_L3), maps)


def stage_L4(inp, h_prev, deltas):
    cf, cb = const_tables()
    rc = rope_consts()
    maps = []
    for c in range(NCORES):
        DS = _c(np.stack([d[c * TOK:(c + 1) * TOK] for d in deltas]))
        maps.append(dict(H1=h_prev[c], DS=DS, pos=_c(inp["positions"][0:1, c * TOK:(c + 1) * TOK]),
                         g_mix=_c(inp["mix_norm"][1:2]), w_m=_c(inp["mla_w_in"][0]), g_q=_c(inp["mla_q_norm"][0:1]),
                         g_kv=_c(inp["mla_kv_norm"][0:1]), w_uq=_c(inp["mla_w_uq"][0]), w_ukv=_c(inp["mla_w_ukv"][0]),
                         rc=rc, cf=cf, cb=cb))
    return run(_prog("L4", build_L4), maps)


def stage_L5(inp, r4):
    cf, cb = const_tables()
    KT = _c(np.concatenate([r4[c]["CKVT"] for c in range(NCORES)], axis=1))
    KR = _c(np.concatenate([r4[c]["KRT"] for c in range(NCORES)], axis=1))
    VK = _c(np.concatenate([r4[c]["CKV"] for c in range(NCORES)], axis=0))
    KN2 = _c(np.concatenate([r4[c]["KN2"] for c in range(NCORES)], axis=0).reshape(128, 128))
    maps = []
    for c in range(NCORES):
        maps.append(dict(QL=r4[c]["QL"], QR=r4[c]["QR"], KT=KT, KR=KR, VK=VK, KN2=KN2, QMAX=r4[c]["QMAX"],
                         H2=r4[c]["H2"], w_ukv=_c(inp["mla_w_ukv"][0]), w_o=_c(inp["mla_w_out"][0]),
                         g_ffn=_c(inp["ffn_norm"][1:2]), w_r=_c(inp["moe_w_router"][1]), cf=cf, cb=cb))
    return run(_prog("L5", build_L5), maps)


def stage_L7(inp, h_prev, deltas):
    maps = []
    for c in range(NCORES):
        DS = _c(np.stack([d[c * TOK:(c + 1) * TOK] for d in deltas]))
        maps.append(dict(H3=h_prev[c], DS=DS, g_fin=_c(inp["final_norm"].reshape(1, D))))
    return run(_prog("L7", build_L7), maps)


def kernel(**inputs):
    inp = {k: np.asarray(v) for k, v in inputs.items()}
    r1 = stage_L1(inp)
    r2 = stage_L2(inp, r1)
    aff0 = _c(np.concatenate([r2[c]["AFF"] for c in range(NCORES)], axis=0))
    hn1 = _c(np.concatenate([r2[c]["HN1"] for c in range(NCORES)], axis=0))
    r3 = stage_L3(inp, 0, aff0, hn1)
    r4 = stage_L4(inp, [r2[c]["H1"] for c in range(NCORES)], [r3[c]["DELTA"] for c in range(NCORES)])
    del r3
    r5 = stage_L5(inp, r4)
    aff1 = _c(np.concatenate([r5[c]["AFF"] for c in range(NCORES)], axis=0))
    hn3 = _c(np.concatenate([r5[c]["HN3"] for c in range(NCORES)], axis=0))
    r6 = stage_L3(inp, 1, aff1, hn3)
    r7 = stage_L7(inp, [r5[c]["H3"] for c in range(NCORES)], [r6[c]["DELTA"] for c in range(NCORES)])
    out = np.concatenate([r7[c]["OUT"] for c in range(NCORES)], axis=0).astype(np.float32)
    return out.reshape(1, SEQ, D)
```

```python
import contextlib
import numpy as np
import ml_dtypes
import concourse.bass as bass
import concourse.mybir as mybir
from concourse.bass_utils import run_bass_kernel_spmd

F32 = mybir.dt.float32
BF16 = mybir.dt.bfloat16
I32 = mybir.dt.int32
AF = mybir.ActivationFunctionType
ALU = mybir.AluOpType
AX = mybir.AxisListType

NCORES = 8
SEQ = 16384
TOK = SEQ // NCORES
NT = TOK // 128
D = 1024
RMS_EPS = 1e-6


class V:
    __slots__ = ("ap", "key")

    def __init__(self, ap, key):
        self.ap = ap
        self.key = key


class Buf:
    def __init__(self, t, name):
        self.t = t
        self.name = name

    def __getitem__(self, idx):
        return V(self.t[idx], self.name)

    def sub(self, s, idx):
        return V(self.t[idx], (self.name, s))

    def v(self, ap, s=None):
        return V(ap, self.name if s is None else (self.name, s))


class Prog:
    ENGS = ("pe", "dve", "act", "pool", "sp")
    NDMA = {"sp": 10, "pool": 8, "act": 4}

    def __init__(self, nc, same_engine_sync=True):
        self.nc = nc
        self.same = same_engine_sync
        self.stack = contextlib.ExitStack()
        self.lists = {e: [] for e in self.ENGS}
        self.cnt = {e: 0 for e in self.ENGS}
        self.seen = {e: {} for e in self.ENGS}
        self.last_w = {}
        self.readers = {}
        self.sem = {}
        self.dma_cnt = {}
        self.dma_rr = {q: 0 for q in self.NDMA}
        for e in ("pe", "dve", "act", "pool"):
            self.sem[("c", e)] = self.stack.enter_context(nc.semaphore("c_" + e))
        for q, n in self.NDMA.items():
            for k in range(n):
                sk = ("d", q, k)
                self.sem[sk] = self.stack.enter_context(nc.semaphore("d_%s%d" % (q, k)))
                self.dma_cnt[sk] = 0
        self.psum = set()
        self.bregs = {}
        self.same_dist = 2

    def sb(self, name, shape, dt):
        t = self.stack.enter_context(self.nc.sbuf_tensor(name, list(shape), dt))
        return Buf(t, name)

    def ps(self, name, shape, dt):
        t = self.stack.enter_context(self.nc.psum_tensor(name, list(shape), dt))
        self.psum.add(name)
        return Buf(t, name)

    def dram(self, name, shape, dt, kind):
        t = self.nc.dram_tensor(name, list(shape), dt, kind=kind)
        return Buf(t.ap(), name)

    def emit(self, eng, fn, reads, writes, dma=False):
        deps = {}

        def need(tok):
            if tok is None:
                return
            sk, val = tok
            if deps.get(sk, 0) < val:
                deps[sk] = val

        for r in reads:
            need(self.last_w.get(r))
            if r in self.psum:
                for sk, val in self.readers.get(r, {}).items():
                    if sk != ("c", eng):
                        need((sk, val))
        for w in writes:
            need(self.last_w.get(w))
            for sk, val in self.readers.get(w, {}).items():
                need((sk, val))
        if dma:
            k = self.dma_rr[eng]
            self.dma_rr[eng] = (k + 1) % self.NDMA[eng]
            sk = ("d", eng, k)
            if self.dma_cnt[sk] > 0:
                need((sk, self.dma_cnt[sk]))
            self.dma_cnt[sk] += 16
            tok = (sk, self.dma_cnt[sk])
        else:
            self.cnt[eng] += 1
            tok = (("c", eng), self.cnt[eng])
        waits = []
        for sk, val in deps.items():
            if sk == ("c", eng) and (eng == "pe" or not self.same):
                continue
            if sk == ("c", eng) and tok[1] - val > self.same_dist:
                continue
            if self.seen[eng].get(sk, 0) >= val:
                continue
            self.seen[eng][sk] = val
            waits.append((sk, val))
        self.lists[eng].append((waits, fn, tok))
        for r in reads:
            d = self.readers.setdefault(r, {})
            if d.get(tok[0], 0) < tok[1]:
                d[tok[0]] = tok[1]
        for w in writes:
            self.last_w[w] = tok
            self.readers[w] = {}

    @staticmethod
    def _keys(*vs):
        return [v.key for v in vs if isinstance(v, V)]

    @staticmethod
    def _ap(v):
        return v.ap if isinstance(v, V) else v

    def dma(self, q, out, in_, reads=None, writes=None, **kw):
        o, i = out.ap, in_.ap
        self.emit(q, lambda e: e.dma_start(out=o, in_=i, **kw), [in_.key] + (reads or []),
                  [out.key] if writes is None else writes, dma=True)

    def _breg(self, e, val):
        if val not in self.bregs:
            r = e.alloc_register("bchk%d" % val)
            e.reg_mov(r, val)
            self.bregs[val] = r
        return self.bregs[val]

    def gather(self, out, src, idx, nrows):
        o, s, ix = out.ap, src.ap, idx.ap
        self.emit("pool", lambda e: e.indirect_dma_start(
            out=o, out_offset=None, in_=s,
            in_offset=bass.IndirectOffsetOnAxis(ap=ix, axis=0),
            bounds_check=self._breg(e, nrows - 1), oob_is_err=False),
            [src.key, idx.key], [out.key], dma=True)

    def scatter(self, dst, src, idx, nrows, accum=False, reads=None, writes=None):
        d, s, ix = dst.ap, src.ap, idx.ap
        kw = {"compute_op": ALU.add} if accum else {}
        self.emit("pool", lambda e: e.indirect_dma_start(
            out=d, out_offset=bass.IndirectOffsetOnAxis(ap=ix, axis=0), in_=s, in_offset=None,
            bounds_check=self._breg(e, nrows - 1), oob_is_err=False, **kw),
            [src.key, idx.key] + ([dst.key] if accum else []) + (reads or []),
            [dst.key] if writes is None else writes, dma=True)

    def matmul(self, out, lhsT, rhs, start=True, stop=True):
        o, l, r = out.ap, lhsT.ap, rhs.ap
        rd = [lhsT.key, rhs.key] + ([] if start else [out.key])
        self.emit("pe", lambda e: e.matmul(o, l, r, start=start, stop=stop), rd, [out.key])

    def transpose(self, out, in_, ident):
        o, i, d = out.ap, in_.ap, ident.ap
        self.emit("pe", lambda e: e.transpose(o, i, d), [in_.key, ident.key], [out.key])

    def act(self, out, in_, func, bias=0.0, scale=1.0, accum_out=None, eng="act"):
        o, i = out.ap, in_.ap
        b, s = self._ap(bias), self._ap(scale)
        kw = {}
        if accum_out is not None:
            kw["accum_out"] = accum_out.ap
        self.emit(eng, lambda e: e.activation(o, i, func, bias=b, scale=s, **kw),
                  self._keys(in_, bias, scale), self._keys(out, accum_out))

    def copy(self, eng, out, in_):
        o, i = out.ap, in_.ap
        if eng == "act":
            self.emit(eng, lambda e: e.copy(o, i), [in_.key], [out.key])
        else:
            self.emit(eng, lambda e: e.tensor_copy(o, i), [in_.key], [out.key])

    def tt(self, eng, out, in0, in1, op):
        o, a, b = out.ap, in0.ap, in1.ap
        self.emit(eng, lambda e: e.tensor_tensor(o, a, b, op), [in0.key, in1.key], [out.key])

    def ts(self, eng, out, in0, s1, s2, op0, op1=None, accum_out=None):
        o, a = out.ap, in0.ap
        x1, x2 = self._ap(s1), self._ap(s2)
        kw = {}
        if op1 is not None:
            kw["op1"] = op1
        if accum_out is not None:
            kw["accum_out"] = accum_out.ap
        self.emit(eng, lambda e: e.tensor_scalar(o, a, x1, x2, op0, **kw),
                  self._keys(in0, s1, s2), self._keys(out, accum_out))

    def stt(self, eng, out, in0, scalar, in1, op0, op1):
        o, a, b = out.ap, in0.ap, in1.ap
        s = self._ap(scalar)
        self.emit(eng, lambda e: e.scalar_tensor_tensor(o, a, s, b, op0, op1),
                  self._keys(in0, scalar, in1), [out.key])

    def reduce(self, eng, out, in_, op, axis=AX.X):
        o, i = out.ap, in_.ap
        self.emit(eng, lambda e: e.tensor_reduce(o, i, axis, op), [in_.key], [out.key])

    def recip(self, out, in_):
        o, i = out.ap, in_.ap
        self.emit("dve", lambda e: e.reciprocal(o, i), [in_.key], [out.key])

    def memset(self, eng, out, val):
        o = out.ap
        self.emit(eng, lambda e: e.memset(o, val), [], [out.key])

    def wait(self, eng, keys):
        waits = []
        for k_ in keys:
            tok = self.last_w.get(k_)
            if tok is None:
                continue
            sk, val = tok
            if sk == ("c", eng) or self.seen[eng].get(sk, 0) >= val:
                continue
            self.seen[eng][sk] = val
            waits.append((sk, val))
        if waits:
            self.lists[eng].append((waits, None, None))

    def barrier(self):
        allv = {("c", e): self.cnt[e] for e in ("pe", "dve", "act", "pool") if self.cnt[e] > 0}
        allv.update({sk: v for sk, v in self.dma_cnt.items() if v > 0})
        for eng in self.ENGS:
            waits = []
            for sk, val in allv.items():
                if sk == ("c", eng) or self.seen[eng].get(sk, 0) >= val:
                    continue
                self.seen[eng][sk] = val
                waits.append((sk, val))
            if waits:
                self.lists[eng].append((waits, None, None))

    def push_scope(self):
        self._outer = self.stack
        self.stack = contextlib.ExitStack()

    def pop_scope(self):
        self.barrier()
        self.stack.close()
        self.stack = self._outer

    def finish(self):
        nc = self.nc
        prog = self

        def mk(name):
            def body(e):
                for waits, fn, tok in prog.lists[name]:
                    for sk, val in waits:
                        e.wait_ge(prog.sem[sk], val)
                    if fn is None:
                        continue
                    ins = fn(e)
                    ins.then_inc(prog.sem[tok[0]], 16 if tok[0][0] == "d" else 1)
                if name == "sp":
                    for sk, val in prog.dma_cnt.items():
                        if val > 0:
                            e.wait_ge(prog.sem[sk], val)
            return body

        with nc.Block() as block:
            block.tensor(mk("pe"))
            block.vector(mk("dve"))
            block.scalar(mk("act"))
            block.gpsimd(mk("pool"))
            block.sync(mk("sp"))
        self.stack.close()
        return nc


def bcast_rows(ap1d_or_row, nparts):
    return ap1d_or_row.broadcast(0, nparts) if hasattr(ap1d_or_row, "broadcast") else ap1d_or_row


def const_tables(ncf=5):
    i = np.arange(128)
    ui = (i[:, None] <= i[None, :]).astype(np.float32)
    li = (i[:, None] >= i[None, :]).astype(np.float32)
    us = (i[:, None] < i[None, :]).astype(np.float32)
    ls = (i[:, None] > i[None, :]).astype(np.float32)
    ident = np.eye(128, dtype=np.float32)
    iota = np.broadcast_to(np.arange(128, dtype=np.float32)[None, :], (128, 128))
    cf = np.stack([ui, li, us, ls, ident, iota], axis=1)
    cb = np.eye(128, dtype=np.float32).astype(ml_dtypes.bfloat16)
    return np.ascontiguousarray(cf[:, :ncf]), cb


GH = 4
GDK = 128
GDV = 256
GIN = 3104


def rmsnorm_tile(p, xt, gbc, hn_out, sq, ss, rstd, width=D):
    p.act(sq, xt, AF.Square, accum_out=ss)
    p.ts("dve", rstd, ss, 1.0 / width, RMS_EPS, ALU.mult, ALU.add)
    p.act(rstd, rstd, AF.Sqrt)
    p.recip(rstd, rstd)
    p.stt("dve", hn_out, xt, rstd, gbc, ALU.mult, ALU.mult)


def build_L1():
    nc = bass.Bass("TRN2", target_bir_lowering=False)
    p = Prog(nc)
    x = p.dram("x", [TOK, D], F32, "ExternalInput")
    g_mix = p.dram("g_mix", [1, D], F32, "ExternalInput")
    w_in = p.dram("w_in", [D, GIN], F32, "ExternalInput")
    wg = p.dram("wg", [2, 16, 512], F32, "ExternalInput")
    bg = p.dram("bg", [2, 512], F32, "ExternalInput")
    cf_d = p.dram("cf", [128, 5, 128], F32, "ExternalInput")
    cb_d = p.dram("cb", [128, 128], BF16, "ExternalInput")
    QP = p.dram("QP", [2, 128, GH, TOK], BF16, "ExternalOutput")
    KP = p.dram("KP", [2, 128, GH, TOK], BF16, "Internal")
    K2 = p.dram("K2", [2, TOK, 512], BF16, "Internal")
    VT = p.dram("VT", [TOK, 1024], BF16, "Internal")
    SR = p.dram("SR", [TOK, 1024], F32, "ExternalOutput")
    OL = p.dram("OL", [TOK, 1024], F32, "ExternalOutput")
    AA = p.dram("AA", [2, 128, NT * GH], F32, "ExternalOutput")
    BE = p.dram("BE", [2, GH, 128, GDV], F32, "ExternalOutput")

    W = p.sb("W", [128, 8, GIN], BF16)
    cf = p.sb("cfs", [128, 5, 128], F32)
    cb = p.sb("cbs", [128, 128], BF16)
    gbc = p.sb("gbc", [128, D], F32)
    bgs = p.sb("bgs", [128, 2, 512], F32)
    wgs = p.sb("wgs", [16, 2, 512], BF16)
    xt = [p.sb("xt%d" % i, [128, D], F32) for i in range(2)]
    sq = p.sb("sq", [128, D], F32)
    ss = p.sb("ss", [128, 1], F32)
    rstd = p.sb("rstd", [128, 1], F32)
    hn = p.sb("hn", [128, D], BF16)
    hnT = p.sb("hnT", [128, D], BF16)
    gdT = p.sb("gdT", [16, 256], BF16)
    zb = p.sb("zb", [128, 512], F32)
    spl = [p.sb("spl%d" % i, [128, 512], F32) for i in range(2)]
    EqT = p.sb("EqT", [128, 512], F32)
    EkT = p.sb("EkT", [128, 512], F32)
    Ek2 = p.sb("Ek2", [128, 512], F32)
    a_all = p.sb("a_all", [128, 2, NT * GH], F32)
    c_all = p.sb("c_all", [128, 2, NT * GH], F32)
    qp = [p.sb("qp%d" % i, [128, 512], BF16) for i in range(2)]
    kp = [p.sb("kp%d" % i, [128, 512], BF16) for i in range(2)]
    k2 = [p.sb("k2%d" % i, [128, 512], BF16) for i in range(2)]
    vb = p.sb("vb", [128, 1024], BF16)
    srt = p.sb("srt", [128, 1024], F32)
    S = p.sb("S", [128, GH, GDV], F32)
    Sb = p.sb("Sb", [128, GH, GDV], BF16)
    s_qp = [p.sb("s_qp%d" % i, [128, 512], BF16) for i in range(2)]
    s_kp = [p.sb("s_kp%d" % i, [128, 512], BF16) for i in range(2)]
    s_k2 = [p.sb("s_k2%d" % i, [128, 512], BF16) for i in range(2)]
    s_v = [p.sb("s_v%d" % i, [128, 1024], BF16) for i in range(2)]
    AT = [p.sb("AT%d" % i, [128, 128], BF16) for i in range(2)]
    ot = [p.sb("ot%d" % i, [128, 1024], F32) for i in range(2)]
    of = [p.sb("of%d" % i, [128, 1024], F32) for i in range(2)]
    banks = [p.ps("bk%d" % i, [128, 512], F32) for i in range(7)]
    pT = p.ps("pT", [128, 1024], BF16)
    bi = [0]

    rot = [3, 4]

    def bank():
        b = banks[rot[0] + bi[0] % rot[1]]
        bi[0] += 1
        return b

    p.dma("sp", cf[:], cf_d[:])
    p.dma("sp", cb[:], cb_d[:])
    p.dma("sp", gbc[:], g_mix.v(g_mix.t[0:1, :].broadcast_to([128, D])))
    p.dma("sp", bgs[:, 0, :], bg.v(bg.t[0:1, :].broadcast_to([128, 512])))
    p.dma("sp", bgs[:, 1, :], bg.v(bg.t[1:2, :].broadcast_to([128, 512])))
    for d_ in range(2):
        p.dma("pool", wgs.sub(d_, (slice(None), d_, slice(None))), wg[d_])
    for dc in range(8):
        p.dma("pool", W.sub(dc, (slice(None), dc, slice(None))), w_in[dc * 128:(dc + 1) * 128, :])
    Wk = lambda dc, lo, hi: W.sub(dc, (slice(None), dc, slice(lo, hi)))
    UI, LI, US, LS = (cf[:, i, :] for i in range(4))
    scale_q = float(GDK) ** -0.5

    def scan_init(d_):
        p.memset("dve", S[:], 0.0)
        p.memset("dve", Sb[:], 0.0)

    def scan_step(d_, j, it, sq_, sk_, sk2_, sv_):
        b = it % 2
        mask = UI if d_ == 0 else LS
        O = ot[b]
        for h in range(GH):
            hs = slice(h * 128, (h + 1) * 128)
            vs = slice(h * GDV, (h + 1) * GDV)
            ba = bank()
            p.matmul(ba[:, 0:128], sk_[:, hs], sq_[:, hs])
            A = AT[h % 2]
            p.tt("dve", A[:], ba[:, 0:128], mask, ALU.mult)
            bo = bank()
            p.matmul(bo[:, 0:GDV], A[:], sv_[:, vs], start=True, stop=False)
            p.matmul(bo[:, 0:GDV], sq_[:, hs], Sb[:, h, :], start=False, stop=True)
            bs = bank()
            p.matmul(bs[:, 0:GDV], sk2_[:, hs], sv_[:, vs])
            if d_ == 0:
                p.copy("act", O[:, vs], bo[:, 0:GDV])
            else:
                p.tt("dve", O[:, vs], bo[:, 0:GDV], of[b][:, vs], ALU.add)
            p.stt("dve", S[:, h, :], S[:, h, :], a_all.v(a_all.t[:, d_, j * GH + h:j * GH + h + 1]),
                  bs[:, 0:GDV], ALU.mult, ALU.add)
            p.copy("act", Sb[:, h, :], S[:, h, :])
        p.dma("pool", OL[j * 128:(j + 1) * 128, :], O[:])

    def scan_fin(d_):
        for h in range(GH):
            p.dma("pool", BE.v(BE.t[d_, h]), S[:, h, :])

    scan_init(0)
    for j in range(NT):
        X = xt[j % 2]
        p.dma("sp", X[:], x[j * 128:(j + 1) * 128, :])
        rmsnorm_tile(p, X[:], gbc[:], hn[:], sq[:], ss[:], rstd[:])
        for dc in range(8):
            p.transpose(pT[:, dc * 128:(dc + 1) * 128], hn[:, dc * 128:(dc + 1) * 128], cb[:])
        p.copy("act", hnT[:], pT[:])
        hT = lambda dc: hnT[:, dc * 128:(dc + 1) * 128]
        bG = bank()
        for d_ in range(2):
            for dc in range(8):
                p.matmul(bG[0:16, d_ * 128:(d_ + 1) * 128], Wk(dc, 3072 + 16 * d_, 3088 + 16 * d_), hT(dc),
                         start=(dc == 0), stop=(dc == 7))
        p.copy("act", gdT[:], bG[0:16, 0:256])
        bq, bk, bkt = banks[0], banks[1], banks[2]
        for n in range(4):
            for dc in range(8):
                p.matmul(bq[:, n * 128:(n + 1) * 128], Wk(dc, n * 128, (n + 1) * 128), hT(dc),
                         start=(dc == 0), stop=(dc == 7))
        for n in range(4):
            for dc in range(8):
                p.matmul(bk[:, n * 128:(n + 1) * 128], Wk(dc, 512 + n * 128, 512 + (n + 1) * 128), hT(dc),
                         start=(dc == 0), stop=(dc == 7))
        for dc in range(8):
            p.matmul(bkt[:], hT(dc), Wk(dc, 512, 1024), start=(dc == 0), stop=(dc == 7))
        for d_ in range(2):
            bz = bank()
            p.matmul(bz[:], gdT[:, d_ * 128:(d_ + 1) * 128], wgs.sub(d_, (slice(None), d_, slice(None))))
            p.tt("dve", zb[:], bz[:], bgs[:, d_, :], ALU.add)
            p.act(zb[:], zb[:], AF.Exp, scale=-1.0)
            sp_ = spl[d_]
            p.act(sp_[:], zb[:], AF.Ln, bias=1.0)
            bc, bd = bank(), bank()
            tri = UI if d_ == 0 else LI
            for h in range(GH):
                p.matmul(bc[:, h * 128:(h + 1) * 128], sp_[:, h * 128:(h + 1) * 128], tri)
            p.matmul(bd[:], LS if d_ == 0 else US, sp_[:])
            p.act(EqT[:], bc[:], AF.Exp, scale=-1.0 / 16)
            p.act(EkT[:], bc[:], AF.Exp, scale=1.0 / 16)
            p.act(Ek2[:], bd[:], AF.Exp, scale=-1.0 / 16)
            last = 127 if d_ == 0 else 0
            p.copy("dve", a_all.v(a_all.t[:, d_, j * GH:(j + 1) * GH]),
                   EqT.v(EqT.t[:].rearrange("p (h t) -> p h t", h=GH)[:, :, last]))
            p.copy("dve", c_all.v(c_all.t[:, d_, j * GH:(j + 1) * GH]),
                   bc.v(bc.t[:].rearrange("p (h t) -> p h t", h=GH)[:, :, last]))
            Q, K, KK = qp[d_], kp[d_], k2[d_]
            p.stt("dve", Q[:], bq[:], scale_q, EqT[:], ALU.mult, ALU.mult)
            p.tt("dve", K[:], bk[:], EkT[:], ALU.mult)
            p.tt("dve", KK[:], bkt[:], Ek2[:], ALU.mult)
            p.dma("pool", QP.v(QP.t[d_, :, :, j * 128:(j + 1) * 128]),
                  Q.v(Q.t[:].rearrange("p (h t) -> p h t", h=GH)))
            p.dma("pool", KP.v(KP.t[d_, :, :, j * 128:(j + 1) * 128]),
                  K.v(K.t[:].rearrange("p (h t) -> p h t", h=GH)))
            p.dma("pool", K2.v(K2.t[d_, j * 128:(j + 1) * 128, :]), KK[:])
        for half in range(2):
            bv = bank()
            for dc in range(8):
                p.matmul(bv[:], hT(dc), Wk(dc, 1024 + half * 512, 1536 + half * 512), start=(dc == 0), stop=(dc == 7))
            p.copy("act", vb[:, half * 512:(half + 1) * 512], bv[:])
        p.dma("pool", VT[j * 128:(j + 1) * 128, :], vb[:])
        scan_step(0, j, j, qp[0], kp[0], k2[0], vb)
        for half in range(2):
            br = bank()
            for dc in range(8):
                p.matmul(br[:], hT(dc), Wk(dc, 2048 + half * 512, 2560 + half * 512), start=(dc == 0), stop=(dc == 7))
            p.act(srt[:, half * 512:(half + 1) * 512], br[:], AF.Silu)
        p.dma("pool", SR[j * 128:(j + 1) * 128, :], srt[:])
    scan_fin(0)
    for d_ in range(2):
        p.dma("pool", AA[d_], c_all[:, d_, :])

    rot[0], rot[1] = 0, 7
    scan_init(1)
    for it, j in enumerate(range(NT - 1, -1, -1)):
        b = it % 2
        sq_, sk_, sk2_, sv_ = s_qp[b], s_kp[b], s_k2[b], s_v[b]
        p.dma("sp", sq_.v(sq_.t[:].rearrange("p (h t) -> p h t", h=GH)), QP.v(QP.t[1, :, :, j * 128:(j + 1) * 128]))
        p.dma("sp", sk_.v(sk_.t[:].rearrange("p (h t) -> p h t", h=GH)), KP.v(KP.t[1, :, :, j * 128:(j + 1) * 128]))
        p.dma("sp", sk2_[:], K2.v(K2.t[1, j * 128:(j + 1) * 128, :]))
        p.dma("sp", sv_[:], VT[j * 128:(j + 1) * 128, :])
        p.dma("sp", of[b][:], OL[j * 128:(j + 1) * 128, :])
        scan_step(1, j, it, sq_, sk_, sk2_, sv_)
    scan_fin(1)
    return p.finish()


def ffn_prep_tile(p, h1, gf, wr, cff, bufs, HN_out, AFF_out, rows, bank):
    sq, ss, rstd, hnf, hnb, hnT, lg, mx, sm = bufs
    rmsnorm_tile(p, h1, gf[:], hnf[:], sq[:], ss[:], rstd[:])
    p.copy("act", hnb[:], hnf[:])
    p.dma("pool", HN_out[rows, :], hnb[:])
    ident_f = cff[:, 4, :]
    for half in range(2):
        bt = bank()
        for q in range(4):
            dc = half * 4 + q
            p.transpose(bt[:, q * 128:(q + 1) * 128], hnf[:, dc * 128:(dc + 1) * 128], ident_f)
        p.copy("act", hnT[:, half * 512:(half + 1) * 512], bt[:])
    bl = bank()
    for dc in range(8):
        p.matmul(bl[:, 0:16], hnT[:, dc * 128:(dc + 1) * 128], wr[:, dc, :], start=(dc == 0), stop=(dc == 7))
    p.reduce("dve", mx[:], bl[:, 0:16], ALU.max)
    p.ts("dve", mx[:], mx[:], -1.0, None, ALU.mult)
    p.act(lg[:], bl[:, 0:16], AF.Exp, bias=mx[:])
    p.reduce("dve", sm[:], lg[:], ALU.add)
    p.recip(sm[:], sm[:])
    p.ts("dve", lg[:], lg[:], sm[:], None, ALU.mult)
    p.dma("pool", AFF_out[rows, :], lg[:])


def ffn_prep_bufs(p):
    return (p.sb("f_sq", [128, D], F32), p.sb("f_ss", [128, 1], F32), p.sb("f_rstd", [128, 1], F32),
            p.sb("f_hnf", [128, D], F32), p.sb("f_hnb", [128, D], BF16), p.sb("f_hnT", [128, D], F32),
            p.sb("f_lg", [128, 16], F32), p.sb("f_mx", [128, 1], F32), p.sb("f_sm", [128, 1], F32))


def build_L2():
    nc = bass.Bass("TRN2", target_bir_lowering=False)
    p = Prog(nc)
    x = p.dram("x", [TOK, D], F32, "ExternalInput")
    OL = p.dram("OL", [TOK, 1024], F32, "ExternalInput")
    QP = p.dram("QP", [2, 128, GH, TOK], BF16, "ExternalInput")
    SR = p.dram("SR", [TOK, 1024], F32, "ExternalInput")
    AA = p.dram("AA", [2, 128, NT * GH], F32, "ExternalInput")
    BP = p.dram("BP", [2, 7, GH, 128, GDV], F32, "ExternalInput")
    APD = p.dram("APD", [2, 7, 128, NT * GH], F32, "ExternalInput")
    g_head = p.dram("g_head", [1, GDV], F32, "ExternalInput")
    w_out = p.dram("w_out", [D, D], F32, "ExternalInput")
    g_ffn = p.dram("g_ffn", [1, D], F32, "ExternalInput")
    w_r = p.dram("w_r", [D, 16], F32, "ExternalInput")
    cf_d = p.dram("cf", [128, 5, 128], F32, "ExternalInput")
    cb_d = p.dram("cb", [128, 128], BF16, "ExternalInput")
    H1 = p.dram("H1", [TOK, D], F32, "ExternalOutput")
    HN1 = p.dram("HN1", [TOK, D], BF16, "ExternalOutput")
    AFF = p.dram("AFF", [TOK, 16], F32, "ExternalOutput")

    Wo = p.sb("Wo", [128, 8, D], BF16)
    wr = p.sb("wr", [128, 8, 16], F32)
    cf = p.sb("cfs", [128, 5, 128], F32)
    cb = p.sb("cbs", [128, 128], BF16)
    gf = p.sb("gf", [128, D], F32)
    gh = p.sb("gh", [128, D], F32)
    apd = p.sb("apd", [128, 2, 7, NT * GH], F32)
    csum = p.sb("csum", [128, 2, 7, GH], F32)
    aown = p.sb("aown", [128, 2, NT * GH], F32)
    Sin = p.sb("Sin", [128, 2, GH, GDV], F32)
    Bt = [p.sb("Bt%d" % i, [128, GH, GDV], F32) for i in range(2)]
    Sbw = p.sb("Sbw", [128, NT, GH, GDV], BF16)
    Sfb = p.sb("Sfb", [128, GH, GDV], BF16)
    xt = [p.sb("xt%d" % i, [128, D], F32) for i in range(2)]
    olt = [p.sb("olt%d" % i, [128, D], F32) for i in range(2)]
    srt = [p.sb("srt%d" % i, [128, D], F32) for i in range(2)]
    qf = [p.sb("qf%d" % i, [128, 512], BF16) for i in range(2)]
    qb = [p.sb("qb%d" % i, [128, 512], BF16) for i in range(2)]
    o = p.sb("o", [128, D], F32)
    osq = p.sb("osq", [128, D], F32)
    hss = p.sb("hss", [128, GH], F32)
    y = p.sb("y", [128, D], BF16)
    yT = p.sb("yT", [128, D], BF16)
    h1 = p.sb("h1", [128, D], F32)
    fb = ffn_prep_bufs(p)
    banks = [p.ps("bk%d" % i, [128, 512], F32) for i in range(7)]
    pT = p.ps("pT", [128, 1024], BF16)
    bi = [0]

    def bank():
        b = banks[bi[0] % 7]
        bi[0] += 1
        return b

    p.dma("sp", cf[:], cf_d[:])
    p.dma("sp", cb[:], cb_d[:])
    p.dma("sp", gf[:], g_ffn.v(g_ffn.t[0:1, :].broadcast_to([128, D])))
    for h in range(GH):
        p.dma("sp", gh[:, h * GDV:(h + 1) * GDV], g_head.v(g_head.t[0:1, :].broadcast_to([128, GDV])))
    p.dma("sp", wr[:], w_r.v(w_r.t.rearrange("(c p) e -> p c e", p=128)))
    for dc in range(8):
        p.dma("pool", Wo.sub(dc, (slice(None), dc, slice(None))), w_out[dc * 128:(dc + 1) * 128, :])
    for d_ in range(2):
        p.dma("sp", apd.v(apd.t[:, d_]), APD.v(APD.t[d_].rearrange("i p c -> p i c")))
        p.dma("sp", aown.v(aown.t[:, d_, :]), AA[d_])
    p.reduce("dve", csum[:], apd.v(apd.t[:].rearrange("p d i (t h) -> p d i h t", h=GH)), ALU.add)
    p.act(csum[:], csum[:], AF.Exp, scale=-1.0 / 16)
    p.act(aown[:], aown[:], AF.Exp, scale=-1.0 / 16)
    p.memset("dve", Sin[:], 0.0)
    n = 0
    for d_ in range(2):
        for i in range(7):
            B = Bt[n % 2]
            n += 1
            p.dma("sp", B[:], BP.v(BP.t[d_, i].rearrange("h p v -> p h v")))
            for h in range(GH):
                p.stt("dve", Sin[:, d_, h, :], Sin[:, d_, h, :], csum.v(csum.t[:, d_, i, h:h + 1]), B[:, h, :],
                      ALU.mult, ALU.add)
    for j in range(NT - 1, -1, -1):
        p.copy("act", Sbw[:, j], Sin[:, 1])
        for h in range(GH):
            p.ts("dve", Sin[:, 1, h, :], Sin[:, 1, h, :], aown.v(aown.t[:, 1, j * GH + h:j * GH + h + 1]), None, ALU.mult)
    for j in range(NT):
        b = j % 2
        rows = slice(j * 128, (j + 1) * 128)
        p.dma("sp", xt[b][:], x[rows, :])
        p.dma("sp", olt[b][:], OL[rows, :])
        p.dma("sp", srt[b][:], SR[rows, :])
        p.dma("sp", qf[b].v(qf[b].t[:].rearrange("p (h t) -> p h t", h=GH)), QP.v(QP.t[0, :, :, rows]))
        p.dma("sp", qb[b].v(qb[b].t[:].rearrange("p (h t) -> p h t", h=GH)), QP.v(QP.t[1, :, :, rows]))
        p.copy("act", Sfb[:], Sin[:, 0])
        for h in range(GH):
            hs = slice(h * 128, (h + 1) * 128)
            vs = slice(h * GDV, (h + 1) * GDV)
            bo = bank()
            p.matmul(bo[:, 0:GDV], qf[b][:, hs], Sfb[:, h, :], start=True, stop=False)
            p.matmul(bo[:, 0:GDV], qb[b][:, hs], Sbw[:, j, h, :], start=False, stop=True)
            p.tt("dve", o[:, vs], bo[:, 0:GDV], olt[b][:, vs], ALU.add)
            p.ts("dve", Sin[:, 0, h, :], Sin[:, 0, h, :], aown.v(aown.t[:, 0, j * GH + h:j * GH + h + 1]), None, ALU.mult)
        p.tt("dve", osq[:], o[:], o[:], ALU.mult)
        p.reduce("dve", hss[:], osq.v(osq.t[:].rearrange("p (h v) -> p h v", h=GH)), ALU.add)
        p.ts("dve", hss[:], hss[:], 1.0 / GDV, RMS_EPS, ALU.mult, ALU.add)
        p.act(hss[:], hss[:], AF.Sqrt)
        p.recip(hss[:], hss[:])
        p.tt("dve", osq[:], srt[b][:], gh[:], ALU.mult)
        for h in range(GH):
            vs = slice(h * GDV, (h + 1) * GDV)
            p.stt("dve", y[:, vs], o[:, vs], hss[:, h:h + 1], osq[:, vs], ALU.mult, ALU.mult)
        for dc in range(8):
            p.transpose(pT[:, dc * 128:(dc + 1) * 128], y[:, dc * 128:(dc + 1) * 128], cb[:])
        p.copy("act", yT[:], pT[:])
        for half in range(2):
            bm = bank()
            for dc in range(8):
                p.matmul(bm[:], yT[:, dc * 128:(dc + 1) * 128],
                         Wo.sub(dc, (slice(None), dc, slice(half * 512, (half + 1) * 512))),
                         start=(dc == 0), stop=(dc == 7))
            p.tt("dve", h1[:, half * 512:(half + 1) * 512], bm[:], xt[b][:, half * 512:(half + 1) * 512], ALU.add)
        p.dma("pool", H1[rows, :], h1[:])
        ffn_prep_tile(p, h1[:], gf, wr, cf, fb, HN1, AFF, rows, bank)
    return p.finish()


NE = 16
CAP = 2 * SEQ // NE
FF = 2048
BISECT_ITERS = 36


def build_L3():
    nc = bass.Bass("TRN2", target_bir_lowering=False)
    p = Prog(nc)
    AFFE = p.dram("AFFE", [2, 128, 128], F32, "ExternalInput")
    HN = p.dram("HN", [SEQ, D], BF16, "ExternalInput")
    wg = p.dram("wg", [2, D, FF], F32, "ExternalInput")
    wu = p.dram("wu", [2, D, FF], F32, "ExternalInput")
    wd = p.dram("wd", [2, FF, D], F32, "ExternalInput")
    cf_d = p.dram("cf", [128, 6, 128], F32, "ExternalInput")
    cb_d = p.dram("cb", [128, 128], BF16, "ExternalInput")
    tok_d = p.dram("tokid", [128, 128], I32, "ExternalInput")
    DELTA = p.dram("DELTA", [SEQ, D], F32, "ExternalOutput")

    Wg = p.sb("Wg", [128, 8, FF], BF16)
    Wu = p.sb("Wu", [128, 8, FF], BF16)
    Wd = p.sb("Wd", [128, 16, D], BF16)
    cf = p.sb("cfs", [128, 6, 128], F32)
    cb = p.sb("cbs", [128, 128], BF16)
    tokid = p.sb("tokid_s", [128, 128], I32)
    zt = p.sb("zt", [128, 4096], F32)
    ones = p.sb("ones", [128, 128], F32)
    aff = p.sb("aff", [128, 2, 128], F32)
    cmp_ = p.sb("cmp", [128, 2, 128], F32)
    st = {n: p.sb("b_" + n, [128, 2], F32) for n in ("lo", "hi", "mid", "cnt", "ge", "nge", "t1", "t2")}
    selT = p.sb("selT", [128, 128], F32)
    rp = p.sb("rp", [128, 1], F32)
    slot = p.sb("slot", [128, 128], F32)
    idx_sb = p.sb("idx_sb", [128, 2, 16, 2], I32)
    banks = [p.ps("bk%d" % i, [128, 512], F32) for i in range(7)]
    pT = p.ps("pT", [128, 1024], BF16)
    bi = [0]

    def bank():
        b = banks[bi[0] % 7]
        bi[0] += 1
        return b

    p.dma("sp", cf[:], cf_d[:])
    p.dma("sp", cb[:], cb_d[:])
    p.dma("sp", tokid[:], tok_d[:])
    p.dma("sp", aff[:], AFFE.v(AFFE.t.rearrange("e p f -> p e f")))
    p.memset("pool", zt[:], 0.0)
    p.memset("dve", ones[:], 1.0)
    zkeys = []
    dz = DELTA.t.rearrange("(k p r) d -> k p (r d)", p=128, r=4)
    for k in range(SEQ // 512):
        zk = ("DELTA", "z", k)
        zkeys.append(zk)
        p.dma("sp", V(dz[k], zk), zt[:])
    US = cf[:, 2, :]
    ident_f = cf[:, 4, :]

    def load_weights(e):
        for dc in range(8):
            p.dma("pool", Wg.sub(dc, (slice(None), dc, slice(None))), V(wg.t[e, dc * 128:(dc + 1) * 128, :], ("wg", e)))
            p.dma("pool", Wu.sub(dc, (slice(None), dc, slice(None))), V(wu.t[e, dc * 128:(dc + 1) * 128, :], ("wu", e)))
        for fc in range(16):
            p.dma("pool", Wd.sub(fc, (slice(None), fc, slice(None))), V(wd.t[e, fc * 128:(fc + 1) * 128, :], ("wd", e)))

    load_weights(0)
    lo, hi, mid, cnt, ge, nge, t1, t2 = (st[n] for n in ("lo", "hi", "mid", "cnt", "ge", "nge", "t1", "t2"))
    p.memset("dve", lo[:], 0.0)
    p.memset("dve", hi[:], 1.0)
    for it in range(BISECT_ITERS):
        p.tt("dve", mid[:], lo[:], hi[:], ALU.add)
        p.ts("dve", mid[:], mid[:], 0.5, None, ALU.mult)
        for e in range(2):
            p.ts("dve", cmp_[:, e, :], aff[:, e, :], mid[:, e:e + 1], None, ALU.is_ge)
        p.reduce("dve", cnt[:], cmp_[:], ALU.add)
        bt = bank()
        p.matmul(bt[:, 0:2], ones[:], cnt[:])
        p.ts("dve", ge[:], bt[:, 0:2], float(CAP) - 0.5, None, ALU.is_ge)
        p.ts("dve", nge[:], ge[:], -1.0, 1.0, ALU.mult, ALU.add)
        p.tt("dve", lo[:], lo[:], nge[:], ALU.mult)
        p.tt("dve", t1[:], mid[:], ge[:], ALU.mult)
        p.tt("dve", lo[:], lo[:], t1[:], ALU.add)
        p.tt("dve", hi[:], hi[:], ge[:], ALU.mult)
        p.tt("dve", t2[:], mid[:], nge[:], ALU.mult)
        p.tt("dve", hi[:], hi[:], t2[:], ALU.add)
    for e in range(2):
        p.ts("dve", cmp_[:, e, :], aff[:, e, :], lo[:, e:e + 1], None, ALU.is_ge)
    p.reduce("dve", cnt[:], cmp_[:], ALU.add)
    wts = idx_sb.t[:].bitcast(F32)
    p.push_scope()
    slot_i = p.sb("slot_i", [128, 128], I32)
    smod_i = p.sb("smod_i", [128, 128], I32)
    sdiv_i = p.sb("sdiv_i", [128, 128], I32)
    smod_f = p.sb("smod_f", [128, 128], F32)
    sdiv_f = p.sb("sdiv_f", [128, 128], F32)
    tokf = p.sb("tokf", [128, 128], F32)
    base = p.sb("base", [128, 128, 16], F32)
    Bcat = p.sb("Bcat", [128, 128, 32], F32)
    OHc = [p.sb("OHc%d" % i, [128, 16, 128], F32) for i in range(2)]
    p.copy("dve", tokf[:], tokid[:])
    iota_r = cf.t[:, 5, :]
    for e in range(2):
        bt = bank()
        p.transpose(bt[:, 0:128], cmp_[:, e, :], ident_f)
        p.copy("act", selT[:], bt[:, 0:128])
        bp = bank()
        p.matmul(bp[:, 0:128], selT[:], US)
        p.matmul(bp[:, 128:129], US, cnt[:, e:e + 1])
        p.copy("act", rp[:], bp[:, 128:129])
        p.ts("dve", slot[:], bp[:, 0:128], rp[:, 0:1], None, ALU.add)
        p.copy("dve", slot_i[:], slot[:])
        p.ts("dve", smod_i[:], slot_i[:], 127, None, ALU.bitwise_and)
        p.ts("dve", sdiv_i[:], slot_i[:], 7, None, ALU.arith_shift_right)
        p.copy("dve", smod_f[:], smod_i[:])
        p.copy("dve", sdiv_f[:], sdiv_i[:])
        p.tt("dve", base[:], cf.v(iota_r[:, 0:16].unsqueeze(1).broadcast_to([128, 128, 16])),
             sdiv_f.v(sdiv_f.t[:].unsqueeze(2).broadcast_to([128, 128, 16])), ALU.is_equal)
        p.tt("dve", base[:], base[:], cmp_.v(cmp_.t[:, e, :].unsqueeze(2).broadcast_to([128, 128, 16])), ALU.mult)
        p.tt("dve", Bcat[:, :, 0:16], base[:], tokf.v(tokf.t[:].unsqueeze(2).broadcast_to([128, 128, 16])), ALU.mult)
        p.tt("dve", Bcat[:, :, 16:32], base[:], aff.v(aff.t[:, e, :].unsqueeze(2).broadcast_to([128, 128, 16])), ALU.mult)
        bacc = bank()
        for c in range(8):
            OH = OHc[c % 2]
            p.tt("dve", OH[:], cf.v(iota_r.unsqueeze(1).broadcast_to([128, 16, 128])),
                 smod_f.v(smod_f.t[:, c * 16:(c + 1) * 16].unsqueeze(2).broadcast_to([128, 16, 128])), ALU.is_equal)
            for fl in range(16):
                f = c * 16 + fl
                p.matmul(bacc[:, 0:32], OH[:, fl, :], Bcat[:, f, :], start=(f == 0), stop=(f == 127))
        p.copy("dve", idx_sb.v(idx_sb.t[:, e, :, 0]), bacc[:, 0:16])
        p.copy("dve", idx_sb.v(wts[:, e, :, 1]), bacc[:, 16:32])
    p.pop_scope()
    xs = [p.sb("xs%d" % i, [128, D], BF16) for i in range(2)]
    xsT = p.sb("xsT", [128, 8, 512], BF16)
    hT = p.sb("hT", [128, 16, 512], BF16)
    sg = [p.sb("sg%d" % i, [128, 512], F32) for i in range(2)]
    y = [p.sb("y%d" % i, [128, D], F32) for i in range(2)]
    first_scatter = True
    for e in range(2):
        if e > 0:
            load_weights(e)
        for tg in range(4):
            for kk in range(4):
                k = tg * 4 + kk
                X = xs[kk % 2]
                p.gather(X[:], HN[:, :], idx_sb[:, e, k, 0:1], SEQ)
                for dc in range(8):
                    p.transpose(pT[:, dc * 128:(dc + 1) * 128], X[:, dc * 128:(dc + 1) * 128], cb[:])
                p.copy("act", xsT[:, :, kk * 128:(kk + 1) * 128], pT.v(pT.t[:].rearrange("p (c t) -> p c t", c=8)))
            for fc in range(16):
                bg_, bu_ = bank(), bank()
                for dc in range(8):
                    p.matmul(bg_[:], Wg.sub(dc, (slice(None), dc, slice(fc * 128, (fc + 1) * 128))), xsT[:, dc, :],
                             start=(dc == 0), stop=(dc == 7))
                for dc in range(8):
                    p.matmul(bu_[:], Wu.sub(dc, (slice(None), dc, slice(fc * 128, (fc + 1) * 128))), xsT[:, dc, :],
                             start=(dc == 0), stop=(dc == 7))
                G = sg[fc % 2]
                p.act(G[:], bg_[:], AF.Silu)
                p.tt("dve", hT[:, fc, :], G[:], bu_[:], ALU.mult)
            for kk in range(4):
                k = tg * 4 + kk
                Y = y[kk % 2]
                for half in range(2):
                    by = bank()
                    for fc in range(16):
                        p.matmul(by[:], hT[:, fc, kk * 128:(kk + 1) * 128],
                                 Wd.sub(fc, (slice(None), fc, slice(half * 512, (half + 1) * 512))),
                                 start=(fc == 0), stop=(fc == 15))
                    p.ts("dve", Y[:, half * 512:(half + 1) * 512], by[:], idx_sb.v(wts[:, e, k, 1:2]), None, ALU.mult)
                p.scatter(DELTA[:, :], Y[:], idx_sb[:, e, k, 0:1], SEQ, accum=True,
                          reads=zkeys if first_scatter else None)
                first_scatter = False
    return p.finish()


MH = 16
QRANK = 256
KVR = 128
NOPE = 128
ROPE = 64
MLA_SCALE = float(NOPE + ROPE) ** -0.5
TWO_PI = 2.0 * np.pi


def rope_consts():
    half = ROPE // 2
    inv = (10000.0 ** (-np.arange(half, dtype=np.float32) / half)).astype(np.float32)
    rc = np.zeros((64, 4), np.float32)
    rc[:, 0] = np.concatenate([inv, inv])
    rc[:, 1] = np.concatenate([np.ones(half), -np.ones(half)])
    rc[:, 2] = -np.pi
    return rc


def build_L4():
    nc = bass.Bass("TRN2", target_bir_lowering=False)
    p = Prog(nc)
    H1 = p.dram("H1", [TOK, D], F32, "ExternalInput")
    DS = p.dram("DS", [NCORES, TOK, D], F32, "ExternalInput")
    pos = p.dram("pos", [1, TOK], I32, "ExternalInput")
    g_mix = p.dram("g_mix", [1, D], F32, "ExternalInput")
    w_m = p.dram("w_m", [D, 448], F32, "ExternalInput")
    g_q = p.dram("g_q", [1, QRANK], F32, "ExternalInput")
    g_kv = p.dram("g_kv", [1, KVR], F32, "ExternalInput")
    w_uq = p.dram("w_uq", [QRANK, MH * 192], F32, "ExternalInput")
    w_ukv = p.dram("w_ukv", [KVR, MH * 256], F32, "ExternalInput")
    rc_d = p.dram("rc", [64, 4], F32, "ExternalInput")
    cf_d = p.dram("cf", [128, 5, 128], F32, "ExternalInput")
    cb_d = p.dram("cb", [128, 128], BF16, "ExternalInput")
    H2 = p.dram("H2", [TOK, D], F32, "ExternalOutput")
    QL = p.dram("QL", [MH, 128, TOK], BF16, "ExternalOutput")
    QR = p.dram("QR", [MH, 64, TOK], BF16, "ExternalOutput")
    CKVT = p.dram("CKVT", [128, TOK], BF16, "ExternalOutput")
    KRT = p.dram("KRT", [64, TOK], BF16, "ExternalOutput")
    CKV = p.dram("CKV", [TOK, 128], BF16, "ExternalOutput")
    KN2 = p.dram("KN2", [TOK, 1], F32, "ExternalOutput")
    QMAX = p.dram("QMAX", [1, MH], F32, "ExternalOutput")

    cf = p.sb("cfs", [128, 5, 128], F32)
    cb = p.sb("cbs", [128, 128], BF16)
    rc = p.sb("rcs", [64, 4], F32)
    gm = p.sb("gm", [128, D], F32)
    gq = p.sb("gq", [128, QRANK], F32)
    gkv = p.sb("gkv", [128, KVR], F32)
    Wm = p.sb("Wm", [128, 8, 448], BF16)
    Wuq = p.sb("Wuq", [128, 2, MH * 192], BF16)
    Wukv = p.sb("Wukv", [128, MH * 256], BF16)
    WukT = p.sb("WukT", [128, MH, 128], BF16)
    posi = p.sb("posi", [64, TOK], I32)
    ang = p.sb("ang", [64, TOK], F32)
    C2 = p.sb("C2", [64, TOK], F32)
    S2 = p.sb("S2", [64, TOK], F32)
    hnT = p.sb("hnT", [128, 8, TOK], BF16)
    cqT = p.sb("cqT", [128, 2, TOK], BF16)
    ht = [p.sb("ht%d" % i, [128, D], F32) for i in range(2)]
    dt_ = [p.sb("dt%d" % i, [128, D], F32) for i in range(3)]
    sq = p.sb("sq", [128, D], F32)
    ss = p.sb("ss", [128, 1], F32)
    rstd = p.sb("rstd", [128, 1], F32)
    hn = p.sb("hn", [128, D], BF16)
    cs = p.sb("cs", [128, 448], F32)
    cn = p.sb("cn", [128, 384], BF16)
    kn = p.sb("kn", [128, 2], F32)
    qn = p.sb("qn", [128, 512], BF16)
    qlf = p.sb("qlf", [128, 512], F32)
    qlb = p.sb("qlb", [128, 512], BF16)
    qsq = p.sb("qsq", [128, 512], F32)
    r1 = p.sb("r1", [64, 512], F32)
    r2 = p.sb("r2", [64, 512], F32)
    rb = p.sb("rb", [64, 512], BF16)
    qmx = p.sb("qmx", [1, MH], F32)
    qm1 = p.sb("qm1", [1, 1], F32)
    ckT = p.sb("ckT", [128, 128], BF16)
    banks = [p.ps("bk%d" % i, [128, 512], F32) for i in range(7)]
    pT = p.ps("pT", [128, 1024], BF16)
    bi = [0]

    def bank():
        b = banks[bi[0] % 7]
        bi[0] += 1
        return b

    p.dma("sp", cf[:], cf_d[:])
    p.dma("sp", cb[:], cb_d[:])
    p.dma("sp", rc[:], rc_d[:])
    p.dma("sp", gm[:], g_mix.v(g_mix.t[0:1, :].broadcast_to([128, D])))
    p.dma("sp", gq[:], g_q.v(g_q.t[0:1, :].broadcast_to([128, QRANK])))
    p.dma("sp", gkv[:], g_kv.v(g_kv.t[0:1, :].broadcast_to([128, KVR])))
    p.dma("sp", posi[:], pos.v(pos.t[0:1, :].broadcast_to([64, TOK])))
    p.dma("pool", Wm[:], w_m.v(w_m.t.rearrange("(c p) n -> p c n", p=128)))
    p.dma("pool", Wuq[:], w_uq.v(w_uq.t.rearrange("(c p) n -> p c n", p=128)))
    p.dma("pool", Wukv[:], w_ukv[:, :])
    onesf = p.sb("onesf", [128, 1], F32)
    p.memset("dve", onesf[:], 1.0)
    p.memset("dve", qmx[:], 0.0)
    Wm_sw = p.sb("Wm_sw", [128, 8, 64], BF16)
    Wuq_sw = p.sb("Wuq_sw", [128, 2, MH, 64], BF16)
    p.copy("dve", Wm_sw[:, :, 0:32], Wm[:, :, 416:448])
    p.copy("dve", Wm_sw[:, :, 32:64], Wm[:, :, 384:416])
    for kc in range(2):
        wv = Wuq.t[:, kc, :].rearrange("p (h c) -> p h c", h=MH)
        p.copy("dve", Wuq_sw[:, kc, :, 0:32], Wuq.v(wv[:, :, 160:192]))
        p.copy("dve", Wuq_sw[:, kc, :, 32:64], Wuq.v(wv[:, :, 128:160]))
    for h in range(MH):
        p.transpose(pT[:, (h % 8) * 128:(h % 8 + 1) * 128], Wukv[:, h * 256:h * 256 + 128], cb[:])
        if h % 8 == 7:
            g0 = h - 7
            p.copy("act", WukT[:, g0:g0 + 8, :], pT.v(pT.t[:].rearrange("p (h r) -> p h r", h=8)))
    p.copy("dve", ang[:], posi[:])
    p.ts("dve", ang[:], ang[:], rc[:, 0:1], None, ALU.mult)
    C1 = 6.28125
    C2_ = float(TWO_PI - 6.28125)
    ki = p.sb("ki", [64, TOK], I32)
    kf = p.sb("kf", [64, TOK], F32)
    gt = p.sb("gt", [64, TOK], F32)
    p.ts("dve", kf[:], ang[:], float(1.0 / TWO_PI), None, ALU.mult)
    p.copy("dve", ki[:], kf[:])
    p.copy("dve", kf[:], ki[:])
    p.stt("dve", ang[:], kf[:], -C1, ang[:], ALU.mult, ALU.add)
    p.stt("dve", ang[:], kf[:], -C2_, ang[:], ALU.mult, ALU.add)

    def fold(t):
        p.ts("dve", gt[:], t[:], float(np.pi), None, ALU.is_gt)
        p.stt("dve", t[:], gt[:], -TWO_PI, t[:], ALU.mult, ALU.add)
        p.ts("dve", gt[:], t[:], float(-np.pi), None, ALU.is_lt)
        p.stt("dve", t[:], gt[:], TWO_PI, t[:], ALU.mult, ALU.add)
        p.ts("dve", t[:], t[:], float(np.pi), float(-np.pi), ALU.min, ALU.max)

    fold(ang)
    p.ts("dve", C2[:], ang[:], float(np.pi / 2), None, ALU.add)
    fold(C2)
    p.act(S2[:], ang[:], AF.Sin)
    p.ts("dve", S2[:], S2[:], rc[:, 1:2], -1.0, ALU.mult, ALU.mult)
    p.act(C2[:], C2[:], AF.Sin)

    for j in range(NT):
        rows = slice(j * 128, (j + 1) * 128)
        Ht = ht[j % 2]
        p.dma("sp", Ht[:], H1[rows, :])
        for c in range(NCORES):
            Dt = dt_[c % 3]
            p.dma("sp", Dt[:], DS.v(DS.t[c, rows, :]))
            p.tt("dve", Ht[:], Ht[:], Dt[:], ALU.add)
        p.dma("pool", H2[rows, :], Ht[:])
        rmsnorm_tile(p, Ht[:], gm[:], hn[:], sq[:], ss[:], rstd[:])
        for dc in range(8):
            p.transpose(pT[:, dc * 128:(dc + 1) * 128], hn[:, dc * 128:(dc + 1) * 128], cb[:])
        p.copy("act", hnT[:, :, rows], pT.v(pT.t[:].rearrange("p (c t) -> p c t", c=8)))
        bc_ = bank()
        for dc in range(8):
            p.matmul(bc_[:, 0:448], hnT[:, dc, rows], Wm[:, dc, :], start=(dc == 0), stop=(dc == 7))
        p.copy("act", cs[:], bc_[:, 0:448])
        rmsnorm_tile(p, cs[:, 0:256], gq[:], cn[:, 0:256], sq[:, 0:256], ss[:], rstd[:], width=QRANK)
        rmsnorm_tile(p, cs[:, 256:384], gkv[:], cn[:, 256:384], sq[:, 0:128], ss[:], rstd[:], width=KVR)
        p.dma("pool", CKV[rows, :], cn[:, 256:384])
        p.tt("dve", sq[:, 0:128], cn[:, 256:384], cn[:, 256:384], ALU.mult)
        p.reduce("dve", kn[:, 0:1], sq[:, 0:128], ALU.add)
        p.tt("dve", sq[:, 0:64], cs[:, 384:448], cs[:, 384:448], ALU.mult)
        p.reduce("dve", kn[:, 1:2], sq[:, 0:64], ALU.add)
        p.tt("dve", kn[:, 0:1], kn[:, 0:1], kn[:, 1:2], ALU.add)
        p.dma("pool", KN2[rows, :], kn[:, 0:1])
        for q in range(3):
            p.transpose(pT[:, q * 128:(q + 1) * 128], cn[:, q * 128:(q + 1) * 128], cb[:])
        p.copy("act", cqT[:, :, rows], pT.v(pT.t[:, 0:256].rearrange("p (c t) -> p c t", c=2)))
        p.copy("act", ckT[:], pT[:, 256:384])
        p.dma("pool", CKVT[:, rows], ckT[:])

    for tg in range(TOK // 512):
        cols = slice(tg * 512, (tg + 1) * 512)
        bk_, bks = bank(), bank()
        for dc in range(8):
            p.matmul(bk_[0:64, :], Wm[:, dc, 384:448], hnT[:, dc, cols], start=(dc == 0), stop=(dc == 7))
        for dc in range(8):
            p.matmul(bks[0:64, :], Wm_sw[:, dc, :], hnT[:, dc, cols], start=(dc == 0), stop=(dc == 7))
        p.tt("dve", r1[:], bk_[0:64, :], C2[:, cols], ALU.mult)
        p.tt("dve", r2[:], bks[0:64, :], S2[:, cols], ALU.mult)
        p.tt("dve", rb[:], r1[:], r2[:], ALU.add)
        p.dma("pool", KRT[:, cols], rb[:])
        for h in range(MH):
            o = h * 192
            bq = bank()
            for kc in range(2):
                p.matmul(bq[:], Wuq[:, kc, o:o + 128], cqT[:, kc, cols], start=(kc == 0), stop=(kc == 1))
            p.copy("act", qn[:], bq[:])
            bl = bank()
            p.matmul(bl[:], WukT[:, h, :], qn[:])
            p.ts("dve", qlf[:], bl[:], MLA_SCALE, None, ALU.mult)
            p.copy("act", qlb[:], qlf[:])
            p.dma("pool", QL.v(QL.t[h, :, cols]), qlb[:])
            br, brs = bank(), bank()
            for kc in range(2):
                p.matmul(br[0:64, :], Wuq[:, kc, o + 128:o + 192], cqT[:, kc, cols], start=(kc == 0), stop=(kc == 1))
            for kc in range(2):
                p.matmul(brs[0:64, :], Wuq_sw[:, kc, h, :], cqT[:, kc, cols], start=(kc == 0), stop=(kc == 1))
            p.tt("dve", r1[:], br[0:64, :], C2[:, cols], ALU.mult)
            p.tt("dve", r2[:], brs[0:64, :], S2[:, cols], ALU.mult)
            p.tt("dve", r1[:], r1[:], r2[:], ALU.add)
            p.ts("dve", r1[:], r1[:], MLA_SCALE, None, ALU.mult)
            p.copy("act", rb[:], r1[:])
            p.dma("pool", QR.v(QR.t[h, :, cols]), rb[:])
            p.tt("dve", qsq[:], qlf[:], qlf[:], ALU.mult)
            p.tt("dve", r2[:], r1[:], r1[:], ALU.mult)
            bn = bank()
            p.matmul(bn[0:1, :], onesf[:, 0:1], qsq[:], start=True, stop=False)
            p.matmul(bn[0:1, :], onesf[0:64, 0:1], r2[:], start=False, stop=True)
            p.reduce("dve", qm1[:], bn[0:1, :], ALU.max)
            p.tt("dve", qmx[:, h:h + 1], qmx[:, h:h + 1], qm1[:], ALU.max)
    p.dma("pool", QMAX[:, :], qmx[:])
    return p.finish()


NKT = SEQ // 128


def build_L5(nheads=None, same=True):
    nc = bass.Bass("TRN2", target_bir_lowering=False)
    p = Prog(nc)
    QL = p.dram("QL", [MH, 128, TOK], BF16, "ExternalInput")
    QR = p.dram("QR", [MH, 64, TOK], BF16, "ExternalInput")
    p.same = same
    KTd = p.dram("KT", [128, SEQ], BF16, "ExternalInput")
    KRd = p.dram("KR", [64, SEQ], BF16, "ExternalInput")
    Vd = p.dram("VK", [SEQ, 128], BF16, "ExternalInput")
    KN2 = p.dram("KN2", [128, 128], F32, "ExternalInput")
    QMAX = p.dram("QMAX", [1, MH], F32, "ExternalInput")
    H2 = p.dram("H2", [TOK, D], F32, "ExternalInput")
    w_ukv = p.dram("w_ukv", [KVR, MH * 256], F32, "ExternalInput")
    w_o = p.dram("w_o", [MH * 128, D], F32, "ExternalInput")
    g_ffn = p.dram("g_ffn", [1, D], F32, "ExternalInput")
    w_r = p.dram("w_r", [D, 16], F32, "ExternalInput")
    cf_d = p.dram("cf", [128, 5, 128], F32, "ExternalInput")
    cb_d = p.dram("cb", [128, 128], BF16, "ExternalInput")
    H3 = p.dram("H3", [TOK, D], F32, "ExternalOutput")
    HN3 = p.dram("HN3", [TOK, D], BF16, "ExternalOutput")
    AFF = p.dram("AFF", [TOK, 16], F32, "ExternalOutput")
    OTD = p.dram("OTD", [MH, 128, TOK], BF16, "Internal")

    KT = p.sb("KTs", [128, SEQ], BF16)
    KR2 = p.sb("KR2", [128, SEQ], BF16)
    Vt = p.sb("Vt", [128, NKT, 128], BF16)
    Wo = p.sb("Wo", [128, MH, D], BF16)
    Wuv = p.sb("Wuv", [128, MH, 128], BF16)
    wr = p.sb("wr", [128, 8, 16], F32)
    cf = p.sb("cfs", [128, 5, 128], F32)
    cb = p.sb("cbs", [128, 128], BF16)
    gf = p.sb("gf", [128, D], F32)
    onesf = p.sb("onesf", [128, 128], F32)
    kn = p.sb("kn", [128, 128], F32)
    km = p.sb("km", [128, 1], F32)
    km1 = p.sb("km1", [1, 1], F32)
    qmx = p.sb("qmx", [1, MH], F32)
    negm = p.sb("negm", [128, MH], F32)
    QW = 1024
    sbk = [p.ps("sbk%d" % i, [128, QW], F32) for i in range(2)]
    obk = p.ps("obk", [128, QW], F32)
    ebk = [p.ps("ebk%d" % i, [128, 512], F32) for i in range(2)]
    mi = [0]

    def bank():
        b = ebk[mi[0] % 2]
        mi[0] += 1
        return b

    p.dma("sp", cf[:], cf_d[:])
    p.dma("sp", cb[:], cb_d[:])
    p.dma("sp", kn[:], KN2[:, :])
    p.dma("sp", qmx[:], QMAX[:, :])
    p.dma("sp", gf[:], g_ffn.v(g_ffn.t[0:1, :].broadcast_to([128, D])))
    p.dma("sp", wr[:], w_r.v(w_r.t.rearrange("(c p) e -> p c e", p=128)))
    for q in range(8):
        cs_ = slice(q * 2048, (q + 1) * 2048)
        p.dma("sp", KT.sub(q, (slice(None), cs_)), V(KTd.t[:, cs_], ("KTd", q)))
    for half in range(2):
        for q in range(8):
            cs_ = slice(q * 2048, (q + 1) * 2048)
            p.dma("sp", KR2.sub((half, q), (slice(half * 64, (half + 1) * 64), cs_)), V(KRd.t[:, cs_], ("KRd", half, q)))
    for q in range(8):
        p.dma("sp", Vt.sub(q, (slice(None), slice(q * 16, (q + 1) * 16), slice(None))),
              V(Vd.t[q * 2048:(q + 1) * 2048, :].rearrange("(k p) r -> p k r", p=128), ("Vd", q)))
    p.dma("pool", Wuv[:], w_ukv.v(w_ukv.t.rearrange("r (h c) -> r h c", h=MH)[:, :, 128:256]))
    for h in range(MH):
        p.dma("pool", Wo.sub(h, (slice(None), h, slice(None))), V(w_o.t[h * 128:(h + 1) * 128, :], ("w_o", h)))
    p.memset("dve", onesf[:], 1.0)
    p.reduce("dve", km[:], kn[:], ALU.max)
    bt = bank()
    p.transpose(bt[0:1, 0:128], km[:, 0:1], cf[:, 4, :])
    p.reduce("dve", km1[:], bt[0:1, 0:128], ALU.max)
    p.ts("dve", qmx[:], qmx[:], km1[0:1, 0:1], None, ALU.mult)
    p.act(qmx[:], qmx[:], AF.Sqrt)
    p.ts("dve", qmx[:], qmx[:], -1.0, None, ALU.mult)
    bb = bank()
    p.matmul(bb[:, 0:MH], onesf[0:1, :], qmx[:])
    p.copy("act", negm[:], bb[:, 0:MH])

    p.push_scope()
    ql = [p.sb("ql%d" % i, [128, TOK], BF16) for i in range(2)]
    qr = [p.sb("qr%d" % i, [128, TOK], BF16) for i in range(2)]
    PT = [p.sb("PT%d" % i, [128, QW], BF16) for i in range(3)]
    accD = [p.sb("accD%d" % i, [128, QW], F32) for i in range(1)]
    accP = [p.sb("accP%d" % i, [128, QW], F32) for i in range(1)]
    rl = p.sb("rl", [128, QW], F32)
    OTn = p.sb("OTn", [128, QW], BF16)
    oTs = [p.sb("oTs%d" % i, [128, QW], BF16) for i in range(2)]
    NH = MH if nheads is None else nheads
    it = 0
    for h in range(NH):
        Ql, Qr = ql[h % 2], qr[h % 2]
        p.dma("sp", Ql[:], QL.v(QL.t[h]))
        p.dma("sp", Qr[0:64, :], QR.v(QR.t[h]))
        p.dma("sp", Qr[64:128, :], QR.v(QR.t[h]))
        for qg in range(TOK // QW):
            q0 = qg * QW
            bo = obk
            AD, AP_ = accD[0], accP[0]
            it += 1
            first = {"dve": True, "pool": True}

            def s_mm(kt):
                bs = sbk[kt % 2]
                ktile = KT.sub(kt // 16, (slice(None), slice(kt * 128, (kt + 1) * 128)))
                rtiles = [KR2.sub((hf, kt // 16), (slice(hf * 64, (hf + 1) * 64), slice(kt * 128, (kt + 1) * 128)))
                          for hf in range(2)]
                for hh in range(QW // 512):
                    p.matmul(bs[:, hh * 512:(hh + 1) * 512], ktile, Ql[:, q0 + hh * 512:q0 + (hh + 1) * 512],
                             start=True, stop=False)
                if kt >= 1:
                    p.wait("pe", [PT[(kt - 1) % 3].name])
                for hh in range(QW // 512):
                    p.matmul(bs[:, hh * 512:(hh + 1) * 512], rtiles[hh],
                             Qr[hh * 64:(hh + 1) * 64, q0 + hh * 512:q0 + (hh + 1) * 512], start=False, stop=True)

            s_mm(0)
            for kt in range(NKT):
                if kt + 1 < NKT:
                    s_mm(kt + 1)
                P_ = PT[kt % 3]
                p.act(P_[:], sbk[kt % 2][:], AF.Exp, bias=negm[:, h:h + 1])
                vt = Vt.sub(kt // 16, (slice(None), kt, slice(None)))
                for hh in range(QW // 512):
                    p.matmul(bo[:, hh * 512:(hh + 1) * 512], vt, P_[:, hh * 512:(hh + 1) * 512],
                             start=(kt == 0), stop=(kt == NKT - 1))
                eng = "pool" if kt % 3 == 2 else "dve"
                A = AP_ if eng == "pool" else AD
                if first[eng]:
                    p.copy(eng, A[:], P_[:])
                    first[eng] = False
                else:
                    p.tt(eng, A[:], A[:], P_[:], ALU.add)
            p.tt("dve", AD[:], AD[:], AP_[:], ALU.add)
            for hh in range(QW // 512):
                hs = slice(hh * 512, (hh + 1) * 512)
                bl = ebk[hh]
                p.matmul(bl[:], onesf[:], AD[:, hs])
                p.recip(rl[:, hs], bl[:])
                p.tt("dve", OTn[:, hs], bo[:, hs], rl[:, hs], ALU.mult)
            oT = oTs[it % 2]
            for hh in range(QW // 512):
                hs = slice(hh * 512, (hh + 1) * 512)
                bv = ebk[hh]
                p.matmul(bv[:], Wuv[:, h, :], OTn[:, hs])
                p.copy("act", oT[:, hs], bv[:])
            p.dma("pool", V(OTD.t[h, :, q0:q0 + QW], ("OTD", h, qg)), oT[:])
    otd_keys = [("OTD", h, qg) for h in range(NH) for qg in range(TOK // QW)]
    p.pop_scope()
    oTt = [p.sb("oTt%d" % i, [128, MH, 128], BF16) for i in range(2)]
    h2t = [p.sb("h2t%d" % i, [128, D], F32) for i in range(2)]
    h3 = p.sb("h3", [128, D], F32)
    fb = ffn_prep_bufs(p)

    for j in range(NT):
        rows = slice(j * 128, (j + 1) * 128)
        b = j % 2
        p.dma("sp", h2t[b][:], H2[rows, :])
        p.dma("sp", oTt[b][:], V(OTD.t[:, :, rows].rearrange("h d t -> d h t"), ("OTD", "rd", j)),
              reads=[k_ for k_ in otd_keys if k_[2] == j // (QW // 128)])
        for half in range(2):
            bm = bank()
            for h in range(MH):
                p.matmul(bm[:], oTt[b][:, h, :], Wo.sub(h, (slice(None), h, slice(half * 512, (half + 1) * 512))),
                         start=(h == 0), stop=(h == MH - 1))
            p.tt("dve", h3[:, half * 512:(half + 1) * 512], bm[:], h2t[b][:, half * 512:(half + 1) * 512], ALU.add)
        p.dma("pool", H3[rows, :], h3[:])
        ffn_prep_tile(p, h3[:], gf, wr, cf, fb, HN3, AFF, rows, bank)
    return p.finish()


def build_L7():
    nc = bass.Bass("TRN2", target_bir_lowering=False)
    p = Prog(nc)
    H3 = p.dram("H3", [TOK, D], F32, "ExternalInput")
    DS = p.dram("DS", [NCORES, TOK, D], F32, "ExternalInput")
    g_fin = p.dram("g_fin", [1, D], F32, "ExternalInput")
    OUT = p.dram("OUT", [TOK, D], F32, "ExternalOutput")
    gm = p.sb("gm", [128, D], F32)
    ht = [p.sb("ht%d" % i, [128, D], F32) for i in range(2)]
    dt_ = [p.sb("dt%d" % i, [128, D], F32) for i in range(3)]
    sq = p.sb("sq", [128, D], F32)
    ss = p.sb("ss", [128, 1], F32)
    rstd = p.sb("rstd", [128, 1], F32)
    ot = [p.sb("ot%d" % i, [128, D], F32) for i in range(2)]
    p.dma("sp", gm[:], g_fin.v(g_fin.t[0:1, :].broadcast_to([128, D])))
    for j in range(NT):
        rows = slice(j * 128, (j + 1) * 128)
        Ht = ht[j % 2]
        p.dma("sp", Ht[:], H3[rows, :])
        for c in range(NCORES):
            Dt = dt_[c % 3]
            p.dma("sp", Dt[:], DS.v(DS.t[c, rows, :]))
            p.tt("dve", Ht[:], Ht[:], Dt[:], ALU.add)
        rmsnorm_tile(p, Ht[:], gm[:], ot[j % 2][:], sq[:], ss[:], rstd[:])
        p.dma("pool", OUT[rows, :], ot[j % 2][:])
    return p.finish()


def run(nc, in_maps):
    res = run_bass_kernel_spmd(nc, in_maps, core_ids=list(range(NCORES)))
    return res.results


_CACHE = {}


def _prog(name, builder):
    if name not in _CACHE:
        _CACHE[name] = builder()
    return _CACHE[name]


def _c(a):
    return np.ascontiguousarray(a)


def stage_L1(inp):
    cf, cb = const_tables()
    x = inp["x"][0]
    wg = _c(np.stack([inp["gla_w_gate_up_f"][0], inp["gla_w_gate_up_b"][0]]))
    bg = _c(np.stack([inp["gla_b_gate_f"][0], inp["gla_b_gate_b"][0]]))
    maps = [dict(x=_c(x[c * TOK:(c + 1) * TOK]), g_mix=_c(inp["mix_norm"][0:1]), w_in=_c(inp["gla_w_in"][0]),
                 wg=wg, bg=bg, cf=cf, cb=cb) for c in range(NCORES)]
    return run(_prog("L1", build_L1), maps)


def stage_L2(inp, r1):
    cf, cb = const_tables()
    x = inp["x"][0]
    maps = []
    for c in range(NCORES):
        BP = np.zeros((2, 7, GH, 128, GDV), np.float32)
        APD = np.zeros((2, 7, 128, NT * GH), np.float32)
        for i in range(7):
            cf_ = c - 7 + i
            if cf_ >= 0:
                BP[0, i] = r1[cf_]["BE"][0]
                APD[0, i] = r1[cf_]["AA"][0]
            cb_ = c + 7 - i
            if cb_ <= NCORES - 1:
                BP[1, i] = r1[cb_]["BE"][1]
                APD[1, i] = r1[cb_]["AA"][1]
        maps.append(dict(x=_c(x[c * TOK:(c + 1) * TOK]), OL=r1[c]["OL"], QP=r1[c]["QP"], SR=r1[c]["SR"],
                         AA=r1[c]["AA"], BP=BP, APD=APD, g_head=_c(inp["gla_head_norm"][0:1]),
                         w_out=_c(inp["gla_w_out"][0]), g_ffn=_c(inp["ffn_norm"][0:1]),
                         w_r=_c(inp["moe_w_router"][0]), cf=cf, cb=cb))
    return run(_prog("L2", build_L2), maps)


def stage_L3(inp, layer, aff_full, hn_full):
    cf, cb = const_tables(6)
    tokid = np.arange(SEQ, dtype=np.int32).reshape(128, 128)
    maps = []
    for c in range(NCORES):
        affe = _c(aff_full[:, 2 * c:2 * c + 2].T.reshape(2, 128, 128))
        maps.append(dict(AFFE=affe, HN=hn_full, wg=_c(inp["moe_w_gate"][layer, 2 * c:2 * c + 2]),
                         wu=_c(inp["moe_w_up"][layer, 2 * c:2 * c + 2]), wd=_c(inp["moe_w_down"][layer, 2 * c:2 * c + 2]),
                         cf=cf, cb=cb, tokid=tokid))
    return run(_prog("L3", build_L3), maps)


def stage_L4(inp, h_prev, deltas):
    cf, cb = const_tables()
    rc = rope_consts()
    maps = []
    for c in range(NCORES):
        DS = _c(np.stack([d[c * TOK:(c + 1) * TOK] for d in deltas]))
        maps.append(dict(H1=h_prev[c], DS=DS, pos=_c(inp["positions"][0:1, c * TOK:(c + 1) * TOK]),
                         g_mix=_c(inp["mix_norm"][1:2]), w_m=_c(inp["mla_w_in"][0]), g_q=_c(inp["mla_q_norm"][0:1]),
                         g_kv=_c(inp["mla_kv_norm"][0:1]), w_uq=_c(inp["mla_w_uq"][0]), w_ukv=_c(inp["mla_w_ukv"][0]),
                         rc=rc, cf=cf, cb=cb))
    return run(_prog("L4", build_L4), maps)


def stage_L5(inp, r4):
    cf, cb = const_tables()
    KT = _c(np.concatenate([r4[c]["CKVT"] for c in range(NCORES)], axis=1))
    KR = _c(np.concatenate([r4[c]["KRT"] for c in range(NCORES)], axis=1))
    VK = _c(np.concatenate([r4[c]["CKV"] for c in range(NCORES)], axis=0))
    KN2 = _c(np.concatenate([r4[c]["KN2"] for c in range(NCORES)], axis=0).reshape(128, 128))
    maps = []
    for c in range(NCORES):
        maps.append(dict(QL=r4[c]["QL"], QR=r4[c]["QR"], KT=KT, KR=KR, VK=VK, KN2=KN2, QMAX=r4[c]["QMAX"],
                         H2=r4[c]["H2"], w_ukv=_c(inp["mla_w_ukv"][0]), w_o=_c(inp["mla_w_out"][0]),
                         g_ffn=_c(inp["ffn_norm"][1:2]), w_r=_c(inp["moe_w_router"][1]), cf=cf, cb=cb))
    return run(_prog("L5", build_L5), maps)


def stage_L7(inp, h_prev, deltas):
    maps = []
    for c in range(NCORES):
        DS = _c(np.stack([d[c * TOK:(c + 1) * TOK] for d in deltas]))
        maps.append(dict(H3=h_prev[c], DS=DS, g_fin=_c(inp["final_norm"].reshape(1, D))))
    return run(_prog("L7", build_L7), maps)


def kernel(**inputs):
    inp = {k: np.asarray(v) for k, v in inputs.items()}
    r1 = stage_L1(inp)
    r2 = stage_L2(inp, r1)
    aff0 = _c(np.concatenate([r2[c]["AFF"] for c in range(NCORES)], axis=0))
    hn1 = _c(np.concatenate([r2[c]["HN1"] for c in range(NCORES)], axis=0))
    r3 = stage_L3(inp, 0, aff0, hn1)
    r4 = stage_L4(inp, [r2[c]["H1"] for c in range(NCORES)], [r3[c]["DELTA"] for c in range(NCORES)])
    del r3
    r5 = stage_L5(inp, r4)
    aff1 = _c(np.concatenate([r5[c]["AFF"] for c in range(NCORES)], axis=0))
    hn3 = _c(np.concatenate([r5[c]["HN3"] for c in range(NCORES)], axis=0))
    r6 = stage_L3(inp, 1, aff1, hn3)
    r7 = stage_L7(inp, [r5[c]["H3"] for c in range(NCORES)], [r6[c]["DELTA"] for c in range(NCORES)])
    out = np.concatenate([r7[c]["OUT"] for c in range(NCORES)], axis=0).astype(np.float32)
    return out.reshape(1, SEQ, D)
```

```python
import contextlib
import numpy as np
import ml_dtypes
import concourse.bass as bass
import concourse.mybir as mybir
from concourse.bass_utils import run_bass_kernel_spmd

F32 = mybir.dt.float32
BF16 = mybir.dt.bfloat16
I32 = mybir.dt.int32
AF = mybir.ActivationFunctionType
ALU = mybir.AluOpType
AX = mybir.AxisListType

NCORES = 8
SEQ = 16384
TOK = SEQ // NCORES
NT = TOK // 128
D = 1024
RMS_EPS = 1e-6


class V:
    __slots__ = ("ap", "key")

    def __init__(self, ap, key):
        self.ap = ap
        self.key = key


class Buf:
    def __init__(self, t, name):
        self.t = t
        self.name = name

    def __getitem__(self, idx):
        return V(self.t[idx], self.name)

    def sub(self, s, idx):
        return V(self.t[idx], (self.name, s))

    def v(self, ap, s=None):
        return V(ap, self.name if s is None else (self.name, s))


class Prog:
    ENGS = ("pe", "dve", "act", "pool", "sp")
    NDMA = {"sp": 10, "pool": 8, "act": 4}

    def __init__(self, nc, same_engine_sync=True):
        self.nc = nc
        self.same = same_engine_sync
        self.stack = contextlib.ExitStack()
        self.lists = {e: [] for e in self.ENGS}
        self.cnt = {e: 0 for e in self.ENGS}
        self.seen = {e: {} for e in self.ENGS}
        self.last_w = {}
        self.readers = {}
        self.sem = {}
        self.dma_cnt = {}
        self.dma_rr = {q: 0 for q in self.NDMA}
        for e in ("pe", "dve", "act", "pool"):
            self.sem[("c", e)] = self.stack.enter_context(nc.semaphore("c_" + e))
        for q, n in self.NDMA.items():
            for k in range(n):
                sk = ("d", q, k)
                self.sem[sk] = self.stack.enter_context(nc.semaphore("d_%s%d" % (q, k)))
                self.dma_cnt[sk] = 0
        self.psum = set()
        self.bregs = {}
        self.same_dist = 2

    def sb(self, name, shape, dt):
        t = self.stack.enter_context(self.nc.sbuf_tensor(name, list(shape), dt))
        return Buf(t, name)

    def ps(self, name, shape, dt):
        t = self.stack.enter_context(self.nc.psum_tensor(name, list(shape), dt))
        self.psum.add(name)
        return Buf(t, name)

    def dram(self, name, shape, dt, kind):
        t = self.nc.dram_tensor(name, list(shape), dt, kind=kind)
        return Buf(t.ap(), name)

    def emit(self, eng, fn, reads, writes, dma=False):
        deps = {}

        def need(tok):
            if tok is None:
                return
            sk, val = tok
            if deps.get(sk, 0) < val:
                deps[sk] = val

        for r in reads:
            need(self.last_w.get(r))
            if r in self.psum:
                for sk, val in self.readers.get(r, {}).items():
                    if sk != ("c", eng):
                        need((sk, val))
        for w in writes:
            need(self.last_w.get(w))
            for sk, val in self.readers.get(w, {}).items():
                need((sk, val))
        if dma:
            k = self.dma_rr[eng]
            self.dma_rr[eng] = (k + 1) % self.NDMA[eng]
            sk = ("d", eng, k)
            if self.dma_cnt[sk] > 0:
                need((sk, self.dma_cnt[sk]))
            self.dma_cnt[sk] += 16
            tok = (sk, self.dma_cnt[sk])
        else:
            self.cnt[eng] += 1
            tok = (("c", eng), self.cnt[eng])
        waits = []
        for sk, val in deps.items():
            if sk == ("c", eng) and (eng == "pe" or not self.same):
                continue
            if sk == ("c", eng) and tok[1] - val > self.same_dist:
                continue
            if self.seen[eng].get(sk, 0) >= val:
                continue
            self.seen[eng][sk] = val
            waits.append((sk, val))
        self.lists[eng].append((waits, fn, tok))
        for r in reads:
            d = self.readers.setdefault(r, {})
            if d.get(tok[0], 0) < tok[1]:
                d[tok[0]] = tok[1]
        for w in writes:
            self.last_w[w] = tok
            self.readers[w] = {}

    @staticmethod
    def _keys(*vs):
        return [v.key for v in vs if isinstance(v, V)]

    @staticmethod
    def _ap(v):
        return v.ap if isinstance(v, V) else v

    def dma(self, q, out, in_, reads=None, writes=None, **kw):
        o, i = out.ap, in_.ap
        self.emit(q, lambda e: e.dma_start(out=o, in_=i, **kw), [in_.key] + (reads or []),
                  [out.key] if writes is None else writes, dma=True)

    def _breg(self, e, val):
        if val not in self.bregs:
            r = e.alloc_register("bchk%d" % val)
            e.reg_mov(r, val)
            self.bregs[val] = r
        return self.bregs[val]

    def gather(self, out, src, idx, nrows):
        o, s, ix = out.ap, src.ap, idx.ap
        self.emit("pool", lambda e: e.indirect_dma_start(
            out=o, out_offset=None, in_=s,
            in_offset=bass.IndirectOffsetOnAxis(ap=ix, axis=0),
            bounds_check=self._breg(e, nrows - 1), oob_is_err=False),
            [src.key, idx.key], [out.key], dma=True)

    def scatter(self, dst, src, idx, nrows, accum=False, reads=None, writes=None):
        d, s, ix = dst.ap, src.ap, idx.ap
        kw = {"compute_op": ALU.add} if accum else {}
        self.emit("pool", lambda e: e.indirect_dma_start(
            out=d, out_offset=bass.IndirectOffsetOnAxis(ap=ix, axis=0), in_=s, in_offset=None,
            bounds_check=self._breg(e, nrows - 1), oob_is_err=False, **kw),
            [src.key, idx.key] + ([dst.key] if accum else []) + (reads or []),
            [dst.key] if writes is None else writes, dma=True)

    def matmul(self, out, lhsT, rhs, start=True, stop=True):
        o, l, r = out.ap, lhsT.ap, rhs.ap
        rd = [lhsT.key, rhs.key] + ([] if start else [out.key])
        self.emit("pe", lambda e: e.matmul(o, l, r, start=start, stop=stop), rd, [out.key])

    def transpose(self, out, in_, ident):
        o, i, d = out.ap, in_.ap, ident.ap
        self.emit("pe", lambda e: e.transpose(o, i, d), [in_.key, ident.key], [out.key])

    def act(self, out, in_, func, bias=0.0, scale=1.0, accum_out=None, eng="act"):
        o, i = out.ap, in_.ap
        b, s = self._ap(bias), self._ap(scale)
        kw = {}
        if accum_out is not None:
            kw["accum_out"] = accum_out.ap
        self.emit(eng, lambda e: e.activation(o, i, func, bias=b, scale=s, **kw),
                  self._keys(in_, bias, scale), self._keys(out, accum_out))

    def copy(self, eng, out, in_):
        o, i = out.ap, in_.ap
        if eng == "act":
            self.emit(eng, lambda e: e.copy(o, i), [in_.key], [out.key])
        else:
            self.emit(eng, lambda e: e.tensor_copy(o, i), [in_.key], [out.key])

    def tt(self, eng, out, in0, in1, op):
        o, a, b = out.ap, in0.ap, in1.ap
        self.emit(eng, lambda e: e.tensor_tensor(o, a, b, op), [in0.key, in1.key], [out.key])

    def ts(self, eng, out, in0, s1, s2, op0, op1=None, accum_out=None):
        o, a = out.ap, in0.ap
        x1, x2 = self._ap(s1), self._ap(s2)
        kw = {}
        if op1 is not None:
            kw["op1"] = op1
        if accum_out is not None:
            kw["accum_out"] = accum_out.ap
        self.emit(eng, lambda e: e.tensor_scalar(o, a, x1, x2, op0, **kw),
                  self._keys(in0, s1, s2), self._keys(out, accum_out))

    def stt(self, eng, out, in0, scalar, in1, op0, op1):
        o, a, b = out.ap, in0.ap, in1.ap
        s = self._ap(scalar)
        self.emit(eng, lambda e: e.scalar_tensor_tensor(o, a, s, b, op0, op1),
                  self._keys(in0, scalar, in1), [out.key])

    def reduce(self, eng, out, in_, op, axis=AX.X):
        o, i = out.ap, in_.ap
        self.emit(eng, lambda e: e.tensor_reduce(o, i, axis, op), [in_.key], [out.key])

    def recip(self, out, in_):
        o, i = out.ap, in_.ap
        self.emit("dve", lambda e: e.reciprocal(o, i), [in_.key], [out.key])

    def memset(self, eng, out, val):
        o = out.ap
        self.emit(eng, lambda e: e.memset(o, val), [], [out.key])

    def wait(self, eng, keys):
        waits = []
        for k_ in keys:
            tok = self.last_w.get(k_)
            if tok is None:
                continue
            sk, val = tok
            if sk == ("c", eng) or self.seen[eng].get(sk, 0) >= val:
                continue
            self.seen[eng][sk] = val
            waits.append((sk, val))
        if waits:
            self.lists[eng].append((waits, None, None))

    def barrier(self):
        allv = {("c", e): self.cnt[e] for e in ("pe", "dve", "act", "pool") if self.cnt[e] > 0}
        allv.update({sk: v for sk, v in self.dma_cnt.items() if v > 0})
        for eng in self.ENGS:
            waits = []
            for sk, val in allv.items():
                if sk == ("c", eng) or self.seen[eng].get(sk, 0) >= val:
                    continue
                self.seen[eng][sk] = val
                waits.append((sk, val))
            if waits:
                self.lists[eng].append((waits, None, None))

    def push_scope(self):
        self._outer = self.stack
        self.stack = contextlib.ExitStack()

    def pop_scope(self):
        self.barrier()
        self.stack.close()
        self.stack = self._outer

    def finish(self):
        nc = self.nc
        prog = self

        def mk(name):
            def body(e):
                for waits, fn, tok in prog.lists[name]:
                    if fn is None:
                        for sk, val in waits:
                            e.wait_ge(prog.sem[sk], val)
                        continue
                    for sk, val in waits[:-1]:
                        e.wait_ge(prog.sem[sk], val)
                    ins = fn(e)
                    if waits:
                        ins._wait_ge(prog.sem[waits[-1][0]], waits[-1][1])
                    ins.then_inc(prog.sem[tok[0]], 16 if tok[0][0] == "d" else 1)
                if name == "sp":
                    for sk, val in prog.dma_cnt.items():
                        if val > 0:
                            e.wait_ge(prog.sem[sk], val)
            return body

        with nc.Block() as block:
            block.tensor(mk("pe"))
            block.vector(mk("dve"))
            block.scalar(mk("act"))
            block.gpsimd(mk("pool"))
            block.sync(mk("sp"))
        self.stack.close()
        return nc


def bcast_rows(ap1d_or_row, nparts):
    return ap1d_or_row.broadcast(0, nparts) if hasattr(ap1d_or_row, "broadcast") else ap1d_or_row


def const_tables(ncf=5):
    i = np.arange(128)
    ui = (i[:, None] <= i[None, :]).astype(np.float32)
    li = (i[:, None] >= i[None, :]).astype(np.float32)
    us = (i[:, None] < i[None, :]).astype(np.float32)
    ls = (i[:, None] > i[None, :]).astype(np.float32)
    ident = np.eye(128, dtype=np.float32)
    iota = np.broadcast_to(np.arange(128, dtype=np.float32)[None, :], (128, 128))
    cf = np.stack([ui, li, us, ls, ident, iota], axis=1)
    cb = np.eye(128, dtype=np.float32).astype(ml_dtypes.bfloat16)
    return np.ascontiguousarray(cf[:, :ncf]), cb


GH = 4
GDK = 128
GDV = 256
GIN = 3104


def rmsnorm_tile(p, xt, gbc, hn_out, sq, ss, rstd, width=D):
    p.act(sq, xt, AF.Square, accum_out=ss)
    p.ts("dve", rstd, ss, 1.0 / width, RMS_EPS, ALU.mult, ALU.add)
    p.act(rstd, rstd, AF.Sqrt)
    p.recip(rstd, rstd)
    p.stt("dve", hn_out, xt, rstd, gbc, ALU.mult, ALU.mult)


def build_L1():
    nc = bass.Bass("TRN2", target_bir_lowering=False)
    p = Prog(nc)
    x = p.dram("x", [TOK, D], F32, "ExternalInput")
    g_mix = p.dram("g_mix", [1, D], F32, "ExternalInput")
    w_in = p.dram("w_in", [D, GIN], F32, "ExternalInput")
    wg = p.dram("wg", [2, 16, 512], F32, "ExternalInput")
    bg = p.dram("bg", [2, 512], F32, "ExternalInput")
    cf_d = p.dram("cf", [128, 5, 128], F32, "ExternalInput")
    cb_d = p.dram("cb", [128, 128], BF16, "ExternalInput")
    QP = p.dram("QP", [2, 128, GH, TOK], BF16, "ExternalOutput")
    KP = p.dram("KP", [2, 128, GH, TOK], BF16, "Internal")
    K2 = p.dram("K2", [2, TOK, 512], BF16, "Internal")
    VT = p.dram("VT", [TOK, 1024], BF16, "Internal")
    SR = p.dram("SR", [TOK, 1024], F32, "ExternalOutput")
    OL = p.dram("OL", [TOK, 1024], F32, "ExternalOutput")
    AA = p.dram("AA", [2, 128, NT * GH], F32, "ExternalOutput")
    BE = p.dram("BE", [2, GH, 128, GDV], F32, "ExternalOutput")

    W = p.sb("W", [128, 8, GIN], BF16)
    cf = p.sb("cfs", [128, 5, 128], F32)
    cb = p.sb("cbs", [128, 128], BF16)
    gbc = p.sb("gbc", [128, D], F32)
    bgs = p.sb("bgs", [128, 2, 512], F32)
    wgs = p.sb("wgs", [16, 2, 512], BF16)
    xt = [p.sb("xt%d" % i, [128, D], F32) for i in range(2)]
    sq = p.sb("sq", [128, D], F32)
    ss = p.sb("ss", [128, 1], F32)
    rstd = p.sb("rstd", [128, 1], F32)
    hn = p.sb("hn", [128, D], BF16)
    hnT = p.sb("hnT", [128, D], BF16)
    gdT = p.sb("gdT", [16, 256], BF16)
    zb = p.sb("zb", [128, 512], F32)
    spl = [p.sb("spl%d" % i, [128, 512], F32) for i in range(2)]
    EqT = p.sb("EqT", [128, 512], F32)
    EkT = p.sb("EkT", [128, 512], F32)
    Ek2 = p.sb("Ek2", [128, 512], F32)
    a_all = p.sb("a_all", [128, 2, NT * GH], F32)
    c_all = p.sb("c_all", [128, 2, NT * GH], F32)
    qp = [p.sb("qp%d" % i, [128, 512], BF16) for i in range(2)]
    kp = [p.sb("kp%d" % i, [128, 512], BF16) for i in range(2)]
    k2 = [p.sb("k2%d" % i, [128, 512], BF16) for i in range(2)]
    vb = p.sb("vb", [128, 1024], BF16)
    srt = p.sb("srt", [128, 1024], F32)
    S = p.sb("S", [128, GH, GDV], F32)
    Sb = p.sb("Sb", [128, GH, GDV], BF16)
    s_qp = [p.sb("s_qp%d" % i, [128, 512], BF16) for i in range(2)]
    s_kp = [p.sb("s_kp%d" % i, [128, 512], BF16) for i in range(2)]
    s_k2 = [p.sb("s_k2%d" % i, [128, 512], BF16) for i in range(2)]
    s_v = [p.sb("s_v%d" % i, [128, 1024], BF16) for i in range(2)]
    AT = [p.sb("AT%d" % i, [128, 128], BF16) for i in range(2)]
    ot = [p.sb("ot%d" % i, [128, 1024], F32) for i in range(2)]
    of = [p.sb("of%d" % i, [128, 1024], F32) for i in range(2)]
    banks = [p.ps("bk%d" % i, [128, 512], F32) for i in range(7)]
    pT = p.ps("pT", [128, 1024], BF16)
    bi = [0]

    rot = [3, 4]

    def bank():
        b = banks[rot[0] + bi[0] % rot[1]]
        bi[0] += 1
        return b

    p.dma("sp", cf[:], cf_d[:])
    p.dma("sp", cb[:], cb_d[:])
    p.dma("sp", gbc[:], g_mix.v(g_mix.t[0:1, :].broadcast_to([128, D])))
    p.dma("sp", bgs[:, 0, :], bg.v(bg.t[0:1, :].broadcast_to([128, 512])))
    p.dma("sp", bgs[:, 1, :], bg.v(bg.t[1:2, :].broadcast_to([128, 512])))
    for d_ in range(2):
        p.dma("pool", wgs.sub(d_, (slice(None), d_, slice(None))), wg[d_])
    for dc in range(8):
        p.dma("pool", W.sub(dc, (slice(None), dc, slice(None))), w_in[dc * 128:(dc + 1) * 128, :])
    Wk = lambda dc, lo, hi: W.sub(dc, (slice(None), dc, slice(lo, hi)))
    UI, LI, US, LS = (cf[:, i, :] for i in range(4))
    scale_q = float(GDK) ** -0.5

    def scan_init(d_):
        p.memset("dve", S[:], 0.0)
        p.memset("dve", Sb[:], 0.0)

    def scan_step(d_, j, it, sq_, sk_, sk2_, sv_):
        b = it % 2
        mask = UI if d_ == 0 else LS
        O = ot[b]
        for h in range(GH):
            hs = slice(h * 128, (h + 1) * 128)
            vs = slice(h * GDV, (h + 1) * GDV)
            ba = bank()
            p.matmul(ba[:, 0:128], sk_[:, hs], sq_[:, hs])
            A = AT[h % 2]
            p.tt("dve", A[:], ba[:, 0:128], mask, ALU.mult)
            bo = bank()
            p.matmul(bo[:, 0:GDV], A[:], sv_[:, vs], start=True, stop=False)
            p.matmul(bo[:, 0:GDV], sq_[:, hs], Sb[:, h, :], start=False, stop=True)
            bs = bank()
            p.matmul(bs[:, 0:GDV], sk2_[:, hs], sv_[:, vs])
            if d_ == 0:
                p.copy("act", O[:, vs], bo[:, 0:GDV])
            else:
                p.tt("dve", O[:, vs], bo[:, 0:GDV], of[b][:, vs], ALU.add)
            p.stt("dve", S[:, h, :], S[:, h, :], a_all.v(a_all.t[:, d_, j * GH + h:j * GH + h + 1]),
                  bs[:, 0:GDV], ALU.mult, ALU.add)
            p.copy("act", Sb[:, h, :], S[:, h, :])
        p.dma("pool", OL[j * 128:(j + 1) * 128, :], O[:])

    def scan_fin(d_):
        for h in range(GH):
            p.dma("pool", BE.v(BE.t[d_, h]), S[:, h, :])

    scan_init(0)
    for j in range(NT):
        X = xt[j % 2]
        p.dma("sp", X[:], x[j * 128:(j + 1) * 128, :])
        rmsnorm_tile(p, X[:], gbc[:], hn[:], sq[:], ss[:], rstd[:])
        for dc in range(8):
            p.transpose(pT[:, dc * 128:(dc + 1) * 128], hn[:, dc * 128:(dc + 1) * 128], cb[:])
        p.copy("act", hnT[:], pT[:])
        hT = lambda dc: hnT[:, dc * 128:(dc + 1) * 128]
        bG = bank()
        for d_ in range(2):
            for dc in range(8):
                p.matmul(bG[0:16, d_ * 128:(d_ + 1) * 128], Wk(dc, 3072 + 16 * d_, 3088 + 16 * d_), hT(dc),
                         start=(dc == 0), stop=(dc == 7))
        p.copy("act", gdT[:], bG[0:16, 0:256])
        bq, bk, bkt = banks[0], banks[1], banks[2]
        for n in range(4):
            for dc in range(8):
                p.matmul(bq[:, n * 128:(n + 1) * 128], Wk(dc, n * 128, (n + 1) * 128), hT(dc),
                         start=(dc == 0), stop=(dc == 7))
        for n in range(4):
            for dc in range(8):
                p.matmul(bk[:, n * 128:(n + 1) * 128], Wk(dc, 512 + n * 128, 512 + (n + 1) * 128), hT(dc),
                         start=(dc == 0), stop=(dc == 7))
        for dc in range(8):
            p.matmul(bkt[:], hT(dc), Wk(dc, 512, 1024), start=(dc == 0), stop=(dc == 7))
        for d_ in range(2):
            bz = bank()
            p.matmul(bz[:], gdT[:, d_ * 128:(d_ + 1) * 128], wgs.sub(d_, (slice(None), d_, slice(None))))
            p.tt("dve", zb[:], bz[:], bgs[:, d_, :], ALU.add)
            p.act(zb[:], zb[:], AF.Exp, scale=-1.0)
            sp_ = spl[d_]
            p.act(sp_[:], zb[:], AF.Ln, bias=1.0)
            bc, bd = bank(), bank()
            tri = UI if d_ == 0 else LI
            for h in range(GH):
                p.matmul(bc[:, h * 128:(h + 1) * 128], sp_[:, h * 128:(h + 1) * 128], tri)
            p.matmul(bd[:], LS if d_ == 0 else US, sp_[:])
            p.act(EqT[:], bc[:], AF.Exp, scale=-1.0 / 16)
            p.act(EkT[:], bc[:], AF.Exp, scale=1.0 / 16)
            p.act(Ek2[:], bd[:], AF.Exp, scale=-1.0 / 16)
            last = 127 if d_ == 0 else 0
            p.copy("dve", a_all.v(a_all.t[:, d_, j * GH:(j + 1) * GH]),
                   EqT.v(EqT.t[:].rearrange("p (h t) -> p h t", h=GH)[:, :, last]))
            p.copy("dve", c_all.v(c_all.t[:, d_, j * GH:(j + 1) * GH]),
                   bc.v(bc.t[:].rearrange("p (h t) -> p h t", h=GH)[:, :, last]))
            Q, K, KK = qp[d_], kp[d_], k2[d_]
            p.stt("dve", Q[:], bq[:], scale_q, EqT[:], ALU.mult, ALU.mult)
            p.tt("dve", K[:], bk[:], EkT[:], ALU.mult)
            p.tt("dve", KK[:], bkt[:], Ek2[:], ALU.mult)
            p.dma("pool", QP.v(QP.t[d_, :, :, j * 128:(j + 1) * 128]),
                  Q.v(Q.t[:].rearrange("p (h t) -> p h t", h=GH)))
            p.dma("pool", KP.v(KP.t[d_, :, :, j * 128:(j + 1) * 128]),
                  K.v(K.t[:].rearrange("p (h t) -> p h t", h=GH)))
            p.dma("pool", K2.v(K2.t[d_, j * 128:(j + 1) * 128, :]), KK[:])
        for half in range(2):
            bv = bank()
            for dc in range(8):
                p.matmul(bv[:], hT(dc), Wk(dc, 1024 + half * 512, 1536 + half * 512), start=(dc == 0), stop=(dc == 7))
            p.copy("act", vb[:, half * 512:(half + 1) * 512], bv[:])
        p.dma("pool", VT[j * 128:(j + 1) * 128, :], vb[:])
        scan_step(0, j, j, qp[0], kp[0], k2[0], vb)
        for half in range(2):
            br = bank()
            for dc in range(8):
                p.matmul(br[:], hT(dc), Wk(dc, 2048 + half * 512, 2560 + half * 512), start=(dc == 0), stop=(dc == 7))
            p.act(srt[:, half * 512:(half + 1) * 512], br[:], AF.Silu)
        p.dma("pool", SR[j * 128:(j + 1) * 128, :], srt[:])
    scan_fin(0)
    for d_ in range(2):
        p.dma("pool", AA[d_], c_all[:, d_, :])

    rot[0], rot[1] = 0, 7
    scan_init(1)
    for it, j in enumerate(range(NT - 1, -1, -1)):
        b = it % 2
        sq_, sk_, sk2_, sv_ = s_qp[b], s_kp[b], s_k2[b], s_v[b]
        p.dma("sp", sq_.v(sq_.t[:].rearrange("p (h t) -> p h t", h=GH)), QP.v(QP.t[1, :, :, j * 128:(j + 1) * 128]))
        p.dma("sp", sk_.v(sk_.t[:].rearrange("p (h t) -> p h t", h=GH)), KP.v(KP.t[1, :, :, j * 128:(j + 1) * 128]))
        p.dma("sp", sk2_[:], K2.v(K2.t[1, j * 128:(j + 1) * 128, :]))
        p.dma("sp", sv_[:], VT[j * 128:(j + 1) * 128, :])
        p.dma("sp", of[b][:], OL[j * 128:(j + 1) * 128, :])
        scan_step(1, j, it, sq_, sk_, sk2_, sv_)
    scan_fin(1)
    return p.finish()


def ffn_prep_tile(p, h1, gf, wr, cff, bufs, HN_out, AFF_out, rows, bank):
    sq, ss, rstd, hnf, hnb, hnT, lg, mx, sm = bufs
    rmsnorm_tile(p, h1, gf[:], hnf[:], sq[:], ss[:], rstd[:])
    p.copy("act", hnb[:], hnf[:])
    p.dma("pool", HN_out[rows, :], hnb[:])
    ident_f = cff[:, 4, :]
    for half in range(2):
        bt = bank()
        for q in range(4):
            dc = half * 4 + q
            p.transpose(bt[:, q * 128:(q + 1) * 128], hnf[:, dc * 128:(dc + 1) * 128], ident_f)
        p.copy("act", hnT[:, half * 512:(half + 1) * 512], bt[:])
    bl = bank()
    for dc in range(8):
        p.matmul(bl[:, 0:16], hnT[:, dc * 128:(dc + 1) * 128], wr[:, dc, :], start=(dc == 0), stop=(dc == 7))
    p.reduce("dve", mx[:], bl[:, 0:16], ALU.max)
    p.ts("dve", mx[:], mx[:], -1.0, None, ALU.mult)
    p.act(lg[:], bl[:, 0:16], AF.Exp, bias=mx[:])
    p.reduce("dve", sm[:], lg[:], ALU.add)
    p.recip(sm[:], sm[:])
    p.ts("dve", lg[:], lg[:], sm[:], None, ALU.mult)
    p.dma("pool", AFF_out[rows, :], lg[:])


def ffn_prep_bufs(p):
    return (p.sb("f_sq", [128, D], F32), p.sb("f_ss", [128, 1], F32), p.sb("f_rstd", [128, 1], F32),
            p.sb("f_hnf", [128, D], F32), p.sb("f_hnb", [128, D], BF16), p.sb("f_hnT", [128, D], F32),
            p.sb("f_lg", [128, 16], F32), p.sb("f_mx", [128, 1], F32), p.sb("f_sm", [128, 1], F32))


def build_L2():
    nc = bass.Bass("TRN2", target_bir_lowering=False)
    p = Prog(nc)
    x = p.dram("x", [TOK, D], F32, "ExternalInput")
    OL = p.dram("OL", [TOK, 1024], F32, "ExternalInput")
    QP = p.dram("QP", [2, 128, GH, TOK], BF16, "ExternalInput")
    SR = p.dram("SR", [TOK, 1024], F32, "ExternalInput")
    AA = p.dram("AA", [2, 128, NT * GH], F32, "ExternalInput")
    BP = p.dram("BP", [2, 7, GH, 128, GDV], F32, "ExternalInput")
    APD = p.dram("APD", [2, 7, 128, NT * GH], F32, "ExternalInput")
    g_head = p.dram("g_head", [1, GDV], F32, "ExternalInput")
    w_out = p.dram("w_out", [D, D], F32, "ExternalInput")
    g_ffn = p.dram("g_ffn", [1, D], F32, "ExternalInput")
    w_r = p.dram("w_r", [D, 16], F32, "ExternalInput")
    cf_d = p.dram("cf", [128, 5, 128], F32, "ExternalInput")
    cb_d = p.dram("cb", [128, 128], BF16, "ExternalInput")
    H1 = p.dram("H1", [TOK, D], F32, "ExternalOutput")
    HN1 = p.dram("HN1", [TOK, D], BF16, "ExternalOutput")
    AFF = p.dram("AFF", [TOK, 16], F32, "ExternalOutput")

    Wo = p.sb("Wo", [128, 8, D], BF16)
    wr = p.sb("wr", [128, 8, 16], F32)
    cf = p.sb("cfs", [128, 5, 128], F32)
    cb = p.sb("cbs", [128, 128], BF16)
    gf = p.sb("gf", [128, D], F32)
    gh = p.sb("gh", [128, D], F32)
    apd = p.sb("apd", [128, 2, 7, NT * GH], F32)
    csum = p.sb("csum", [128, 2, 7, GH], F32)
    aown = p.sb("aown", [128, 2, NT * GH], F32)
    Sin = p.sb("Sin", [128, 2, GH, GDV], F32)
    Bt = [p.sb("Bt%d" % i, [128, GH, GDV], F32) for i in range(2)]
    Sbw = p.sb("Sbw", [128, NT, GH, GDV], BF16)
    Sfb = p.sb("Sfb", [128, GH, GDV], BF16)
    xt = [p.sb("xt%d" % i, [128, D], F32) for i in range(2)]
    olt = [p.sb("olt%d" % i, [128, D], F32) for i in range(2)]
    srt = [p.sb("srt%d" % i, [128, D], F32) for i in range(2)]
    qf = [p.sb("qf%d" % i, [128, 512], BF16) for i in range(2)]
    qb = [p.sb("qb%d" % i, [128, 512], BF16) for i in range(2)]
    o = p.sb("o", [128, D], F32)
    osq = p.sb("osq", [128, D], F32)
    hss = p.sb("hss", [128, GH], F32)
    y = p.sb("y", [128, D], BF16)
    yT = p.sb("yT", [128, D], BF16)
    h1 = p.sb("h1", [128, D], F32)
    fb = ffn_prep_bufs(p)
    banks = [p.ps("bk%d" % i, [128, 512], F32) for i in range(7)]
    pT = p.ps("pT", [128, 1024], BF16)
    bi = [0]

    def bank():
        b = banks[bi[0] % 7]
        bi[0] += 1
        return b

    p.dma("sp", cf[:], cf_d[:])
    p.dma("sp", cb[:], cb_d[:])
    p.dma("sp", gf[:], g_ffn.v(g_ffn.t[0:1, :].broadcast_to([128, D])))
    for h in range(GH):
        p.dma("sp", gh[:, h * GDV:(h + 1) * GDV], g_head.v(g_head.t[0:1, :].broadcast_to([128, GDV])))
    p.dma("sp", wr[:], w_r.v(w_r.t.rearrange("(c p) e -> p c e", p=128)))
    for dc in range(8):
        p.dma("pool", Wo.sub(dc, (slice(None), dc, slice(None))), w_out[dc * 128:(dc + 1) * 128, :])
    for d_ in range(2):
        p.dma("sp", apd.v(apd.t[:, d_]), APD.v(APD.t[d_].rearrange("i p c -> p i c")))
        p.dma("sp", aown.v(aown.t[:, d_, :]), AA[d_])
    p.reduce("dve", csum[:], apd.v(apd.t[:].rearrange("p d i (t h) -> p d i h t", h=GH)), ALU.add)
    p.act(csum[:], csum[:], AF.Exp, scale=-1.0 / 16)
    p.act(aown[:], aown[:], AF.Exp, scale=-1.0 / 16)
    p.memset("dve", Sin[:], 0.0)
    n = 0
    for d_ in range(2):
        for i in range(7):
            B = Bt[n % 2]
            n += 1
            p.dma("sp", B[:], BP.v(BP.t[d_, i].rearrange("h p v -> p h v")))
            for h in range(GH):
                p.stt("dve", Sin[:, d_, h, :], Sin[:, d_, h, :], csum.v(csum.t[:, d_, i, h:h + 1]), B[:, h, :],
                      ALU.mult, ALU.add)
    for j in range(NT - 1, -1, -1):
        p.copy("act", Sbw[:, j], Sin[:, 1])
        for h in range(GH):
            p.ts("dve", Sin[:, 1, h, :], Sin[:, 1, h, :], aown.v(aown.t[:, 1, j * GH + h:j * GH + h + 1]), None, ALU.mult)
    for j in range(NT):
        b = j % 2
        rows = slice(j * 128, (j + 1) * 128)
        p.dma("sp", xt[b][:], x[rows, :])
        p.dma("sp", olt[b][:], OL[rows, :])
        p.dma("sp", srt[b][:], SR[rows, :])
        p.dma("sp", qf[b].v(qf[b].t[:].rearrange("p (h t) -> p h t", h=GH)), QP.v(QP.t[0, :, :, rows]))
        p.dma("sp", qb[b].v(qb[b].t[:].rearrange("p (h t) -> p h t", h=GH)), QP.v(QP.t[1, :, :, rows]))
        p.copy("act", Sfb[:], Sin[:, 0])
        for h in range(GH):
            hs = slice(h * 128, (h + 1) * 128)
            vs = slice(h * GDV, (h + 1) * GDV)
            bo = bank()
            p.matmul(bo[:, 0:GDV], qf[b][:, hs], Sfb[:, h, :], start=True, stop=False)
            p.matmul(bo[:, 0:GDV], qb[b][:, hs], Sbw[:, j, h, :], start=False, stop=True)
            p.tt("dve", o[:, vs], bo[:, 0:GDV], olt[b][:, vs], ALU.add)
            p.ts("dve", Sin[:, 0, h, :], Sin[:, 0, h, :], aown.v(aown.t[:, 0, j * GH + h:j * GH + h + 1]), None, ALU.mult)
        p.tt("dve", osq[:], o[:], o[:], ALU.mult)
        p.reduce("dve", hss[:], osq.v(osq.t[:].rearrange("p (h v) -> p h v", h=GH)), ALU.add)
        p.ts("dve", hss[:], hss[:], 1.0 / GDV, RMS_EPS, ALU.mult, ALU.add)
        p.act(hss[:], hss[:], AF.Sqrt)
        p.recip(hss[:], hss[:])
        p.tt("dve", osq[:], srt[b][:], gh[:], ALU.mult)
        for h in range(GH):
            vs = slice(h * GDV, (h + 1) * GDV)
            p.stt("dve", y[:, vs], o[:, vs], hss[:, h:h + 1], osq[:, vs], ALU.mult, ALU.mult)
        for dc in range(8):
            p.transpose(pT[:, dc * 128:(dc + 1) * 128], y[:, dc * 128:(dc + 1) * 128], cb[:])
        p.copy("act", yT[:], pT[:])
        for half in range(2):
            bm = bank()
            for dc in range(8):
                p.matmul(bm[:], yT[:, dc * 128:(dc + 1) * 128],
                         Wo.sub(dc, (slice(None), dc, slice(half * 512, (half + 1) * 512))),
                         start=(dc == 0), stop=(dc == 7))
            p.tt("dve", h1[:, half * 512:(half + 1) * 512], bm[:], xt[b][:, half * 512:(half + 1) * 512], ALU.add)
        p.dma("pool", H1[rows, :], h1[:])
        ffn_prep_tile(p, h1[:], gf, wr, cf, fb, HN1, AFF, rows, bank)
    return p.finish()


NE = 16
CAP = 2 * SEQ // NE
FF = 2048
BISECT_ITERS = 36


def build_L3():
    nc = bass.Bass("TRN2", target_bir_lowering=False)
    p = Prog(nc)
    AFFE = p.dram("AFFE", [2, 128, 128], F32, "ExternalInput")
    HN = p.dram("HN", [SEQ, D], BF16, "ExternalInput")
    wg = p.dram("wg", [2, D, FF], F32, "ExternalInput")
    wu = p.dram("wu", [2, D, FF], F32, "ExternalInput")
    wd = p.dram("wd", [2, FF, D], F32, "ExternalInput")
    cf_d = p.dram("cf", [128, 6, 128], F32, "ExternalInput")
    cb_d = p.dram("cb", [128, 128], BF16, "ExternalInput")
    tok_d = p.dram("tokid", [128, 128], I32, "ExternalInput")
    DELTA = p.dram("DELTA", [SEQ, D], F32, "ExternalOutput")

    Wg = p.sb("Wg", [128, 8, FF], BF16)
    Wu = p.sb("Wu", [128, 8, FF], BF16)
    Wd = p.sb("Wd", [128, 16, D], BF16)
    cf = p.sb("cfs", [128, 6, 128], F32)
    cb = p.sb("cbs", [128, 128], BF16)
    tokid = p.sb("tokid_s", [128, 128], I32)
    zt = p.sb("zt", [128, 4096], F32)
    ones = p.sb("ones", [128, 128], F32)
    aff = p.sb("aff", [128, 2, 128], F32)
    cmp_ = p.sb("cmp", [128, 2, 128], F32)
    st = {n: p.sb("b_" + n, [128, 2], F32) for n in ("lo", "hi", "mid", "cnt", "ge", "nge", "t1", "t2")}
    selT = p.sb("selT", [128, 128], F32)
    rp = p.sb("rp", [128, 1], F32)
    slot = p.sb("slot", [128, 128], F32)
    idx_sb = p.sb("idx_sb", [128, 2, 16, 2], I32)
    banks = [p.ps("bk%d" % i, [128, 512], F32) for i in range(7)]
    pT = p.ps("pT", [128, 1024], BF16)
    bi = [0]

    def bank():
        b = banks[bi[0] % 7]
        bi[0] += 1
        return b

    p.dma("sp", cf[:], cf_d[:])
    p.dma("sp", cb[:], cb_d[:])
    p.dma("sp", tokid[:], tok_d[:])
    p.dma("sp", aff[:], AFFE.v(AFFE.t.rearrange("e p f -> p e f")))
    p.memset("pool", zt[:], 0.0)
    p.memset("dve", ones[:], 1.0)
    zkeys = []
    dz = DELTA.t.rearrange("(k p r) d -> k p (r d)", p=128, r=4)
    for k in range(SEQ // 512):
        zk = ("DELTA", "z", k)
        zkeys.append(zk)
        p.dma("sp", V(dz[k], zk), zt[:])
    US = cf[:, 2, :]
    ident_f = cf[:, 4, :]

    def load_weights(e):
        for dc in range(8):
            p.dma("pool", Wg.sub(dc, (slice(None), dc, slice(None))), V(wg.t[e, dc * 128:(dc + 1) * 128, :], ("wg", e)))
            p.dma("pool", Wu.sub(dc, (slice(None), dc, slice(None))), V(wu.t[e, dc * 128:(dc + 1) * 128, :], ("wu", e)))
        for fc in range(16):
            p.dma("pool", Wd.sub(fc, (slice(None), fc, slice(None))), V(wd.t[e, fc * 128:(fc + 1) * 128, :], ("wd", e)))

    load_weights(0)
    lo, hi, mid, cnt, ge, nge, t1, t2 = (st[n] for n in ("lo", "hi", "mid", "cnt", "ge", "nge", "t1", "t2"))
    p.memset("dve", lo[:], 0.0)
    p.memset("dve", hi[:], 1.0)
    for it in range(BISECT_ITERS):
        p.tt("dve", mid[:], lo[:], hi[:], ALU.add)
        p.ts("dve", mid[:], mid[:], 0.5, None, ALU.mult)
        for e in range(2):
            p.ts("dve", cmp_[:, e, :], aff[:, e, :], mid[:, e:e + 1], None, ALU.is_ge)
        p.reduce("dve", cnt[:], cmp_[:], ALU.add)
        bt = bank()
        p.matmul(bt[:, 0:2], ones[:], cnt[:])
        p.ts("dve", ge[:], bt[:, 0:2], float(CAP) - 0.5, None, ALU.is_ge)
        p.ts("dve", nge[:], ge[:], -1.0, 1.0, ALU.mult, ALU.add)
        p.tt("dve", lo[:], lo[:], nge[:], ALU.mult)
        p.tt("dve", t1[:], mid[:], ge[:], ALU.mult)
        p.tt("dve", lo[:], lo[:], t1[:], ALU.add)
        p.tt("dve", hi[:], hi[:], ge[:], ALU.mult)
        p.tt("dve", t2[:], mid[:], nge[:], ALU.mult)
        p.tt("dve", hi[:], hi[:], t2[:], ALU.add)
    for e in range(2):
        p.ts("dve", cmp_[:, e, :], aff[:, e, :], lo[:, e:e + 1], None, ALU.is_ge)
    p.reduce("dve", cnt[:], cmp_[:], ALU.add)
    wts = idx_sb.t[:].bitcast(F32)
    p.push_scope()
    slot_i = p.sb("slot_i", [128, 128], I32)
    smod_i = p.sb("smod_i", [128, 128], I32)
    sdiv_i = p.sb("sdiv_i", [128, 128], I32)
    smod_f = p.sb("smod_f", [128, 128], F32)
    sdiv_f = p.sb("sdiv_f", [128, 128], F32)
    tokf = p.sb("tokf", [128, 128], F32)
    base = p.sb("base", [128, 128, 16], F32)
    Bcat = p.sb("Bcat", [128, 128, 32], F32)
    OHc = [p.sb("OHc%d" % i, [128, 16, 128], F32) for i in range(2)]
    p.copy("dve", tokf[:], tokid[:])
    iota_r = cf.t[:, 5, :]
    for e in range(2):
        bt = bank()
        p.transpose(bt[:, 0:128], cmp_[:, e, :], ident_f)
        p.copy("act", selT[:], bt[:, 0:128])
        bp = bank()
        p.matmul(bp[:, 0:128], selT[:], US)
        p.matmul(bp[:, 128:129], US, cnt[:, e:e + 1])
        p.copy("act", rp[:], bp[:, 128:129])
        p.ts("dve", slot[:], bp[:, 0:128], rp[:, 0:1], None, ALU.add)
        p.copy("dve", slot_i[:], slot[:])
        p.ts("dve", smod_i[:], slot_i[:], 127, None, ALU.bitwise_and)
        p.ts("dve", sdiv_i[:], slot_i[:], 7, None, ALU.arith_shift_right)
        p.copy("dve", smod_f[:], smod_i[:])
        p.copy("dve", sdiv_f[:], sdiv_i[:])
        p.tt("dve", base[:], cf.v(iota_r[:, 0:16].unsqueeze(1).broadcast_to([128, 128, 16])),
             sdiv_f.v(sdiv_f.t[:].unsqueeze(2).broadcast_to([128, 128, 16])), ALU.is_equal)
        p.tt("dve", base[:], base[:], cmp_.v(cmp_.t[:, e, :].unsqueeze(2).broadcast_to([128, 128, 16])), ALU.mult)
        p.tt("dve", Bcat[:, :, 0:16], base[:], tokf.v(tokf.t[:].unsqueeze(2).broadcast_to([128, 128, 16])), ALU.mult)
        p.tt("dve", Bcat[:, :, 16:32], base[:], aff.v(aff.t[:, e, :].unsqueeze(2).broadcast_to([128, 128, 16])), ALU.mult)
        bacc = bank()
        for c in range(8):
            OH = OHc[c % 2]
            p.tt("dve", OH[:], cf.v(iota_r.unsqueeze(1).broadcast_to([128, 16, 128])),
                 smod_f.v(smod_f.t[:, c * 16:(c + 1) * 16].unsqueeze(2).broadcast_to([128, 16, 128])), ALU.is_equal)
            for fl in range(16):
                f = c * 16 + fl
                p.matmul(bacc[:, 0:32], OH[:, fl, :], Bcat[:, f, :], start=(f == 0), stop=(f == 127))
        p.copy("dve", idx_sb.v(idx_sb.t[:, e, :, 0]), bacc[:, 0:16])
        p.copy("dve", idx_sb.v(wts[:, e, :, 1]), bacc[:, 16:32])
    p.pop_scope()
    xs = [p.sb("xs%d" % i, [128, D], BF16) for i in range(2)]
    xsT = p.sb("xsT", [128, 8, 512], BF16)
    hT = p.sb("hT", [128, 16, 512], BF16)
    sg = [p.sb("sg%d" % i, [128, 512], F32) for i in range(2)]
    y = [p.sb("y%d" % i, [128, D], F32) for i in range(2)]
    first_scatter = True
    for e in range(2):
        if e > 0:
            load_weights(e)
        for tg in range(4):
            for kk in range(4):
                k = tg * 4 + kk
                X = xs[kk % 2]
                p.gather(X[:], HN[:, :], idx_sb[:, e, k, 0:1], SEQ)
                for dc in range(8):
                    p.transpose(pT[:, dc * 128:(dc + 1) * 128], X[:, dc * 128:(dc + 1) * 128], cb[:])
                p.copy("act", xsT[:, :, kk * 128:(kk + 1) * 128], pT.v(pT.t[:].rearrange("p (c t) -> p c t", c=8)))
            for fc in range(16):
                bg_, bu_ = bank(), bank()
                for dc in range(8):
                    p.matmul(bg_[:], Wg.sub(dc, (slice(None), dc, slice(fc * 128, (fc + 1) * 128))), xsT[:, dc, :],
                             start=(dc == 0), stop=(dc == 7))
                for dc in range(8):
                    p.matmul(bu_[:], Wu.sub(dc, (slice(None), dc, slice(fc * 128, (fc + 1) * 128))), xsT[:, dc, :],
                             start=(dc == 0), stop=(dc == 7))
                G = sg[fc % 2]
                p.act(G[:], bg_[:], AF.Silu)
                p.tt("dve", hT[:, fc, :], G[:], bu_[:], ALU.mult)
            for kk in range(4):
                k = tg * 4 + kk
                Y = y[kk % 2]
                for half in range(2):
                    by = bank()
                    for fc in range(16):
                        p.matmul(by[:], hT[:, fc, kk * 128:(kk + 1) * 128],
                                 Wd.sub(fc, (slice(None), fc, slice(half * 512, (half + 1) * 512))),
                                 start=(fc == 0), stop=(fc == 15))
                    p.ts("dve", Y[:, half * 512:(half + 1) * 512], by[:], idx_sb.v(wts[:, e, k, 1:2]), None, ALU.mult)
                p.scatter(DELTA[:, :], Y[:], idx_sb[:, e, k, 0:1], SEQ, accum=True,
                          reads=zkeys if first_scatter else None)
                first_scatter = False
    return p.finish()


MH = 16
QRANK = 256
KVR = 128
NOPE = 128
ROPE = 64
MLA_SCALE = float(NOPE + ROPE) ** -0.5
TWO_PI = 2.0 * np.pi


def rope_consts():
    half = ROPE // 2
    inv = (10000.0 ** (-np.arange(half, dtype=np.float32) / half)).astype(np.float32)
    rc = np.zeros((64, 4), np.float32)
    rc[:, 0] = np.concatenate([inv, inv])
    rc[:, 1] = np.concatenate([np.ones(half), -np.ones(half)])
    rc[:, 2] = -np.pi
    return rc


def build_L4():
    nc = bass.Bass("TRN2", target_bir_lowering=False)
    p = Prog(nc)
    H1 = p.dram("H1", [TOK, D], F32, "ExternalInput")
    DS = p.dram("DS", [NCORES, TOK, D], F32, "ExternalInput")
    pos = p.dram("pos", [1, TOK], I32, "ExternalInput")
    g_mix = p.dram("g_mix", [1, D], F32, "ExternalInput")
    w_m = p.dram("w_m", [D, 448], F32, "ExternalInput")
    g_q = p.dram("g_q", [1, QRANK], F32, "ExternalInput")
    g_kv = p.dram("g_kv", [1, KVR], F32, "ExternalInput")
    w_uq = p.dram("w_uq", [QRANK, MH * 192], F32, "ExternalInput")
    w_ukv = p.dram("w_ukv", [KVR, MH * 256], F32, "ExternalInput")
    rc_d = p.dram("rc", [64, 4], F32, "ExternalInput")
    cf_d = p.dram("cf", [128, 5, 128], F32, "ExternalInput")
    cb_d = p.dram("cb", [128, 128], BF16, "ExternalInput")
    H2 = p.dram("H2", [TOK, D], F32, "ExternalOutput")
    QL = p.dram("QL", [MH, 128, TOK], BF16, "ExternalOutput")
    QR = p.dram("QR", [MH, 64, TOK], BF16, "ExternalOutput")
    CKVT = p.dram("CKVT", [128, TOK], BF16, "ExternalOutput")
    KRT = p.dram("KRT", [64, TOK], BF16, "ExternalOutput")
    CKV = p.dram("CKV", [TOK, 128], BF16, "ExternalOutput")
    KN2 = p.dram("KN2", [TOK, 1], F32, "ExternalOutput")
    QMAX = p.dram("QMAX", [1, MH], F32, "ExternalOutput")

    cf = p.sb("cfs", [128, 5, 128], F32)
    cb = p.sb("cbs", [128, 128], BF16)
    rc = p.sb("rcs", [64, 4], F32)
    gm = p.sb("gm", [128, D], F32)
    gq = p.sb("gq", [128, QRANK], F32)
    gkv = p.sb("gkv", [128, KVR], F32)
    Wm = p.sb("Wm", [128, 8, 448], BF16)
    Wuq = p.sb("Wuq", [128, 2, MH * 192], BF16)
    Wukv = p.sb("Wukv", [128, MH * 256], BF16)
    WukT = p.sb("WukT", [128, MH, 128], BF16)
    posi = p.sb("posi", [64, TOK], I32)
    ang = p.sb("ang", [64, TOK], F32)
    C2 = p.sb("C2", [64, TOK], F32)
    S2 = p.sb("S2", [64, TOK], F32)
    hnT = p.sb("hnT", [128, 8, TOK], BF16)
    cqT = p.sb("cqT", [128, 2, TOK], BF16)
    ht = [p.sb("ht%d" % i, [128, D], F32) for i in range(2)]
    dt_ = [p.sb("dt%d" % i, [128, D], F32) for i in range(3)]
    sq = p.sb("sq", [128, D], F32)
    ss = p.sb("ss", [128, 1], F32)
    rstd = p.sb("rstd", [128, 1], F32)
    hn = p.sb("hn", [128, D], BF16)
    cs = p.sb("cs", [128, 448], F32)
    cn = p.sb("cn", [128, 384], BF16)
    kn = p.sb("kn", [128, 2], F32)
    qn = p.sb("qn", [128, 512], BF16)
    qlf = p.sb("qlf", [128, 512], F32)
    qlb = p.sb("qlb", [128, 512], BF16)
    qsq = p.sb("qsq", [128, 512], F32)
    r1 = p.sb("r1", [64, 512], F32)
    r2 = p.sb("r2", [64, 512], F32)
    rb = p.sb("rb", [64, 512], BF16)
    qmx = p.sb("qmx", [1, MH], F32)
    qm1 = p.sb("qm1", [1, 1], F32)
    ckT = p.sb("ckT", [128, 128], BF16)
    banks = [p.ps("bk%d" % i, [128, 512], F32) for i in range(7)]
    pT = p.ps("pT", [128, 1024], BF16)
    bi = [0]

    def bank():
        b = banks[bi[0] % 7]
        bi[0] += 1
        return b

    p.dma("sp", cf[:], cf_d[:])
    p.dma("sp", cb[:], cb_d[:])
    p.dma("sp", rc[:], rc_d[:])
    p.dma("sp", gm[:], g_mix.v(g_mix.t[0:1, :].broadcast_to([128, D])))
    p.dma("sp", gq[:], g_q.v(g_q.t[0:1, :].broadcast_to([128, QRANK])))
    p.dma("sp", gkv[:], g_kv.v(g_kv.t[0:1, :].broadcast_to([128, KVR])))
    p.dma("sp", posi[:], pos.v(pos.t[0:1, :].broadcast_to([64, TOK])))
    p.dma("pool", Wm[:], w_m.v(w_m.t.rearrange("(c p) n -> p c n", p=128)))
    p.dma("pool", Wuq[:], w_uq.v(w_uq.t.rearrange("(c p) n -> p c n", p=128)))
    p.dma("pool", Wukv[:], w_ukv[:, :])
    onesf = p.sb("onesf", [128, 1], F32)
    p.memset("dve", onesf[:], 1.0)
    p.memset("dve", qmx[:], 0.0)
    Wm_sw = p.sb("Wm_sw", [128, 8, 64], BF16)
    Wuq_sw = p.sb("Wuq_sw", [128, 2, MH, 64], BF16)
    p.copy("dve", Wm_sw[:, :, 0:32], Wm[:, :, 416:448])
    p.copy("dve", Wm_sw[:, :, 32:64], Wm[:, :, 384:416])
    for kc in range(2):
        wv = Wuq.t[:, kc, :].rearrange("p (h c) -> p h c", h=MH)
        p.copy("dve", Wuq_sw[:, kc, :, 0:32], Wuq.v(wv[:, :, 160:192]))
        p.copy("dve", Wuq_sw[:, kc, :, 32:64], Wuq.v(wv[:, :, 128:160]))
    for h in range(MH):
        p.transpose(pT[:, (h % 8) * 128:(h % 8 + 1) * 128], Wukv[:, h * 256:h * 256 + 128], cb[:])
        if h % 8 == 7:
            g0 = h - 7
            p.copy("act", WukT[:, g0:g0 + 8, :], pT.v(pT.t[:].rearrange("p (h r) -> p h r", h=8)))
    p.copy("dve", ang[:], posi[:])
    p.ts("dve", ang[:], ang[:], rc[:, 0:1], None, ALU.mult)
    C1 = 6.28125
    C2_ = float(TWO_PI - 6.28125)
    ki = p.sb("ki", [64, TOK], I32)
    kf = p.sb("kf", [64, TOK], F32)
    gt = p.sb("gt", [64, TOK], F32)
    p.ts("dve", kf[:], ang[:], float(1.0 / TWO_PI), None, ALU.mult)
    p.copy("dve", ki[:], kf[:])
    p.copy("dve", kf[:], ki[:])
    p.stt("dve", ang[:], kf[:], -C1, ang[:], ALU.mult, ALU.add)
    p.stt("dve", ang[:], kf[:], -C2_, ang[:], ALU.mult, ALU.add)

    def fold(t):
        p.ts("dve", gt[:], t[:], float(np.pi), None, ALU.is_gt)
        p.stt("dve", t[:], gt[:], -TWO_PI, t[:], ALU.mult, ALU.add)
        p.ts("dve", gt[:], t[:], float(-np.pi), None, ALU.is_lt)
        p.stt("dve", t[:], gt[:], TWO_PI, t[:], ALU.mult, ALU.add)
        p.ts("dve", t[:], t[:], float(np.pi), float(-np.pi), ALU.min, ALU.max)

    fold(ang)
    p.ts("dve", C2[:], ang[:], float(np.pi / 2), None, ALU.add)
    fold(C2)
    p.act(S2[:], ang[:], AF.Sin)
    p.ts("dve", S2[:], S2[:], rc[:, 1:2], -1.0, ALU.mult, ALU.mult)
    p.act(C2[:], C2[:], AF.Sin)

    for j in range(NT):
        rows = slice(j * 128, (j + 1) * 128)
        Ht = ht[j % 2]
        p.dma("sp", Ht[:], H1[rows, :])
        for c in range(NCORES):
            Dt = dt_[c % 3]
            p.dma("sp", Dt[:], DS.v(DS.t[c, rows, :]))
            p.tt("dve", Ht[:], Ht[:], Dt[:], ALU.add)
        p.dma("pool", H2[rows, :], Ht[:])
        rmsnorm_tile(p, Ht[:], gm[:], hn[:], sq[:], ss[:], rstd[:])
        for dc in range(8):
            p.transpose(pT[:, dc * 128:(dc + 1) * 128], hn[:, dc * 128:(dc + 1) * 128], cb[:])
        p.copy("act", hnT[:, :, rows], pT.v(pT.t[:].rearrange("p (c t) -> p c t", c=8)))
        bc_ = bank()
        for dc in range(8):
            p.matmul(bc_[:, 0:448], hnT[:, dc, rows], Wm[:, dc, :], start=(dc == 0), stop=(dc == 7))
        p.copy("act", cs[:], bc_[:, 0:448])
        rmsnorm_tile(p, cs[:, 0:256], gq[:], cn[:, 0:256], sq[:, 0:256], ss[:], rstd[:], width=QRANK)
        rmsnorm_tile(p, cs[:, 256:384], gkv[:], cn[:, 256:384], sq[:, 0:128], ss[:], rstd[:], width=KVR)
        p.dma("pool", CKV[rows, :], cn[:, 256:384])
        p.tt("dve", sq[:, 0:128], cn[:, 256:384], cn[:, 256:384], ALU.mult)
        p.reduce("dve", kn[:, 0:1], sq[:, 0:128], ALU.add)
        p.tt("dve", sq[:, 0:64], cs[:, 384:448], cs[:, 384:448], ALU.mult)
        p.reduce("dve", kn[:, 1:2], sq[:, 0:64], ALU.add)
        p.tt("dve", kn[:, 0:1], kn[:, 0:1], kn[:, 1:2], ALU.add)
        p.dma("pool", KN2[rows, :], kn[:, 0:1])
        for q in range(3):
            p.transpose(pT[:, q * 128:(q + 1) * 128], cn[:, q * 128:(q + 1) * 128], cb[:])
        p.copy("act", cqT[:, :, rows], pT.v(pT.t[:, 0:256].rearrange("p (c t) -> p c t", c=2)))
        p.copy("act", ckT[:], pT[:, 256:384])
        p.dma("pool", CKVT[:, rows], ckT[:])

    for tg in range(TOK // 512):
        cols = slice(tg * 512, (tg + 1) * 512)
        bk_, bks = bank(), bank()
        for dc in range(8):
            p.matmul(bk_[0:64, :], Wm[:, dc, 384:448], hnT[:, dc, cols], start=(dc == 0), stop=(dc == 7))
        for dc in range(8):
            p.matmul(bks[0:64, :], Wm_sw[:, dc, :], hnT[:, dc, cols], start=(dc == 0), stop=(dc == 7))
        p.tt("dve", r1[:], bk_[0:64, :], C2[:, cols], ALU.mult)
        p.tt("dve", r2[:], bks[0:64, :], S2[:, cols], ALU.mult)
        p.tt("dve", rb[:], r1[:], r2[:], ALU.add)
        p.dma("pool", KRT[:, cols], rb[:])
        for h in range(MH):
            o = h * 192
            bq = bank()
            for kc in range(2):
                p.matmul(bq[:], Wuq[:, kc, o:o + 128], cqT[:, kc, cols], start=(kc == 0), stop=(kc == 1))
            p.copy("act", qn[:], bq[:])
            bl = bank()
            p.matmul(bl[:], WukT[:, h, :], qn[:])
            p.ts("dve", qlf[:], bl[:], MLA_SCALE, None, ALU.mult)
            p.copy("act", qlb[:], qlf[:])
            p.dma("pool", QL.v(QL.t[h, :, cols]), qlb[:])
            br, brs = bank(), bank()
            for kc in range(2):
                p.matmul(br[0:64, :], Wuq[:, kc, o + 128:o + 192], cqT[:, kc, cols], start=(kc == 0), stop=(kc == 1))
            for kc in range(2):
                p.matmul(brs[0:64, :], Wuq_sw[:, kc, h, :], cqT[:, kc, cols], start=(kc == 0), stop=(kc == 1))
            p.tt("dve", r1[:], br[0:64, :], C2[:, cols], ALU.mult)
            p.tt("dve", r2[:], brs[0:64, :], S2[:, cols], ALU.mult)
            p.tt("dve", r1[:], r1[:], r2[:], ALU.add)
            p.ts("dve", r1[:], r1[:], MLA_SCALE, None, ALU.mult)
            p.copy("act", rb[:], r1[:])
            p.dma("pool", QR.v(QR.t[h, :, cols]), rb[:])
            p.tt("dve", qsq[:], qlf[:], qlf[:], ALU.mult)
            p.tt("dve", r2[:], r1[:], r1[:], ALU.mult)
            bn = bank()
            p.matmul(bn[0:1, :], onesf[:, 0:1], qsq[:], start=True, stop=False)
            p.matmul(bn[0:1, :], onesf[0:64, 0:1], r2[:], start=False, stop=True)
            p.reduce("dve", qm1[:], bn[0:1, :], ALU.max)
            p.tt("dve", qmx[:, h:h + 1], qmx[:, h:h + 1], qm1[:], ALU.max)
    p.dma("pool", QMAX[:, :], qmx[:])
    return p.finish()


NKT = SEQ // 128


def build_L5(nheads=None, same=True):
    nc = bass.Bass("TRN2", target_bir_lowering=False)
    p = Prog(nc)
    QL = p.dram("QL", [MH, 128, TOK], BF16, "ExternalInput")
    QR = p.dram("QR", [MH, 64, TOK], BF16, "ExternalInput")
    p.same = same
    KTd = p.dram("KT", [128, SEQ], BF16, "ExternalInput")
    KRd = p.dram("KR", [64, SEQ], BF16, "ExternalInput")
    Vd = p.dram("VK", [SEQ, 128], BF16, "ExternalInput")
    KN2 = p.dram("KN2", [128, 128], F32, "ExternalInput")
    QMAX = p.dram("QMAX", [1, MH], F32, "ExternalInput")
    H2 = p.dram("H2", [TOK, D], F32, "ExternalInput")
    w_ukv = p.dram("w_ukv", [KVR, MH * 256], F32, "ExternalInput")
    w_o = p.dram("w_o", [MH * 128, D], F32, "ExternalInput")
    g_ffn = p.dram("g_ffn", [1, D], F32, "ExternalInput")
    w_r = p.dram("w_r", [D, 16], F32, "ExternalInput")
    cf_d = p.dram("cf", [128, 5, 128], F32, "ExternalInput")
    cb_d = p.dram("cb", [128, 128], BF16, "ExternalInput")
    H3 = p.dram("H3", [TOK, D], F32, "ExternalOutput")
    HN3 = p.dram("HN3", [TOK, D], BF16, "ExternalOutput")
    AFF = p.dram("AFF", [TOK, 16], F32, "ExternalOutput")
    OTD = p.dram("OTD", [MH, 128, TOK], BF16, "Internal")

    KT = p.sb("KTs", [128, SEQ], BF16)
    KR2 = p.sb("KR2", [128, SEQ], BF16)
    Vt = p.sb("Vt", [128, NKT, 128], BF16)
    Wo = p.sb("Wo", [128, MH, D], BF16)
    Wuv = p.sb("Wuv", [128, MH, 128], BF16)
    wr = p.sb("wr", [128, 8, 16], F32)
    cf = p.sb("cfs", [128, 5, 128], F32)
    cb = p.sb("cbs", [128, 128], BF16)
    gf = p.sb("gf", [128, D], F32)
    onesf = p.sb("onesf", [128, 128], F32)
    kn = p.sb("kn", [128, 128], F32)
    km = p.sb("km", [128, 1], F32)
    km1 = p.sb("km1", [1, 1], F32)
    qmx = p.sb("qmx", [1, MH], F32)
    negm = p.sb("negm", [128, MH], F32)
    QW = 1024
    sbk = [p.ps("sbk%d" % i, [128, QW], F32) for i in range(2)]
    obk = p.ps("obk", [128, QW], F32)
    ebk = [p.ps("ebk%d" % i, [128, 512], F32) for i in range(2)]
    mi = [0]

    def bank():
        b = ebk[mi[0] % 2]
        mi[0] += 1
        return b

    p.dma("sp", cf[:], cf_d[:])
    p.dma("sp", cb[:], cb_d[:])
    p.dma("sp", kn[:], KN2[:, :])
    p.dma("sp", qmx[:], QMAX[:, :])
    p.dma("sp", gf[:], g_ffn.v(g_ffn.t[0:1, :].broadcast_to([128, D])))
    p.dma("sp", wr[:], w_r.v(w_r.t.rearrange("(c p) e -> p c e", p=128)))
    for q in range(8):
        cs_ = slice(q * 2048, (q + 1) * 2048)
        p.dma("sp", KT.sub(q, (slice(None), cs_)), V(KTd.t[:, cs_], ("KTd", q)))
    for half in range(2):
        for q in range(8):
            cs_ = slice(q * 2048, (q + 1) * 2048)
            p.dma("sp", KR2.sub((half, q), (slice(half * 64, (half + 1) * 64), cs_)), V(KRd.t[:, cs_], ("KRd", half, q)))
    for q in range(8):
        p.dma("sp", Vt.sub(q, (slice(None), slice(q * 16, (q + 1) * 16), slice(None))),
              V(Vd.t[q * 2048:(q + 1) * 2048, :].rearrange("(k p) r -> p k r", p=128), ("Vd", q)))
    p.dma("pool", Wuv[:], w_ukv.v(w_ukv.t.rearrange("r (h c) -> r h c", h=MH)[:, :, 128:256]))
    for h in range(MH):
        p.dma("pool", Wo.sub(h, (slice(None), h, slice(None))), V(w_o.t[h * 128:(h + 1) * 128, :], ("w_o", h)))
    p.memset("dve", onesf[:], 1.0)
    p.reduce("dve", km[:], kn[:], ALU.max)
    bt = bank()
    p.transpose(bt[0:1, 0:128], km[:, 0:1], cf[:, 4, :])
    p.reduce("dve", km1[:], bt[0:1, 0:128], ALU.max)
    p.ts("dve", qmx[:], qmx[:], km1[0:1, 0:1], None, ALU.mult)
    p.act(qmx[:], qmx[:], AF.Sqrt)
    p.ts("dve", qmx[:], qmx[:], -1.0, None, ALU.mult)
    bb = bank()
    p.matmul(bb[:, 0:MH], onesf[0:1, :], qmx[:])
    p.copy("act", negm[:], bb[:, 0:MH])

    p.push_scope()
    ql = [p.sb("ql%d" % i, [128, TOK], BF16) for i in range(2)]
    qr = [p.sb("qr%d" % i, [128, TOK], BF16) for i in range(2)]
    PT = [p.sb("PT%d" % i, [128, QW], BF16) for i in range(3)]
    accD = [p.sb("accD%d" % i, [128, QW], F32) for i in range(1)]
    accP = [p.sb("accP%d" % i, [128, QW], F32) for i in range(1)]
    rl = p.sb("rl", [128, QW], F32)
    OTn = p.sb("OTn", [128, QW], BF16)
    oTs = [p.sb("oTs%d" % i, [128, QW], BF16) for i in range(2)]
    NH = MH if nheads is None else nheads
    it = 0
    for h in range(NH):
        Ql, Qr = ql[h % 2], qr[h % 2]
        p.dma("sp", Ql[:], QL.v(QL.t[h]))
        p.dma("sp", Qr[0:64, :], QR.v(QR.t[h]))
        p.dma("sp", Qr[64:128, :], QR.v(QR.t[h]))
        for qg in range(TOK // QW):
            q0 = qg * QW
            bo = obk
            AD, AP_ = accD[0], accP[0]
            it += 1
            first = {"dve": True, "pool": True}

            def s_mm(kt):
                bs = sbk[kt % 2]
                ktile = KT.sub(kt // 16, (slice(None), slice(kt * 128, (kt + 1) * 128)))
                rtiles = [KR2.sub((hf, kt // 16), (slice(hf * 64, (hf + 1) * 64), slice(kt * 128, (kt + 1) * 128)))
                          for hf in range(2)]
                for hh in range(QW // 512):
                    p.matmul(bs[:, hh * 512:(hh + 1) * 512], ktile, Ql[:, q0 + hh * 512:q0 + (hh + 1) * 512],
                             start=True, stop=False)
                if kt >= 1:
                    p.wait("pe", [PT[(kt - 1) % 3].name])
                for hh in range(QW // 512):
                    p.matmul(bs[:, hh * 512:(hh + 1) * 512], rtiles[hh],
                             Qr[hh * 64:(hh + 1) * 64, q0 + hh * 512:q0 + (hh + 1) * 512], start=False, stop=True)

            s_mm(0)
            for kt in range(NKT):
                if kt + 1 < NKT:
                    s_mm(kt + 1)
                P_ = PT[kt % 3]
                p.act(P_[:], sbk[kt % 2][:], AF.Exp, bias=negm[:, h:h + 1])
                vt = Vt.sub(kt // 16, (slice(None), kt, slice(None)))
                for hh in range(QW // 512):
                    p.matmul(bo[:, hh * 512:(hh + 1) * 512], vt, P_[:, hh * 512:(hh + 1) * 512],
                             start=(kt == 0), stop=(kt == NKT - 1))
                eng = "pool" if kt % 3 == 2 else "dve"
                A = AP_ if eng == "pool" else AD
                if first[eng]:
                    p.copy(eng, A[:], P_[:])
                    first[eng] = False
                else:
                    p.tt(eng, A[:], A[:], P_[:], ALU.add)
            p.tt("dve", AD[:], AD[:], AP_[:], ALU.add)
            for hh in range(QW // 512):
                hs = slice(hh * 512, (hh + 1) * 512)
                bl = ebk[hh]
                p.matmul(bl[:], onesf[:], AD[:, hs])
                p.recip(rl[:, hs], bl[:])
                p.tt("dve", OTn[:, hs], bo[:, hs], rl[:, hs], ALU.mult)
            oT = oTs[it % 2]
            for hh in range(QW // 512):
                hs = slice(hh * 512, (hh + 1) * 512)
                bv = ebk[hh]
                p.matmul(bv[:], Wuv[:, h, :], OTn[:, hs])
                p.copy("act", oT[:, hs], bv[:])
            p.dma("pool", V(OTD.t[h, :, q0:q0 + QW], ("OTD", h, qg)), oT[:])
    otd_keys = [("OTD", h, qg) for h in range(NH) for qg in range(TOK // QW)]
    p.pop_scope()
    oTt = [p.sb("oTt%d" % i, [128, MH, 128], BF16) for i in range(2)]
    h2t = [p.sb("h2t%d" % i, [128, D], F32) for i in range(2)]
    h3 = p.sb("h3", [128, D], F32)
    fb = ffn_prep_bufs(p)

    for j in range(NT):
        rows = slice(j * 128, (j + 1) * 128)
        b = j % 2
        p.dma("sp", h2t[b][:], H2[rows, :])
        p.dma("sp", oTt[b][:], V(OTD.t[:, :, rows].rearrange("h d t -> d h t"), ("OTD", "rd", j)),
              reads=[k_ for k_ in otd_keys if k_[2] == j // (QW // 128)])
        for half in range(2):
            bm = bank()
            for h in range(MH):
                p.matmul(bm[:], oTt[b][:, h, :], Wo.sub(h, (slice(None), h, slice(half * 512, (half + 1) * 512))),
                         start=(h == 0), stop=(h == MH - 1))
            p.tt("dve", h3[:, half * 512:(half + 1) * 512], bm[:], h2t[b][:, half * 512:(half + 1) * 512], ALU.add)
        p.dma("pool", H3[rows, :], h3[:])
        ffn_prep_tile(p, h3[:], gf, wr, cf, fb, HN3, AFF, rows, bank)
    return p.finish()


def build_L7():
    nc = bass.Bass("TRN2", target_bir_lowering=False)
    p = Prog(nc)
    H3 = p.dram("H3", [TOK, D], F32, "ExternalInput")
    DS = p.dram("DS", [NCORES, TOK, D], F32, "ExternalInput")
    g_fin = p.dram("g_fin", [1, D], F32, "ExternalInput")
    OUT = p.dram("OUT", [TOK, D], F32, "ExternalOutput")
    gm = p.sb("gm", [128, D], F32)
    ht = [p.sb("ht%d" % i, [128, D], F32) for i in range(2)]
    dt_ = [p.sb("dt%d" % i, [128, D], F32) for i in range(3)]
    sq = p.sb("sq", [128, D], F32)
    ss = p.sb("ss", [128, 1], F32)
    rstd = p.sb("rstd", [128, 1], F32)
    ot = [p.sb("ot%d" % i, [128, D], F32) for i in range(2)]
    p.dma("sp", gm[:], g_fin.v(g_fin.t[0:1, :].broadcast_to([128, D])))
    for j in range(NT):
        rows = slice(j * 128, (j + 1) * 128)
        Ht = ht[j % 2]
        p.dma("sp", Ht[:], H3[rows, :])
        for c in range(NCORES):
            Dt = dt_[c % 3]
            p.dma("sp", Dt[:], DS.v(DS.t[c, rows, :]))
            p.tt("dve", Ht[:], Ht[:], Dt[:], ALU.add)
        rmsnorm_tile(p, Ht[:], gm[:], ot[j % 2][:], sq[:], ss[:], rstd[:])
        p.dma("pool", OUT[rows, :], ot[j % 2][:])
    return p.finish()


def run(nc, in_maps):
    res = run_bass_kernel_spmd(nc, in_maps, core_ids=list(range(NCORES)))
    return res.results


_CACHE = {}


def _prog(name, builder):
    if name not in _CACHE:
        _CACHE[name] = builder()
    return _CACHE[name]


def _c(a):
    return np.ascontiguousarray(a)


def stage_L1(inp):
    cf, cb = const_tables()
    x = inp["x"][0]
    wg = _c(np.stack([inp["gla_w_gate_up_f"][0], inp["gla_w_gate_up_b"][0]]))
    bg = _c(np.stack([inp["gla_b_gate_f"][0], inp["gla_b_gate_b"][0]]))
    maps = [dict(x=_c(x[c * TOK:(c + 1) * TOK]), g_mix=_c(inp["mix_norm"][0:1]), w_in=_c(inp["gla_w_in"][0]),
                 wg=wg, bg=bg, cf=cf, cb=cb) for c in range(NCORES)]
    return run(_prog("L1", build_L1), maps)


def stage_L2(inp, r1):
    cf, cb = const_tables()
    x = inp["x"][0]
    maps = []
    for c in range(NCORES):
        BP = np.zeros((2, 7, GH, 128, GDV), np.float32)
        APD = np.zeros((2, 7, 128, NT * GH), np.float32)
        for i in range(7):
            cf_ = c - 7 + i
            if cf_ >= 0:
                BP[0, i] = r1[cf_]["BE"][0]
                APD[0, i] = r1[cf_]["AA"][0]
            cb_ = c + 7 - i
            if cb_ <= NCORES - 1:
                BP[1, i] = r1[cb_]["BE"][1]
                APD[1, i] = r1[cb_]["AA"][1]
        maps.append(dict(x=_c(x[c * TOK:(c + 1) * TOK]), OL=r1[c]["OL"], QP=r1[c]["QP"], SR=r1[c]["SR"],
                         AA=r1[c]["AA"], BP=BP, APD=APD, g_head=_c(inp["gla_head_norm"][0:1]),
                         w_out=_c(inp["gla_w_out"][0]), g_ffn=_c(inp["ffn_norm"][0:1]),
                         w_r=_c(inp["moe_w_router"][0]), cf=cf, cb=cb))
    return run(_prog("L2", build_L2), maps)


def stage_L3(inp, layer, aff_full, hn_full):
    cf, cb = const_tables(6)
    tokid = np.arange(SEQ, dtype=np.int32).reshape(128, 128)
    maps = []
    for c in range(NCORES):
        affe = _c(aff_full[:, 2 * c:2 * c + 2].T.reshape(2, 128, 128))
        maps.append(dict(AFFE=affe, HN=hn_full, wg=_c(inp["moe_w_gate"][layer, 2 * c:2 * c + 2]),
                         wu=_c(inp["moe_w_up"][layer, 2 * c:2 * c + 2]), wd=_c(inp["moe_w_down"][layer, 2 * c:2 * c + 2]),
                         cf=cf, cb=cb, tokid=tokid))
    return run(_prog("L3", build_L3), maps)


def stage_L4(inp, h_prev, deltas):
    cf, cb = const_tables()
    rc = rope_consts()
    maps = []
    for c in range(NCORES):
        DS = _c(np.stack([d[c * TOK:(c + 1) * TOK] for d in deltas]))
        maps.append(dict(H1=h_prev[c], DS=DS, pos=_c(inp["positions"][0:1, c * TOK:(c + 1) * TOK]),
                         g_mix=_c(inp["mix_norm"][1:2]), w_m=_c(inp["mla_w_in"][0]), g_q=_c(inp["mla_q_norm"][0:1]),
                         g_kv=_c(inp["mla_kv_norm"][0:1]), w_uq=_c(inp["mla_w_uq"][0]), w_ukv=_c(inp["mla_w_ukv"][0]),
                         rc=rc, cf=cf, cb=cb))
    return run(_prog("L4", build_L4), maps)


def stage_L5(inp, r4):
    cf, cb = const_tables()
    KT = _c(np.concatenate([r4[c]["CKVT"] for c in range(NCORES)], axis=1))
    KR = _c(np.concatenate([r4[c]["KRT"] for c in range(NCORES)], axis=1))
    VK = _c(np.concatenate([r4[c]["CKV"] for c in range(NCORES)], axis=0))
    KN2 = _c(np.concatenate([r4[c]["KN2"] for c in range(NCORES)], axis=0).reshape(128, 128))
    maps = []
    for c in range(NCORES):
        maps.append(dict(QL=r4[c]["QL"], QR=r4[c]["QR"], KT=KT, KR=KR, VK=VK, KN2=KN2, QMAX=r4[c]["QMAX"],
                         H2=r4[c]["H2"], w_ukv=_c(inp["mla_w_ukv"][0]), w_o=_c(inp["mla_w_out"][0]),
                         g_ffn=_c(inp["ffn_norm"][1:2]), w_r=_c(inp["moe_w_router"][1]), cf=cf, cb=cb))
    return run(_prog("L5", build_L5), maps)


def stage_L7(inp, h_prev, deltas):
    maps = []
    for c in range(NCORES):
        DS = _c(np.stack([d[c * TOK:(c + 1) * TOK] for d in deltas]))
        maps.append(dict(H3=h_prev[c], DS=DS, g_fin=_c(inp["final_norm"].reshape(1, D))))
    return run(_prog("L7", build_L7), maps)


def kernel(**inputs):
    inp = {k: np.asarray(v) for k, v in inputs.items()}
    r1 = stage_L1(inp)
    r2 = stage_L2(inp, r1)
    aff0 = _c(np.concatenate([r2[c]["AFF"] for c in range(NCORES)], axis=0))
    hn1 = _c(np.concatenate([r2[c]["HN1"] for c in range(NCORES)], axis=0))
    r3 = stage_L3(inp, 0, aff0, hn1)
    r4 = stage_L4(inp, [r2[c]["H1"] for c in range(NCORES)], [r3[c]["DELTA"] for c in range(NCORES)])
    del r3
    r5 = stage_L5(inp, r4)
    aff1 = _c(np.concatenate([r5[c]["AFF"] for c in range(NCORES)], axis=0))
    hn3 = _c(np.concatenate([r5[c]["HN3"] for c in range(NCORES)], axis=0))
    r6 = stage_L3(inp, 1, aff1, hn3)
    r7 = stage_L7(inp, [r5[c]["H3"] for c in range(NCORES)], [r6[c]["DELTA"] for c in range(NCORES)])
    out = np.concatenate([r7[c]["OUT"] for c in range(NCORES)], axis=0).astype(np.float32)
    return out.reshape(1, SEQ, D)
```

```python
import contextlib
import numpy as np
import ml_dtypes
import concourse.bass as bass
import concourse.mybir as mybir
from concourse.bass_utils import run_bass_kernel_spmd

F32 = mybir.dt.float32
BF16 = mybir.dt.bfloat16
I32 = mybir.dt.int32
AF = mybir.ActivationFunctionType
ALU = mybir.AluOpType
AX = mybir.AxisListType

NCORES = 8
SEQ = 16384
TOK = SEQ // NCORES
NT = TOK // 128
D = 1024
RMS_EPS = 1e-6


class V:
    __slots__ = ("ap", "key")

    def __init__(self, ap, key):
        self.ap = ap
        self.key = key


class Buf:
    def __init__(self, t, name):
        self.t = t
        self.name = name

    def __getitem__(self, idx):
        return V(self.t[idx], self.name)

    def sub(self, s, idx):
        return V(self.t[idx], (self.name, s))

    def v(self, ap, s=None):
        return V(ap, self.name if s is None else (self.name, s))


class Prog:
    ENGS = ("pe", "dve", "act", "pool", "sp")
    NDMA = {"sp": 10, "pool": 8, "act": 4}

    def __init__(self, nc, same_engine_sync=True):
        self.nc = nc
        self.same = same_engine_sync
        self.stack = contextlib.ExitStack()
        self.lists = {e: [] for e in self.ENGS}
        self.cnt = {e: 0 for e in self.ENGS}
        self.seen = {e: {} for e in self.ENGS}
        self.last_w = {}
        self.readers = {}
        self.sem = {}
        self.dma_cnt = {}
        self.dma_rr = {q: 0 for q in self.NDMA}
        for e in ("pe", "dve", "act", "pool"):
            self.sem[("c", e)] = self.stack.enter_context(nc.semaphore("c_" + e))
        for q, n in self.NDMA.items():
            for k in range(n):
                sk = ("d", q, k)
                self.sem[sk] = self.stack.enter_context(nc.semaphore("d_%s%d" % (q, k)))
                self.dma_cnt[sk] = 0
        self.psum = set()
        self.bregs = {}
        self.same_dist = 2

    def sb(self, name, shape, dt):
        t = self.stack.enter_context(self.nc.sbuf_tensor(name, list(shape), dt))
        return Buf(t, name)

    def ps(self, name, shape, dt):
        t = self.stack.enter_context(self.nc.psum_tensor(name, list(shape), dt))
        self.psum.add(name)
        return Buf(t, name)

    def dram(self, name, shape, dt, kind):
        t = self.nc.dram_tensor(name, list(shape), dt, kind=kind)
        return Buf(t.ap(), name)

    def emit(self, eng, fn, reads, writes, dma=False):
        deps = {}

        def need(tok):
            if tok is None:
                return
            sk, val = tok
            if deps.get(sk, 0) < val:
                deps[sk] = val

        for r in reads:
            need(self.last_w.get(r))
            if r in self.psum:
                for sk, val in self.readers.get(r, {}).items():
                    if sk != ("c", eng):
                        need((sk, val))
        for w in writes:
            need(self.last_w.get(w))
            for sk, val in self.readers.get(w, {}).items():
                need((sk, val))
        if dma:
            k = self.dma_rr[eng]
            self.dma_rr[eng] = (k + 1) % self.NDMA[eng]
            sk = ("d", eng, k)
            if self.dma_cnt[sk] > 0:
                need((sk, self.dma_cnt[sk]))
            self.dma_cnt[sk] += 16
            tok = (sk, self.dma_cnt[sk])
        else:
            self.cnt[eng] += 1
            tok = (("c", eng), self.cnt[eng])
        waits = []
        for sk, val in deps.items():
            if sk == ("c", eng) and (eng == "pe" or not self.same):
                continue
            if sk == ("c", eng) and tok[1] - val > self.same_dist:
                continue
            if self.seen[eng].get(sk, 0) >= val:
                continue
            self.seen[eng][sk] = val
            waits.append((sk, val))
        self.lists[eng].append((waits, fn, tok))
        for r in reads:
            d = self.readers.setdefault(r, {})
            if d.get(tok[0], 0) < tok[1]:
                d[tok[0]] = tok[1]
        for w in writes:
            self.last_w[w] = tok
            self.readers[w] = {}

    @staticmethod
    def _keys(*vs):
        return [v.key for v in vs if isinstance(v, V)]

    @staticmethod
    def _ap(v):
        return v.ap if isinstance(v, V) else v

    def dma(self, q, out, in_, reads=None, writes=None, **kw):
        o, i = out.ap, in_.ap
        self.emit(q, lambda e: e.dma_start(out=o, in_=i, **kw), [in_.key] + (reads or []),
                  [out.key] if writes is None else writes, dma=True)

    def _breg(self, e, val):
        if val not in self.bregs:
            r = e.alloc_register("bchk%d" % val)
            e.reg_mov(r, val)
            self.bregs[val] = r
        return self.bregs[val]

    def gather(self, out, src, idx, nrows):
        o, s, ix = out.ap, src.ap, idx.ap
        self.emit("pool", lambda e: e.indirect_dma_start(
            out=o, out_offset=None, in_=s,
            in_offset=bass.IndirectOffsetOnAxis(ap=ix, axis=0),
            bounds_check=self._breg(e, nrows - 1), oob_is_err=False),
            [src.key, idx.key], [out.key], dma=True)

    def scatter(self, dst, src, idx, nrows, accum=False, reads=None, writes=None):
        d, s, ix = dst.ap, src.ap, idx.ap
        kw = {"compute_op": ALU.add} if accum else {}
        self.emit("pool", lambda e: e.indirect_dma_start(
            out=d, out_offset=bass.IndirectOffsetOnAxis(ap=ix, axis=0), in_=s, in_offset=None,
            bounds_check=self._breg(e, nrows - 1), oob_is_err=False, **kw),
            [src.key, idx.key] + ([dst.key] if accum else []) + (reads or []),
            [dst.key] if writes is None else writes, dma=True)

    def matmul(self, out, lhsT, rhs, start=True, stop=True):
        o, l, r = out.ap, lhsT.ap, rhs.ap
        rd = [lhsT.key, rhs.key] + ([] if start else [out.key])
        self.emit("pe", lambda e: e.matmul(o, l, r, start=start, stop=stop), rd, [out.key])

    def transpose(self, out, in_, ident):
        o, i, d = out.ap, in_.ap, ident.ap
        self.emit("pe", lambda e: e.transpose(o, i, d), [in_.key, ident.key], [out.key])

    def act(self, out, in_, func, bias=0.0, scale=1.0, accum_out=None, eng="act"):
        o, i = out.ap, in_.ap
        b, s = self._ap(bias), self._ap(scale)
        kw = {}
        if accum_out is not None:
            kw["accum_out"] = accum_out.ap
        self.emit(eng, lambda e: e.activation(o, i, func, bias=b, scale=s, **kw),
                  self._keys(in_, bias, scale), self._keys(out, accum_out))

    def copy(self, eng, out, in_):
        o, i = out.ap, in_.ap
        if eng == "act":
            self.emit(eng, lambda e: e.copy(o, i), [in_.key], [out.key])
        else:
            self.emit(eng, lambda e: e.tensor_copy(o, i), [in_.key], [out.key])

    def tt(self, eng, out, in0, in1, op):
        o, a, b = out.ap, in0.ap, in1.ap
        self.emit(eng, lambda e: e.tensor_tensor(o, a, b, op), [in0.key, in1.key], [out.key])

    def ts(self, eng, out, in0, s1, s2, op0, op1=None, accum_out=None):
        o, a = out.ap, in0.ap
        x1, x2 = self._ap(s1), self._ap(s2)
        kw = {}
        if op1 is not None:
            kw["op1"] = op1
        if accum_out is not None:
            kw["accum_out"] = accum_out.ap
        self.emit(eng, lambda e: e.tensor_scalar(o, a, x1, x2, op0, **kw),
                  self._keys(in0, s1, s2), self._keys(out, accum_out))

    def stt(self, eng, out, in0, scalar, in1, op0, op1):
        o, a, b = out.ap, in0.ap, in1.ap
        s = self._ap(scalar)
        self.emit(eng, lambda e: e.scalar_tensor_tensor(o, a, s, b, op0, op1),
                  self._keys(in0, scalar, in1), [out.key])

    def reduce(self, eng, out, in_, op, axis=AX.X):
        o, i = out.ap, in_.ap
        self.emit(eng, lambda e: e.tensor_reduce(o, i, axis, op), [in_.key], [out.key])

    def recip(self, out, in_):
        o, i = out.ap, in_.ap
        self.emit("dve", lambda e: e.reciprocal(o, i), [in_.key], [out.key])

    def memset(self, eng, out, val):
        o = out.ap
        self.emit(eng, lambda e: e.memset(o, val), [], [out.key])

    def wait(self, eng, keys):
        waits = []
        for k_ in keys:
            tok = self.last_w.get(k_)
            if tok is None:
                continue
            sk, val = tok
            if sk == ("c", eng) or self.seen[eng].get(sk, 0) >= val:
                continue
            self.seen[eng][sk] = val
            waits.append((sk, val))
        if waits:
            self.lists[eng].append((waits, None, None))

    def barrier(self):
        allv = {("c", e): self.cnt[e] for e in ("pe", "dve", "act", "pool") if self.cnt[e] > 0}
        allv.update({sk: v for sk, v in self.dma_cnt.items() if v > 0})
        for eng in self.ENGS:
            waits = []
            for sk, val in allv.items():
                if sk == ("c", eng) or self.seen[eng].get(sk, 0) >= val:
                    continue
                self.seen[eng][sk] = val
                waits.append((sk, val))
            if waits:
                self.lists[eng].append((waits, None, None))

    def push_scope(self):
        self._outer = self.stack
        self.stack = contextlib.ExitStack()

    def pop_scope(self):
        self.barrier()
        self.stack.close()
        self.stack = self._outer

    def finish(self):
        nc = self.nc
        prog = self

        def mk(name):
            def body(e):
                for waits, fn, tok in prog.lists[name]:
                    if fn is None:
                        for sk, val in waits:
                            e.wait_ge(prog.sem[sk], val)
                        continue
                    for sk, val in waits[:-1]:
                        e.wait_ge(prog.sem[sk], val)
                    ins = fn(e)
                    if waits:
                        ins._wait_ge(prog.sem[waits[-1][0]], waits[-1][1])
                    ins.then_inc(prog.sem[tok[0]], 16 if tok[0][0] == "d" else 1)
                if name == "sp":
                    for sk, val in prog.dma_cnt.items():
                        if val > 0:
                            e.wait_ge(prog.sem[sk], val)
            return body

        with nc.Block() as block:
            block.tensor(mk("pe"))
            block.vector(mk("dve"))
            block.scalar(mk("act"))
            block.gpsimd(mk("pool"))
            block.sync(mk("sp"))
        self.stack.close()
        return nc


def bcast_rows(ap1d_or_row, nparts):
    return ap1d_or_row.broadcast(0, nparts) if hasattr(ap1d_or_row, "broadcast") else ap1d_or_row


def const_tables(ncf=5):
    i = np.arange(128)
    ui = (i[:, None] <= i[None, :]).astype(np.float32)
    li = (i[:, None] >= i[None, :]).astype(np.float32)
    us = (i[:, None] < i[None, :]).astype(np.float32)
    ls = (i[:, None] > i[None, :]).astype(np.float32)
    ident = np.eye(128, dtype=np.float32)
    iota = np.broadcast_to(np.arange(128, dtype=np.float32)[None, :], (128, 128))
    cf = np.stack([ui, li, us, ls, ident, iota], axis=1)
    cb = np.eye(128, dtype=np.float32).astype(ml_dtypes.bfloat16)
    return np.ascontiguousarray(cf[:, :ncf]), cb


GH = 4
GDK = 128
GDV = 256
GIN = 3104


def rmsnorm_tile(p, xt, gbc, hn_out, sq, ss, rstd, width=D):
    p.act(sq, xt, AF.Square, accum_out=ss)
    p.ts("dve", rstd, ss, 1.0 / width, RMS_EPS, ALU.mult, ALU.add)
    p.act(rstd, rstd, AF.Sqrt)
    p.recip(rstd, rstd)
    p.stt("dve", hn_out, xt, rstd, gbc, ALU.mult, ALU.mult)


def build_L1():
    nc = bass.Bass("TRN2", target_bir_lowering=False)
    p = Prog(nc)
    x = p.dram("x", [TOK, D], F32, "ExternalInput")
    g_mix = p.dram("g_mix", [1, D], F32, "ExternalInput")
    w_in = p.dram("w_in", [D, GIN], F32, "ExternalInput")
    wg = p.dram("wg", [2, 16, 512], F32, "ExternalInput")
    bg = p.dram("bg", [2, 512], F32, "ExternalInput")
    cf_d = p.dram("cf", [128, 5, 128], F32, "ExternalInput")
    cb_d = p.dram("cb", [128, 128], BF16, "ExternalInput")
    QP = p.dram("QP", [2, 128, GH, TOK], BF16, "ExternalOutput")
    KP = p.dram("KP", [2, 128, GH, TOK], BF16, "Internal")
    K2 = p.dram("K2", [2, TOK, 512], BF16, "Internal")
    VT = p.dram("VT", [TOK, 1024], BF16, "Internal")
    SR = p.dram("SR", [TOK, 1024], F32, "ExternalOutput")
    OL = p.dram("OL", [TOK, 1024], F32, "ExternalOutput")
    AA = p.dram("AA", [2, 128, NT * GH], F32, "ExternalOutput")
    BE = p.dram("BE", [2, GH, 128, GDV], F32, "ExternalOutput")

    W = p.sb("W", [128, 8, GIN], BF16)
    cf = p.sb("cfs", [128, 5, 128], F32)
    cb = p.sb("cbs", [128, 128], BF16)
    gbc = p.sb("gbc", [128, D], F32)
    bgs = p.sb("bgs", [128, 2, 512], F32)
    wgs = p.sb("wgs", [16, 2, 512], BF16)
    xt = [p.sb("xt%d" % i, [128, D], F32) for i in range(2)]
    sq = p.sb("sq", [128, D], F32)
    ss = p.sb("ss", [128, 1], F32)
    rstd = p.sb("rstd", [128, 1], F32)
    hn = p.sb("hn", [128, D], BF16)
    hnT = p.sb("hnT", [128, D], BF16)
    gdT = p.sb("gdT", [16, 256], BF16)
    zb = p.sb("zb", [128, 512], F32)
    spl = [p.sb("spl%d" % i, [128, 512], F32) for i in range(2)]
    EqT = p.sb("EqT", [128, 512], F32)
    EkT = p.sb("EkT", [128, 512], F32)
    Ek2 = p.sb("Ek2", [128, 512], F32)
    a_all = p.sb("a_all", [128, 2, NT * GH], F32)
    c_all = p.sb("c_all", [128, 2, NT * GH], F32)
    qp = [p.sb("qp%d" % i, [128, 512], BF16) for i in range(2)]
    kp = [p.sb("kp%d" % i, [128, 512], BF16) for i in range(2)]
    k2 = [p.sb("k2%d" % i, [128, 512], BF16) for i in range(2)]
    vb = p.sb("vb", [128, 1024], BF16)
    srt = p.sb("srt", [128, 1024], F32)
    S = p.sb("S", [128, GH, GDV], F32)
    Sb = p.sb("Sb", [128, GH, GDV], BF16)
    s_qp = [p.sb("s_qp%d" % i, [128, 512], BF16) for i in range(2)]
    s_kp = [p.sb("s_kp%d" % i, [128, 512], BF16) for i in range(2)]
    s_k2 = [p.sb("s_k2%d" % i, [128, 512], BF16) for i in range(2)]
    s_v = [p.sb("s_v%d" % i, [128, 1024], BF16) for i in range(2)]
    AT = [p.sb("AT%d" % i, [128, 128], BF16) for i in range(2)]
    ot = [p.sb("ot%d" % i, [128, 1024], F32) for i in range(2)]
    of = [p.sb("of%d" % i, [128, 1024], F32) for i in range(2)]
    banks = [p.ps("bk%d" % i, [128, 512], F32) for i in range(7)]
    pT = p.ps("pT", [128, 1024], BF16)
    bi = [0]

    rot = [3, 4]

    def bank():
        b = banks[rot[0] + bi[0] % rot[1]]
        bi[0] += 1
        return b

    p.dma("sp", cf[:], cf_d[:])
    p.dma("sp", cb[:], cb_d[:])
    p.dma("sp", gbc[:], g_mix.v(g_mix.t[0:1, :].broadcast_to([128, D])))
    p.dma("sp", bgs[:, 0, :], bg.v(bg.t[0:1, :].broadcast_to([128, 512])))
    p.dma("sp", bgs[:, 1, :], bg.v(bg.t[1:2, :].broadcast_to([128, 512])))
    for d_ in range(2):
        p.dma("pool", wgs.sub(d_, (slice(None), d_, slice(None))), wg[d_])
    for dc in range(8):
        p.dma("pool", W.sub(dc, (slice(None), dc, slice(None))), w_in[dc * 128:(dc + 1) * 128, :])
    Wk = lambda dc, lo, hi: W.sub(dc, (slice(None), dc, slice(lo, hi)))
    UI, LI, US, LS = (cf[:, i, :] for i in range(4))
    scale_q = float(GDK) ** -0.5

    def scan_init(d_):
        p.memset("dve", S[:], 0.0)
        p.memset("dve", Sb[:], 0.0)

    def scan_step(d_, j, it, sq_, sk_, sk2_, sv_):
        b = it % 2
        mask = UI if d_ == 0 else LS
        O = ot[b]
        for h in range(GH):
            hs = slice(h * 128, (h + 1) * 128)
            vs = slice(h * GDV, (h + 1) * GDV)
            ba = bank()
            p.matmul(ba[:, 0:128], sk_[:, hs], sq_[:, hs])
            A = AT[h % 2]
            p.tt("dve", A[:], ba[:, 0:128], mask, ALU.mult)
            bo = bank()
            p.matmul(bo[:, 0:GDV], A[:], sv_[:, vs], start=True, stop=False)
            p.matmul(bo[:, 0:GDV], sq_[:, hs], Sb[:, h, :], start=False, stop=True)
            bs = bank()
            p.matmul(bs[:, 0:GDV], sk2_[:, hs], sv_[:, vs])
            if d_ == 0:
                p.copy("act", O[:, vs], bo[:, 0:GDV])
            else:
                p.tt("dve", O[:, vs], bo[:, 0:GDV], of[b][:, vs], ALU.add)
            p.stt("dve", S[:, h, :], S[:, h, :], a_all.v(a_all.t[:, d_, j * GH + h:j * GH + h + 1]),
                  bs[:, 0:GDV], ALU.mult, ALU.add)
            p.copy("act", Sb[:, h, :], S[:, h, :])
        p.dma("pool", OL[j * 128:(j + 1) * 128, :], O[:])

    def scan_fin(d_):
        for h in range(GH):
            p.dma("pool", BE.v(BE.t[d_, h]), S[:, h, :])

    scan_init(0)
    for j in range(NT):
        X = xt[j % 2]
        p.dma("sp", X[:], x[j * 128:(j + 1) * 128, :])
        rmsnorm_tile(p, X[:], gbc[:], hn[:], sq[:], ss[:], rstd[:])
        for dc in range(8):
            p.transpose(pT[:, dc * 128:(dc + 1) * 128], hn[:, dc * 128:(dc + 1) * 128], cb[:])
        p.copy("act", hnT[:], pT[:])
        hT = lambda dc: hnT[:, dc * 128:(dc + 1) * 128]
        bG = bank()
        for d_ in range(2):
            for dc in range(8):
                p.matmul(bG[0:16, d_ * 128:(d_ + 1) * 128], Wk(dc, 3072 + 16 * d_, 3088 + 16 * d_), hT(dc),
                         start=(dc == 0), stop=(dc == 7))
        p.copy("act", gdT[:], bG[0:16, 0:256])
        bq, bk, bkt = banks[0], banks[1], banks[2]
        for n in range(4):
            for dc in range(8):
                p.matmul(bq[:, n * 128:(n + 1) * 128], Wk(dc, n * 128, (n + 1) * 128), hT(dc),
                         start=(dc == 0), stop=(dc == 7))
        for n in range(4):
            for dc in range(8):
                p.matmul(bk[:, n * 128:(n + 1) * 128], Wk(dc, 512 + n * 128, 512 + (n + 1) * 128), hT(dc),
                         start=(dc == 0), stop=(dc == 7))
        for dc in range(8):
            p.matmul(bkt[:], hT(dc), Wk(dc, 512, 1024), start=(dc == 0), stop=(dc == 7))
        for d_ in range(2):
            bz = bank()
            p.matmul(bz[:], gdT[:, d_ * 128:(d_ + 1) * 128], wgs.sub(d_, (slice(None), d_, slice(None))))
            p.tt("dve", zb[:], bz[:], bgs[:, d_, :], ALU.add)
            p.act(zb[:], zb[:], AF.Exp, scale=-1.0)
            sp_ = spl[d_]
            p.act(sp_[:], zb[:], AF.Ln, bias=1.0)
            bc, bd = bank(), bank()
            tri = UI if d_ == 0 else LI
            for h in range(GH):
                p.matmul(bc[:, h * 128:(h + 1) * 128], sp_[:, h * 128:(h + 1) * 128], tri)
            p.matmul(bd[:], LS if d_ == 0 else US, sp_[:])
            p.act(EqT[:], bc[:], AF.Exp, scale=-1.0 / 16)
            p.act(EkT[:], bc[:], AF.Exp, scale=1.0 / 16)
            p.act(Ek2[:], bd[:], AF.Exp, scale=-1.0 / 16)
            last = 127 if d_ == 0 else 0
            p.copy("dve", a_all.v(a_all.t[:, d_, j * GH:(j + 1) * GH]),
                   EqT.v(EqT.t[:].rearrange("p (h t) -> p h t", h=GH)[:, :, last]))
            p.copy("dve", c_all.v(c_all.t[:, d_, j * GH:(j + 1) * GH]),
                   bc.v(bc.t[:].rearrange("p (h t) -> p h t", h=GH)[:, :, last]))
            Q, K, KK = qp[d_], kp[d_], k2[d_]
            p.stt("dve", Q[:], bq[:], scale_q, EqT[:], ALU.mult, ALU.mult)
            p.tt("dve", K[:], bk[:], EkT[:], ALU.mult)
            p.tt("dve", KK[:], bkt[:], Ek2[:], ALU.mult)
            p.dma("pool", QP.v(QP.t[d_, :, :, j * 128:(j + 1) * 128]),
                  Q.v(Q.t[:].rearrange("p (h t) -> p h t", h=GH)))
            p.dma("pool", KP.v(KP.t[d_, :, :, j * 128:(j + 1) * 128]),
                  K.v(K.t[:].rearrange("p (h t) -> p h t", h=GH)))
            p.dma("pool", K2.v(K2.t[d_, j * 128:(j + 1) * 128, :]), KK[:])
        for half in range(2):
            bv = bank()
            for dc in range(8):
                p.matmul(bv[:], hT(dc), Wk(dc, 1024 + half * 512, 1536 + half * 512), start=(dc == 0), stop=(dc == 7))
            p.copy("act", vb[:, half * 512:(half + 1) * 512], bv[:])
        p.dma("pool", VT[j * 128:(j + 1) * 128, :], vb[:])
        scan_step(0, j, j, qp[0], kp[0], k2[0], vb)
        for half in range(2):
            br = bank()
            for dc in range(8):
                p.matmul(br[:], hT(dc), Wk(dc, 2048 + half * 512, 2560 + half * 512), start=(dc == 0), stop=(dc == 7))
            p.act(srt[:, half * 512:(half + 1) * 512], br[:], AF.Silu)
        p.dma("pool", SR[j * 128:(j + 1) * 128, :], srt[:])
    scan_fin(0)
    for d_ in range(2):
        p.dma("pool", AA[d_], c_all[:, d_, :])

    rot[0], rot[1] = 0, 7
    scan_init(1)
    for it, j in enumerate(range(NT - 1, -1, -1)):
        b = it % 2
        sq_, sk_, sk2_, sv_ = s_qp[b], s_kp[b], s_k2[b], s_v[b]
        p.dma("sp", sq_.v(sq_.t[:].rearrange("p (h t) -> p h t", h=GH)), QP.v(QP.t[1, :, :, j * 128:(j + 1) * 128]))
        p.dma("sp", sk_.v(sk_.t[:].rearrange("p (h t) -> p h t", h=GH)), KP.v(KP.t[1, :, :, j * 128:(j + 1) * 128]))
        p.dma("sp", sk2_[:], K2.v(K2.t[1, j * 128:(j + 1) * 128, :]))
        p.dma("sp", sv_[:], VT[j * 128:(j + 1) * 128, :])
        p.dma("sp", of[b][:], OL[j * 128:(j + 1) * 128, :])
        scan_step(1, j, it, sq_, sk_, sk2_, sv_)
    scan_fin(1)
    return p.finish()


def ffn_prep_tile(p, h1, gf, wr, cff, bufs, HN_out, AFF_out, rows, bank):
    sq, ss, rstd, hnf, hnb, hnT, lg, mx, sm = bufs
    rmsnorm_tile(p, h1, gf[:], hnf[:], sq[:], ss[:], rstd[:])
    p.copy("act", hnb[:], hnf[:])
    p.dma("pool", HN_out[rows, :], hnb[:])
    ident_f = cff[:, 4, :]
    for half in range(2):
        bt = bank()
        for q in range(4):
            dc = half * 4 + q
            p.transpose(bt[:, q * 128:(q + 1) * 128], hnf[:, dc * 128:(dc + 1) * 128], ident_f)
        p.copy("act", hnT[:, half * 512:(half + 1) * 512], bt[:])
    bl = bank()
    for dc in range(8):
        p.matmul(bl[:, 0:16], hnT[:, dc * 128:(dc + 1) * 128], wr[:, dc, :], start=(dc == 0), stop=(dc == 7))
    p.reduce("dve", mx[:], bl[:, 0:16], ALU.max)
    p.ts("dve", mx[:], mx[:], -1.0, None, ALU.mult)
    p.act(lg[:], bl[:, 0:16], AF.Exp, bias=mx[:])
    p.reduce("dve", sm[:], lg[:], ALU.add)
    p.recip(sm[:], sm[:])
    p.ts("dve", lg[:], lg[:], sm[:], None, ALU.mult)
    p.dma("pool", AFF_out[rows, :], lg[:])


def ffn_prep_bufs(p):
    return (p.sb("f_sq", [128, D], F32), p.sb("f_ss", [128, 1], F32), p.sb("f_rstd", [128, 1], F32),
            p.sb("f_hnf", [128, D], F32), p.sb("f_hnb", [128, D], BF16), p.sb("f_hnT", [128, D], F32),
            p.sb("f_lg", [128, 16], F32), p.sb("f_mx", [128, 1], F32), p.sb("f_sm", [128, 1], F32))


def build_L2():
    nc = bass.Bass("TRN2", target_bir_lowering=False)
    p = Prog(nc)
    x = p.dram("x", [TOK, D], F32, "ExternalInput")
    OL = p.dram("OL", [TOK, 1024], F32, "ExternalInput")
    QP = p.dram("QP", [2, 128, GH, TOK], BF16, "ExternalInput")
    SR = p.dram("SR", [TOK, 1024], F32, "ExternalInput")
    AA = p.dram("AA", [2, 128, NT * GH], F32, "ExternalInput")
    BP = p.dram("BP", [2, 7, GH, 128, GDV], F32, "ExternalInput")
    APD = p.dram("APD", [2, 7, 128, NT * GH], F32, "ExternalInput")
    g_head = p.dram("g_head", [1, GDV], F32, "ExternalInput")
    w_out = p.dram("w_out", [D, D], F32, "ExternalInput")
    g_ffn = p.dram("g_ffn", [1, D], F32, "ExternalInput")
    w_r = p.dram("w_r", [D, 16], F32, "ExternalInput")
    cf_d = p.dram("cf", [128, 5, 128], F32, "ExternalInput")
    cb_d = p.dram("cb", [128, 128], BF16, "ExternalInput")
    H1 = p.dram("H1", [TOK, D], F32, "ExternalOutput")
    HN1 = p.dram("HN1", [TOK, D], BF16, "ExternalOutput")
    AFF = p.dram("AFF", [TOK, 16], F32, "ExternalOutput")

    Wo = p.sb("Wo", [128, 8, D], BF16)
    wr = p.sb("wr", [128, 8, 16], F32)
    cf = p.sb("cfs", [128, 5, 128], F32)
    cb = p.sb("cbs", [128, 128], BF16)
    gf = p.sb("gf", [128, D], F32)
    gh = p.sb("gh", [128, D], F32)
    apd = p.sb("apd", [128, 2, 7, NT * GH], F32)
    csum = p.sb("csum", [128, 2, 7, GH], F32)
    aown = p.sb("aown", [128, 2, NT * GH], F32)
    Sin = p.sb("Sin", [128, 2, GH, GDV], F32)
    Bt = [p.sb("Bt%d" % i, [128, GH, GDV], F32) for i in range(2)]
    Sbw = p.sb("Sbw", [128, NT, GH, GDV], BF16)
    Sfb = p.sb("Sfb", [128, GH, GDV], BF16)
    xt = [p.sb("xt%d" % i, [128, D], F32) for i in range(2)]
    olt = [p.sb("olt%d" % i, [128, D], F32) for i in range(2)]
    srt = [p.sb("srt%d" % i, [128, D], F32) for i in range(2)]
    qf = [p.sb("qf%d" % i, [128, 512], BF16) for i in range(2)]
    qb = [p.sb("qb%d" % i, [128, 512], BF16) for i in range(2)]
    o = p.sb("o", [128, D], F32)
    osq = p.sb("osq", [128, D], F32)
    hss = p.sb("hss", [128, GH], F32)
    y = p.sb("y", [128, D], BF16)
    yT = p.sb("yT", [128, D], BF16)
    h1 = p.sb("h1", [128, D], F32)
    fb = ffn_prep_bufs(p)
    banks = [p.ps("bk%d" % i, [128, 512], F32) for i in range(7)]
    pT = p.ps("pT", [128, 1024], BF16)
    bi = [0]

    def bank():
        b = banks[bi[0] % 7]
        bi[0] += 1
        return b

    p.dma("sp", cf[:], cf_d[:])
    p.dma("sp", cb[:], cb_d[:])
    p.dma("sp", gf[:], g_ffn.v(g_ffn.t[0:1, :].broadcast_to([128, D])))
    for h in range(GH):
        p.dma("sp", gh[:, h * GDV:(h + 1) * GDV], g_head.v(g_head.t[0:1, :].broadcast_to([128, GDV])))
    p.dma("sp", wr[:], w_r.v(w_r.t.rearrange("(c p) e -> p c e", p=128)))
    for dc in range(8):
        p.dma("pool", Wo.sub(dc, (slice(None), dc, slice(None))), w_out[dc * 128:(dc + 1) * 128, :])
    for d_ in range(2):
        p.dma("sp", apd.v(apd.t[:, d_]), APD.v(APD.t[d_].rearrange("i p c -> p i c")))
        p.dma("sp", aown.v(aown.t[:, d_, :]), AA[d_])
    p.reduce("dve", csum[:], apd.v(apd.t[:].rearrange("p d i (t h) -> p d i h t", h=GH)), ALU.add)
    p.act(csum[:], csum[:], AF.Exp, scale=-1.0 / 16)
    p.act(aown[:], aown[:], AF.Exp, scale=-1.0 / 16)
    p.memset("dve", Sin[:], 0.0)
    n = 0
    for d_ in range(2):
        for i in range(7):
            B = Bt[n % 2]
            n += 1
            p.dma("sp", B[:], BP.v(BP.t[d_, i].rearrange("h p v -> p h v")))
            for h in range(GH):
                p.stt("dve", Sin[:, d_, h, :], Sin[:, d_, h, :], csum.v(csum.t[:, d_, i, h:h + 1]), B[:, h, :],
                      ALU.mult, ALU.add)
    for j in range(NT - 1, -1, -1):
        p.copy("act", Sbw[:, j], Sin[:, 1])
        for h in range(GH):
            p.ts("dve", Sin[:, 1, h, :], Sin[:, 1, h, :], aown.v(aown.t[:, 1, j * GH + h:j * GH + h + 1]), None, ALU.mult)
    for j in range(NT):
        b = j % 2
        rows = slice(j * 128, (j + 1) * 128)
        p.dma("sp", xt[b][:], x[rows, :])
        p.dma("sp", olt[b][:], OL[rows, :])
        p.dma("sp", srt[b][:], SR[rows, :])
        p.dma("sp", qf[b].v(qf[b].t[:].rearrange("p (h t) -> p h t", h=GH)), QP.v(QP.t[0, :, :, rows]))
        p.dma("sp", qb[b].v(qb[b].t[:].rearrange("p (h t) -> p h t", h=GH)), QP.v(QP.t[1, :, :, rows]))
        p.copy("act", Sfb[:], Sin[:, 0])
        for h in range(GH):
            hs = slice(h * 128, (h + 1) * 128)
            vs = slice(h * GDV, (h + 1) * GDV)
            bo = bank()
            p.matmul(bo[:, 0:GDV], qf[b][:, hs], Sfb[:, h, :], start=True, stop=False)
            p.matmul(bo[:, 0:GDV], qb[b][:, hs], Sbw[:, j, h, :], start=False, stop=True)
            p.tt("dve", o[:, vs], bo[:, 0:GDV], olt[b][:, vs], ALU.add)
            p.ts("dve", Sin[:, 0, h, :], Sin[:, 0, h, :], aown.v(aown.t[:, 0, j * GH + h:j * GH + h + 1]), None, ALU.mult)
        p.tt("dve", osq[:], o[:], o[:], ALU.mult)
        p.reduce("dve", hss[:], osq.v(osq.t[:].rearrange("p (h v) -> p h v", h=GH)), ALU.add)
        p.ts("dve", hss[:], hss[:], 1.0 / GDV, RMS_EPS, ALU.mult, ALU.add)
        p.act(hss[:], hss[:], AF.Sqrt)
        p.recip(hss[:], hss[:])
        p.tt("dve", osq[:], srt[b][:], gh[:], ALU.mult)
        for h in range(GH):
            vs = slice(h * GDV, (h + 1) * GDV)
            p.stt("dve", y[:, vs], o[:, vs], hss[:, h:h + 1], osq[:, vs], ALU.mult, ALU.mult)
        for dc in range(8):
            p.transpose(pT[:, dc * 128:(dc + 1) * 128], y[:, dc * 128:(dc + 1) * 128], cb[:])
        p.copy("act", yT[:], pT[:])
        for half in range(2):
            bm = bank()
            for dc in range(8):
                p.matmul(bm[:], yT[:, dc * 128:(dc + 1) * 128],
                         Wo.sub(dc, (slice(None), dc, slice(half * 512, (half + 1) * 512))),
                         start=(dc == 0), stop=(dc == 7))
            p.tt("dve", h1[:, half * 512:(half + 1) * 512], bm[:], xt[b][:, half * 512:(half + 1) * 512], ALU.add)
        p.dma("pool", H1[rows, :], h1[:])
        ffn_prep_tile(p, h1[:], gf, wr, cf, fb, HN1, AFF, rows, bank)
    return p.finish()


NE = 16
CAP = 2 * SEQ // NE
FF = 2048
BISECT_ITERS = 36


def build_L3():
    nc = bass.Bass("TRN2", target_bir_lowering=False)
    p = Prog(nc)
    AFFE = p.dram("AFFE", [2, 128, 128], F32, "ExternalInput")
    HN = p.dram("HN", [SEQ, D], BF16, "ExternalInput")
    wg = p.dram("wg", [2, D, FF], F32, "ExternalInput")
    wu = p.dram("wu", [2, D, FF], F32, "ExternalInput")
    wd = p.dram("wd", [2, FF, D], F32, "ExternalInput")
    cf_d = p.dram("cf", [128, 6, 128], F32, "ExternalInput")
    cb_d = p.dram("cb", [128, 128], BF16, "ExternalInput")
    tok_d = p.dram("tokid", [128, 128], I32, "ExternalInput")
    DELTA = p.dram("DELTA", [SEQ, D], F32, "ExternalOutput")

    Wg = p.sb("Wg", [128, 8, FF], BF16)
    Wu = p.sb("Wu", [128, 8, FF], BF16)
    Wd = p.sb("Wd", [128, 16, D], BF16)
    cf = p.sb("cfs", [128, 6, 128], F32)
    cb = p.sb("cbs", [128, 128], BF16)
    tokid = p.sb("tokid_s", [128, 128], I32)
    zt = p.sb("zt", [128, 4096], F32)
    ones = p.sb("ones", [128, 128], F32)
    aff = p.sb("aff", [128, 2, 128], F32)
    cmp_ = p.sb("cmp", [128, 2, 128], F32)
    st = {n: p.sb("b_" + n, [128, 2], F32) for n in ("lo", "hi", "mid", "cnt", "ge", "nge", "t1", "t2")}
    selT = p.sb("selT", [128, 128], F32)
    rp = p.sb("rp", [128, 1], F32)
    slot = p.sb("slot", [128, 128], F32)
    idx_sb = p.sb("idx_sb", [128, 2, 16, 2], I32)
    banks = [p.ps("bk%d" % i, [128, 512], F32) for i in range(7)]
    pT = p.ps("pT", [128, 1024], BF16)
    bi = [0]

    def bank():
        b = banks[bi[0] % 7]
        bi[0] += 1
        return b

    p.dma("sp", cf[:], cf_d[:])
    p.dma("sp", cb[:], cb_d[:])
    p.dma("sp", tokid[:], tok_d[:])
    p.dma("sp", aff[:], AFFE.v(AFFE.t.rearrange("e p f -> p e f")))
    p.memset("pool", zt[:], 0.0)
    p.memset("dve", ones[:], 1.0)
    zkeys = []
    dz = DELTA.t.rearrange("(k p r) d -> k p (r d)", p=128, r=4)
    for k in range(SEQ // 512):
        zk = ("DELTA", "z", k)
        zkeys.append(zk)
        p.dma("sp", V(dz[k], zk), zt[:])
    US = cf[:, 2, :]
    ident_f = cf[:, 4, :]

    def load_weights(e):
        for dc in range(8):
            p.dma("pool", Wg.sub(dc, (slice(None), dc, slice(None))), V(wg.t[e, dc * 128:(dc + 1) * 128, :], ("wg", e)))
            p.dma("pool", Wu.sub(dc, (slice(None), dc, slice(None))), V(wu.t[e, dc * 128:(dc + 1) * 128, :], ("wu", e)))
        for fc in range(16):
            p.dma("pool", Wd.sub(fc, (slice(None), fc, slice(None))), V(wd.t[e, fc * 128:(fc + 1) * 128, :], ("wd", e)))

    load_weights(0)
    lo, hi, mid, cnt, ge, nge, t1, t2 = (st[n] for n in ("lo", "hi", "mid", "cnt", "ge", "nge", "t1", "t2"))
    p.memset("dve", lo[:], 0.0)
    p.memset("dve", hi[:], 1.0)
    for it in range(BISECT_ITERS):
        p.tt("dve", mid[:], lo[:], hi[:], ALU.add)
        p.ts("dve", mid[:], mid[:], 0.5, None, ALU.mult)
        for e in range(2):
            p.ts("dve", cmp_[:, e, :], aff[:, e, :], mid[:, e:e + 1], None, ALU.is_ge)
        p.reduce("dve", cnt[:], cmp_[:], ALU.add)
        bt = bank()
        p.matmul(bt[:, 0:2], ones[:], cnt[:])
        p.ts("dve", ge[:], bt[:, 0:2], float(CAP) - 0.5, None, ALU.is_ge)
        p.ts("dve", nge[:], ge[:], -1.0, 1.0, ALU.mult, ALU.add)
        p.tt("dve", lo[:], lo[:], nge[:], ALU.mult)
        p.tt("dve", t1[:], mid[:], ge[:], ALU.mult)
        p.tt("dve", lo[:], lo[:], t1[:], ALU.add)
        p.tt("dve", hi[:], hi[:], ge[:], ALU.mult)
        p.tt("dve", t2[:], mid[:], nge[:], ALU.mult)
        p.tt("dve", hi[:], hi[:], t2[:], ALU.add)
    for e in range(2):
        p.ts("dve", cmp_[:, e, :], aff[:, e, :], lo[:, e:e + 1], None, ALU.is_ge)
    p.reduce("dve", cnt[:], cmp_[:], ALU.add)
    wts = idx_sb.t[:].bitcast(F32)
    p.push_scope()
    slot_i = p.sb("slot_i", [128, 128], I32)
    smod_i = p.sb("smod_i", [128, 128], I32)
    sdiv_i = p.sb("sdiv_i", [128, 128], I32)
    smod_f = p.sb("smod_f", [128, 128], F32)
    sdiv_f = p.sb("sdiv_f", [128, 128], F32)
    tokf = p.sb("tokf", [128, 128], F32)
    base = p.sb("base", [128, 128, 16], F32)
    Bcat = p.sb("Bcat", [128, 128, 32], F32)
    OHc = [p.sb("OHc%d" % i, [128, 16, 128], F32) for i in range(2)]
    p.copy("dve", tokf[:], tokid[:])
    iota_r = cf.t[:, 5, :]
    for e in range(2):
        bt = bank()
        p.transpose(bt[:, 0:128], cmp_[:, e, :], ident_f)
        p.copy("act", selT[:], bt[:, 0:128])
        bp = bank()
        p.matmul(bp[:, 0:128], selT[:], US)
        p.matmul(bp[:, 128:129], US, cnt[:, e:e + 1])
        p.copy("act", rp[:], bp[:, 128:129])
        p.ts("dve", slot[:], bp[:, 0:128], rp[:, 0:1], None, ALU.add)
        p.copy("dve", slot_i[:], slot[:])
        p.ts("dve", smod_i[:], slot_i[:], 127, None, ALU.bitwise_and)
        p.ts("dve", sdiv_i[:], slot_i[:], 7, None, ALU.arith_shift_right)
        p.copy("dve", smod_f[:], smod_i[:])
        p.copy("dve", sdiv_f[:], sdiv_i[:])
        p.tt("dve", base[:], cf.v(iota_r[:, 0:16].unsqueeze(1).broadcast_to([128, 128, 16])),
             sdiv_f.v(sdiv_f.t[:].unsqueeze(2).broadcast_to([128, 128, 16])), ALU.is_equal)
        p.tt("dve", base[:], base[:], cmp_.v(cmp_.t[:, e, :].unsqueeze(2).broadcast_to([128, 128, 16])), ALU.mult)
        p.tt("dve", Bcat[:, :, 0:16], base[:], tokf.v(tokf.t[:].unsqueeze(2).broadcast_to([128, 128, 16])), ALU.mult)
        p.tt("dve", Bcat[:, :, 16:32], base[:], aff.v(aff.t[:, e, :].unsqueeze(2).broadcast_to([128, 128, 16])), ALU.mult)
        bacc = bank()
        for c in range(8):
            OH = OHc[c % 2]
            p.tt("dve", OH[:], cf.v(iota_r.unsqueeze(1).broadcast_to([128, 16, 128])),
                 smod_f.v(smod_f.t[:, c * 16:(c + 1) * 16].unsqueeze(2).broadcast_to([128, 16, 128])), ALU.is_equal)
            for fl in range(16):
                f = c * 16 + fl
                p.matmul(bacc[:, 0:32], OH[:, fl, :], Bcat[:, f, :], start=(f == 0), stop=(f == 127))
        p.copy("dve", idx_sb.v(idx_sb.t[:, e, :, 0]), bacc[:, 0:16])
        p.copy("dve", idx_sb.v(wts[:, e, :, 1]), bacc[:, 16:32])
    p.pop_scope()
    xs = [p.sb("xs%d" % i, [128, D], BF16) for i in range(2)]
    xsT = p.sb("xsT", [128, 8, 512], BF16)
    hT = p.sb("hT", [128, 16, 512], BF16)
    sg = [p.sb("sg%d" % i, [128, 512], F32) for i in range(2)]
    y = [p.sb("y%d" % i, [128, D], F32) for i in range(2)]
    first_scatter = True
    for e in range(2):
        if e > 0:
            load_weights(e)
        for tg in range(4):
            for kk in range(4):
                k = tg * 4 + kk
                X = xs[kk % 2]
                p.gather(X[:], HN[:, :], idx_sb[:, e, k, 0:1], SEQ)
                for dc in range(8):
                    p.transpose(pT[:, dc * 128:(dc + 1) * 128], X[:, dc * 128:(dc + 1) * 128], cb[:])
                p.copy("act", xsT[:, :, kk * 128:(kk + 1) * 128], pT.v(pT.t[:].rearrange("p (c t) -> p c t", c=8)))
            for fc in range(16):
                bg_, bu_ = bank(), bank()
                for dc in range(8):
                    p.matmul(bg_[:], Wg.sub(dc, (slice(None), dc, slice(fc * 128, (fc + 1) * 128))), xsT[:, dc, :],
                             start=(dc == 0), stop=(dc == 7))
                for dc in range(8):
                    p.matmul(bu_[:], Wu.sub(dc, (slice(None), dc, slice(fc * 128, (fc + 1) * 128))), xsT[:, dc, :],
                             start=(dc == 0), stop=(dc == 7))
                G = sg[fc % 2]
                p.act(G[:], bg_[:], AF.Silu)
                p.tt("dve", hT[:, fc, :], G[:], bu_[:], ALU.mult)
            for kk in range(4):
                k = tg * 4 + kk
                Y = y[kk % 2]
                for half in range(2):
                    by = bank()
                    for fc in range(16):
                        p.matmul(by[:], hT[:, fc, kk * 128:(kk + 1) * 128],
                                 Wd.sub(fc, (slice(None), fc, slice(half * 512, (half + 1) * 512))),
                                 start=(fc == 0), stop=(fc == 15))
                    p.ts("dve", Y[:, half * 512:(half + 1) * 512], by[:], idx_sb.v(wts[:, e, k, 1:2]), None, ALU.mult)
                p.scatter(DELTA[:, :], Y[:], idx_sb[:, e, k, 0:1], SEQ, accum=True,
                          reads=zkeys if first_scatter else None)
                first_scatter = False
    return p.finish()


MH = 16
QRANK = 256
KVR = 128
NOPE = 128
ROPE = 64
MLA_SCALE = float(NOPE + ROPE) ** -0.5
TWO_PI = 2.0 * np.pi


def rope_consts():
    half = ROPE // 2
    inv = (10000.0 ** (-np.arange(half, dtype=np.float32) / half)).astype(np.float32)
    rc = np.zeros((64, 4), np.float32)
    rc[:, 0] = np.concatenate([inv, inv])
    rc[:, 1] = np.concatenate([np.ones(half), -np.ones(half)])
    rc[:, 2] = -np.pi
    return rc


def build_L4():
    nc = bass.Bass("TRN2", target_bir_lowering=False)
    p = Prog(nc)
    H1 = p.dram("H1", [TOK, D], F32, "ExternalInput")
    DS = p.dram("DS", [NCORES, TOK, D], F32, "ExternalInput")
    pos = p.dram("pos", [1, TOK], I32, "ExternalInput")
    g_mix = p.dram("g_mix", [1, D], F32, "ExternalInput")
    w_m = p.dram("w_m", [D, 448], F32, "ExternalInput")
    g_q = p.dram("g_q", [1, QRANK], F32, "ExternalInput")
    g_kv = p.dram("g_kv", [1, KVR], F32, "ExternalInput")
    w_uq = p.dram("w_uq", [QRANK, MH * 192], F32, "ExternalInput")
    w_ukv = p.dram("w_ukv", [KVR, MH * 256], F32, "ExternalInput")
    rc_d = p.dram("rc", [64, 4], F32, "ExternalInput")
    cf_d = p.dram("cf", [128, 5, 128], F32, "ExternalInput")
    cb_d = p.dram("cb", [128, 128], BF16, "ExternalInput")
    H2 = p.dram("H2", [TOK, D], F32, "ExternalOutput")
    QL = p.dram("QL", [MH, 128, TOK], BF16, "ExternalOutput")
    QR = p.dram("QR", [MH, 64, TOK], BF16, "ExternalOutput")
    CKVT = p.dram("CKVT", [128, TOK], BF16, "ExternalOutput")
    KRT = p.dram("KRT", [64, TOK], BF16, "ExternalOutput")
    CKV = p.dram("CKV", [TOK, 128], BF16, "ExternalOutput")
    KN2 = p.dram("KN2", [TOK, 1], F32, "ExternalOutput")
    QMAX = p.dram("QMAX", [1, MH], F32, "ExternalOutput")

    cf = p.sb("cfs", [128, 5, 128], F32)
    cb = p.sb("cbs", [128, 128], BF16)
    rc = p.sb("rcs", [64, 4], F32)
    gm = p.sb("gm", [128, D], F32)
    gq = p.sb("gq", [128, QRANK], F32)
    gkv = p.sb("gkv", [128, KVR], F32)
    Wm = p.sb("Wm", [128, 8, 448], BF16)
    Wuq = p.sb("Wuq", [128, 2, MH * 192], BF16)
    Wukv = p.sb("Wukv", [128, MH * 256], BF16)
    WukT = p.sb("WukT", [128, MH, 128], BF16)
    posi = p.sb("posi", [64, TOK], I32)
    ang = p.sb("ang", [64, TOK], F32)
    C2 = p.sb("C2", [64, TOK], F32)
    S2 = p.sb("S2", [64, TOK], F32)
    hnT = p.sb("hnT", [128, 8, TOK], BF16)
    cqT = p.sb("cqT", [128, 2, TOK], BF16)
    ht = [p.sb("ht%d" % i, [128, D], F32) for i in range(2)]
    dt_ = [p.sb("dt%d" % i, [128, D], F32) for i in range(3)]
    sq = p.sb("sq", [128, D], F32)
    ss = p.sb("ss", [128, 1], F32)
    rstd = p.sb("rstd", [128, 1], F32)
    hn = p.sb("hn", [128, D], BF16)
    cs = p.sb("cs", [128, 448], F32)
    cn = p.sb("cn", [128, 384], BF16)
    kn = p.sb("kn", [128, 2], F32)
    qn = p.sb("qn", [128, 512], BF16)
    qlf = p.sb("qlf", [128, 512], F32)
    qlb = p.sb("qlb", [128, 512], BF16)
    qsq = p.sb("qsq", [128, 512], F32)
    r1 = p.sb("r1", [64, 512], F32)
    r2 = p.sb("r2", [64, 512], F32)
    rb = p.sb("rb", [64, 512], BF16)
    qmx = p.sb("qmx", [1, MH], F32)
    qm1 = p.sb("qm1", [1, 1], F32)
    ckT = p.sb("ckT", [128, 128], BF16)
    banks = [p.ps("bk%d" % i, [128, 512], F32) for i in range(7)]
    pT = p.ps("pT", [128, 1024], BF16)
    bi = [0]

    def bank():
        b = banks[bi[0] % 7]
        bi[0] += 1
        return b

    p.dma("sp", cf[:], cf_d[:])
    p.dma("sp", cb[:], cb_d[:])
    p.dma("sp", rc[:], rc_d[:])
    p.dma("sp", gm[:], g_mix.v(g_mix.t[0:1, :].broadcast_to([128, D])))
    p.dma("sp", gq[:], g_q.v(g_q.t[0:1, :].broadcast_to([128, QRANK])))
    p.dma("sp", gkv[:], g_kv.v(g_kv.t[0:1, :].broadcast_to([128, KVR])))
    p.dma("sp", posi[:], pos.v(pos.t[0:1, :].broadcast_to([64, TOK])))
    p.dma("pool", Wm[:], w_m.v(w_m.t.rearrange("(c p) n -> p c n", p=128)))
    p.dma("pool", Wuq[:], w_uq.v(w_uq.t.rearrange("(c p) n -> p c n", p=128)))
    p.dma("pool", Wukv[:], w_ukv[:, :])
    onesf = p.sb("onesf", [128, 1], F32)
    p.memset("dve", onesf[:], 1.0)
    p.memset("dve", qmx[:], 0.0)
    Wm_sw = p.sb("Wm_sw", [128, 8, 64], BF16)
    Wuq_sw = p.sb("Wuq_sw", [128, 2, MH, 64], BF16)
    p.copy("dve", Wm_sw[:, :, 0:32], Wm[:, :, 416:448])
    p.copy("dve", Wm_sw[:, :, 32:64], Wm[:, :, 384:416])
    for kc in range(2):
        wv = Wuq.t[:, kc, :].rearrange("p (h c) -> p h c", h=MH)
        p.copy("dve", Wuq_sw[:, kc, :, 0:32], Wuq.v(wv[:, :, 160:192]))
        p.copy("dve", Wuq_sw[:, kc, :, 32:64], Wuq.v(wv[:, :, 128:160]))
    for h in range(MH):
        p.transpose(pT[:, (h % 8) * 128:(h % 8 + 1) * 128], Wukv[:, h * 256:h * 256 + 128], cb[:])
        if h % 8 == 7:
            g0 = h - 7
            p.copy("act", WukT[:, g0:g0 + 8, :], pT.v(pT.t[:].rearrange("p (h r) -> p h r", h=8)))
    p.copy("dve", ang[:], posi[:])
    p.ts("dve", ang[:], ang[:], rc[:, 0:1], None, ALU.mult)
    C1 = 6.28125
    C2_ = float(TWO_PI - 6.28125)
    ki = p.sb("ki", [64, TOK], I32)
    kf = p.sb("kf", [64, TOK], F32)
    gt = p.sb("gt", [64, TOK], F32)
    p.ts("dve", kf[:], ang[:], float(1.0 / TWO_PI), None, ALU.mult)
    p.copy("dve", ki[:], kf[:])
    p.copy("dve", kf[:], ki[:])
    p.stt("dve", ang[:], kf[:], -C1, ang[:], ALU.mult, ALU.add)
    p.stt("dve", ang[:], kf[:], -C2_, ang[:], ALU.mult, ALU.add)

    def fold(t):
        p.ts("dve", gt[:], t[:], float(np.pi), None, ALU.is_gt)
        p.stt("dve", t[:], gt[:], -TWO_PI, t[:], ALU.mult, ALU.add)
        p.ts("dve", gt[:], t[:], float(-np.pi), None, ALU.is_lt)
        p.stt("dve", t[:], gt[:], TWO_PI, t[:], ALU.mult, ALU.add)
        p.ts("dve", t[:], t[:], float(np.pi), float(-np.pi), ALU.min, ALU.max)

    fold(ang)
    p.ts("dve", C2[:], ang[:], float(np.pi / 2), None, ALU.add)
    fold(C2)
    p.act(S2[:], ang[:], AF.Sin)
    p.ts("dve", S2[:], S2[:], rc[:, 1:2], -1.0, ALU.mult, ALU.mult)
    p.act(C2[:], C2[:], AF.Sin)

    for j in range(NT):
        rows = slice(j * 128, (j + 1) * 128)
        Ht = ht[j % 2]
        p.dma("sp", Ht[:], H1[rows, :])
        for c in range(NCORES):
            Dt = dt_[c % 3]
            p.dma("sp", Dt[:], DS.v(DS.t[c, rows, :]))
            p.tt("dve", Ht[:], Ht[:], Dt[:], ALU.add)
        p.dma("pool", H2[rows, :], Ht[:])
        rmsnorm_tile(p, Ht[:], gm[:], hn[:], sq[:], ss[:], rstd[:])
        for dc in range(8):
            p.transpose(pT[:, dc * 128:(dc + 1) * 128], hn[:, dc * 128:(dc + 1) * 128], cb[:])
        p.copy("act", hnT[:, :, rows], pT.v(pT.t[:].rearrange("p (c t) -> p c t", c=8)))
        bc_ = bank()
        for dc in range(8):
            p.matmul(bc_[:, 0:448], hnT[:, dc, rows], Wm[:, dc, :], start=(dc == 0), stop=(dc == 7))
        p.copy("act", cs[:], bc_[:, 0:448])
        rmsnorm_tile(p, cs[:, 0:256], gq[:], cn[:, 0:256], sq[:, 0:256], ss[:], rstd[:], width=QRANK)
        rmsnorm_tile(p, cs[:, 256:384], gkv[:], cn[:, 256:384], sq[:, 0:128], ss[:], rstd[:], width=KVR)
        p.dma("pool", CKV[rows, :], cn[:, 256:384])
        p.tt("dve", sq[:, 0:128], cn[:, 256:384], cn[:, 256:384], ALU.mult)
        p.reduce("dve", kn[:, 0:1], sq[:, 0:128], ALU.add)
        p.tt("dve", sq[:, 0:64], cs[:, 384:448], cs[:, 384:448], ALU.mult)
        p.reduce("dve", kn[:, 1:2], sq[:, 0:64], ALU.add)
        p.tt("dve", kn[:, 0:1], kn[:, 0:1], kn[:, 1:2], ALU.add)
        p.dma("pool", KN2[rows, :], kn[:, 0:1])
        for q in range(3):
            p.transpose(pT[:, q * 128:(q + 1) * 128], cn[:, q * 128:(q + 1) * 128], cb[:])
        p.copy("act", cqT[:, :, rows], pT.v(pT.t[:, 0:256].rearrange("p (c t) -> p c t", c=2)))
        p.copy("act", ckT[:], pT[:, 256:384])
        p.dma("pool", CKVT[:, rows], ckT[:])

    for tg in range(TOK // 512):
        cols = slice(tg * 512, (tg + 1) * 512)
        bk_, bks = bank(), bank()
        for dc in range(8):
            p.matmul(bk_[0:64, :], Wm[:, dc, 384:448], hnT[:, dc, cols], start=(dc == 0), stop=(dc == 7))
        for dc in range(8):
            p.matmul(bks[0:64, :], Wm_sw[:, dc, :], hnT[:, dc, cols], start=(dc == 0), stop=(dc == 7))
        p.tt("dve", r1[:], bk_[0:64, :], C2[:, cols], ALU.mult)
        p.tt("dve", r2[:], bks[0:64, :], S2[:, cols], ALU.mult)
        p.tt("dve", rb[:], r1[:], r2[:], ALU.add)
        p.dma("pool", KRT[:, cols], rb[:])
        for h in range(MH):
            o = h * 192
            bq = bank()
            for kc in range(2):
                p.matmul(bq[:], Wuq[:, kc, o:o + 128], cqT[:, kc, cols], start=(kc == 0), stop=(kc == 1))
            p.copy("act", qn[:], bq[:])
            bl = bank()
            p.matmul(bl[:], WukT[:, h, :], qn[:])
            p.ts("dve", qlf[:], bl[:], MLA_SCALE, None, ALU.mult)
            p.copy("act", qlb[:], qlf[:])
            p.dma("pool", QL.v(QL.t[h, :, cols]), qlb[:])
            br, brs = bank(), bank()
            for kc in range(2):
                p.matmul(br[0:64, :], Wuq[:, kc, o + 128:o + 192], cqT[:, kc, cols], start=(kc == 0), stop=(kc == 1))
            for kc in range(2):
                p.matmul(brs[0:64, :], Wuq_sw[:, kc, h, :], cqT[:, kc, cols], start=(kc == 0), stop=(kc == 1))
            p.tt("dve", r1[:], br[0:64, :], C2[:, cols], ALU.mult)
            p.tt("dve", r2[:], brs[0:64, :], S2[:, cols], ALU.mult)
            p.tt("dve", r1[:], r1[:], r2[:], ALU.add)
            p.ts("dve", r1[:], r1[:], MLA_SCALE, None, ALU.mult)
            p.copy("act", rb[:], r1[:])
            p.dma("pool", QR.v(QR.t[h, :, cols]), rb[:])
            p.tt("dve", qsq[:], qlf[:], qlf[:], ALU.mult)
            p.tt("dve", r2[:], r1[:], r1[:], ALU.mult)
            bn = bank()
            p.matmul(bn[0:1, :], onesf[:, 0:1], qsq[:], start=True, stop=False)
            p.matmul(bn[0:1, :], onesf[0:64, 0:1], r2[:], start=False, stop=True)
            p.reduce("dve", qm1[:], bn[0:1, :], ALU.max)
            p.tt("dve", qmx[:, h:h + 1], qmx[:, h:h + 1], qm1[:], ALU.max)
    p.dma("pool", QMAX[:, :], qmx[:])
    return p.finish()


NKT = SEQ // 128


def build_L5(nheads=None, same=True):
    nc = bass.Bass("TRN2", target_bir_lowering=False)
    p = Prog(nc)
    QL = p.dram("QL", [MH, 128, TOK], BF16, "ExternalInput")
    QR = p.dram("QR", [MH, 64, TOK], BF16, "ExternalInput")
    p.same = same
    KTd = p.dram("KT", [128, SEQ], BF16, "ExternalInput")
    KRd = p.dram("KR", [64, SEQ], BF16, "ExternalInput")
    Vd = p.dram("VK", [SEQ, 128], BF16, "ExternalInput")
    KN2 = p.dram("KN2", [128, 128], F32, "ExternalInput")
    QMAX = p.dram("QMAX", [1, MH], F32, "ExternalInput")
    H2 = p.dram("H2", [TOK, D], F32, "ExternalInput")
    w_ukv = p.dram("w_ukv", [KVR, MH * 256], F32, "ExternalInput")
    w_o = p.dram("w_o", [MH * 128, D], F32, "ExternalInput")
    g_ffn = p.dram("g_ffn", [1, D], F32, "ExternalInput")
    w_r = p.dram("w_r", [D, 16], F32, "ExternalInput")
    cf_d = p.dram("cf", [128, 5, 128], F32, "ExternalInput")
    cb_d = p.dram("cb", [128, 128], BF16, "ExternalInput")
    H3 = p.dram("H3", [TOK, D], F32, "ExternalOutput")
    HN3 = p.dram("HN3", [TOK, D], BF16, "ExternalOutput")
    AFF = p.dram("AFF", [TOK, 16], F32, "ExternalOutput")
    OTD = p.dram("OTD", [MH, 128, TOK], BF16, "Internal")

    KT = p.sb("KTs", [128, SEQ], BF16)
    KR2 = p.sb("KR2", [128, SEQ], BF16)
    Vt = p.sb("Vt", [128, NKT, 128], BF16)
    Wo = p.sb("Wo", [128, MH, D], BF16)
    Wuv = p.sb("Wuv", [128, MH, 128], BF16)
    wr = p.sb("wr", [128, 8, 16], F32)
    cf = p.sb("cfs", [128, 5, 128], F32)
    cb = p.sb("cbs", [128, 128], BF16)
    gf = p.sb("gf", [128, D], F32)
    onesf = p.sb("onesf", [128, 128], F32)
    kn = p.sb("kn", [128, 128], F32)
    km = p.sb("km", [128, 1], F32)
    km1 = p.sb("km1", [1, 1], F32)
    qmx = p.sb("qmx", [1, MH], F32)
    negm = p.sb("negm", [128, MH], F32)
    QW = 1024
    sbk = [p.ps("sbk%d" % i, [128, QW], F32) for i in range(2)]
    obk = p.ps("obk", [128, QW], F32)
    ebk = [p.ps("ebk%d" % i, [128, 512], F32) for i in range(2)]
    mi = [0]

    def bank():
        b = ebk[mi[0] % 2]
        mi[0] += 1
        return b

    p.dma("sp", cf[:], cf_d[:])
    p.dma("sp", cb[:], cb_d[:])
    p.dma("sp", kn[:], KN2[:, :])
    p.dma("sp", qmx[:], QMAX[:, :])
    p.dma("sp", gf[:], g_ffn.v(g_ffn.t[0:1, :].broadcast_to([128, D])))
    p.dma("sp", wr[:], w_r.v(w_r.t.rearrange("(c p) e -> p c e", p=128)))
    for q in range(8):
        cs_ = slice(q * 2048, (q + 1) * 2048)
        p.dma("sp", KT.sub(q, (slice(None), cs_)), V(KTd.t[:, cs_], ("KTd", q)))
    for half in range(2):
        for q in range(8):
            cs_ = slice(q * 2048, (q + 1) * 2048)
            p.dma("sp", KR2.sub((half, q), (slice(half * 64, (half + 1) * 64), cs_)), V(KRd.t[:, cs_], ("KRd", half, q)))
    for q in range(8):
        p.dma("sp", Vt.sub(q, (slice(None), slice(q * 16, (q + 1) * 16), slice(None))),
              V(Vd.t[q * 2048:(q + 1) * 2048, :].rearrange("(k p) r -> p k r", p=128), ("Vd", q)))
    p.dma("pool", Wuv[:], w_ukv.v(w_ukv.t.rearrange("r (h c) -> r h c", h=MH)[:, :, 128:256]))
    for h in range(MH):
        p.dma("pool", Wo.sub(h, (slice(None), h, slice(None))), V(w_o.t[h * 128:(h + 1) * 128, :], ("w_o", h)))
    p.memset("dve", onesf[:], 1.0)
    p.reduce("dve", km[:], kn[:], ALU.max)
    bt = bank()
    p.transpose(bt[0:1, 0:128], km[:, 0:1], cf[:, 4, :])
    p.reduce("dve", km1[:], bt[0:1, 0:128], ALU.max)
    p.ts("dve", qmx[:], qmx[:], km1[0:1, 0:1], None, ALU.mult)
    p.act(qmx[:], qmx[:], AF.Sqrt)
    p.ts("dve", qmx[:], qmx[:], -1.0, None, ALU.mult)
    bb = bank()
    p.matmul(bb[:, 0:MH], onesf[0:1, :], qmx[:])
    p.copy("act", negm[:], bb[:, 0:MH])

    p.push_scope()
    ql = [p.sb("ql%d" % i, [128, TOK], BF16) for i in range(2)]
    qr = [p.sb("qr%d" % i, [128, TOK], BF16) for i in range(2)]
    PT = [p.sb("PT%d" % i, [128, QW], BF16) for i in range(3)]
    accD = [p.sb("accD%d" % i, [128, QW], F32) for i in range(1)]
    accP = [p.sb("accP%d" % i, [128, QW], F32) for i in range(1)]
    rl = p.sb("rl", [128, QW], F32)
    OTn = p.sb("OTn", [128, QW], BF16)
    oTs = [p.sb("oTs%d" % i, [128, QW], BF16) for i in range(2)]
    NH = MH if nheads is None else nheads
    it = 0
    for h in range(NH):
        Ql, Qr = ql[h % 2], qr[h % 2]
        p.dma("sp", Ql[:], QL.v(QL.t[h]))
        p.dma("sp", Qr[0:64, :], QR.v(QR.t[h]))
        p.dma("sp", Qr[64:128, :], QR.v(QR.t[h]))
        for qg in range(TOK // QW):
            q0 = qg * QW
            bo = obk
            AD, AP_ = accD[0], accP[0]
            it += 1
            first = {"dve": True, "pool": True}

            def s_mm(kt):
                bs = sbk[kt % 2]
                ktile = KT.sub(kt // 16, (slice(None), slice(kt * 128, (kt + 1) * 128)))
                rtiles = [KR2.sub((hf, kt // 16), (slice(hf * 64, (hf + 1) * 64), slice(kt * 128, (kt + 1) * 128)))
                          for hf in range(2)]
                for hh in range(QW // 512):
                    p.matmul(bs[:, hh * 512:(hh + 1) * 512], ktile, Ql[:, q0 + hh * 512:q0 + (hh + 1) * 512],
                             start=True, stop=False)
                if kt >= 1:
                    p.wait("pe", [PT[(kt - 1) % 3].name])
                for hh in range(QW // 512):
                    p.matmul(bs[:, hh * 512:(hh + 1) * 512], rtiles[hh],
                             Qr[hh * 64:(hh + 1) * 64, q0 + hh * 512:q0 + (hh + 1) * 512], start=False, stop=True)

            s_mm(0)
            for kt in range(NKT):
                if kt + 1 < NKT:
                    s_mm(kt + 1)
                P_ = PT[kt % 3]
                p.act(P_[:], sbk[kt % 2][:], AF.Exp, bias=negm[:, h:h + 1])
                vt = Vt.sub(kt // 16, (slice(None), kt, slice(None)))
                for hh in range(QW // 512):
                    p.matmul(bo[:, hh * 512:(hh + 1) * 512], vt, P_[:, hh * 512:(hh + 1) * 512],
                             start=(kt == 0), stop=(kt == NKT - 1))
                eng = "pool" if kt % 3 == 2 else "dve"
                A = AP_ if eng == "pool" else AD
                if first[eng]:
                    p.copy(eng, A[:], P_[:])
                    first[eng] = False
                else:
                    p.tt(eng, A[:], A[:], P_[:], ALU.add)
            p.tt("dve", AD[:], AD[:], AP_[:], ALU.add)
            for hh in range(QW // 512):
                hs = slice(hh * 512, (hh + 1) * 512)
                bl = ebk[hh]
                p.matmul(bl[:], onesf[:], AD[:, hs])
                p.recip(rl[:, hs], bl[:])
                p.tt("dve", OTn[:, hs], bo[:, hs], rl[:, hs], ALU.mult)
            oT = oTs[it % 2]
            for hh in range(QW // 512):
                hs = slice(hh * 512, (hh + 1) * 512)
                bv = ebk[hh]
                p.matmul(bv[:], Wuv[:, h, :], OTn[:, hs])
                p.copy("act", oT[:, hs], bv[:])
            p.dma("pool", V(OTD.t[h, :, q0:q0 + QW], ("OTD", h, qg)), oT[:])
    otd_keys = [("OTD", h, qg) for h in range(NH) for qg in range(TOK // QW)]
    p.pop_scope()
    oTt = [p.sb("oTt%d" % i, [128, MH, 128], BF16) for i in range(2)]
    h2t = [p.sb("h2t%d" % i, [128, D], F32) for i in range(2)]
    h3 = p.sb("h3", [128, D], F32)
    fb = ffn_prep_bufs(p)

    for j in range(NT):
        rows = slice(j * 128, (j + 1) * 128)
        b = j % 2
        p.dma("sp", h2t[b][:], H2[rows, :])
        p.dma("sp", oTt[b][:], V(OTD.t[:, :, rows].rearrange("h d t -> d h t"), ("OTD", "rd", j)),
              reads=[k_ for k_ in otd_keys if k_[2] == j // (QW // 128)])
        for half in range(2):
            bm = bank()
            for h in range(MH):
                p.matmul(bm[:], oTt[b][:, h, :], Wo.sub(h, (slice(None), h, slice(half * 512, (half + 1) * 512))),
                         start=(h == 0), stop=(h == MH - 1))
            p.tt("dve", h3[:, half * 512:(half + 1) * 512], bm[:], h2t[b][:, half * 512:(half + 1) * 512], ALU.add)
        p.dma("pool", H3[rows, :], h3[:])
        ffn_prep_tile(p, h3[:], gf, wr, cf, fb, HN3, AFF, rows, bank)
    return p.finish()


def build_L7():
    nc = bass.Bass("TRN2", target_bir_lowering=False)
    p = Prog(nc)
    H3 = p.dram("H3", [TOK, D], F32, "ExternalInput")
    DS = p.dram("DS", [NCORES, TOK, D], F32, "ExternalInput")
    g_fin = p.dram("g_fin", [1, D], F32, "ExternalInput")
    OUT = p.dram("OUT", [TOK, D], F32, "ExternalOutput")
    gm = p.sb("gm", [128, D], F32)
    ht = [p.sb("ht%d" % i, [128, D], F32) for i in range(2)]
    dt_ = [[p.sb("dt%d_%d" % (i, c), [128, D], F32) for c in range(NCORES)] for i in range(2)]
    sq = p.sb("sq", [128, D], F32)
    ss = p.sb("ss", [128, 1], F32)
    rstd = p.sb("rstd", [128, 1], F32)
    ot = [p.sb("ot%d" % i, [128, D], F32) for i in range(2)]
    p.dma("sp", gm[:], g_fin.v(g_fin.t[0:1, :].broadcast_to([128, D])))
    def loads(j):
        rows = slice(j * 128, (j + 1) * 128)
        p.dma("sp", ht[j % 2][:], H3[rows, :])
        for c in range(NCORES):
            p.dma("sp" if c % 2 == 0 else "act", dt_[j % 2][c][:], DS.v(DS.t[c, rows, :]))

    loads(0)
    for j in range(NT):
        rows = slice(j * 128, (j + 1) * 128)
        Ht = ht[j % 2]
        if j + 1 < NT:
            loads(j + 1)
        for c in range(NCORES):
            p.tt("dve", Ht[:], Ht[:], dt_[j % 2][c][:], ALU.add)
        rmsnorm_tile(p, Ht[:], gm[:], ot[j % 2][:], sq[:], ss[:], rstd[:])
        p.dma("pool", OUT[rows, :], ot[j % 2][:])
    return p.finish()


def run(nc, in_maps):
    res = run_bass_kernel_spmd(nc, in_maps, core_ids=list(range(NCORES)))
    return res.results


_CACHE = {}


def _prog(name, builder):
    if name not in _CACHE:
        _CACHE[name] = builder()
    return _CACHE[name]


def _c(a):
    return np.ascontiguousarray(a)


def stage_L1(inp):
    cf, cb = const_tables()
    x = inp["x"][0]
    wg = _c(np.stack([inp["gla_w_gate_up_f"][0], inp["gla_w_gate_up_b"][0]]))
    bg = _c(np.stack([inp["gla_b_gate_f"][0], inp["gla_b_gate_b"][0]]))
    maps = [dict(x=_c(x[c * TOK:(c + 1) * TOK]), g_mix=_c(inp["mix_norm"][0:1]), w_in=_c(inp["gla_w_in"][0]),
                 wg=wg, bg=bg, cf=cf, cb=cb) for c in range(NCORES)]
    return run(_prog("L1", build_L1), maps)


def stage_L2(inp, r1):
    cf, cb = const_tables()
    x = inp["x"][0]
    maps = []
    for c in range(NCORES):
        BP = np.zeros((2, 7, GH, 128, GDV), np.float32)
        APD = np.zeros((2, 7, 128, NT * GH), np.float32)
        for i in range(7):
            cf_ = c - 7 + i
            if cf_ >= 0:
                BP[0, i] = r1[cf_]["BE"][0]
                APD[0, i] = r1[cf_]["AA"][0]
            cb_ = c + 7 - i
            if cb_ <= NCORES - 1:
                BP[1, i] = r1[cb_]["BE"][1]
                APD[1, i] = r1[cb_]["AA"][1]
        maps.append(dict(x=_c(x[c * TOK:(c + 1) * TOK]), OL=r1[c]["OL"], QP=r1[c]["QP"], SR=r1[c]["SR"],
                         AA=r1[c]["AA"], BP=BP, APD=APD, g_head=_c(inp["gla_head_norm"][0:1]),
                         w_out=_c(inp["gla_w_out"][0]), g_ffn=_c(inp["ffn_norm"][0:1]),
                         w_r=_c(inp["moe_w_router"][0]), cf=cf, cb=cb))
    return run(_prog("L2", build_L2), maps)


def stage_L3(inp, layer, aff_full, hn_full):
    cf, cb = const_tables(6)
    tokid = np.arange(SEQ, dtype=np.int32).reshape(128, 128)
    maps = []
    for c in range(NCORES):
        affe = _c(aff_full[:, 2 * c:2 * c + 2].T.reshape(2, 128, 128))
        maps.append(dict(AFFE=affe, HN=hn_full, wg=_c(inp["moe_w_gate"][layer, 2 * c:2 * c + 2]),
                         wu=_c(inp["moe_w_up"][layer, 2 * c:2 * c + 2]), wd=_c(inp["moe_w_down"][layer, 2 * c:2 * c + 2]),
                         cf=cf, cb=cb, tokid=tokid))
    return run(_prog("L3", build_L3), maps)


def stage_L4(inp, h_prev, deltas):
    cf, cb = const_tables()
    rc = rope_consts()
    maps = []
    for c in range(NCORES):
        DS = _c(np.stack([d[c * TOK:(c + 1) * TOK] for d in deltas]))
        maps.append(dict(H1=h_prev[c], DS=DS, pos=_c(inp["positions"][0:1, c * TOK:(c + 1) * TOK]),
                         g_mix=_c(inp["mix_norm"][1:2]), w_m=_c(inp["mla_w_in"][0]), g_q=_c(inp["mla_q_norm"][0:1]),
                         g_kv=_c(inp["mla_kv_norm"][0:1]), w_uq=_c(inp["mla_w_uq"][0]), w_ukv=_c(inp["mla_w_ukv"][0]),
                         rc=rc, cf=cf, cb=cb))
    return run(_prog("L4", build_L4), maps)


def stage_L5(inp, r4):
    cf, cb = const_tables()
    KT = _c(np.concatenate([r4[c]["CKVT"] for c in range(NCORES)], axis=1))
    KR = _c(np.concatenate([r4[c]["KRT"] for c in range(NCORES)], axis=1))
    VK = _c(np.concatenate([r4[c]["CKV"] for c in range(NCORES)], axis=0))
    KN2 = _c(np.concatenate([r4[c]["KN2"] for c in range(NCORES)], axis=0).reshape(128, 128))
    maps = []
    for c in range(NCORES):
        maps.append(dict(QL=r4[c]["QL"], QR=r4[c]["QR"], KT=KT, KR=KR, VK=VK, KN2=KN2, QMAX=r4[c]["QMAX"],
                         H2=r4[c]["H2"], w_ukv=_c(inp["mla_w_ukv"][0]), w_o=_c(inp["mla_w_out"][0]),
                         g_ffn=_c(inp["ffn_norm"][1:2]), w_r=_c(inp["moe_w_router"][1]), cf=cf, cb=cb))
    return run(_prog("L5", build_L5), maps)


def stage_L7(inp, h_prev, deltas):
    maps = []
    for c in range(NCORES):
        DS = _c(np.stack([d[c * TOK:(c + 1) * TOK] for d in deltas]))
        maps.append(dict(H3=h_prev[c], DS=DS, g_fin=_c(inp["final_norm"].reshape(1, D))))
    return run(_prog("L7", build_L7), maps)


def kernel(**inputs):
    inp = {k: np.asarray(v) for k, v in inputs.items()}
    r1 = stage_L1(inp)
    r2 = stage_L2(inp, r1)
    aff0 = _c(np.concatenate([r2[c]["AFF"] for c in range(NCORES)], axis=0))
    hn1 = _c(np.concatenate([r2[c]["HN1"] for c in range(NCORES)], axis=0))
    r3 = stage_L3(inp, 0, aff0, hn1)
    r4 = stage_L4(inp, [r2[c]["H1"] for c in range(NCORES)], [r3[c]["DELTA"] for c in range(NCORES)])
    del r3
    r5 = stage_L5(inp, r4)
    aff1 = _c(np.concatenate([r5[c]["AFF"] for c in range(NCORES)], axis=0))
    hn3 = _c(np.concatenate([r5[c]["HN3"] for c in range(NCORES)], axis=0))
    r6 = stage_L3(inp, 1, aff1, hn3)
    r7 = stage_L7(inp, [r5[c]["H3"] for c in range(NCORES)], [r6[c]["DELTA"] for c in range(NCORES)])
    out = np.concatenate([r7[c]["OUT"] for c in range(NCORES)], axis=0).astype(np.float32)
    return out.reshape(1, SEQ, D)
```
